# Optimizing a Trainium2 kernel written in Bass

```python
import math
import jax
import jax.numpy as jnp
from jax import lax
import numpy as np

D_MODEL = 1024
BATCH = 2
SEQ = 16384
DEPTH = 2

GRID_W = 64
CTX_LEN = 256

HEAD_DIM = 64
MIX_WIDTH = D_MODEL
ATT_WIDTH = MIX_WIDTH // 2
RWKV_WIDTH = MIX_WIDTH // 4
HY_WIDTH = MIX_WIDTH - ATT_WIDTH - RWKV_WIDTH

ATT_HEADS = ATT_WIDTH // HEAD_DIM
ATT_KV_HEADS = 2
ATT_REP = ATT_HEADS // ATT_KV_HEADS
ATT_KV_WIDTH = ATT_KV_HEADS * HEAD_DIM
ROPE_THETA = 10000.0
QK_EPS = 1e-6
Q_BLOCK = 128

RWKV_HEADS = RWKV_WIDTH // HEAD_DIM
W_LORA = 16
A_LORA = 16
G_LORA = 32
RWKV_GN_EPS = 64e-5

HY_ORDER = 2
HY_EMB = 33
HY_FFN = 64
HY_SHORT = 3

FF_DENSE = 2816
N_EXPERTS = 8
TOP_K = 2
FF_EXPERT = 2816
LN_EPS = 1e-6

IN_ATT = ATT_WIDTH + 2 * ATT_KV_WIDTH
RWKV_LORA = 2 * W_LORA + 2 * A_LORA + G_LORA
IN_RWKV = 3 * RWKV_WIDTH + RWKV_LORA
IN_HY = (HY_ORDER + 1) * HY_WIDTH
IN_WIDTH = IN_ATT + IN_RWKV + IN_HY

kernel_name = 'hybrid_diffusion_trunk_attn_rwkv7_hyena_moe'


def layer_norm(x):
    xf = x.astype(jnp.float32)
    xc = xf - jnp.mean(xf, -1, keepdims=True)
    var = jnp.mean(xc * xc, -1, keepdims=True)
    return (xc * lax.rsqrt(var + LN_EPS)).astype(x.dtype)


def modulate(x, shift, scale):
    return layer_norm(x) * (1 + scale) + shift


def post_norm(x, g, b):
    return layer_norm(x) * g + b


def rms_norm(x, g):
    xf = x.astype(jnp.float32)
    return (xf * lax.rsqrt(jnp.mean(xf * xf, -1, keepdims=True) + QK_EPS)).astype(x.dtype) * g


def axial_rope_tables(L):
    rows = L // GRID_W
    row = jnp.repeat(jnp.arange(rows, dtype=jnp.float32), GRID_W)
    col = jnp.tile(jnp.arange(GRID_W, dtype=jnp.float32), rows)
    n_freq = HEAD_DIM // 4
    inv_freq = ROPE_THETA ** (-jnp.arange(n_freq, dtype=jnp.float32) / n_freq)
    ang = jnp.concatenate([row[:, None] * inv_freq, col[:, None] * inv_freq], -1)
    return jnp.cos(ang), jnp.sin(ang)


def apply_rope(x, cos, sin):
    B, L, H, _ = x.shape
    xp = x.astype(jnp.float32).reshape(B, L, H, HEAD_DIM // 2, 2)
    x0, x1 = xp[..., 0], xp[..., 1]
    c, s = cos[None, :, None, :], sin[None, :, None, :]
    return jnp.stack([x0 * c - x1 * s, x0 * s + x1 * c], -1).reshape(x.shape).astype(x.dtype)


def qkv_heads(u, q_gain, k_gain):
    B, L, _ = u.shape
    q = u[..., :ATT_WIDTH].reshape(B, L, ATT_HEADS, HEAD_DIM)
    k = u[..., ATT_WIDTH:ATT_WIDTH + ATT_KV_WIDTH].reshape(B, L, ATT_KV_HEADS, HEAD_DIM)
    v = u[..., ATT_WIDTH + ATT_KV_WIDTH:IN_ATT].reshape(B, L, ATT_KV_HEADS, HEAD_DIM)
    return rms_norm(q, q_gain), rms_norm(k, k_gain), v


def gqa_attend(q, k, v):
    s = jnp.einsum('bqgrd,bkgd->bgrqk', q, k, preferred_element_type=jnp.float32) * (HEAD_DIM ** -0.5)
    p = jax.nn.softmax(s, axis=-1).astype(v.dtype)
    return jnp.einsum('bgrqk,bkgd->bqgrd', p, v)


def attention_group(u_lat, u_ctx, lp, rope, need_ctx):
    ql, kl, vl = qkv_heads(u_lat, lp['q_gain'], lp['k_gain'])
    qc, kc, vc = qkv_heads(u_ctx, lp['q_gain'], lp['k_gain'])
    cos, sin = rope
    ql = apply_rope(ql, cos, sin)
    kl = apply_rope(kl, cos, sin)
    k_all = jnp.concatenate([kl, kc], 1)
    v_all = jnp.concatenate([vl, vc], 1)
    B, L = ql.shape[:2]
    n_blk = L // Q_BLOCK
    qb = ql.reshape(B, n_blk, Q_BLOCK, ATT_KV_HEADS, ATT_REP, HEAD_DIM).swapaxes(0, 1)
    ob = lax.map(lambda q: gqa_attend(q, k_all, v_all), qb)
    out_lat = ob.swapaxes(0, 1).reshape(B, L, ATT_WIDTH)
    if not need_ctx:
        return out_lat, None
    Lc = qc.shape[1]
    out_ctx = gqa_attend(qc.reshape(B, Lc, ATT_KV_HEADS, ATT_REP, HEAD_DIM), kc, vc).reshape(B, Lc, ATT_WIDTH)
    return out_lat, out_ctx


def token_shift(u, mu):
    prev = jnp.pad(u[:, :-1], ((0, 0), (1, 0), (0, 0)))
    nxt = jnp.pad(u[:, 1:], ((0, 0), (0, 1), (0, 0)))
    return u + mu[0] * (prev - u) + mu[1] * (nxt - u)


def rwkv_prep(u, lp):
    B, L, _ = u.shape
    C, H, N = RWKV_WIDTH, RWKV_HEADS, HEAD_DIM
    u = token_shift(u, lp['rwkv_mu']).astype(jnp.float32)
    r, k, v = u[..., :C], u[..., C:2 * C], u[..., 2 * C:3 * C]
    o = 3 * C
    lw = u[..., o:o + 2 * W_LORA].reshape(B, L, 2, W_LORA)
    o += 2 * W_LORA
    la = u[..., o:o + 2 * A_LORA].reshape(B, L, 2, A_LORA)
    o += 2 * A_LORA
    lg = u[..., o:o + G_LORA]
    w_raw = lp['rwkv_w0'] + jnp.einsum('bldr,drc->bldc', jnp.tanh(lw), lp['rwkv_wB'])
    w = jnp.exp(-jnp.exp(-jax.nn.softplus(-w_raw) - 0.5))
    a = jax.nn.sigmoid(lp['rwkv_a0'] + jnp.einsum('bldr,drc->bldc', la, lp['rwkv_aB']))
    g = jax.nn.sigmoid(lg) @ lp['rwkv_gB']
    kk = (k * lp['rwkv_kk']).reshape(B, L, H, N)
    kk = kk * lax.rsqrt(jnp.sum(kk * kk, -1, keepdims=True) + 1e-12)
    kd = k[:, :, None, :] * (1 + (a - 1) * lp['rwkv_ka'])
    return {'r': r.reshape(B, L, H, N), 'v': v.reshape(B, L, H, N), 'kk': kk, 'g': g,
            'w': w.reshape(B, L, 2, H, N), 'a': a.reshape(B, L, 2, H, N), 'kd': kd.reshape(B, L, 2, H, N)}


def wkv_scan(S0, pr, d, reverse):
    def step(S, inp):
        r_t, w_t, k_t, v_t, kk_t, a_t = inp
        sa = jnp.einsum('bhvk,bhk->bhv', S, kk_t)
        S = S * w_t[:, :, None, :] - sa[..., None] * (kk_t * a_t)[:, :, None, :] + v_t[..., None] * k_t[:, :, None, :]
        return S, jnp.einsum('bhvk,bhk->bhv', S, r_t)
    xs = tuple(jnp.moveaxis(t, 1, 0) for t in
               (pr['r'], pr['w'][:, :, d], pr['kd'][:, :, d], pr['v'], pr['kk'], pr['a'][:, :, d]))
    S, y = lax.scan(step, S0, xs, reverse=reverse)
    return S, jnp.moveaxis(y, 0, 1)


def rwkv_readout(pr, y_f, y_b, lp):
    bonus = jnp.sum(pr['r'][:, :, None] * pr['kd'] * lp['rwkv_rk'], -1, keepdims=True) * pr['v'][:, :, None]
    y = y_f + y_b + jnp.sum(bonus, 2)
    mu = jnp.mean(y, -1, keepdims=True)
    yc = y - mu
    yn = yc * lax.rsqrt(jnp.mean(yc * yc, -1, keepdims=True) + RWKV_GN_EPS)
    B, L = y.shape[:2]
    yn = yn.reshape(B, L, RWKV_WIDTH) * lp['rwkv_gn_g'] + lp['rwkv_gn_b']
    return yn * pr['g']


def rwkv_group(u_lat, u_ctx, lp, need_ctx):
    pl = rwkv_prep(u_lat, lp)
    pc = rwkv_prep(u_ctx, lp)
    S0 = jnp.zeros((u_lat.shape[0], RWKV_HEADS, HEAD_DIM, HEAD_DIM), jnp.float32)
    S_f, yc_f = wkv_scan(S0, pc, 0, False)
    S_b, yc_b = wkv_scan(S0, pc, 1, True)
    _, yl_f = wkv_scan(S_f, pl, 0, False)
    _, yl_b = wkv_scan(S_b, pl, 1, True)
    out_lat = rwkv_readout(pl, yl_f, yl_b, lp).astype(u_lat.dtype)
    if not need_ctx:
        return out_lat, None
    return out_lat, rwkv_readout(pc, yc_f, yc_b, lp).astype(u_ctx.dtype)


def short_conv3(u, w, b):
    up = jnp.pad(u, ((0, 0), (1, 1), (0, 0)))
    return up[:, :-2] * w[0] + up[:, 1:-1] * w[1] + up[:, 2:] * w[2] + b


def hyena_filter_spectrum(L, lp):
    bands = (HY_EMB - 1) // 2
    t = jnp.linspace(0.0, 1.0, L, dtype=jnp.float32)[:, None]
    f = jnp.linspace(1e-4, bands - 1, bands, dtype=jnp.float32)[None, :]
    wt = 2.0 * math.pi * jnp.arange(L, dtype=jnp.float32)[:, None] / L
    z = jnp.concatenate([t, jnp.cos(f * wt), -jnp.sin(f * wt)], -1)
    h = jnp.sin(lp['hy_freq1'] * (z @ lp['hy_w1'] + lp['hy_b1']))
    h = jnp.sin(lp['hy_freq2'] * (h @ lp['hy_w2'] + lp['hy_b2']))
    h = ((h @ lp['hy_w3']) * jnp.exp(-t * lp['hy_decay'])).astype(jnp.float32)
    h = h.reshape(L, HY_ORDER, 2, HY_WIDTH)
    fwd, bwd = h[:, :, 0], h[:, :, 1]
    kern = jnp.concatenate([fwd, jnp.zeros_like(fwd[:1]), bwd[:0:-1]], 0)
    kern = kern / jnp.sum(jnp.abs(kern), axis=0, keepdims=True)
    return jnp.fft.rfft(kern, axis=0)


def fft_long_conv(z, kf, d):
    L = z.shape[1]
    zf = z.astype(jnp.float32)
    y = jnp.fft.irfft(jnp.fft.rfft(zf, n=2 * L, axis=1) * kf[None], n=2 * L, axis=1)[:, :L]
    return (y + zf * d).astype(z.dtype)


def hyena_group(u, lp):
    L = u.shape[1]
    u = short_conv3(u, lp['hy_short_w'], lp['hy_short_b'])
    C = HY_WIDTH
    x1, x2, v = u[..., :C], u[..., C:2 * C], u[..., 2 * C:]
    kf = hyena_filter_spectrum(L, lp)
    z = x1 * fft_long_conv(v, kf[:, 0], lp['hy_bias'][0])
    return x2 * fft_long_conv(z, kf[:, 1], lp['hy_bias'][1])


def token_mixer(h_lat, h_ctx, lp, rope, need_ctx):
    u_lat = h_lat @ lp['w_in']
    u_ctx = h_ctx @ lp['w_in']
    s_att = slice(0, IN_ATT)
    s_rwkv = slice(IN_ATT, IN_ATT + IN_RWKV)
    s_hy = slice(IN_ATT + IN_RWKV, IN_WIDTH)
    att_l, att_c = attention_group(u_lat[..., s_att], u_ctx[..., s_att], lp, rope, need_ctx)
    rw_l, rw_c = rwkv_group(u_lat[..., s_rwkv], u_ctx[..., s_rwkv], lp, need_ctx)
    hy_l = hyena_group(u_lat[..., s_hy], lp)
    o_lat = jnp.concatenate([att_l, rw_l, hy_l], -1) @ lp['w_out']
    if not need_ctx:
        return o_lat, None
    hy_c = hyena_group(u_ctx[..., s_hy], lp)
    o_ctx = jnp.concatenate([att_c, rw_c, hy_c], -1) @ lp['w_out']
    return o_lat, o_ctx


def swiglu(h, w1, w3, w2):
    return (jax.nn.silu(h @ w1) * (h @ w3)) @ w2


def moe_swiglu(h, w_router, w1, w3, w2):
    logits = jnp.einsum('bld,de->ble', h, w_router).astype(jnp.float32)
    top_v, top_i = lax.top_k(logits, TOP_K)
    gates = jax.nn.softmax(top_v, axis=-1)
    dense_gate = jnp.sum(jax.nn.one_hot(top_i, N_EXPERTS, dtype=jnp.float32) * gates[..., None], -2)
    dense_gate = dense_gate.astype(h.dtype)
    out = jnp.zeros_like(h)
    for e in range(N_EXPERTS):
        out = out + dense_gate[..., e:e + 1] * swiglu(h, w1[e], w3[e], w2[e])
    return out


def setup_inputs(seed: int = 0) -> dict:
    key = jax.random.key(seed)
    keys = iter(jax.random.split(key, 64))

    def nrm(shape, scale=1.0):
        return scale * jax.random.normal(next(keys), shape, jnp.float32)

    def uni(shape, lo, hi):
        return jax.random.uniform(next(keys), shape, jnp.float32, lo, hi)

    def gain(shape):
        return 1.0 + nrm(shape, 0.02)

    D = D_MODEL
    beta = (8 * DEPTH) ** -0.25
    n_dense = (DEPTH + 1) // 2
    n_moe = DEPTH // 2
    n_filt = HY_ORDER * 2 * HY_WIDTH
    return {
        'x': nrm((BATCH, SEQ, D)),
        'c': nrm((BATCH, D)),
        'ctx': nrm((BATCH, CTX_LEN, D)),
        'c_ctx': nrm((D,)),
        'ada_w': nrm((DEPTH, D, 6 * D), 0.5 * D ** -0.5),
        'ada_b': nrm((DEPTH, 6 * D), 0.02),
        'w_in': nrm((DEPTH, D, IN_WIDTH), D ** -0.5),
        'w_out': nrm((DEPTH, MIX_WIDTH, D), beta * MIX_WIDTH ** -0.5),
        'q_gain': gain((DEPTH, HEAD_DIM)),
        'k_gain': gain((DEPTH, HEAD_DIM)),
        'rwkv_mu': uni((DEPTH, 2, IN_RWKV), 0.0, 0.5),
        'rwkv_w0': uni((DEPTH, 2, RWKV_WIDTH), -5.0, 1.0),
        'rwkv_wB': nrm((DEPTH, 2, W_LORA, RWKV_WIDTH), W_LORA ** -0.5),
        'rwkv_a0': nrm((DEPTH, 2, RWKV_WIDTH), 0.1),
        'rwkv_aB': nrm((DEPTH, 2, A_LORA, RWKV_WIDTH), A_LORA ** -0.5),
        'rwkv_gB': nrm((DEPTH, G_LORA, RWKV_WIDTH), G_LORA ** -0.5),
        'rwkv_kk': 0.85 + nrm((DEPTH, RWKV_WIDTH), 0.02),
        'rwkv_ka': gain((DEPTH, RWKV_WIDTH)),
        'rwkv_rk': nrm((DEPTH, RWKV_HEADS, HEAD_DIM), 0.1),
        'rwkv_gn_g': gain((DEPTH, RWKV_WIDTH)),
        'rwkv_gn_b': nrm((DEPTH, RWKV_WIDTH), 0.02),
        'hy_short_w': nrm((DEPTH, HY_SHORT, IN_HY), HY_SHORT ** -0.5),
        'hy_short_b': nrm((DEPTH, IN_HY), 0.02),
        'hy_w1': nrm((DEPTH, HY_EMB, HY_FFN), HY_EMB ** -0.5),
        'hy_b1': nrm((DEPTH, HY_FFN), 0.1),
        'hy_freq1': gain((DEPTH, HY_FFN)),
        'hy_w2': nrm((DEPTH, HY_FFN, HY_FFN), HY_FFN ** -0.5),
        'hy_b2': nrm((DEPTH, HY_FFN), 0.1),
        'hy_freq2': gain((DEPTH, HY_FFN)),
        'hy_w3': nrm((DEPTH, HY_FFN, n_filt), HY_FFN ** -0.5),
        'hy_decay': uni((DEPTH, n_filt), 3.07, 15.35),
        'hy_bias': nrm((DEPTH, HY_ORDER, HY_WIDTH)),
        'ln1_g': gain((DEPTH, D)),
        'ln1_b': nrm((DEPTH, D), 0.02),
        'ln2_g': gain((DEPTH, D)),
        'ln2_b': nrm((DEPTH, D), 0.02),
        'ffn_w1': nrm((n_dense, D, FF_DENSE), D ** -0.5),
        'ffn_w3': nrm((n_dense, D, FF_DENSE), D ** -0.5),
        'ffn_w2': nrm((n_dense, FF_DENSE, D), beta * FF_DENSE ** -0.5),
        'moe_router': nrm((n_moe, D, N_EXPERTS), D ** -0.5),
        'moe_w1': nrm((n_moe, N_EXPERTS, D, FF_EXPERT), D ** -0.5),
        'moe_w3': nrm((n_moe, N_EXPERTS, D, FF_EXPERT), D ** -0.5),
        'moe_w2': nrm((n_moe, N_EXPERTS, FF_EXPERT, D), beta * FF_EXPERT ** -0.5),
    }


def reference(x, c, ctx, c_ctx, ada_w, ada_b, w_in, w_out, q_gain, k_gain,
              rwkv_mu, rwkv_w0, rwkv_wB, rwkv_a0, rwkv_aB, rwkv_gB, rwkv_kk, rwkv_ka, rwkv_rk,
              rwkv_gn_g, rwkv_gn_b, hy_short_w, hy_short_b, hy_w1, hy_b1, hy_freq1, hy_w2, hy_b2,
              hy_freq2, hy_w3, hy_decay, hy_bias, ln1_g, ln1_b, ln2_g, ln2_b,
              ffn_w1, ffn_w3, ffn_w2, moe_router, moe_w1, moe_w3, moe_w2):
    alpha = float((2 * DEPTH) ** 0.25)
    rope = axial_rope_tables(x.shape[1])
    cond_lat = jax.nn.silu(c)
    cond_ctx = jax.nn.silu(c_ctx)
    xc = ctx
    for l in range(DEPTH):
        need_ctx = l < DEPTH - 1
        mod_l = (cond_lat @ ada_w[l] + ada_b[l])[:, None, :]
        mod_c = (cond_ctx @ ada_w[l] + ada_b[l])[None, None, :]
        sh1, sc1, g1, sh2, sc2, g2 = jnp.split(mod_l, 6, axis=-1)
        csh1, csc1, cg1, csh2, csc2, cg2 = jnp.split(mod_c, 6, axis=-1)
        lp = {
            'w_in': w_in[l], 'w_out': w_out[l], 'q_gain': q_gain[l], 'k_gain': k_gain[l],
            'rwkv_mu': rwkv_mu[l], 'rwkv_w0': rwkv_w0[l], 'rwkv_wB': rwkv_wB[l],
            'rwkv_a0': rwkv_a0[l], 'rwkv_aB': rwkv_aB[l], 'rwkv_gB': rwkv_gB[l],
            'rwkv_kk': rwkv_kk[l], 'rwkv_ka': rwkv_ka[l], 'rwkv_rk': rwkv_rk[l],
            'rwkv_gn_g': rwkv_gn_g[l], 'rwkv_gn_b': rwkv_gn_b[l],
            'hy_short_w': hy_short_w[l], 'hy_short_b': hy_short_b[l],
            'hy_w1': hy_w1[l], 'hy_b1': hy_b1[l], 'hy_freq1': hy_freq1[l],
            'hy_w2': hy_w2[l], 'hy_b2': hy_b2[l], 'hy_freq2': hy_freq2[l],
            'hy_w3': hy_w3[l], 'hy_decay': hy_decay[l], 'hy_bias': hy_bias[l],
        }
        o_lat, o_ctx = token_mixer(modulate(x, sh1, sc1), modulate(xc, csh1, csc1), lp, rope, need_ctx)
        x = post_norm(alpha * x + g1 * o_lat, ln1_g[l], ln1_b[l])
        if need_ctx:
            xc = post_norm(alpha * xc + cg1 * o_ctx, ln1_g[l], ln1_b[l])

        j = l // 2
        if l % 2 == 0:
            f_lat = swiglu(modulate(x, sh2, sc2), ffn_w1[j], ffn_w3[j], ffn_w2[j])
        else:
            f_lat = moe_swiglu(modulate(x, sh2, sc2), moe_router[j], moe_w1[j], moe_w3[j], moe_w2[j])
        if need_ctx:
            if l % 2 == 0:
                f_ctx = swiglu(modulate(xc, csh2, csc2), ffn_w1[j], ffn_w3[j], ffn_w2[j])
            else:
                f_ctx = moe_swiglu(modulate(xc, csh2, csc2), moe_router[j], moe_w1[j], moe_w3[j], moe_w2[j])
            xc = post_norm(alpha * xc + cg2 * f_ctx, ln2_g[l], ln2_b[l])
        x = post_norm(alpha * x + g2 * f_lat, ln2_g[l], ln2_b[l])
    return x
```

```python
import math


import numpy as np
from contextlib import ExitStack
import concourse.bass as bass
import concourse.mybir as mybir
from concourse.bass_utils import run_bass_kernel_spmd

F32 = mybir.dt.float32
BF16 = mybir.dt.bfloat16
AF = mybir.ActivationFunctionType
ALU = mybir.AluOpType
AX = mybir.AxisListType
NDS = 24


class Reg:
    __slots__ = ("w", "r", "name", "excl")

    def __init__(self, name="", excl=False):
        self.w = None
        self.r = {}
        self.name = name
        self.excl = excl


class Prog:
    def __init__(self):
        self.nc = bass.Bass("TRN2", target_bir_lowering=False)
        self.es = ExitStack()
        nc = self.nc
        self.eng = {"pe": nc.tensor, "act": nc.scalar, "dve": nc.vector, "pool": nc.gpsimd, "sp": nc.sync}
        self.sem = {k: self.es.enter_context(nc.semaphore("s_" + k)) for k in self.eng}
        self.seq = {k: 0 for k in self.eng}
        self.known = {k: {} for k in self.eng}
        self.pend = {k: ([], []) for k in self.eng}
        self.dsem = [self.es.enter_context(nc.semaphore("d%d" % i)) for i in range(NDS)]
        self.dval = [0] * NDS
        self.dnext = 0
        self.ninst = 0
        self._n = 0
        self.scopes = []

    def sb(self, shape, dt, name=None):
        self._n += 1
        es = self.scopes[-1] if self.scopes else self.es
        return es.enter_context(self.nc.sbuf_tensor(name or "t%d" % self._n, list(shape), dt))

    def push_scope(self):
        self.scopes.append(ExitStack())

    def pop_scope(self, regs):
        for r in regs:
            for e in self.eng:
                if r.w is not None:
                    self._wait1(e, r.w)
        self.scopes.pop().close()

    def ps(self, shape, dt, name=None):
        self._n += 1
        nbytes = int(np.prod(shape[1:])) * (4 if dt == F32 else 2)
        assert nbytes % 2048 == 0, "PSUM tensors must be whole banks"
        return self.es.enter_context(self.nc.psum_tensor(name or "p%d" % self._n, list(shape), dt)), Reg(excl=True)

    def dram(self, name, shape, dt, kind):
        return self.nc.dram_tensor(name, list(shape), dt, kind=kind).ap()

    def _wait1(self, e, tok):
        key, sem, val = tok
        if self.known[e].get(key, 0) < val:
            self.eng[e].wait_ge(sem, val)
            self.known[e][key] = val
            self.ninst += 1

    def _deps(self, e, reads, writes, is_dma):
        for r in reads:
            if r.w is not None:
                t = r.w
                if t[0] == e and e == "pe" and not is_dma:
                    continue
                self._wait1(e, t)
            if r.excl:
                for k, t in r.r.items():
                    if k != e or is_dma:
                        self._wait1(e, t)
        for w in writes:
            if w.w is not None:
                t = w.w
                if not (t[0] == e and not is_dma):
                    self._wait1(e, t)
            for k, t in w.r.items():
                if k == e and not is_dma:
                    continue
                self._wait1(e, t)

    def op(self, e, fn, reads=(), writes=(), inc=True):
        self._deps(e, reads, writes, False)
        inst = fn(self.eng[e])
        self.ninst += 1
        pr, pw = self.pend[e]
        pr.extend(reads)
        pw.extend(writes)
        if inc:
            self.seq[e] += 1
            inst.then_inc(self.sem[e], 1)
            tok = (e, self.sem[e], self.seq[e])
            for r in pr:
                r.r[e] = tok
            for w in pw:
                w.w = tok
                w.r = {}
            self.pend[e] = ([], [])
        return inst

    def dma(self, q, out, in_, reads=(), writes=(), **kw):
        self._deps(q, reads, writes, True)
        slot = self.dnext
        self.dnext = (slot + 1) % NDS
        key = ("d", slot)
        if self.dval[slot] > 0:
            self._wait1(q, (key, self.dsem[slot], self.dval[slot]))
        inst = self.eng[q].dma_start(out=out, in_=in_, **kw)
        self.ninst += 1
        self.dval[slot] += 16
        inst.then_inc(self.dsem[slot], 16)
        tok = (key, self.dsem[slot], self.dval[slot])
        for r in reads:
            r.r[key] = tok
        for w in writes:
            w.w = tok
            w.r = {}
        return inst

    def finish(self, regs):
        for r in regs:
            if r.w is not None:
                self._wait1("sp", r.w)
        for slot in range(NDS):
            if self.dval[slot] > 0:
                self._wait1("sp", (("d", slot), self.dsem[slot], self.dval[slot]))

    def close(self):
        self.es.close()
        return self.nc


D = 1024
INW = 2400
LN_EPS = 1e-6


def mods_block(P, cT, adaw, adab, ncol, ones_row, psum_banks, plus_one_ranges):
    ncb = ncol // 512
    assert ncb <= len(psum_banks)
    bc = [P.sb([128, ncol], F32) for _ in range(2)]
    r_bc = [Reg(), Reg()]
    P.push_scope()
    c_sb = P.sb([128, 16], F32)
    cs_sb = P.sb([128, 16], F32)
    r_c = Reg()
    r_cs = Reg()
    P.dma("sp", c_sb[:], cT[:, :], writes=[r_c])
    P.op("act", lambda e: e.activation(out=cs_sb[:], in_=c_sb[:], func=AF.Silu), reads=[r_c], writes=[r_cs])
    ab_sb = P.sb([1, ncol], F32)
    r_ab = Reg()
    P.dma("sp", ab_sb[:], adab[:, :], writes=[r_ab])
    aw = [P.sb([128, ncol], F32) for _ in range(2)]
    r_aw = [Reg(), Reg()]
    modrow = P.sb([1, ncol], F32)
    r_mr = Reg()
    it = 0
    for s in range(2):
        for k in range(8):
            b = it % 2
            it += 1
            P.dma("sp", aw[b][:], adaw[k * 128:(k + 1) * 128, :], writes=[r_aw[b]])
            for cb in range(ncb):
                bank, rb = psum_banks[cb]
                P.op("pe", lambda e, s=s, cb=cb, bank=bank, k=k, b=b: e.matmul(
                    bank[0:1, 0:512], cs_sb[:, s * 8 + k:s * 8 + k + 1], aw[b][:, cb * 512:(cb + 1) * 512],
                    start=(k == 0), stop=(k == 7)),
                    reads=[r_cs, r_aw[b]], writes=[rb], inc=(cb == ncb - 1))
        for cb in range(ncb):
            bank, rb = psum_banks[cb]
            P.op("dve", lambda e, cb=cb, bank=bank: e.tensor_tensor(
                out=modrow[0:1, cb * 512:(cb + 1) * 512], in0=bank[0:1, 0:512],
                in1=ab_sb[0:1, cb * 512:(cb + 1) * 512], op=ALU.add),
                reads=[rb, r_ab], writes=[r_mr])
        for (a, b2) in plus_one_ranges:
            P.op("dve", lambda e, a=a, b2=b2: e.tensor_scalar_add(out=modrow[0:1, a:b2], in0=modrow[0:1, a:b2], scalar1=1.0),
                 reads=[r_mr], writes=[r_mr])
        for cb in range(ncb):
            bank, rb = psum_banks[cb]
            P.op("pe", lambda e, cb=cb, bank=bank: e.matmul(
                bank[:, 0:512], ones_row[0:1, 0:128], modrow[0:1, cb * 512:(cb + 1) * 512], start=True, stop=True),
                reads=[r_mr], writes=[rb])
            P.op("act", lambda e, s=s, cb=cb, bank=bank: e.copy(out=bc[s][:, cb * 512:(cb + 1) * 512], in_=bank[:, 0:512]),
                 reads=[rb], writes=[r_bc[s]])
    P.pop_scope([r_bc[1]])
    return bc, r_bc


def ln_tile(P, rows, x_ap, r_x, tmp, r_tmp, stat, r_stat):
    st = stat
    P.op("dve", lambda e: e.bn_stats(out=st[:rows, 0:6], in_=x_ap[:, 0:512]), reads=[r_x], writes=[r_stat])
    P.op("dve", lambda e: e.bn_stats(out=st[:rows, 6:12], in_=x_ap[:, 512:1024]), reads=[r_x], writes=[r_stat])
    P.op("dve", lambda e: e.bn_aggr(out=st[:rows, 12:14], in_=st[:rows, 0:12]), reads=[r_stat], writes=[r_stat])
    P.op("dve", lambda e: e.tensor_scalar_add(out=st[:rows, 15:16], in0=st[:rows, 13:14], scalar1=LN_EPS), reads=[r_stat], writes=[r_stat])
    P.op("act", lambda e: e.activation(out=st[:rows, 14:15], in_=st[:rows, 15:16], func=AF.Sqrt), reads=[r_stat], writes=[r_stat])
    P.op("dve", lambda e: e.reciprocal(out=st[:rows, 14:15], in_=st[:rows, 14:15]), reads=[r_stat], writes=[r_stat])
    P.op("dve", lambda e: e.tensor_scalar(out=tmp[:rows, :], in0=x_ap, scalar1=st[:rows, 12:13], scalar2=st[:rows, 14:15],
                                          op0=ALU.subtract, op1=ALU.mult), reads=[r_x, r_stat], writes=[r_tmp])


def make_ident(P, dt=BF16):
    ident = P.sb([128, 128], dt)
    r_id = Reg()
    P.op("pool", lambda e: e.memset(ident[:], 0.0), writes=[r_id])
    P.op("pool", lambda e: e.affine_select(out=ident[:], in_=ident[:], pattern=[[-1, 128]], compare_op=ALU.not_equal,
                                           fill=1.0, base=0, channel_multiplier=1), reads=[r_id], writes=[r_id])
    return ident, r_id


def build_p1(n_lat=4096, n_ctx=64):
    P = Prog()
    ntok = n_lat + n_ctx
    x = P.dram("x", [ntok, D], F32, "ExternalInput")
    cT = P.dram("cT", [128, 16], F32, "ExternalInput")
    adaw = P.dram("adaw", [D, 2048], F32, "ExternalInput")
    adab = P.dram("adab", [1, 2048], F32, "ExternalInput")
    win = P.dram("win", [D, INW], F32, "ExternalInput")
    u = P.dram("u", [ntok, INW], F32, "ExternalOutput")

    fb = [P.ps([128, 512], F32) for i in range(6)]
    tb = [P.ps([128, 8, 128], BF16) for i in range(2)]
    ones_row = P.sb([1, 128], F32)
    r_ones = Reg()
    P.op("dve", lambda e: e.memset(ones_row[:], 1.0), writes=[r_ones])
    ident, r_id = make_ident(P)
    wb = P.sb([128, 8, INW], BF16)
    r_wb = Reg()
    for k in range(8):
        P.dma("pool", wb[:, k, :], win[k * 128:(k + 1) * 128, :], writes=[r_wb])
    bc, r_bc = mods_block(P, cT, adaw, adab, 2048, ones_row, fb[:4], [(1024, 2048)])

    NX = 3
    xs = [P.sb([128, D], F32) for _ in range(NX)]
    r_xs = [Reg() for _ in range(NX)]
    tmp = [P.sb([128, D], F32) for _ in range(2)]
    r_tmp = [Reg() for _ in range(2)]
    tmp2 = [P.sb([128, D], F32) for _ in range(2)]
    r_tmp2 = [Reg() for _ in range(2)]
    hb = [P.sb([128, D], BF16) for _ in range(2)]
    r_hb = [Reg() for _ in range(2)]
    stat = [P.sb([128, 16], F32) for _ in range(2)]
    r_stat = [Reg() for _ in range(2)]
    hT = [P.sb([128, 8, 128], BF16) for _ in range(2)]
    r_hT = [Reg() for _ in range(2)]
    uo = [P.sb([128, INW], F32) for _ in range(2)]
    r_uo = [Reg() for _ in range(2)]
    r_u_out = Reg()

    tiles = [(i * 128, 128, 0) for i in range(n_lat // 128)]
    t0 = n_lat
    while t0 < ntok:
        rows = min(128, ntok - t0)
        tiles.append((t0, rows, 1))
        t0 += rows
    for i, (t0, rows, s) in enumerate(tiles):
        a = i % NX
        b = i % 2
        P.dma("sp", xs[a][:rows, :], x[t0:t0 + rows, :], writes=[r_xs[a]])
        ln_tile(P, rows, xs[a][:rows, :], r_xs[a], tmp[b], r_tmp[b], stat[b], r_stat[b])
        P.op("pool", lambda e: e.tensor_tensor(out=tmp2[b][:rows, :], in0=tmp[b][:rows, :], in1=bc[s][:rows, 1024:2048], op=ALU.mult),
             reads=[r_tmp[b], r_bc[s]], writes=[r_tmp2[b]])
        P.op("dve", lambda e: e.tensor_tensor(out=hb[b][:rows, :], in0=tmp2[b][:rows, :], in1=bc[s][:rows, 0:1024], op=ALU.add),
             reads=[r_tmp2[b], r_bc[s]], writes=[r_hb[b]])
        tp, r_tp = tb[b]
        for k in range(8):
            P.op("pe", lambda e, k=k: e.transpose(tp[:, k, :rows], hb[b][:rows, k * 128:(k + 1) * 128], ident[:rows, :rows]),
                 reads=[r_hb[b], r_id], writes=[r_tp], inc=(k == 7))
        P.op("act", lambda e: e.copy(out=hT[b][:, :, :rows], in_=tp[:, :, :rows]), reads=[r_tp], writes=[r_hT[b]])
        for cb in range(5):
            bank, rb = fb[1 + cb]
            for k in range(8):
                P.op("pe", lambda e, k=k, cb=cb, bank=bank: e.matmul(bank[:rows, 0:480], hT[b][:, k, :rows], wb[:, k, cb * 480:(cb + 1) * 480],
                                                       start=(k == 0), stop=(k == 7)),
                     reads=[r_hT[b], r_wb], writes=[rb], inc=(k == 7))
            eng = "act" if cb % 2 == 0 else "dve"
            if eng == "act":
                P.op("act", lambda e, cb=cb, bank=bank: e.copy(out=uo[b][:rows, cb * 480:(cb + 1) * 480], in_=bank[:rows, 0:480]),
                     reads=[rb], writes=[r_uo[b]])
            else:
                P.op("dve", lambda e, cb=cb, bank=bank: e.tensor_copy(out=uo[b][:rows, cb * 480:(cb + 1) * 480], in_=bank[:rows, 0:480]),
                     reads=[rb], writes=[r_uo[b]])
        P.dma("sp", u[t0:t0 + rows, :], uo[b][:rows, :], reads=[r_uo[b]], writes=[r_u_out])
    P.finish([r_u_out])
    return P


HD = 64
QK_EPS = 1e-6


def build_a1(n_lat=16384, n_ctx=256, stage=2):
    P = Prog()
    ntok = n_lat + n_ctx
    NT = ntok // 128
    NTL = n_lat // 128
    qk = P.dram("qk", [ntok, 192], F32, "ExternalInput")
    v = P.dram("v", [ntok, 64], F32, "ExternalInput")
    gains = P.dram("gains", [128, 192], F32, "ExternalInput")
    cs = P.dram("cs", [n_lat, 192], F32, "ExternalInput")
    att = P.dram("att", [ntok, 128], F32, "ExternalOutput")

    identb, r_idb = make_ident(P, BF16)
    identf, r_idf = make_ident(P, F32)
    g_sb = P.sb([128, 192], F32)
    r_g = Reg()
    P.dma("sp", g_sb[:], gains[:, :], writes=[r_g])

    QT = P.sb([64, 2, ntok], BF16)
    KT = P.sb([64, ntok], BF16)
    VA = P.sb([128, NT, 65], BF16)
    r_QT = Reg()
    r_KT = Reg()
    r_VA = Reg()
    P.op("pool", lambda e: e.memset(VA[:, :, 64:65], 1.0), writes=[r_VA])

    sps = [P.ps([128, 1024], F32) for _ in range(2)]
    ops_ = [P.ps([128, 512], F32) for _ in range(2)]
    tpb = P.ps([128, 8, 128], BF16)
    tpf = P.ps([128, 4, 128], F32)

    NB = 2
    qk_sb = [P.sb([128, 192], F32) for _ in range(NB)]
    r_qk = [Reg() for _ in range(NB)]
    v_sb = [P.sb([128, 64], F32) for _ in range(NB)]
    r_v = [Reg() for _ in range(NB)]
    cs_sb = [P.sb([128, 192], F32) for _ in range(NB)]
    r_cs = [Reg() for _ in range(NB)]
    junk = [P.sb([128, 64], F32) for _ in range(NB)]
    r_junk = [Reg() for _ in range(NB)]
    ss = [P.sb([128, 8], F32) for _ in range(NB)]
    r_ss = [Reg() for _ in range(NB)]
    qn = [P.sb([128, 192], F32) for _ in range(NB)]
    r_qn = [Reg() for _ in range(NB)]
    ra = [P.sb([128, 96], F32) for _ in range(NB)]
    rb_ = [P.sb([128, 96], F32) for _ in range(NB)]
    r_ra = [Reg() for _ in range(NB)]
    r_rb = [Reg() for _ in range(NB)]
    qr = [P.sb([128, 192], BF16) for _ in range(NB)]
    r_qr = [Reg() for _ in range(NB)]

    for t in range(NT):
        b = t % NB
        t0 = t * 128
        lat = t < NTL
        P.dma("sp", qk_sb[b][:], qk[t0:t0 + 128, :], writes=[r_qk[b]])
        P.dma("sp", v_sb[b][:], v[t0:t0 + 128, :], writes=[r_v[b]])
        if lat:
            P.dma("sp", cs_sb[b][:], cs[t0:t0 + 128, :], writes=[r_cs[b]])
        for h in range(3):
            P.op("act", lambda e, h=h: e.activation(out=junk[b][:], in_=qk_sb[b][:, h * 64:(h + 1) * 64], func=AF.Square,
                                                    accum_out=ss[b][:, h:h + 1]), reads=[r_qk[b]], writes=[r_junk[b], r_ss[b]])
        P.op("dve", lambda e: e.tensor_scalar(out=ss[b][:, 3:6], in0=ss[b][:, 0:3], scalar1=1.0 / 64, scalar2=QK_EPS,
                                              op0=ALU.mult, op1=ALU.add), reads=[r_ss[b]], writes=[r_ss[b]])
        P.op("act", lambda e: e.activation(out=ss[b][:, 3:6], in_=ss[b][:, 3:6], func=AF.Sqrt), reads=[r_ss[b]], writes=[r_ss[b]])
        P.op("dve", lambda e: e.reciprocal(out=ss[b][:, 3:6], in_=ss[b][:, 3:6]), reads=[r_ss[b]], writes=[r_ss[b]])
        for h in range(3):
            P.op("dve", lambda e, h=h: e.scalar_tensor_tensor(out=qn[b][:, h * 64:(h + 1) * 64], in0=qk_sb[b][:, h * 64:(h + 1) * 64],
                                                              scalar=ss[b][:, 3 + h:4 + h], in1=g_sb[:, h * 64:(h + 1) * 64],
                                                              op0=ALU.mult, op1=ALU.mult),
                 reads=[r_qk[b], r_ss[b], r_g], writes=[r_qn[b]])
        if lat:
            x0 = qn[b][:].rearrange("p (i two) -> p i two", two=2)[:, :, 0]
            x1 = qn[b][:].rearrange("p (i two) -> p i two", two=2)[:, :, 1]
            o0 = qr[b][:].rearrange("p (i two) -> p i two", two=2)[:, :, 0]
            o1 = qr[b][:].rearrange("p (i two) -> p i two", two=2)[:, :, 1]
            c_ = cs_sb[b][:, 0:96]
            s_ = cs_sb[b][:, 96:192]
            P.op("dve", lambda e: e.tensor_tensor(out=ra[b][:], in0=x0, in1=c_, op=ALU.mult), reads=[r_qn[b], r_cs[b]], writes=[r_ra[b]])
            P.op("pool", lambda e: e.tensor_tensor(out=rb_[b][:], in0=x1, in1=s_, op=ALU.mult), reads=[r_qn[b], r_cs[b]], writes=[r_rb[b]])
            P.op("dve", lambda e: e.tensor_tensor(out=o0, in0=ra[b][:], in1=rb_[b][:], op=ALU.subtract), reads=[r_ra[b], r_rb[b]], writes=[r_qr[b]])
            P.op("dve", lambda e: e.tensor_tensor(out=ra[b][:], in0=x0, in1=s_, op=ALU.mult), reads=[r_qn[b], r_cs[b]], writes=[r_ra[b]])
            P.op("pool", lambda e: e.tensor_tensor(out=rb_[b][:], in0=x1, in1=c_, op=ALU.mult), reads=[r_qn[b], r_cs[b]], writes=[r_rb[b]])
            P.op("dve", lambda e: e.tensor_tensor(out=o1, in0=ra[b][:], in1=rb_[b][:], op=ALU.add), reads=[r_ra[b], r_rb[b]], writes=[r_qr[b]])
        else:
            P.op("dve", lambda e: e.tensor_copy(out=qr[b][:], in_=qn[b][:]), reads=[r_qn[b]], writes=[r_qr[b]])
        P.op("pool", lambda e: e.tensor_copy(out=VA[:, t, 0:64], in_=v_sb[b][:]), reads=[r_v[b]], writes=[r_VA])
        tp, r_tp = tpb
        for h in range(3):
            P.op("pe", lambda e, h=h: e.transpose(tp[0:64, h, :], qr[b][:, h * 64:(h + 1) * 64], identb[:, :]),
                 reads=[r_qr[b], r_idb], writes=[r_tp], inc=(h == 2))
        P.op("act", lambda e: e.copy(out=QT[:, :, t0:t0 + 128], in_=tp[0:64, 0:2, :]), reads=[r_tp], writes=[r_QT])
        P.op("dve", lambda e: e.tensor_copy(out=KT[:, t0:t0 + 128], in_=tp[0:64, 2, :]), reads=[r_tp], writes=[r_KT])

    pt = [P.sb([128, 1024], BF16) for _ in range(2)]
    r_pt = [Reg() for _ in range(2)]
    osb = [P.sb([65, 512], F32) for _ in range(2)]
    r_osb = [Reg() for _ in range(2)]
    ot = [P.sb([128, 4, 64], F32) for _ in range(2)]
    r_ot = [Reg() for _ in range(2)]
    rc = [P.sb([128, 4], F32) for _ in range(2)]
    r_rc = [Reg() for _ in range(2)]
    r_att = Reg()
    blocks = [(qb * 512, 512, list(range(NT))) for qb in range(n_lat // 512)]
    blocks.append((n_lat, n_ctx, list(range(NTL, NT))))
    if stage < 2:
        blocks = []
    items = []
    groups = []
    for (q0, nq, kts) in blocks:
        for h in range(2):
            g = len(groups)
            groups.append((q0, nq, h))
            npair = len(kts) // 2
            for pi in range(npair):
                items.append((g, (kts[2 * pi], kts[2 * pi + 1]), pi == 0, pi == npair - 1))

    def emit_st(n):
        g, kt2, first, last = items[n]
        q0, nq, h = groups[g]
        sb_, r_sb = sps[n % 2]
        for j in range(2):
            kt = kt2[j]
            P.op("pe", lambda e, j=j, kt=kt: e.matmul(sb_[:, j * 512:j * 512 + nq], KT[:, kt * 128:(kt + 1) * 128],
                                                      QT[:, h, q0:q0 + nq], start=True, stop=True),
                 reads=[r_KT, r_QT], writes=[r_sb], inc=(j == 1))

    def emit_rest(n):
        g, kt2, first, last = items[n]
        q0, nq, h = groups[g]
        sb_, r_sb = sps[n % 2]
        p2 = n % 2
        o2 = g % 2
        obank, r_ob = ops_[o2]
        for j in range(2):
            P.op("act", lambda e, j=j: e.activation(out=pt[p2][:, j * 512:j * 512 + nq], in_=sb_[:, j * 512:j * 512 + nq],
                                                    func=AF.Exp, scale=0.125),
                 reads=[r_sb], writes=[r_pt[p2]])
        for j in range(2):
            kt = kt2[j]
            P.op("pe", lambda e, j=j, kt=kt: e.matmul(obank[0:65, 0:nq], VA[:, kt, :], pt[p2][:, j * 512:j * 512 + nq],
                                                      start=(first and j == 0), stop=(last and j == 1)),
                 reads=[r_VA, r_pt[p2]], writes=[r_ob], inc=(j == 1))
        if last:
            P.op("dve", lambda e: e.tensor_copy(out=osb[o2][:, 0:nq], in_=obank[0:65, 0:nq]), reads=[r_ob], writes=[r_osb[o2]])
            tf, r_tf = tpf
            nj = nq // 128
            for j in range(nj):
                P.op("pe", lambda e, j=j: e.transpose(tf[:, j, 0:65], osb[o2][0:65, j * 128:(j + 1) * 128], identf[0:65, 0:65]),
                     reads=[r_osb[o2], r_idf], writes=[r_tf], inc=(j == nj - 1))
            P.op("dve", lambda e: e.reciprocal(out=rc[o2][:, 0:nj], in_=tf[:, 0:nj, 64]), reads=[r_tf], writes=[r_rc[o2]])
            for j in range(nj):
                P.op("dve", lambda e, j=j: e.tensor_scalar(out=ot[o2][:, j, :], in0=tf[:, j, 0:64], scalar1=rc[o2][:, j:j + 1], scalar2=None,
                                                           op0=ALU.mult), reads=[r_tf, r_rc[o2]], writes=[r_ot[o2]])
            dst = att[q0:q0 + nq, h * 64:(h + 1) * 64].rearrange("(j p) d -> p j d", p=128)
            P.dma("sp", dst, ot[o2][:, 0:nj, :], reads=[r_ot[o2]], writes=[r_att])

    if items:
        emit_st(0)
    for n in range(len(items)):
        if n + 1 < len(items):
            emit_st(n + 1)
        emit_rest(n)
    P.finish([r_att])
    return P


GN_EPS = 64e-5
WSC = -0.6065306597126334


def rwkv_orders(n_lat, n_ctx, C=64):
    ncl, ncc = n_lat // C, n_ctx // C
    fwd = [n_lat + c * C for c in range(ncc)] + [c * C for c in range(ncl)]
    bwd = [n_lat + c * C for c in range(ncc - 1, -1, -1)] + [c * C for c in range(ncl - 1, -1, -1)]
    return fwd, bwd


def build_a2(n_lat=16384, n_ctx=256, stop=99):
    P = Prog()
    ntok = n_lat + n_ctx
    fwd, bwd = rwkv_orders(n_lat, n_ctx)
    NS = len(fwd)
    U3 = P.dram("U3", [NS * 128, 864], F32, "ExternalInput")
    coefmu = P.dram("coefmu", [128, 576], F32, "ExternalInput")
    rowp = P.dram("rowp", [128, 128], F32, "ExternalInput")
    hv = P.dram("hv", [128, 320], F32, "ExternalInput")
    Wl = P.dram("Wl", [96, 192], F32, "ExternalInput")
    cmask = P.dram("cmask", [128, 128 + 256 + 128 + 96], F32, "ExternalInput")
    rw = P.dram("rw", [ntok, 64], F32, "ExternalOutput")
    yfs = P.dram("yfs", [ntok, 64], F32, "Internal")
    ybs = P.dram("ybs", [ntok, 64], F32, "Internal")
    gs = P.dram("gs", [ntok, 64], F32, "Internal")
    r_yfs, r_ybs, r_gs, r_rw = Reg(), Reg(), Reg(), Reg()

    ident, r_id = make_ident(P, F32)
    cm = P.sb([128, 608], F32)
    r_cm = Reg()
    P.dma("sp", cm[:], cmask[:, :], writes=[r_cm])
    MIT = cm[:, 0:128]
    MM = cm[:, 128:384]
    MS = cm[:, 384:512]
    LM = cm[:, 512:608]
    coef = P.sb([128, 864], F32)
    r_coef = Reg()
    P.dma("sp", coef[:, 288:864], coefmu[:, :], writes=[r_coef])
    P.op("dve", lambda e: e.tensor_tensor(out=coef[:, 0:288], in0=coef[:, 288:576], in1=coef[:, 576:864], op=ALU.add), reads=[r_coef], writes=[r_coef])
    P.op("dve", lambda e: e.tensor_scalar(out=coef[:, 0:288], in0=coef[:, 0:288], scalar1=-1.0, scalar2=1.0, op0=ALU.mult, op1=ALU.add),
         reads=[r_coef], writes=[r_coef])
    rp = P.sb([128, 128], F32)
    hvs = P.sb([128, 320], F32)
    wl = P.sb([96, 192], F32)
    r_par = Reg()
    P.dma("sp", rp[:], rowp[:, :], writes=[r_par])
    P.dma("sp", hvs[:], hv[:, :], writes=[r_par])
    P.dma("sp", wl[:], Wl[:, :], writes=[r_par])
    KKW, KA, RK, GNG, GNB = (hvs[:, i * 64:(i + 1) * 64] for i in range(5))
    ones = P.sb([128, 128], F32)
    r_ones = Reg()
    P.op("pool", lambda e: e.memset(ones[:], 1.0), writes=[r_ones])

    banks = [P.ps([128, 512], F32) for _ in range(8)]
    bk = [0]

    def nb():
        b = banks[bk[0] % 8]
        bk[0] += 1
        return b

    ev = [0]

    def evac(out, in_, reads, writes):
        ev[0] += 1
        if ev[0] % 2 == 0:
            P.op("act", lambda e: e.copy(out=out, in_=in_), reads=reads, writes=writes)
        else:
            P.op("dve", lambda e: e.tensor_copy(out=out, in_=in_), reads=reads, writes=writes)

    def T(shape=(128, 128)):
        return [P.sb(list(shape), F32) for _ in range(2)], [Reg() for _ in range(2)]

    u3, r_u3 = T((128, 864))
    prod, r_prod = T((128, 864))
    us, r_us = T((128, 288))
    lo, r_lo = T((128, 96))
    loT, r_loT = T((96, 128))
    wa, r_wa = T((128, 128))
    gg, r_gg = T((128, 64))
    kk, r_kk = T((128, 64))
    sm, r_sm = T((128, 8))
    tmp, r_tmp = T((128, 64))
    tmp2, r_tmp2 = T((128, 64))
    kd, r_kd = T((128, 64))
    bdn = ["lw", "a", "b", "kd", "r", "v"]
    bd = {n: T() for n in bdn}
    for n in bdn:
        for i in range(2):
            P.op("pool", lambda e, n=n, i=i: e.memset(bd[n][0][i][:], 0.0), writes=[bd[n][1][i]])
    ex, r_ex = T((128, 512))
    ee, r_ee = T((128, 256))
    At, r_At = T()
    BKt, r_BKt = T((128, 256))
    Rt, r_Rt = T()
    BKG, r_BKG = T((128, 256))
    gcc, r_gcc = T((128, 1))
    BKT, r_BKT = T((128, 256))
    ART, r_ART = T((128, 256))
    LA, r_LA = T((128, 256))
    LK, r_LK = T((128, 256))
    X, r_X = T()
    XT, r_XT = T()
    X2, r_X2 = T()
    XT2, r_XT2 = T()
    TT, r_TT = T()
    Pm, r_Pm = T()
    LV, r_LV = T()
    Q, r_Q = T()
    RpT, r_RpT = T()
    Mm, r_Mm = T()
    yo, r_yo = T()
    ST = [P.sb([128, 128], F32) for _ in range(2)]
    r_ST = [Reg(), Reg()]
    P.op("pool", lambda e: e.memset(ST[0][:], 0.0), writes=[r_ST[0]])

    for s in range(NS):
        i = s % 2
        LW, r_LW = bd["lw"][0][i], bd["lw"][1][i]
        Ab, r_Ab = bd["a"][0][i], bd["a"][1][i]
        Bb, r_Bb = bd["b"][0][i], bd["b"][1][i]
        KDb, r_KDb = bd["kd"][0][i], bd["kd"][1][i]
        Rb, r_Rb = bd["r"][0][i], bd["r"][1][i]
        Vb, r_Vb = bd["v"][0][i], bd["v"][1][i]
        P.dma("sp", u3[i][:], U3[s * 128:(s + 1) * 128, :], writes=[r_u3[i]])
        P.op("dve", lambda e: e.tensor_tensor(out=prod[i][:], in0=u3[i][:], in1=coef[:], op=ALU.mult), reads=[r_u3[i], r_coef], writes=[r_prod[i]])
        P.op("pool", lambda e: e.tensor_tensor(out=us[i][:], in0=prod[i][:, 0:288], in1=prod[i][:, 288:576], op=ALU.add), reads=[r_prod[i]], writes=[r_us[i]])
        P.op("dve", lambda e: e.tensor_tensor(out=us[i][:], in0=us[i][:], in1=prod[i][:, 576:864], op=ALU.add), reads=[r_prod[i], r_us[i]], writes=[r_us[i]])
        r_ = us[i][:, 0:64]
        k_ = us[i][:, 64:128]
        v_ = us[i][:, 128:192]
        if stop <= 1:
            continue
        P.op("act", lambda e: e.activation(out=lo[i][:, 0:32], in_=us[i][:, 192:224], func=AF.Tanh), reads=[r_us[i]], writes=[r_lo[i]])
        P.op("act", lambda e: e.activation(out=lo[i][:, 64:96], in_=us[i][:, 256:288], func=AF.Sigmoid), reads=[r_us[i]], writes=[r_lo[i]])
        P.op("pool", lambda e: e.tensor_copy(out=lo[i][:, 32:64], in_=us[i][:, 224:256]), reads=[r_us[i]], writes=[r_lo[i]])
        P.op("dve", lambda e: e.tensor_tensor(out=lo[i][:], in0=lo[i][:], in1=LM, op=ALU.mult), reads=[r_lo[i], r_cm], writes=[r_lo[i]])
        b1, rb1 = nb()
        P.op("pe", lambda e: e.transpose(b1[0:96, 0:128], lo[i][:, :], ident[:, :]), reads=[r_lo[i], r_id], writes=[rb1])
        evac(loT[i][:, :], b1[0:96, 0:128], [rb1], [r_loT[i]])
        b2, rb2 = nb()
        P.op("pe", lambda e: e.matmul(b2[:, 0:192], loT[i][:, :], wl[:, :], start=True, stop=True), reads=[r_loT[i], r_par], writes=[rb2])
        P.op("dve", lambda e: e.tensor_tensor(out=wa[i][:], in0=b2[:, 0:128], in1=rp[:], op=ALU.add), reads=[rb2, r_par], writes=[r_wa[i]])
        P.op("act", lambda e: e.copy(out=gg[i][:], in_=b2[:, 128:192]), reads=[rb2], writes=[r_gg[i]])
        P.op("act", lambda e: e.activation(out=wa[i][:], in_=wa[i][:], func=AF.Sigmoid), reads=[r_wa[i]], writes=[r_wa[i]])
        sw = wa[i][:, 0:64]
        asg = wa[i][:, 64:128]
        if stop <= 2:
            continue
        P.op("dve", lambda e: e.tensor_tensor(out=kk[i][:], in0=k_, in1=KKW, op=ALU.mult), reads=[r_us[i], r_par], writes=[r_kk[i]])
        P.op("act", lambda e: e.activation(out=tmp[i][:], in_=kk[i][:], func=AF.Square, accum_out=sm[i][:, 0:1]), reads=[r_kk[i]], writes=[r_tmp[i], r_sm[i]])
        P.op("dve", lambda e: e.tensor_scalar_add(out=sm[i][:, 1:2], in0=sm[i][:, 0:1], scalar1=1e-12), reads=[r_sm[i]], writes=[r_sm[i]])
        P.op("act", lambda e: e.activation(out=sm[i][:, 1:2], in_=sm[i][:, 1:2], func=AF.Sqrt), reads=[r_sm[i]], writes=[r_sm[i]])
        P.op("dve", lambda e: e.reciprocal(out=sm[i][:, 2:3], in_=sm[i][:, 1:2]), reads=[r_sm[i]], writes=[r_sm[i]])
        P.op("dve", lambda e: e.tensor_scalar(out=kk[i][:], in0=kk[i][:], scalar1=sm[i][:, 2:3], scalar2=None, op0=ALU.mult), reads=[r_kk[i], r_sm[i]], writes=[r_kk[i]])
        P.op("dve", lambda e: e.scalar_tensor_tensor(out=tmp2[i][:], in0=asg, scalar=-1.0, in1=KA, op0=ALU.add, op1=ALU.mult), reads=[r_wa[i], r_par], writes=[r_tmp2[i]])
        P.op("dve", lambda e: e.scalar_tensor_tensor(out=kd[i][:], in0=tmp2[i][:], scalar=1.0, in1=k_, op0=ALU.add, op1=ALU.mult), reads=[r_tmp2[i], r_us[i]], writes=[r_kd[i]])
        P.op("dve", lambda e: e.tensor_tensor(out=tmp[i][:], in0=r_, in1=RK, op=ALU.mult), reads=[r_us[i], r_par], writes=[r_tmp[i]])
        P.op("dve", lambda e: e.tensor_tensor(out=tmp[i][:], in0=tmp[i][:], in1=kd[i][:], op=ALU.mult), reads=[r_tmp[i], r_kd[i]], writes=[r_tmp[i]])
        P.op("dve", lambda e: e.tensor_reduce(out=sm[i][:, 3:4], in_=tmp[i][:], axis=AX.X, op=ALU.add), reads=[r_tmp[i]], writes=[r_sm[i]])
        if stop <= 3:
            continue
        for h in range(2):
            ps_ = slice(h * 64, (h + 1) * 64)
            ce = "act" if h == 0 else "pool"
            P.op("dve", lambda e: e.tensor_scalar(out=LW[ps_, ps_], in0=wa[i][ps_, 0:64], scalar1=WSC, scalar2=None, op0=ALU.mult), reads=[r_wa[i]], writes=[r_LW])
            P.op("dve", lambda e: e.tensor_scalar(out=Ab[ps_, ps_], in0=kk[i][ps_, :], scalar1=-1.0, scalar2=None, op0=ALU.mult), reads=[r_kk[i]], writes=[r_Ab])
            P.op("pool", lambda e: e.tensor_tensor(out=Bb[ps_, ps_], in0=kk[i][ps_, :], in1=wa[i][ps_, 64:128], op=ALU.mult), reads=[r_kk[i], r_wa[i]], writes=[r_Bb])
            for (dst, r_dst, src, r_src) in ((KDb, r_KDb, kd[i][ps_, :], r_kd[i]), (Rb, r_Rb, us[i][ps_, 0:64], r_us[i]), (Vb, r_Vb, us[i][ps_, 128:192], r_us[i])):
                if ce == "act":
                    P.op("act", lambda e, dst=dst, src=src: e.copy(out=dst[ps_, ps_], in_=src), reads=[r_src], writes=[r_dst])
                else:
                    P.op("pool", lambda e, dst=dst, src=src: e.tensor_copy(out=dst[ps_, ps_], in_=src), reads=[r_src], writes=[r_dst])
        if stop <= 4:
            continue
        b3, rb3 = nb()
        P.op("pe", lambda e: e.matmul(b3[:, 0:128], MIT, LW[:, :], start=True, stop=True), reads=[r_cm, r_LW], writes=[rb3], inc=False)
        P.op("pe", lambda e: e.matmul(b3[:, 128:256], ones[:, :], LW[:, :], start=True, stop=True), reads=[r_ones, r_LW], writes=[rb3], inc=False)
        P.op("pe", lambda e: e.matmul(b3[:, 256:257], LW[:, :], ones[:, 0:1], start=True, stop=True), reads=[r_ones, r_LW], writes=[rb3])
        P.op("act", lambda e: e.activation(out=ex[i][:, 0:128], in_=b3[:, 0:128], func=AF.Exp), reads=[rb3], writes=[r_ex[i]])
        P.op("act", lambda e: e.activation(out=ex[i][:, 128:256], in_=b3[:, 0:128], func=AF.Exp, scale=-1.0), reads=[rb3], writes=[r_ex[i]])
        P.op("act", lambda e: e.activation(out=ex[i][:, 256:384], in_=b3[:, 128:256], func=AF.Exp), reads=[rb3], writes=[r_ex[i]])
        P.op("act", lambda e: e.activation(out=gcc[i][:, 0:1], in_=b3[:, 256:257], func=AF.Exp), reads=[rb3], writes=[r_gcc[i]])
        P.op("act", lambda e: e.activation(out=ex[i][:, 384:512], in_=LW[:, :], func=AF.Exp, scale=-1.0), reads=[r_LW], writes=[r_ex[i]])
        EI = ex[i][:, 0:128]
        EN = ex[i][:, 128:256]
        P.op("dve", lambda e: e.tensor_tensor(out=ee[i][:, 0:128], in0=EI, in1=ex[i][:, 384:512], op=ALU.mult), reads=[r_ex[i]], writes=[r_ee[i]])
        P.op("pool", lambda e: e.tensor_tensor(out=ee[i][:, 128:256], in0=ex[i][:, 256:384], in1=EN, op=ALU.mult), reads=[r_ex[i]], writes=[r_ee[i]])
        P.op("dve", lambda e: e.tensor_tensor(out=At[i][:], in0=Ab[:, :], in1=ee[i][:, 0:128], op=ALU.mult), reads=[r_Ab, r_ee[i]], writes=[r_At[i]])
        P.op("pool", lambda e: e.tensor_tensor(out=BKt[i][:, 0:128], in0=Bb[:, :], in1=EN, op=ALU.mult), reads=[r_Bb, r_ex[i]], writes=[r_BKt[i]])
        P.op("dve", lambda e: e.tensor_tensor(out=BKt[i][:, 128:256], in0=KDb[:, :], in1=EN, op=ALU.mult), reads=[r_KDb, r_ex[i]], writes=[r_BKt[i]])
        P.op("pool", lambda e: e.tensor_tensor(out=Rt[i][:], in0=Rb[:, :], in1=EI, op=ALU.mult), reads=[r_Rb, r_ex[i]], writes=[r_Rt[i]])
        P.op("dve", lambda e: e.tensor_tensor(out=BKG[i][:, 0:128], in0=Bb[:, :], in1=ee[i][:, 128:256], op=ALU.mult), reads=[r_Bb, r_ee[i]], writes=[r_BKG[i]])
        P.op("pool", lambda e: e.tensor_tensor(out=BKG[i][:, 128:256], in0=KDb[:, :], in1=ee[i][:, 128:256], op=ALU.mult), reads=[r_KDb, r_ee[i]], writes=[r_BKG[i]])
        if stop <= 5:
            continue
        b4, rb4 = nb()
        P.op("pe", lambda e: e.transpose(b4[:, 0:128], BKt[i][:, 0:128], ident[:, :]), reads=[r_BKt[i], r_id], writes=[rb4], inc=False)
        P.op("pe", lambda e: e.transpose(b4[:, 128:256], BKt[i][:, 128:256], ident[:, :]), reads=[r_BKt[i], r_id], writes=[rb4])
        evac(BKT[i][:, :], b4[:, 0:256], [rb4], [r_BKT[i]])
        b5, rb5 = nb()
        P.op("pe", lambda e: e.transpose(b5[:, 0:128], At[i][:, :], ident[:, :]), reads=[r_At[i], r_id], writes=[rb5], inc=False)
        P.op("pe", lambda e: e.transpose(b5[:, 128:256], Rt[i][:, :], ident[:, :]), reads=[r_Rt[i], r_id], writes=[rb5])
        evac(ART[i][:, :], b5[:, 0:256], [rb5], [r_ART[i]])
        b6, rb6 = nb()
        P.op("pe", lambda e: e.matmul(b6[:, 0:256], BKT[i][:, 0:128], ART[i][:, :], start=True, stop=True), reads=[r_BKT[i], r_ART[i]], writes=[rb6])
        P.op("dve", lambda e: e.tensor_tensor(out=LA[i][:], in0=b6[:, 0:256], in1=MM, op=ALU.mult), reads=[rb6, r_cm], writes=[r_LA[i]])
        b7, rb7 = nb()
        P.op("pe", lambda e: e.matmul(b7[:, 0:256], BKT[i][:, 128:256], ART[i][:, :], start=True, stop=True), reads=[r_BKT[i], r_ART[i]], writes=[rb7])
        P.op("dve", lambda e: e.tensor_tensor(out=LK[i][:], in0=b7[:, 0:256], in1=MM, op=ALU.mult), reads=[rb7, r_cm], writes=[r_LK[i]])
        b8, rb8 = nb()
        P.op("pe", lambda e: e.matmul(b8[:, 0:128], ART[i][:, 0:128], BKT[i][:, 0:128], start=True, stop=True), reads=[r_BKT[i], r_ART[i]], writes=[rb8])
        P.op("dve", lambda e: e.tensor_tensor(out=XT[i][:], in0=b8[:, 0:128], in1=MS, op=ALU.mult), reads=[rb8, r_cm], writes=[r_XT[i]])
        if stop <= 6:
            continue
        P.op("pool", lambda e: e.tensor_tensor(out=TT[i][:], in0=LA[i][:, 0:128], in1=ident[:, :], op=ALU.add), reads=[r_LA[i], r_id], writes=[r_TT[i]])
        cx, r_cx = LA[i][:, 0:128], r_LA[i]
        cxt, r_cxt = XT[i][:, :], r_XT[i]
        nxt = [(X2[i], r_X2[i], XT2[i], r_XT2[i]), (X[i], r_X[i], XT[i], r_XT[i])]
        for lv in range(5):
            nX, r_nX, nXT, r_nXT = nxt[lv % 2]
            if lv < 4:
                ba, rba = nb()
                P.op("pe", lambda e, cx=cx, cxt=cxt, ba=ba: e.matmul(ba[:, 0:128], cxt, cx, start=True, stop=True), reads=[r_cx, r_cxt], writes=[rba])
            bb, rbb = nb()
            P.op("pe", lambda e, cx=cx, cxt=cxt, bb=bb: e.matmul(bb[:, 0:128], cx, cxt, start=True, stop=True), reads=[r_cx, r_cxt], writes=[rbb])
            if lv < 4:
                P.op("act", lambda e, nX=nX, ba=ba: e.copy(out=nX[:, :], in_=ba[:, 0:128]), reads=[rba], writes=[r_nX])
            P.op("dve", lambda e, nXT=nXT, bb=bb: e.tensor_copy(out=nXT[:, :], in_=bb[:, 0:128]), reads=[rbb], writes=[r_nXT])
            bc, rbc = nb()
            P.op("pe", lambda e, nXT=nXT, bc=bc: e.matmul(bc[:, 0:128], nXT[:, :], TT[i][:, :], start=True, stop=True), reads=[r_nXT, r_TT[i]], writes=[rbc])
            P.op("dve", lambda e, bc=bc: e.tensor_tensor(out=TT[i][:], in0=bc[:, 0:128], in1=TT[i][:], op=ALU.add), reads=[rbc, r_TT[i]], writes=[r_TT[i]])
            cx, r_cx, cxt, r_cxt = nX[:, :], r_nX, nXT[:, :], r_nXT
        if stop <= 7:
            continue
        b9, rb9 = nb()
        P.op("pe", lambda e: e.matmul(b9[:, 0:128], TT[i][:, :], At[i][:, :], start=True, stop=True), reads=[r_TT[i], r_At[i]], writes=[rb9], inc=False)
        P.op("pe", lambda e: e.matmul(b9[:, 128:256], LK[i][:, 0:128], Vb[:, :], start=True, stop=True), reads=[r_LK[i], r_Vb], writes=[rb9])
        P.op("act", lambda e: e.copy(out=Pm[i][:, :], in_=b9[:, 0:128]), reads=[rb9], writes=[r_Pm[i]])
        P.op("dve", lambda e: e.tensor_copy(out=LV[i][:, :], in_=b9[:, 128:256]), reads=[rb9], writes=[r_LV[i]])
        b10, rb10 = nb()
        P.op("pe", lambda e: e.matmul(b10[:, 0:128], TT[i][:, :], LV[i][:, :], start=True, stop=True), reads=[r_TT[i], r_LV[i]], writes=[rb10], inc=False)
        P.op("pe", lambda e: e.matmul(b10[:, 128:256], Pm[i][:, :], LA[i][:, 128:256], start=True, stop=True), reads=[r_Pm[i], r_LA[i]], writes=[rb10], inc=False)
        P.op("pe", lambda e: e.matmul(b10[:, 256:384], Pm[i][:, :], BKG[i][:, 0:128], start=True, stop=True), reads=[r_Pm[i], r_BKG[i]], writes=[rb10])
        P.op("act", lambda e: e.copy(out=Q[i][:, :], in_=b10[:, 0:128]), reads=[rb10], writes=[r_Q[i]])
        P.op("dve", lambda e: e.tensor_tensor(out=RpT[i][:], in0=b10[:, 128:256], in1=ART[i][:, 128:256], op=ALU.add), reads=[rb10, r_ART[i]], writes=[r_RpT[i]])
        P.op("dve", lambda e: e.scalar_tensor_tensor(out=Mm[i][:], in0=ident[:, :], scalar=gcc[i][:, 0:1], in1=b10[:, 256:384], op0=ALU.mult, op1=ALU.add),
             reads=[rb10, r_id, r_gcc[i]], writes=[r_Mm[i]])
        if stop <= 8:
            continue
        sc, sn = s % 2, (s + 1) % 2
        b11, rb11 = nb()
        P.op("pe", lambda e: e.matmul(b11[:, 0:128], LA[i][:, 128:256], Q[i][:, :], start=True, stop=False), reads=[r_LA[i], r_Q[i]], writes=[rb11], inc=False)
        P.op("pe", lambda e: e.matmul(b11[:, 0:128], LK[i][:, 128:256], Vb[:, :], start=False, stop=False), reads=[r_LK[i], r_Vb], writes=[rb11], inc=False)
        P.op("pe", lambda e: e.matmul(b11[:, 0:128], RpT[i][:, :], ST[sc][:, :], start=False, stop=True), reads=[r_RpT[i], r_ST[sc]], writes=[rb11])
        b12, rb12 = nb()
        P.op("pe", lambda e: e.matmul(b12[:, 0:128], BKG[i][:, 0:128], Q[i][:, :], start=True, stop=False), reads=[r_BKG[i], r_Q[i]], writes=[rb12], inc=False)
        P.op("pe", lambda e: e.matmul(b12[:, 0:128], BKG[i][:, 128:256], Vb[:, :], start=False, stop=False), reads=[r_BKG[i], r_Vb], writes=[rb12], inc=False)
        P.op("pe", lambda e: e.matmul(b12[:, 0:128], Mm[i][:, :], ST[sc][:, :], start=False, stop=True), reads=[r_Mm[i], r_ST[sc]], writes=[rb12])
        P.op("act", lambda e: e.copy(out=ST[sn][:, :], in_=b12[:, 0:128]), reads=[rb12], writes=[r_ST[sn]])
        P.op("dve", lambda e: e.scalar_tensor_tensor(out=yo[i][:], in0=Vb[:, :], scalar=sm[i][:, 3:4], in1=b11[:, 0:128], op0=ALU.mult, op1=ALU.add),
             reads=[rb11, r_Vb, r_sm[i]], writes=[r_yo[i]])
        tf, tb_ = fwd[s], bwd[s]
        P.dma("sp", yfs[tf:tf + 64, :], yo[i][0:64, 0:64], reads=[r_yo[i]], writes=[r_yfs])
        P.dma("sp", ybs[tb_:tb_ + 64, :], yo[i][64:128, 64:128], reads=[r_yo[i]], writes=[r_ybs])
        P.dma("sp", gs[tf:tf + 64, :], gg[i][0:64, :], reads=[r_gg[i]], writes=[r_gs])

    if stop < 99:
        P.finish([])
        return P
    yf_, r_yf_ = T((128, 64))
    yb_, r_yb_ = T((128, 64))
    g_, r_g_ = T((128, 64))
    st, r_st = T((128, 16))
    yn, r_yn = T((128, 64))
    for t in range(ntok // 128):
        i = t % 2
        t0 = t * 128
        P.dma("sp", yf_[i][:], yfs[t0:t0 + 128, :], reads=[r_yfs], writes=[r_yf_[i]])
        P.dma("sp", yb_[i][:], ybs[t0:t0 + 128, :], reads=[r_ybs], writes=[r_yb_[i]])
        P.dma("sp", g_[i][:], gs[t0:t0 + 128, :], reads=[r_gs], writes=[r_g_[i]])
        P.op("dve", lambda e: e.tensor_tensor(out=yf_[i][:], in0=yf_[i][:], in1=yb_[i][:], op=ALU.add), reads=[r_yf_[i], r_yb_[i]], writes=[r_yf_[i]])
        P.op("dve", lambda e: e.bn_stats(out=st[i][:, 0:6], in_=yf_[i][:]), reads=[r_yf_[i]], writes=[r_st[i]])
        P.op("dve", lambda e: e.bn_aggr(out=st[i][:, 6:8], in_=st[i][:, 0:6]), reads=[r_st[i]], writes=[r_st[i]])
        P.op("dve", lambda e: e.tensor_scalar_add(out=st[i][:, 8:9], in0=st[i][:, 7:8], scalar1=GN_EPS), reads=[r_st[i]], writes=[r_st[i]])
        P.op("act", lambda e: e.activation(out=st[i][:, 8:9], in_=st[i][:, 8:9], func=AF.Sqrt), reads=[r_st[i]], writes=[r_st[i]])
        P.op("dve", lambda e: e.reciprocal(out=st[i][:, 9:10], in_=st[i][:, 8:9]), reads=[r_st[i]], writes=[r_st[i]])
        P.op("dve", lambda e: e.tensor_scalar(out=yn[i][:], in0=yf_[i][:], scalar1=st[i][:, 6:7], scalar2=st[i][:, 9:10], op0=ALU.subtract, op1=ALU.mult),
             reads=[r_yf_[i], r_st[i]], writes=[r_yn[i]])
        P.op("pool", lambda e: e.tensor_tensor(out=yn[i][:], in0=yn[i][:], in1=GNG, op=ALU.mult), reads=[r_yn[i], r_par], writes=[r_yn[i]])
        P.op("pool", lambda e: e.tensor_tensor(out=yn[i][:], in0=yn[i][:], in1=GNB, op=ALU.add), reads=[r_yn[i], r_par], writes=[r_yn[i]])
        P.op("dve", lambda e: e.tensor_tensor(out=yn[i][:], in0=yn[i][:], in1=g_[i][:], op=ALU.mult), reads=[r_yn[i], r_g_[i]], writes=[r_yn[i]])
        P.dma("sp", rw[t0:t0 + 128, :], yn[i][:], reads=[r_yn[i]], writes=[r_rw])
    P.finish([r_rw])
    return P


PI = math.pi


def build_a3(n_lat=16384, n_ctx=256, stop=99):
    P = Prog()
    ntok = n_lat + n_ctx
    NT = ntok // 128
    H3 = P.dram("H3", [ntok, 576], F32, "ExternalInput")
    cw = P.dram("cw", [128, 768], F32, "ExternalInput")
    ztl = P.dram("ztl", [2, 33, n_lat], F32, "ExternalInput")
    ztc = P.dram("ztc", [2, 33, n_ctx], F32, "ExternalInput")
    w1d = P.dram("w1", [33, 64], F32, "ExternalInput")
    w2d = P.dram("w2", [64, 64], F32, "ExternalInput")
    w3d = P.dram("w3", [64, 256], F32, "ExternalInput")
    colp = P.dram("colp", [64, 4], F32, "ExternalInput")
    decd = P.dram("dec", [1, 256], F32, "ExternalInput")
    hbd = P.dram("hb", [128, 128], F32, "ExternalInput")
    hy = P.dram("hy", [ntok, 64], F32, "ExternalOutput")
    WL = 2 * n_lat - 1
    WC = 2 * n_ctx - 1
    KDl = P.dram("KDl", [128, WL + 1], BF16, "Internal")
    KDc = P.dram("KDc", [128, WC + 1], BF16, "Internal")
    r_KD = {0: Reg(), 1: Reg()}
    r_hy = Reg()

    identf, r_idf = make_ident(P, F32)
    J = P.sb([128, 128], BF16)
    r_J = Reg()
    P.op("pool", lambda e: e.memset(J[:], 0.0), writes=[r_J])
    P.op("pool", lambda e: e.affine_select(out=J[:], in_=J[:], pattern=[[1, 128]], compare_op=ALU.not_equal, fill=1.0, base=-127, channel_multiplier=1),
         reads=[r_J], writes=[r_J])
    ones = P.sb([128, 128], F32)
    r_ones = Reg()
    P.op("pool", lambda e: e.memset(ones[:], 1.0), writes=[r_ones])
    npi = P.sb([128, 1], F32)
    P.op("pool", lambda e: e.memset(npi[:], PI / 2), writes=[r_ones])

    cws = P.sb([128, 768], F32)
    w1s = P.sb([33, 64], F32)
    w2s = P.sb([64, 64], F32)
    w3s = P.sb([64, 256], F32)
    cps = P.sb([64, 4], F32)
    decs = P.sb([1, 256], F32)
    hbs = P.sb([128, 128], F32)
    r_par = Reg()
    for dst, src in ((cws, cw), (w1s, w1d), (w2s, w2d), (w3s, w3d), (cps, colp), (decs, decd), (hbs, hbd)):
        P.dma("sp", dst[:], src[:, :], writes=[r_par])
    P.op("dve", lambda e: e.tensor_scalar(out=decs[:], in0=decs[:], scalar1=-1.0, scalar2=None, op0=ALU.mult), reads=[r_par], writes=[r_par])

    banks = [P.ps([128, 512], F32) for _ in range(8)]
    bk = [0]

    def nb():
        b = banks[bk[0] % 6]
        bk[0] += 1
        return b
    ybanks = banks[6:8]

    SC = P.sb([128, 192, NT], F32)
    r_SC = Reg()
    RN = P.sb([128, 2, 128], F32)
    r_RN = Reg()
    P.push_scope()
    h3 = [P.sb([128, 576], F32) for _ in range(2)]
    r_h3 = [Reg(), Reg()]
    pr = [P.sb([128, 576], F32) for _ in range(2)]
    r_pr = [Reg(), Reg()]
    s1 = [P.sb([128, 192], F32) for _ in range(2)]
    r_s1 = [Reg(), Reg()]
    for t in range(NT):
        i = t % 2
        P.dma("sp", h3[i][:], H3[t * 128:(t + 1) * 128, :], writes=[r_h3[i]])
        P.op("dve", lambda e: e.tensor_tensor(out=pr[i][:], in0=h3[i][:], in1=cws[:, 0:576], op=ALU.mult), reads=[r_h3[i], r_par], writes=[r_pr[i]])
        P.op("pool", lambda e: e.tensor_tensor(out=s1[i][:], in0=pr[i][:, 0:192], in1=pr[i][:, 192:384], op=ALU.add), reads=[r_pr[i]], writes=[r_s1[i]])
        P.op("pool", lambda e: e.tensor_tensor(out=s1[i][:], in0=s1[i][:], in1=pr[i][:, 384:576], op=ALU.add), reads=[r_pr[i], r_s1[i]], writes=[r_s1[i]])
        P.op("dve", lambda e: e.tensor_tensor(out=SC[:, :, t], in0=s1[i][:], in1=cws[:, 576:768], op=ALU.add), reads=[r_s1[i], r_par], writes=[r_SC])

    nacc = 2 * max(n_lat // 512, 1) + 2
    acc = P.sb([128, 2, 2 * (n_lat // 512 + 1)], F32)
    r_acc = Reg()
    P.op("pool", lambda e: e.memset(acc[:], 0.0), writes=[r_acc])
    zt = [P.sb([33, 512], F32) for _ in range(2)]
    r_zt = [Reg(), Reg()]
    hA = [P.sb([64, 512], F32) for _ in range(2)]
    r_hA = [Reg(), Reg()]
    hB = [P.sb([64, 512], F32) for _ in range(2)]
    r_hB = [Reg(), Reg()]
    win = [P.sb([128, 512], F32) for _ in range(2)]
    r_win = [Reg(), Reg()]
    hw = [P.sb([128, 512], F32) for _ in range(2)]
    r_hw = [Reg(), Reg()]
    hwb = [P.sb([128, 512], BF16) for _ in range(2)]
    r_hwb = [Reg(), Reg()]
    junk = [P.sb([128, 512], F32) for _ in range(2)]
    r_junk = [Reg(), Reg()]
    sS = [P.sb([64, 512], F32) for _ in range(2)]
    sC = [P.sb([64, 512], F32) for _ in range(2)]
    sQ = [P.sb([64, 512], F32) for _ in range(2)]
    r_sS = [Reg(), Reg()]
    r_sC = [Reg(), Reg()]
    r_sQ = [Reg(), Reg()]

    def sin_big(h, r_h, N, i):
        S, C, Q = sS[i], sC[i], sQ[i]
        P.op("act", lambda e: e.activation(out=S[:, 0:N], in_=h[:, 0:N], func=AF.Sin, scale=0.125), reads=[r_h], writes=[r_sS[i]])
        P.op("act", lambda e: e.activation(out=C[:, 0:N], in_=h[:, 0:N], func=AF.Sin, scale=0.125, bias=npi[0:64, 0:1]), reads=[r_h, r_ones], writes=[r_sC[i]])
        for lv in range(3):
            dst = h if lv == 2 else S
            r_dst = r_h if lv == 2 else r_sS[i]
            if lv < 2:
                P.op("pool", lambda e: e.tensor_tensor(out=Q[:, 0:N], in0=S[:, 0:N], in1=S[:, 0:N], op=ALU.mult), reads=[r_sS[i]], writes=[r_sQ[i]])
            P.op("dve", lambda e, dst=dst: e.scalar_tensor_tensor(out=dst[:, 0:N], in0=S[:, 0:N], scalar=2.0, in1=C[:, 0:N], op0=ALU.mult, op1=ALU.mult),
                 reads=[r_sS[i], r_sC[i]], writes=[r_dst])
            if lv < 2:
                P.op("dve", lambda e: e.tensor_scalar(out=C[:, 0:N], in0=Q[:, 0:N], scalar1=-2.0, scalar2=1.0, op0=ALU.mult, op1=ALU.add),
                     reads=[r_sQ[i]], writes=[r_sC[i]])

    it = 0
    for seq, (L, ztd, KD) in enumerate(((n_lat, ztl, KDl), (n_ctx, ztc, KDc))):
        N = min(512, L)
        nblk = L // N
        for ps in range(2):
            for bl in range(nblk):
                i = it % 2
                it += 1
                P.dma("sp", zt[i][:, 0:N], ztd[ps, :, bl * N:(bl + 1) * N], writes=[r_zt[i]])
                b1, rb1 = nb()
                P.op("pe", lambda e: e.matmul(b1[0:64, 0:N], w1s[:, :], zt[i][:, 0:N], start=True, stop=True), reads=[r_par, r_zt[i]], writes=[rb1])
                P.op("dve", lambda e: e.tensor_scalar(out=hA[i][:, 0:N], in0=b1[0:64, 0:N], scalar1=cps[:, 0:1], scalar2=cps[:, 1:2], op0=ALU.add, op1=ALU.mult),
                     reads=[rb1, r_par], writes=[r_hA[i]])
                sin_big(hA[i], r_hA[i], N, i)
                b2, rb2 = nb()
                P.op("pe", lambda e: e.matmul(b2[0:64, 0:N], w2s[:, :], hA[i][:, 0:N], start=True, stop=True), reads=[r_par, r_hA[i]], writes=[rb2])
                P.op("dve", lambda e: e.tensor_scalar(out=hB[i][:, 0:N], in0=b2[0:64, 0:N], scalar1=cps[:, 2:3], scalar2=cps[:, 3:4], op0=ALU.add, op1=ALU.mult),
                     reads=[rb2, r_par], writes=[r_hB[i]])
                sin_big(hB[i], r_hB[i], N, i)
                b3, rb3 = nb()
                P.op("pe", lambda e: e.matmul(b3[:, 0:N], w3s[:, ps * 128:(ps + 1) * 128], hB[i][:, 0:N], start=True, stop=True), reads=[r_par, r_hB[i]], writes=[rb3])
                b4, rb4 = nb()
                P.op("pe", lambda e: e.matmul(b4[:, 0:N], decs[0:1, ps * 128:(ps + 1) * 128], zt[i][0:1, 0:N], start=True, stop=True), reads=[r_par, r_zt[i]], writes=[rb4])
                P.op("act", lambda e: e.activation(out=win[i][:, 0:N], in_=b4[:, 0:N], func=AF.Exp), reads=[rb4], writes=[r_win[i]])
                P.op("dve", lambda e: e.tensor_tensor(out=hw[i][:, 0:N], in0=b3[:, 0:N], in1=win[i][:, 0:N], op=ALU.mult), reads=[rb3, r_win[i]], writes=[r_hw[i]])
                if ps == 1 and bl == nblk - 1:
                    P.op("dve", lambda e: e.memset(hw[i][:, N - 1:N], 0.0), reads=[r_hw[i]], writes=[r_hw[i]])
                P.op("act", lambda e: e.activation(out=junk[i][:, 0:N], in_=hw[i][:, 0:N], func=AF.Abs, accum_out=acc[:, seq, ps * nblk + bl:ps * nblk + bl + 1]),
                     reads=[r_hw[i]], writes=[r_junk[i], r_acc])
                P.op("pool", lambda e: e.tensor_copy(out=hwb[i][:, 0:N], in_=hw[i][:, 0:N]), reads=[r_hw[i]], writes=[r_hwb[i]])
                if ps == 0:
                    q0 = L - 1 + bl * N
                    P.dma("sp", KD[:, q0:q0 + N], hwb[i][:, 0:N], reads=[r_hwb[i]], writes=[r_KD[seq]])
                else:
                    q0 = bl * N
                    nw = N - 1 if bl == nblk - 1 else N
                    P.dma("sp", KD[:, q0:q0 + nw], hwb[i][:, 0:nw], reads=[r_hwb[i]], writes=[r_KD[seq]])
    nrm = P.sb([128, 2], F32)
    r_nrm = Reg()
    dg = P.sb([128, 128], F32)
    r_dg = Reg()
    for seq in range(2):
        P.op("dve", lambda e: e.tensor_reduce(out=nrm[:, seq:seq + 1], in_=acc[:, seq, :], axis=AX.X, op=ALU.add), reads=[r_acc], writes=[r_nrm])
        P.op("dve", lambda e: e.tensor_scalar(out=dg[:], in0=identf[:], scalar1=nrm[:, seq:seq + 1], scalar2=None, op0=ALU.mult), reads=[r_nrm, r_idf], writes=[r_dg])
        b5, rb5 = nb()
        P.op("pe", lambda e: e.matmul(b5[:, 0:128], ones[:, :], dg[:, :], start=True, stop=True), reads=[r_ones, r_dg], writes=[rb5])
        P.op("dve", lambda e: e.reciprocal(out=RN[:, seq, :], in_=b5[:, 0:128]), reads=[rb5], writes=[r_RN])

    P.pop_scope([r_KD[0], r_KD[1], r_RN, r_SC])
    hsk = [P.sb([128, n_lat], BF16) for _ in range(2)]
    r_hsk = [Reg(), Reg()]
    zb = [P.sb([128, 128], BF16) for _ in range(2)]
    r_zb = [Reg(), Reg()]
    zr = [P.sb([128, 128], BF16) for _ in range(2)]
    r_zr = [Reg(), Reg()]
    tm = [P.sb([128, 128], F32) for _ in range(2)]
    r_tm = [Reg(), Reg()]
    hk = 0
    cv = 0
    for seq, (L, KD, W, j0) in enumerate(((n_lat, KDl, WL + 1, 0), (n_ctx, KDc, WC + 1, n_lat // 128))):
        NB = L // 128
        for ch in range(64):
            for o in range(2):
                c2 = cv % 2
                cv += 1
                row = o * 64 + ch
                src_c = (128 + ch) if o == 0 else ch
                Zs = SC[:, src_c, j0:j0 + NB]
                P.op("pool", lambda e: e.tensor_copy(out=zb[c2][:, 0:NB], in_=Zs), reads=[r_SC], writes=[r_zb[c2]])
                bz, rbz = nb()
                P.op("pe", lambda e: e.matmul(bz[:, 0:NB], J[:, :], zb[c2][:, 0:NB], start=True, stop=True), reads=[r_J, r_zb[c2]], writes=[rbz])
                P.op("act", lambda e: e.copy(out=zr[c2][:, 0:NB], in_=bz[:, 0:NB]), reads=[rbz], writes=[r_zr[c2]])
                yb_, r_yb = ybanks[c2]
                first = True
                for h in (1, 0):
                    hb_ = hk % 2
                    hk += 1
                    if h == 1:
                        x0, wd = L - 128, L
                        deltas = list(range(0, NB))
                    else:
                        x0, wd = 0, L - 128
                        deltas = list(range(-(NB - 1), 0))
                    if wd == 0:
                        continue
                    src = bass.AP(KD.tensor, row * W + x0, [[1, 128], [1, wd]])
                    P.dma("sp", hsk[hb_][:, 0:wd], src, reads=[r_KD[seq]], writes=[r_hsk[hb_]])
                    for di, d in enumerate(deltas):
                        xo = 128 * d + L - 128 - x0
                        lo_i, hi_i = max(0, d), NB + min(0, d)
                        lo_j, hi_j = max(0, -d), NB - max(0, d)
                        last = (h == 0 and di == len(deltas) - 1) or (NB == 1)
                        P.op("pe", lambda e, xo=xo, lo_i=lo_i, hi_i=hi_i, lo_j=lo_j, hi_j=hi_j, first=first, last=last: e.matmul(
                            yb_[:, lo_i:hi_i], hsk[hb_][:, xo:xo + 128], zr[c2][:, lo_j:hi_j], start=first, stop=last),
                            reads=[r_hsk[hb_], r_zr[c2]], writes=[r_yb], inc=(di == len(deltas) - 1))
                        first = False
                col = o * 64 + ch
                P.op("dve", lambda e: e.tensor_scalar(out=tm[c2][:, 0:NB], in0=yb_[:, 0:NB], scalar1=RN[:, seq, col:col + 1], scalar2=None, op0=ALU.mult),
                     reads=[r_yb, r_RN], writes=[r_tm[c2]])
                P.op("dve", lambda e: e.scalar_tensor_tensor(out=tm[c2][:, 0:NB], in0=Zs, scalar=hbs[:, col:col + 1], in1=tm[c2][:, 0:NB], op0=ALU.mult, op1=ALU.add),
                     reads=[r_SC, r_par, r_tm[c2]], writes=[r_tm[c2]])
                if o == 0:
                    P.op("dve", lambda e: e.tensor_tensor(out=SC[:, ch, j0:j0 + NB], in0=SC[:, ch, j0:j0 + NB], in1=tm[c2][:, 0:NB], op=ALU.mult),
                         reads=[r_SC, r_tm[c2]], writes=[r_SC])
                else:
                    P.op("dve", lambda e: e.tensor_tensor(out=SC[:, 64 + ch, j0:j0 + NB], in0=SC[:, 64 + ch, j0:j0 + NB], in1=tm[c2][:, 0:NB], op=ALU.mult),
                         reads=[r_SC, r_tm[c2]], writes=[r_SC])
    if stop <= 4:
        P.finish([])
        return P
    ot = [P.sb([128, 64], F32) for _ in range(2)]
    r_ot = [Reg(), Reg()]
    for t in range(NT):
        i = t % 2
        eng = "act" if t % 2 == 0 else "pool"
        if eng == "act":
            P.op("act", lambda e: e.copy(out=ot[i][:], in_=SC[:, 64:128, t]), reads=[r_SC], writes=[r_ot[i]])
        else:
            P.op("pool", lambda e: e.tensor_copy(out=ot[i][:], in_=SC[:, 64:128, t]), reads=[r_SC], writes=[r_ot[i]])
        P.dma("sp", hy[t * 128:(t + 1) * 128, :], ot[i][:], reads=[r_ot[i]], writes=[r_hy])
    P.finish([r_hy])
    return P


D = 1024
FF = 2816
ALPHA = float(4 ** 0.25)


def build_p2(n_lat=4096, n_ctx=64, moe=False):
    P = Prog()
    E = 8 if moe else 1
    ntok = n_lat + n_ctx
    mix = P.dram("mix", [ntok, D], F32, "ExternalInput")
    x = P.dram("x", [ntok, D], F32, "ExternalInput")
    cT = P.dram("cT", [128, 16], F32, "ExternalInput")
    adaw = P.dram("adaw", [D, 4096], F32, "ExternalInput")
    adab = P.dram("adab", [1, 4096], F32, "ExternalInput")
    wout = P.dram("wout", [D, D], F32, "ExternalInput")
    lnp = P.dram("lnp", [128, 4096], F32, "ExternalInput")
    w1 = P.dram("w1", [E, D, FF], F32, "ExternalInput")
    w3 = P.dram("w3", [E, D, FF], F32, "ExternalInput")
    w2 = P.dram("w2", [E, FF, D], F32, "ExternalInput")
    if moe:
        wr = P.dram("wr", [D, 8], F32, "ExternalInput")
    xo = P.dram("xo", [ntok, D], F32, "ExternalOutput")
    r_xo = Reg()

    fb = [P.ps([128, 512], F32) for _ in range(6)]
    tb = [P.ps([128, 8, 128], BF16) for _ in range(1)]
    tf = [P.ps([128, 4, 128], F32) for _ in range(1)]
    ones_row = P.sb([1, 128], F32)
    P.op("dve", lambda e: e.memset(ones_row[:], 1.0), writes=[Reg()])
    ident, r_id = make_ident(P)
    if moe:
        identf, r_idf = make_ident(P, F32)
        wrs = P.sb([128, 8, 8], F32)
        r_wrs = Reg()
        P.dma("sp", wrs[:], wr.rearrange("(k p) e -> p k e", p=128), writes=[r_wrs])
    wob = P.sb([128, 8, D], BF16)
    r_wob = Reg()
    for k in range(8):
        P.dma("pool", wob[:, k, :], wout[k * 128:(k + 1) * 128, :], writes=[r_wob])
    lns = P.sb([128, 4096], F32)
    r_lns = Reg()
    P.dma("sp", lns[:], lnp[:, :], writes=[r_lns])
    bcA, r_bcA = mods_block(P, cT, adaw[:, 0:2048], adab[:, 0:2048], 2048, ones_row, fb[:4], [])
    bcB, r_bcB = mods_block(P, cT, adaw[:, 2048:4096], adab[:, 2048:4096], 2048, ones_row, fb[:4], [(0, 1024)])

    tiles_all = [(i * 128, 128, 0) for i in range(n_lat // 128)]
    if n_ctx:
        tiles_all.append((n_lat, n_ctx, 1))
    SB = 4
    sblocks = [tiles_all[i:i + SB] for i in range(0, n_lat // 128, SB)]
    if n_ctx:
        sblocks[-1] = sblocks[-1] + [tiles_all[-1]]
    MT = SB + 1
    NTOK = SB * 128 + n_ctx

    def T(shape, dt=F32, n=2):
        return [P.sb(list(shape), dt) for _ in range(n)], [Reg() for _ in range(n)]
    xs, r_xs = T((128, D))
    ms, r_ms = T((128, D))
    mb, r_mb = T((128, D), BF16)
    mT, r_mT = T((128, 8, 128), BF16)
    yv, r_yv = T((128, D))
    tmp, r_tmp = T((128, D))
    stat, r_stat = T((128, 16))
    hb, r_hb = T((128, D), BF16)
    x1 = P.sb([128, MT, D], F32)
    r_x1 = [Reg() for _ in range(MT)]
    acc = P.sb([128, MT, D], F32)
    r_acc = [Reg() for _ in range(MT)]
    h2T = P.sb([128, 8, NTOK], BF16)
    r_h2T = Reg()
    if moe:
        hf, r_hf = T((128, D))
        hTf, r_hTf = T((128, 8, 128))
        lg, r_lg = T((128, 32))
        gates = P.sb([128, MT, 8], F32)
        r_gates = [Reg() for _ in range(MT)]
    UF = 2
    units = [(f0, min(UF, 22 - f0)) for f0 in range(0, 22, UF)]
    w1u, r_w1u = T((128, 8, UF * 128), BF16)
    w3u, r_w3u = T((128, 8, UF * 128), BF16)
    w2u, r_w2u = T((128, UF, D), BF16)
    GT, r_GT = T((128, UF, NTOK), BF16)
    sa, r_sa = T((128, 512))
    wq = [0]
    it = [0]

    for sbk in sblocks:
        ntk = sum(r for (_, r, _) in sbk)
        col = 0
        for ti, (t0, rows, s) in enumerate(sbk):
            i = it[0] % 2
            it[0] += 1
            P.dma("sp", ms[i][:rows, :], mix[t0:t0 + rows, :], writes=[r_ms[i]])
            P.dma("sp", xs[i][:rows, :], x[t0:t0 + rows, :], writes=[r_xs[i]])
            P.op("pool", lambda e: e.tensor_copy(out=mb[i][:rows, :], in_=ms[i][:rows, :]), reads=[r_ms[i]], writes=[r_mb[i]])
            tp, r_tp = tb[0]
            for k in range(8):
                P.op("pe", lambda e, k=k: e.transpose(tp[:, k, :rows], mb[i][:rows, k * 128:(k + 1) * 128], ident[:rows, :rows]),
                     reads=[r_mb[i], r_id], writes=[r_tp], inc=(k == 7))
            P.op("act", lambda e: e.copy(out=mT[i][:, :, :rows], in_=tp[:, :, :rows]), reads=[r_tp], writes=[r_mT[i]])
            P.op("act", lambda e: e.mul(out=yv[i][:rows, :], in_=xs[i][:rows, :], mul=ALPHA), reads=[r_xs[i]], writes=[r_yv[i]])
            for cb in range(2):
                bank, rb = fb[4 + cb]
                for k in range(8):
                    P.op("pe", lambda e, k=k, cb=cb, bank=bank: e.matmul(bank[:rows, :], mT[i][:, k, :rows], wob[:, k, cb * 512:(cb + 1) * 512],
                                                                   start=(k == 0), stop=(k == 7)), reads=[r_mT[i], r_wob], writes=[rb], inc=(k == 7))
                P.op("dve", lambda e, cb=cb, bank=bank: e.tensor_tensor(out=tmp[i][:rows, cb * 512:(cb + 1) * 512], in0=bank[:rows, :],
                                                                in1=bcA[s][:rows, cb * 512:(cb + 1) * 512], op=ALU.mult),
                     reads=[rb, r_bcA[s]], writes=[r_tmp[i]])
            P.op("pool", lambda e: e.tensor_tensor(out=yv[i][:rows, :], in0=yv[i][:rows, :], in1=tmp[i][:rows, :], op=ALU.add), reads=[r_yv[i], r_tmp[i]], writes=[r_yv[i]])
            ln_tile(P, rows, yv[i][:rows, :], r_yv[i], tmp[i], r_tmp[i], stat[i], r_stat[i])
            P.op("pool", lambda e: e.tensor_tensor(out=tmp[i][:rows, :], in0=tmp[i][:rows, :], in1=lns[:rows, 0:1024], op=ALU.mult), reads=[r_tmp[i], r_lns], writes=[r_tmp[i]])
            P.op("dve", lambda e: e.tensor_tensor(out=x1[:rows, ti, :], in0=tmp[i][:rows, :], in1=lns[:rows, 1024:2048], op=ALU.add), reads=[r_tmp[i], r_lns], writes=[r_x1[ti]])
            ln_tile(P, rows, x1[:rows, ti, :], r_x1[ti], tmp[i], r_tmp[i], stat[i], r_stat[i])
            P.op("pool", lambda e: e.tensor_tensor(out=tmp[i][:rows, :], in0=tmp[i][:rows, :], in1=bcB[s][:rows, 0:1024], op=ALU.mult), reads=[r_tmp[i], r_bcB[s]], writes=[r_tmp[i]])
            if moe:
                P.op("dve", lambda e: e.tensor_tensor(out=hf[i][:rows, :], in0=tmp[i][:rows, :], in1=bcA[s][:rows, 1024:2048], op=ALU.add), reads=[r_tmp[i], r_bcA[s]], writes=[r_hf[i]])
                P.op("pool", lambda e: e.tensor_copy(out=hb[i][:rows, :], in_=hf[i][:rows, :]), reads=[r_hf[i]], writes=[r_hb[i]])
            else:
                P.op("dve", lambda e: e.tensor_tensor(out=hb[i][:rows, :], in0=tmp[i][:rows, :], in1=bcA[s][:rows, 1024:2048], op=ALU.add), reads=[r_tmp[i], r_bcA[s]], writes=[r_hb[i]])
            for k in range(8):
                P.op("pe", lambda e, k=k: e.transpose(tp[:, k, :rows], hb[i][:rows, k * 128:(k + 1) * 128], ident[:rows, :rows]),
                     reads=[r_hb[i], r_id], writes=[r_tp], inc=(k == 7))
            P.op("act", lambda e, col=col: e.copy(out=h2T[:, :, col:col + rows], in_=tp[:, :, :rows]), reads=[r_tp], writes=[r_h2T])
            if moe:
                tq, r_tq = tf[0]
                for half in range(2):
                    for k in range(4):
                        kk = half * 4 + k
                        P.op("pe", lambda e, k=k, kk=kk: e.transpose(tq[:, k, :rows], hf[i][:rows, kk * 128:(kk + 1) * 128], identf[:rows, :rows]),
                             reads=[r_hf[i], r_idf], writes=[r_tq], inc=(k == 3))
                    P.op("dve", lambda e, half=half: e.tensor_copy(out=hTf[i][:, half * 4:half * 4 + 4, :rows], in_=tq[:, :, :rows]), reads=[r_tq], writes=[r_hTf[i]])
                bank, rb = fb[4]
                for k in range(8):
                    P.op("pe", lambda e, k=k, bank=bank: e.matmul(bank[:rows, 0:8], hTf[i][:, k, :rows], wrs[:, k, :], start=(k == 0), stop=(k == 7)),
                         reads=[r_hTf[i], r_wrs], writes=[rb], inc=(k == 7))
                L = lg[i]
                rl = r_lg[i]
                P.op("dve", lambda e, bank=bank: e.tensor_copy(out=L[:rows, 0:8], in_=bank[:rows, 0:8]), reads=[rb], writes=[rl])
                P.op("dve", lambda e: e.tensor_reduce(out=L[:rows, 24:25], in_=L[:rows, 0:8], axis=AX.X, op=ALU.max), reads=[rl], writes=[rl])
                P.op("dve", lambda e: e.tensor_scalar(out=L[:rows, 8:16], in0=L[:rows, 0:8], scalar1=L[:rows, 24:25], scalar2=None, op0=ALU.is_equal), reads=[rl], writes=[rl])
                P.op("dve", lambda e: e.scalar_tensor_tensor(out=L[:rows, 16:24], in0=L[:rows, 8:16], scalar=-1e30, in1=L[:rows, 0:8], op0=ALU.mult, op1=ALU.add), reads=[rl], writes=[rl])
                P.op("dve", lambda e: e.tensor_reduce(out=L[:rows, 25:26], in_=L[:rows, 16:24], axis=AX.X, op=ALU.max), reads=[rl], writes=[rl])
                P.op("dve", lambda e: e.tensor_scalar(out=L[:rows, 16:24], in0=L[:rows, 16:24], scalar1=L[:rows, 25:26], scalar2=None, op0=ALU.is_equal), reads=[rl], writes=[rl])
                P.op("dve", lambda e: e.tensor_tensor(out=L[:rows, 26:27], in0=L[:rows, 25:26], in1=L[:rows, 24:25], op=ALU.subtract), reads=[rl], writes=[rl])
                P.op("act", lambda e: e.activation(out=L[:rows, 26:27], in_=L[:rows, 26:27], func=AF.Exp), reads=[rl], writes=[rl])
                P.op("dve", lambda e: e.tensor_scalar_add(out=L[:rows, 27:28], in0=L[:rows, 26:27], scalar1=1.0), reads=[rl], writes=[rl])
                P.op("dve", lambda e: e.reciprocal(out=L[:rows, 27:28], in_=L[:rows, 27:28]), reads=[rl], writes=[rl])
                P.op("dve", lambda e: e.tensor_tensor(out=L[:rows, 28:29], in0=L[:rows, 26:27], in1=L[:rows, 27:28], op=ALU.mult), reads=[rl], writes=[rl])
                P.op("dve", lambda e: e.tensor_scalar(out=L[:rows, 8:16], in0=L[:rows, 8:16], scalar1=L[:rows, 27:28], scalar2=None, op0=ALU.mult), reads=[rl], writes=[rl])
                P.op("dve", lambda e, ti=ti: e.scalar_tensor_tensor(out=gates[:rows, ti, :], in0=L[:rows, 16:24], scalar=L[:rows, 28:29], in1=L[:rows, 8:16],
                                                                    op0=ALU.mult, op1=ALU.add), reads=[rl], writes=[r_gates[ti]])
            col += rows
        tblocks = []
        c0 = 0
        while c0 < ntk:
            n = min(512, ntk - c0)
            tblocks.append((c0, n))
            c0 += n
        for e_ in range(E):
            for (f0, nf) in units:
                q = wq[0] % 2
                wq[0] += 1
                for k in range(8):
                    P.dma("pool", w1u[q][:, k, 0:nf * 128], w1[e_, k * 128:(k + 1) * 128, f0 * 128:(f0 + nf) * 128], writes=[r_w1u[q]])
                    P.dma("pool", w3u[q][:, k, 0:nf * 128], w3[e_, k * 128:(k + 1) * 128, f0 * 128:(f0 + nf) * 128], writes=[r_w3u[q]])
                for f in range(nf):
                    P.dma("pool", w2u[q][:, f, :], w2[e_, (f0 + f) * 128:(f0 + f + 1) * 128, :], writes=[r_w2u[q]])
                for (c0, n) in tblocks:
                    for f in range(nf):
                        ba, rba = fb[0 + (f % 2) * 2]
                        bb, rbb = fb[1 + (f % 2) * 2]
                        for k in range(8):
                            P.op("pe", lambda e, k=k, f=f, ba=ba: e.matmul(ba[:, 0:n], w1u[q][:, k, f * 128:(f + 1) * 128], h2T[:, k, c0:c0 + n],
                                                                     start=(k == 0), stop=(k == 7)), reads=[r_w1u[q], r_h2T], writes=[rba], inc=(k == 7))
                        for k in range(8):
                            P.op("pe", lambda e, k=k, f=f, bb=bb: e.matmul(bb[:, 0:n], w3u[q][:, k, f * 128:(f + 1) * 128], h2T[:, k, c0:c0 + n],
                                                                     start=(k == 0), stop=(k == 7)), reads=[r_w3u[q], r_h2T], writes=[rbb], inc=(k == 7))
                        j = f % 2
                        P.op("act", lambda e, ba=ba, j=j: e.activation(out=sa[j][:, 0:n], in_=ba[:, 0:n], func=AF.Silu), reads=[rba], writes=[r_sa[j]])
                        P.op("dve", lambda e, bb=bb, j=j, f=f: e.tensor_tensor(out=GT[q][:, f, c0:c0 + n], in0=bb[:, 0:n], in1=sa[j][:, 0:n], op=ALU.mult),
                             reads=[rbb, r_sa[j]], writes=[r_GT[q]])
                col = 0
                for ti, (t0, rows, s) in enumerate(sbk):
                    for cb in range(2):
                        bank, rb = fb[4 + cb]
                        for f in range(nf):
                            P.op("pe", lambda e, f=f, cb=cb, bank=bank, col=col: e.matmul(bank[:rows, :], GT[q][:, f, col:col + rows], w2u[q][:, f, cb * 512:(cb + 1) * 512],
                                                                                  start=(f == 0), stop=(f == nf - 1)),
                                 reads=[r_GT[q], r_w2u[q]], writes=[rb], inc=(f == nf - 1))
                        first = (e_ == 0 and f0 == 0)
                        dst = acc[:rows, ti, cb * 512:(cb + 1) * 512]
                        if moe:
                            gsc = gates[:rows, ti, e_:e_ + 1]
                            if first:
                                P.op("dve", lambda e, bank=bank, dst=dst, gsc=gsc: e.tensor_scalar(out=dst, in0=bank[:rows, :], scalar1=gsc, scalar2=None, op0=ALU.mult),
                                     reads=[rb, r_gates[ti]], writes=[r_acc[ti]])
                            else:
                                P.op("dve", lambda e, bank=bank, dst=dst, gsc=gsc: e.scalar_tensor_tensor(out=dst, in0=bank[:rows, :], scalar=gsc, in1=dst, op0=ALU.mult, op1=ALU.add),
                                     reads=[rb, r_gates[ti], r_acc[ti]], writes=[r_acc[ti]])
                        else:
                            if first:
                                P.op("act", lambda e, bank=bank, dst=dst: e.copy(out=dst, in_=bank[:rows, :]), reads=[rb], writes=[r_acc[ti]])
                            else:
                                P.op("dve", lambda e, bank=bank, dst=dst: e.tensor_tensor(out=dst, in0=bank[:rows, :], in1=dst, op=ALU.add),
                                     reads=[rb, r_acc[ti]], writes=[r_acc[ti]])
                    col += rows
        for ti, (t0, rows, s) in enumerate(sbk):
            i = it[0] % 2
            it[0] += 1
            P.op("pool", lambda e: e.tensor_tensor(out=acc[:rows, ti, :], in0=acc[:rows, ti, :], in1=bcB[s][:rows, 1024:2048], op=ALU.mult), reads=[r_acc[ti], r_bcB[s]], writes=[r_acc[ti]])
            P.op("dve", lambda e: e.scalar_tensor_tensor(out=yv[i][:rows, :], in0=x1[:rows, ti, :], scalar=ALPHA, in1=acc[:rows, ti, :], op0=ALU.mult, op1=ALU.add),
                 reads=[r_x1[ti], r_acc[ti]], writes=[r_yv[i]])
            ln_tile(P, rows, yv[i][:rows, :], r_yv[i], tmp[i], r_tmp[i], stat[i], r_stat[i])
            P.op("pool", lambda e: e.tensor_tensor(out=tmp[i][:rows, :], in0=tmp[i][:rows, :], in1=lns[:rows, 2048:3072], op=ALU.mult), reads=[r_tmp[i], r_lns], writes=[r_tmp[i]])
            P.op("dve", lambda e: e.tensor_tensor(out=yv[i][:rows, :], in0=tmp[i][:rows, :], in1=lns[:rows, 3072:4096], op=ALU.add), reads=[r_tmp[i], r_lns], writes=[r_yv[i]])
            P.dma("sp", xo[t0:t0 + rows, :], yv[i][:rows, :], reads=[r_yv[i]], writes=[r_xo])
    P.finish([r_xo])
    return P


def a2_consts():
    i = np.arange(128); half = i // 64
    same = half[:, None] == half[None, :]
    MST = (same & np.where(half[:, None] == 0, i[:, None] < i[None, :], i[:, None] > i[None, :])).astype(np.float32)
    MIT = MST + np.eye(128, dtype=np.float32)
    MS = np.ascontiguousarray(MST.T)
    LM = np.zeros((128, 96), np.float32)
    LM[:64, 0:16] = 1; LM[64:, 16:32] = 1; LM[:64, 32:48] = 1; LM[64:, 48:64] = 1; LM[:, 64:96] = 1
    return np.concatenate([MIT, MST, MIT, MS, LM], 1).astype(np.float32)

def a2_inputs(ur, n_lat, n_ctx, hd, mu, w0, wB, a0, aB, gB, kkw, ka, rk, gng, gnb):
    C = 256
    cols = np.concatenate([np.arange(hd * 64, hd * 64 + 64), C + np.arange(hd * 64, hd * 64 + 64), 2 * C + np.arange(hd * 64, hd * 64 + 64),
                           np.arange(768, 864)])
    u = ur[:, cols]
    def shifted(x, d):
        o = np.zeros_like(x)
        if d == 1: o[1:] = x[:-1]
        else: o[:-1] = x[1:]
        return o
    prev = np.concatenate([shifted(u[:n_lat], 1), shifted(u[n_lat:], 1)], 0)
    nxt = np.concatenate([shifted(u[:n_lat], -1), shifted(u[n_lat:], -1)], 0)
    u3 = np.concatenate([u, prev, nxt], 1)
    fwd, bwd = rwkv_orders(n_lat, n_ctx)
    idx = np.concatenate([np.concatenate([np.arange(f, f + 64), np.arange(b, b + 64)]) for f, b in zip(fwd, bwd)])
    U3 = np.ascontiguousarray(u3[idx])
    coefmu = np.tile(np.concatenate([mu[0][cols], mu[1][cols]])[None], (128, 1)).astype(np.float32)
    hs = slice(hd * 64, hd * 64 + 64)
    rowp = np.zeros((128, 128), np.float32)
    rowp[:64, :64] = w0[0][hs]; rowp[64:, :64] = w0[1][hs]; rowp[:64, 64:] = a0[0][hs]; rowp[64:, 64:] = a0[1][hs]
    hv = np.tile(np.concatenate([kkw[hs], ka[hs], rk[hd], gng[hs], gnb[hs]])[None], (128, 1)).astype(np.float32)
    Wl = np.zeros((96, 192), np.float32)
    Wl[0:16, 0:64] = wB[0][:, hs]; Wl[16:32, 0:64] = wB[1][:, hs]; Wl[32:48, 64:128] = aB[0][:, hs]; Wl[48:64, 64:128] = aB[1][:, hs]
    Wl[64:96, 128:192] = gB[:, hs]
    return dict(U3=U3, coefmu=coefmu, rowp=rowp, hv=hv, Wl=Wl, cmask=a2_consts())


def hy_ztab(L):
    bands = 16
    t = np.linspace(0.0, 1.0, L, dtype=np.float32)[:, None]
    f = np.linspace(1e-4, bands - 1, bands, dtype=np.float32)[None, :]
    wt = (np.float32(2.0 * math.pi) * np.arange(L, dtype=np.float32)[:, None] / np.float32(L)).astype(np.float32)
    z = np.concatenate([t, np.cos(f * wt), -np.sin(f * wt)], -1).astype(np.float32)
    return np.ascontiguousarray(np.stack([z.T, z[::-1].T], 0))

def a3_inputs(uh, n_lat, n_ctx, j, sw, sb, w1, b1, f1, w2, b2, f2, w3, dec, hbias):
    cs = slice(j * 64, j * 64 + 64)
    cols = np.concatenate([np.arange(256)[cs], 256 + np.arange(256)[cs], 512 + np.arange(256)[cs]])
    u = uh[:, cols]
    def shifted(x, d):
        o = np.zeros_like(x)
        if d == 1: o[1:] = x[:-1]
        else: o[:-1] = x[1:]
        return o
    prev = np.concatenate([shifted(u[:n_lat], 1), shifted(u[n_lat:], 1)], 0)
    nxt = np.concatenate([shifted(u[:n_lat], -1), shifted(u[n_lat:], -1)], 0)
    H3 = np.ascontiguousarray(np.concatenate([u, prev, nxt], 1), dtype=np.float32)
    cw = np.tile(np.concatenate([sw[1][cols], sw[0][cols], sw[2][cols], sb[cols]])[None], (128, 1)).astype(np.float32)
    fc = np.array([o * 512 + d * 256 + j * 64 + c for d in range(2) for o in range(2) for c in range(64)])
    colp = np.stack([b1, f1, b2, f2], 1).astype(np.float32)
    hb = np.tile(np.concatenate([hbias[0][cs], hbias[1][cs]])[None], (128, 1)).astype(np.float32)
    return dict(H3=H3, cw=cw, ztl=hy_ztab(n_lat), ztc=hy_ztab(n_ctx), w1=np.ascontiguousarray(w1, dtype=np.float32),
                w2=np.ascontiguousarray(w2, dtype=np.float32), w3=np.ascontiguousarray(w3[:, fc], dtype=np.float32), colp=colp,
                dec=np.ascontiguousarray(dec[fc][None], dtype=np.float32), hb=hb)


N_LAT = 16384
N_CTX = 256
_PROGS = {}


def _prog(name):
    if name not in _PROGS:
        if name == "p1":
            P = build_p1(4096, 64)
        elif name == "a1":
            P = build_a1(N_LAT, N_CTX)
        elif name == "a2":
            P = build_a2(N_LAT, N_CTX)
        elif name == "a3":
            P = build_a3(N_LAT, N_CTX)
        elif name == "p2d":
            P = build_p2(4096, 64, False)
        elif name == "p2m":
            P = build_p2(4096, 64, True)
        _PROGS[name] = P.close()
    return _PROGS[name]


def _run(name, in_maps, out_name):
    nc = _prog(name)
    in_maps = [{k: np.ascontiguousarray(v, dtype=np.float32) for k, v in m.items()} for m in in_maps]
    res = run_bass_kernel_spmd(nc, in_maps, core_ids=list(range(8)))
    return [np.asarray(r[out_name]) for r in res.results]


def _rope_cs(L):
    rows = L // 64
    row = np.repeat(np.arange(rows, dtype=np.float32), 64)
    col = np.tile(np.arange(64, dtype=np.float32), rows)
    inv = (np.float32(10000.0) ** (-np.arange(16, dtype=np.float32) / np.float32(16))).astype(np.float32)
    ang = np.concatenate([row[:, None] * inv, col[:, None] * inv], -1).astype(np.float32)
    cos, sin = np.cos(ang).astype(np.float32), np.sin(ang).astype(np.float32)
    return np.concatenate([cos, cos, cos, sin, sin, sin], -1).astype(np.float32)


def _cT(cb, cctx):
    c2 = np.stack([cb, cctx], 0).astype(np.float32)
    return np.ascontiguousarray(c2.reshape(2, 8, 128).transpose(2, 0, 1).reshape(128, 16))


def kernel(x, c, ctx, c_ctx, ada_w, ada_b, w_in, w_out, q_gain, k_gain,
           rwkv_mu, rwkv_w0, rwkv_wB, rwkv_a0, rwkv_aB, rwkv_gB, rwkv_kk, rwkv_ka, rwkv_rk,
           rwkv_gn_g, rwkv_gn_b, hy_short_w, hy_short_b, hy_w1, hy_b1, hy_freq1, hy_w2, hy_b2,
           hy_freq2, hy_w3, hy_decay, hy_bias, ln1_g, ln1_b, ln2_g, ln2_b,
           ffn_w1, ffn_w3, ffn_w2, moe_router, moe_w1, moe_w3, moe_w2):
    f32 = lambda a: np.asarray(a, dtype=np.float32)
    x = f32(x).copy()
    xc = f32(ctx).copy()
    c, c_ctx = f32(c), f32(c_ctx)
    cs = _rope_cs(N_LAT)
    depth = 2
    for l in range(depth):
        aw, ab = f32(ada_w[l]), f32(ada_b[l])
        cores = [(b, q) for b in range(2) for q in range(4)]
        ins = []
        for (b, q) in cores:
            xt = np.concatenate([x[b, q * 4096:(q + 1) * 4096], xc[b, q * 64:(q + 1) * 64]], 0)
            ins.append(dict(x=xt, cT=_cT(c[b], c_ctx), adaw=aw[:, 0:2048], adab=ab[None, 0:2048], win=f32(w_in[l])))
        us = _run("p1", ins, "u")
        u = np.empty((2, N_LAT + N_CTX, 2400), np.float32)
        for (b, q), uu in zip(cores, us):
            u[b, q * 4096:(q + 1) * 4096] = uu[:4096]
            u[b, N_LAT + q * 64:N_LAT + (q + 1) * 64] = uu[4096:]
        mixo = np.empty((2, N_LAT + N_CTX, 1024), np.float32)
        gains = np.tile(np.concatenate([f32(q_gain[l]), f32(q_gain[l]), f32(k_gain[l])])[None], (128, 1))
        ins = []
        for (b, j) in cores:
            g = j // 2
            qk = np.concatenate([u[b][:, 128 * j:128 * j + 128], u[b][:, 512 + 64 * g:512 + 64 * g + 64]], 1)
            ins.append(dict(qk=qk, v=u[b][:, 640 + 64 * g:640 + 64 * g + 64], gains=gains, cs=cs))
        for (b, j), o in zip(cores, _run("a1", ins, "att")):
            mixo[b][:, 128 * j:128 * j + 128] = o
        ins = []
        for (b, j) in cores:
            ins.append(a2_inputs(u[b][:, 768:1632], N_LAT, N_CTX, j, f32(rwkv_mu[l]), f32(rwkv_w0[l]), f32(rwkv_wB[l]), f32(rwkv_a0[l]),
                                 f32(rwkv_aB[l]), f32(rwkv_gB[l]), f32(rwkv_kk[l]), f32(rwkv_ka[l]), f32(rwkv_rk[l]),
                                 f32(rwkv_gn_g[l]), f32(rwkv_gn_b[l])))
        for (b, j), o in zip(cores, _run("a2", ins, "rw")):
            mixo[b][:, 512 + 64 * j:512 + 64 * j + 64] = o
        ins = []
        for (b, j) in cores:
            ins.append(a3_inputs(u[b][:, 1632:2400], N_LAT, N_CTX, j, f32(hy_short_w[l]), f32(hy_short_b[l]), f32(hy_w1[l]), f32(hy_b1[l]),
                                 f32(hy_freq1[l]), f32(hy_w2[l]), f32(hy_b2[l]), f32(hy_freq2[l]), f32(hy_w3[l]), f32(hy_decay[l]),
                                 f32(hy_bias[l])))
        for (b, j), o in zip(cores, _run("a3", ins, "hy")):
            mixo[b][:, 768 + 64 * j:768 + 64 * j + 64] = o
        lnp = np.tile(np.concatenate([f32(ln1_g[l]), f32(ln1_b[l]), f32(ln2_g[l]), f32(ln2_b[l])])[None], (128, 1))
        jj = l // 2
        ins = []
        for (b, q) in cores:
            mt = np.concatenate([mixo[b, q * 4096:(q + 1) * 4096], mixo[b, N_LAT + q * 64:N_LAT + (q + 1) * 64]], 0)
            xt = np.concatenate([x[b, q * 4096:(q + 1) * 4096], xc[b, q * 64:(q + 1) * 64]], 0)
            d = dict(mix=mt, x=xt, cT=_cT(c[b], c_ctx), adaw=aw[:, 2048:6144], adab=ab[None, 2048:6144], wout=f32(w_out[l]), lnp=lnp)
            if l % 2 == 0:
                d.update(w1=f32(ffn_w1[jj])[None], w3=f32(ffn_w3[jj])[None], w2=f32(ffn_w2[jj])[None])
            else:
                d.update(w1=f32(moe_w1[jj]), w3=f32(moe_w3[jj]), w2=f32(moe_w2[jj]), wr=f32(moe_router[jj]))
            ins.append(d)
        outs = _run("p2d" if l % 2 == 0 else "p2m", ins, "xo")
        xn = np.empty_like(x)
        xcn = np.empty_like(xc)
        for (b, q), o in zip(cores, outs):
            xn[b, q * 4096:(q + 1) * 4096] = o[:4096]
            xcn[b, q * 64:(q + 1) * 64] = o[4096:]
        x, xc = xn, xcn
    return x
```

```python
import math


import numpy as np
from contextlib import ExitStack
import concourse.bass as bass
import concourse.mybir as mybir
from concourse.bass_utils import run_bass_kernel_spmd

F32 = mybir.dt.float32
BF16 = mybir.dt.bfloat16
AF = mybir.ActivationFunctionType
ALU = mybir.AluOpType
AX = mybir.AxisListType
NDS = 24


class Reg:
    __slots__ = ("w", "r", "name", "excl")

    def __init__(self, name="", excl=False):
        self.w = []
        self.r = {}
        self.name = name
        self.excl = excl


class _EngRec:
    def __init__(self):
        self.call = None

    def __getattr__(self, name):
        def f(*a, **k):
            self.call = (name, a, k)
        return f


class Prog:
    def __init__(self):
        self.nc = bass.Bass("TRN2", target_bir_lowering=False)
        self.es = ExitStack()
        nc = self.nc
        self.eng = {"pe": nc.tensor, "act": nc.scalar, "dve": nc.vector, "pool": nc.gpsimd, "sp": nc.sync}
        self.sem = {k: self.es.enter_context(nc.semaphore("s_" + k)) for k in self.eng}
        self.seq = {k: 0 for k in self.eng}
        self.known = {k: {} for k in self.eng}
        self.pend = {k: ([], []) for k in self.eng}
        self.dsem = [self.es.enter_context(nc.semaphore("d%d" % i)) for i in range(NDS)]
        self.dval = [0] * NDS
        self.dnext = 0
        self.ninst = 0
        self._n = 0
        self.scopes = []
        self.ccsem = None
        self.rec = None

    def sb(self, shape, dt, name=None):
        self._n += 1
        es = self.scopes[-1] if self.scopes else self.es
        return es.enter_context(self.nc.sbuf_tensor(name or "t%d" % self._n, list(shape), dt))

    def push_scope(self):
        self.scopes.append(ExitStack())

    def pop_scope(self, regs):
        for r in regs:
            for e in self.eng:
                for t in r.w:
                    self._wait1(e, t)
        self.scopes.pop().close()

    def ps(self, shape, dt, name=None):
        self._n += 1
        nbytes = int(np.prod(shape[1:])) * (4 if dt == F32 else 2)
        assert nbytes % 2048 == 0, "PSUM tensors must be whole banks"
        es = self.scopes[-1] if self.scopes else self.es
        return es.enter_context(self.nc.psum_tensor(name or "p%d" % self._n, list(shape), dt)), Reg(excl=True)

    def dram(self, name, shape, dt, kind):
        return self.nc.dram_tensor(name, list(shape), dt, kind=kind).ap()

    def _wait1(self, e, tok):
        key, sem, val = tok
        if self.known[e].get(key, 0) < val:
            self.eng[e].wait_ge(sem, val)
            self.known[e][key] = val
            self.ninst += 1

    def _deps(self, e, reads, writes, is_dma):
        for r in reads:
            for t in r.w:
                if t[0] == e and e == "pe" and not is_dma:
                    continue
                self._wait1(e, t)
            if r.excl:
                for k, t in r.r.items():
                    if k != e or is_dma:
                        self._wait1(e, t)
        for w in writes:
            for t in w.w:
                if not (t[0] == e and not is_dma):
                    self._wait1(e, t)
            for k, t in w.r.items():
                if k == e and not is_dma:
                    continue
                self._wait1(e, t)

    def op(self, e, fn, reads=(), writes=(), inc=True):
        if self.rec is not None:
            prox = _EngRec()
            fn(prox)
            name, a, k = prox.call
            self.rec.append(("op", e, (lambda eng, name=name, a=a, k=k: getattr(eng, name)(*a, **k)), tuple(reads), tuple(writes), inc))
            return None
        self._deps(e, reads, writes, False)
        inst = fn(self.eng[e])
        self.ninst += 1
        pr, pw = self.pend[e]
        pr.extend(reads)
        pw.extend(writes)
        if inc:
            self.seq[e] += 1
            inst.then_inc(self.sem[e], 1)
            tok = (e, self.sem[e], self.seq[e])
            for r in pr:
                r.r[e] = tok
            for w in pw:
                w.w = [tok]
                w.r = {}
            self.pend[e] = ([], [])
        return inst

    def dma(self, q, out, in_, reads=(), writes=(), **kw):
        if self.rec is not None:
            self.rec.append(("dma", q, out, in_, tuple(reads), tuple(writes), kw))
            return None
        self._deps(q, reads, writes, True)
        slot = self.dnext
        self.dnext = (slot + 1) % NDS
        key = ("d", slot)
        if self.dval[slot] > 0:
            self._wait1(q, (key, self.dsem[slot], self.dval[slot]))
        inst = self.eng[q].dma_start(out=out, in_=in_, **kw)
        self.ninst += 1
        self.dval[slot] += 16
        inst.then_inc(self.dsem[slot], 16)
        tok = (key, self.dsem[slot], self.dval[slot])
        for r in reads:
            r.r[key] = tok
        for w in writes:
            w.w = [t for t in w.w if isinstance(t[0], tuple) and t[0] != key] + [tok]
            w.r = {}
        return inst

    def allgather(self, out, in_, groups, reads=(), writes=()):
        q = "pool"
        self._deps(q, reads, writes, True)
        if self.ccsem is None:
            self.ccsem = self.es.enter_context(self.nc.semaphore("ccsem"))
            self.ccval = 0
        inst = self.eng[q].collective_compute("AllGather", ALU.bypass, replica_groups=groups, ins=[in_], outs=[out])
        self.ninst += 1
        self.ccval += 1
        inst.then_inc(self.ccsem, 1)
        tok = ("cc", self.ccsem, self.ccval)
        for r in reads:
            r.r["cc"] = tok
        for w in writes:
            w.w = [tok]
            w.r = {}
        return inst

    def record(self):
        self.rec = []

    def stop_record(self):
        r, self.rec = self.rec, None
        return r

    def replay_interleaved(self, lists):
        n = max(len(l) for l in lists)
        for i in range(n):
            for l in lists:
                if i < len(l):
                    it = l[i]
                    if it[0] == "op":
                        self.op(it[1], it[2], it[3], it[4], it[5])
                    else:
                        self.dma(it[1], it[2], it[3], it[4], it[5], **it[6])

    def replay_skewed(self, lists, nact):
        L = max(len(l) for l in lists)
        D = -(-L // nact)
        T = (len(lists) - 1) * D + L
        for t in range(T):
            c0 = max(0, (t - L) // D)
            for c in range(c0, min(len(lists), t // D + 1)):
                k = t - c * D
                l = lists[c]
                if 0 <= k < len(l):
                    it = l[k]
                    if it[0] == "op":
                        self.op(it[1], it[2], it[3], it[4], it[5])
                    else:
                        self.dma(it[1], it[2], it[3], it[4], it[5], **it[6])

    def finish(self, regs):
        for r in regs:
            for t in r.w:
                self._wait1("sp", t)
        for slot in range(NDS):
            if self.dval[slot] > 0:
                self._wait1("sp", (("d", slot), self.dsem[slot], self.dval[slot]))

    def close(self):
        self.es.close()
        return self.nc


D = 1024
INW = 2400
LN_EPS = 1e-6


def mods_block(P, cT, adaw, adab, ncol, ones_row, psum_banks, plus_one_ranges):
    ncb = ncol // 512
    assert ncb <= len(psum_banks)
    bc = [P.sb([128, ncol], F32) for _ in range(2)]
    r_bc = [Reg(), Reg()]
    P.push_scope()
    c_sb = P.sb([128, 16], F32)
    cs_sb = P.sb([128, 16], F32)
    r_c = Reg()
    r_cs = Reg()
    P.dma("sp", c_sb[:], cT[:, :], writes=[r_c])
    P.op("act", lambda e: e.activation(out=cs_sb[:], in_=c_sb[:], func=AF.Silu), reads=[r_c], writes=[r_cs])
    ab_sb = P.sb([1, ncol], F32)
    r_ab = Reg()
    P.dma("sp", ab_sb[:], adab[:, :], writes=[r_ab])
    aw = [P.sb([128, ncol], F32) for _ in range(2)]
    r_aw = [Reg(), Reg()]
    modrow = P.sb([1, ncol], F32)
    r_mr = Reg()
    it = 0
    for s in range(2):
        for k in range(8):
            b = it % 2
            it += 1
            P.dma("sp", aw[b][:], adaw[k * 128:(k + 1) * 128, :], writes=[r_aw[b]])
            for cb in range(ncb):
                bank, rb = psum_banks[cb]
                P.op("pe", lambda e, s=s, cb=cb, bank=bank, k=k, b=b: e.matmul(
                    bank[0:1, 0:512], cs_sb[:, s * 8 + k:s * 8 + k + 1], aw[b][:, cb * 512:(cb + 1) * 512],
                    start=(k == 0), stop=(k == 7)),
                    reads=[r_cs, r_aw[b]], writes=[rb], inc=(cb == ncb - 1))
        for cb in range(ncb):
            bank, rb = psum_banks[cb]
            P.op("dve", lambda e, cb=cb, bank=bank: e.tensor_tensor(
                out=modrow[0:1, cb * 512:(cb + 1) * 512], in0=bank[0:1, 0:512],
                in1=ab_sb[0:1, cb * 512:(cb + 1) * 512], op=ALU.add),
                reads=[rb, r_ab], writes=[r_mr])
        for (a, b2) in plus_one_ranges:
            P.op("dve", lambda e, a=a, b2=b2: e.tensor_scalar_add(out=modrow[0:1, a:b2], in0=modrow[0:1, a:b2], scalar1=1.0),
                 reads=[r_mr], writes=[r_mr])
        for cb in range(ncb):
            bank, rb = psum_banks[cb]
            P.op("pe", lambda e, cb=cb, bank=bank: e.matmul(
                bank[:, 0:512], ones_row[0:1, 0:128], modrow[0:1, cb * 512:(cb + 1) * 512], start=True, stop=True),
                reads=[r_mr], writes=[rb])
            P.op("act", lambda e, s=s, cb=cb, bank=bank: e.copy(out=bc[s][:, cb * 512:(cb + 1) * 512], in_=bank[:, 0:512]),
                 reads=[rb], writes=[r_bc[s]])
    P.pop_scope([r_bc[1]])
    return bc, r_bc


def ln_tile(P, rows, x_ap, r_x, tmp, r_tmp, stat, r_stat):
    st = stat
    P.op("dve", lambda e: e.bn_stats(out=st[:rows, 0:6], in_=x_ap[:, 0:512]), reads=[r_x], writes=[r_stat])
    P.op("dve", lambda e: e.bn_stats(out=st[:rows, 6:12], in_=x_ap[:, 512:1024]), reads=[r_x], writes=[r_stat])
    P.op("dve", lambda e: e.bn_aggr(out=st[:rows, 12:14], in_=st[:rows, 0:12]), reads=[r_stat], writes=[r_stat])
    P.op("dve", lambda e: e.tensor_scalar_add(out=st[:rows, 15:16], in0=st[:rows, 13:14], scalar1=LN_EPS), reads=[r_stat], writes=[r_stat])
    P.op("act", lambda e: e.activation(out=st[:rows, 14:15], in_=st[:rows, 15:16], func=AF.Sqrt), reads=[r_stat], writes=[r_stat])
    P.op("dve", lambda e: e.reciprocal(out=st[:rows, 14:15], in_=st[:rows, 14:15]), reads=[r_stat], writes=[r_stat])
    P.op("dve", lambda e: e.tensor_scalar(out=tmp[:rows, :], in0=x_ap, scalar1=st[:rows, 12:13], scalar2=st[:rows, 14:15],
                                          op0=ALU.subtract, op1=ALU.mult), reads=[r_x, r_stat], writes=[r_tmp])


def make_ident(P, dt=BF16):
    ident = P.sb([128, 128], dt)
    r_id = Reg()
    P.op("pool", lambda e: e.memset(ident[:], 0.0), writes=[r_id])
    P.op("pool", lambda e: e.affine_select(out=ident[:], in_=ident[:], pattern=[[-1, 128]], compare_op=ALU.not_equal,
                                           fill=1.0, base=0, channel_multiplier=1), reads=[r_id], writes=[r_id])
    return ident, r_id


def build_p1(n_lat=4096, n_ctx=64):
    P = Prog()
    ntok = n_lat + n_ctx
    x = P.dram("x", [ntok, D], F32, "ExternalInput")
    cT = P.dram("cT", [128, 16], F32, "ExternalInput")
    adaw = P.dram("adaw", [D, 2048], F32, "ExternalInput")
    adab = P.dram("adab", [1, 2048], F32, "ExternalInput")
    win = P.dram("win", [D, INW], F32, "ExternalInput")
    u = P.dram("u", [ntok, INW], F32, "ExternalOutput")

    fb = [P.ps([128, 512], F32) for i in range(6)]
    tb = [P.ps([128, 8, 128], BF16) for i in range(2)]
    ones_row = P.sb([1, 128], F32)
    r_ones = Reg()
    P.op("dve", lambda e: e.memset(ones_row[:], 1.0), writes=[r_ones])
    ident, r_id = make_ident(P)
    wb = P.sb([128, 8, INW], BF16)
    r_wb = Reg()
    for k in range(8):
        P.dma("pool", wb[:, k, :], win[k * 128:(k + 1) * 128, :], writes=[r_wb])
    bc, r_bc = mods_block(P, cT, adaw, adab, 2048, ones_row, fb[:4], [(1024, 2048)])

    NX = 3
    xs = [P.sb([128, D], F32) for _ in range(NX)]
    r_xs = [Reg() for _ in range(NX)]
    tmp = [P.sb([128, D], F32) for _ in range(2)]
    r_tmp = [Reg() for _ in range(2)]
    tmp2 = [P.sb([128, D], F32) for _ in range(2)]
    r_tmp2 = [Reg() for _ in range(2)]
    hb = [P.sb([128, D], BF16) for _ in range(2)]
    r_hb = [Reg() for _ in range(2)]
    stat = [P.sb([128, 16], F32) for _ in range(2)]
    r_stat = [Reg() for _ in range(2)]
    hT = [P.sb([128, 8, 128], BF16) for _ in range(2)]
    r_hT = [Reg() for _ in range(2)]
    uo = [P.sb([128, INW], F32) for _ in range(2)]
    r_uo = [Reg() for _ in range(2)]
    r_u_out = Reg()

    tiles = [(i * 128, 128, 0) for i in range(n_lat // 128)]
    t0 = n_lat
    while t0 < ntok:
        rows = min(128, ntok - t0)
        tiles.append((t0, rows, 1))
        t0 += rows
    for i, (t0, rows, s) in enumerate(tiles):
        a = i % NX
        b = i % 2
        P.dma("sp", xs[a][:rows, :], x[t0:t0 + rows, :], writes=[r_xs[a]])
        ln_tile(P, rows, xs[a][:rows, :], r_xs[a], tmp[b], r_tmp[b], stat[b], r_stat[b])
        P.op("pool", lambda e: e.tensor_tensor(out=tmp2[b][:rows, :], in0=tmp[b][:rows, :], in1=bc[s][:rows, 1024:2048], op=ALU.mult),
             reads=[r_tmp[b], r_bc[s]], writes=[r_tmp2[b]])
        P.op("dve", lambda e: e.tensor_tensor(out=hb[b][:rows, :], in0=tmp2[b][:rows, :], in1=bc[s][:rows, 0:1024], op=ALU.add),
             reads=[r_tmp2[b], r_bc[s]], writes=[r_hb[b]])
        tp, r_tp = tb[b]
        for k in range(8):
            P.op("pe", lambda e, k=k: e.transpose(tp[:, k, :rows], hb[b][:rows, k * 128:(k + 1) * 128], ident[:rows, :rows]),
                 reads=[r_hb[b], r_id], writes=[r_tp], inc=(k == 7))
        P.op("act", lambda e: e.copy(out=hT[b][:, :, :rows], in_=tp[:, :, :rows]), reads=[r_tp], writes=[r_hT[b]])
        for cb in range(5):
            bank, rb = fb[1 + cb]
            for k in range(8):
                P.op("pe", lambda e, k=k, cb=cb, bank=bank: e.matmul(bank[:rows, 0:480], hT[b][:, k, :rows], wb[:, k, cb * 480:(cb + 1) * 480],
                                                       start=(k == 0), stop=(k == 7)),
                     reads=[r_hT[b], r_wb], writes=[rb], inc=(k == 7))
            eng = "act" if cb % 2 == 0 else "dve"
            if eng == "act":
                P.op("act", lambda e, cb=cb, bank=bank: e.copy(out=uo[b][:rows, cb * 480:(cb + 1) * 480], in_=bank[:rows, 0:480]),
                     reads=[rb], writes=[r_uo[b]])
            else:
                P.op("dve", lambda e, cb=cb, bank=bank: e.tensor_copy(out=uo[b][:rows, cb * 480:(cb + 1) * 480], in_=bank[:rows, 0:480]),
                     reads=[rb], writes=[r_uo[b]])
        P.dma("sp", u[t0:t0 + rows, :], uo[b][:rows, :], reads=[r_uo[b]], writes=[r_u_out])
    P.finish([r_u_out])
    return P


HD = 64
QK_EPS = 1e-6


def build_a1(n_lat=16384, n_ctx=256, stage=2):
    P = Prog()
    ntok = n_lat + n_ctx
    NT = ntok // 128
    NTL = n_lat // 128
    qk = P.dram("qk", [ntok, 192], F32, "ExternalInput")
    v = P.dram("v", [ntok, 64], F32, "ExternalInput")
    gains = P.dram("gains", [128, 192], F32, "ExternalInput")
    cs = P.dram("cs", [n_lat, 192], F32, "ExternalInput")
    att = P.dram("att", [ntok, 128], F32, "ExternalOutput")

    identb, r_idb = make_ident(P, BF16)
    identf, r_idf = make_ident(P, F32)
    g_sb = P.sb([128, 192], F32)
    r_g = Reg()
    P.dma("sp", g_sb[:], gains[:, :], writes=[r_g])

    QT = P.sb([64, 2, ntok], BF16)
    KT = P.sb([64, ntok], BF16)
    VA = P.sb([128, NT, 65], BF16)
    r_QT = Reg()
    r_KT = Reg()
    r_VA = Reg()
    P.op("pool", lambda e: e.memset(VA[:, :, 64:65], 1.0), writes=[r_VA])

    NSB = 3

    P.push_scope()
    tpb = P.ps([128, 8, 128], BF16)
    NB = 2
    qk_sb = [P.sb([128, 192], F32) for _ in range(NB)]
    r_qk = [Reg() for _ in range(NB)]
    v_sb = [P.sb([128, 64], F32) for _ in range(NB)]
    r_v = [Reg() for _ in range(NB)]
    cs_sb = [P.sb([128, 192], F32) for _ in range(NB)]
    r_cs = [Reg() for _ in range(NB)]
    junk = [P.sb([128, 64], F32) for _ in range(NB)]
    r_junk = [Reg() for _ in range(NB)]
    ss = [P.sb([128, 8], F32) for _ in range(NB)]
    r_ss = [Reg() for _ in range(NB)]
    qn = [P.sb([128, 192], F32) for _ in range(NB)]
    r_qn = [Reg() for _ in range(NB)]
    ra = [P.sb([128, 96], F32) for _ in range(NB)]
    rb_ = [P.sb([128, 96], F32) for _ in range(NB)]
    r_ra = [Reg() for _ in range(NB)]
    r_rb = [Reg() for _ in range(NB)]
    qr = [P.sb([128, 192], BF16) for _ in range(NB)]
    r_qr = [Reg() for _ in range(NB)]

    for t in range(NT):
        b = t % NB
        t0 = t * 128
        lat = t < NTL
        P.dma("sp", qk_sb[b][:], qk[t0:t0 + 128, :], writes=[r_qk[b]])
        P.dma("sp", v_sb[b][:], v[t0:t0 + 128, :], writes=[r_v[b]])
        if lat:
            P.dma("sp", cs_sb[b][:], cs[t0:t0 + 128, :], writes=[r_cs[b]])
        for h in range(3):
            P.op("act", lambda e, h=h: e.activation(out=junk[b][:], in_=qk_sb[b][:, h * 64:(h + 1) * 64], func=AF.Square,
                                                    accum_out=ss[b][:, h:h + 1]), reads=[r_qk[b]], writes=[r_junk[b], r_ss[b]])
        P.op("dve", lambda e: e.tensor_scalar(out=ss[b][:, 3:6], in0=ss[b][:, 0:3], scalar1=1.0 / 64, scalar2=QK_EPS,
                                              op0=ALU.mult, op1=ALU.add), reads=[r_ss[b]], writes=[r_ss[b]])
        P.op("act", lambda e: e.activation(out=ss[b][:, 3:6], in_=ss[b][:, 3:6], func=AF.Sqrt), reads=[r_ss[b]], writes=[r_ss[b]])
        P.op("dve", lambda e: e.reciprocal(out=ss[b][:, 3:6], in_=ss[b][:, 3:6]), reads=[r_ss[b]], writes=[r_ss[b]])
        for h in range(3):
            P.op("dve", lambda e, h=h: e.scalar_tensor_tensor(out=qn[b][:, h * 64:(h + 1) * 64], in0=qk_sb[b][:, h * 64:(h + 1) * 64],
                                                              scalar=ss[b][:, 3 + h:4 + h], in1=g_sb[:, h * 64:(h + 1) * 64],
                                                              op0=ALU.mult, op1=ALU.mult),
                 reads=[r_qk[b], r_ss[b], r_g], writes=[r_qn[b]])
        if lat:
            x0 = qn[b][:].rearrange("p (i two) -> p i two", two=2)[:, :, 0]
            x1 = qn[b][:].rearrange("p (i two) -> p i two", two=2)[:, :, 1]
            o0 = qr[b][:].rearrange("p (i two) -> p i two", two=2)[:, :, 0]
            o1 = qr[b][:].rearrange("p (i two) -> p i two", two=2)[:, :, 1]
            c_ = cs_sb[b][:, 0:96]
            s_ = cs_sb[b][:, 96:192]
            P.op("dve", lambda e: e.tensor_tensor(out=ra[b][:], in0=x0, in1=c_, op=ALU.mult), reads=[r_qn[b], r_cs[b]], writes=[r_ra[b]])
            P.op("pool", lambda e: e.tensor_tensor(out=rb_[b][:], in0=x1, in1=s_, op=ALU.mult), reads=[r_qn[b], r_cs[b]], writes=[r_rb[b]])
            P.op("dve", lambda e: e.tensor_tensor(out=o0, in0=ra[b][:], in1=rb_[b][:], op=ALU.subtract), reads=[r_ra[b], r_rb[b]], writes=[r_qr[b]])
            P.op("dve", lambda e: e.tensor_tensor(out=ra[b][:], in0=x0, in1=s_, op=ALU.mult), reads=[r_qn[b], r_cs[b]], writes=[r_ra[b]])
            P.op("pool", lambda e: e.tensor_tensor(out=rb_[b][:], in0=x1, in1=c_, op=ALU.mult), reads=[r_qn[b], r_cs[b]], writes=[r_rb[b]])
            P.op("dve", lambda e: e.tensor_tensor(out=o1, in0=ra[b][:], in1=rb_[b][:], op=ALU.add), reads=[r_ra[b], r_rb[b]], writes=[r_qr[b]])
        else:
            P.op("dve", lambda e: e.tensor_copy(out=qr[b][:], in_=qn[b][:]), reads=[r_qn[b]], writes=[r_qr[b]])
        P.op("pool", lambda e: e.tensor_copy(out=VA[:, t, 0:64], in_=v_sb[b][:]), reads=[r_v[b]], writes=[r_VA])
        tp, r_tp = tpb
        for h in range(3):
            P.op("pe", lambda e, h=h: e.transpose(tp[0:64, h, :], qr[b][:, h * 64:(h + 1) * 64], identb[:, :]),
                 reads=[r_qr[b], r_idb], writes=[r_tp], inc=(h == 2))
        P.op("act", lambda e: e.copy(out=QT[:, :, t0:t0 + 128], in_=tp[0:64, 0:2, :]), reads=[r_tp], writes=[r_QT])
        P.op("dve", lambda e: e.tensor_copy(out=KT[:, t0:t0 + 128], in_=tp[0:64, 2, :]), reads=[r_tp], writes=[r_KT])

    P.pop_scope([r_QT, r_KT, r_VA])
    sps = [P.ps([128, 1024], F32) for _ in range(NSB)]
    ops_ = [P.ps([128, 512], F32) for _ in range(1)]
    tpf = P.ps([128, 4, 128], F32)
    pt = [P.sb([128, 1024], BF16) for _ in range(NSB)]
    r_pt = [Reg() for _ in range(NSB)]
    osb = [P.sb([65, 512], F32) for _ in range(2)]
    r_osb = [Reg() for _ in range(2)]
    ot = [P.sb([128, 4, 64], F32) for _ in range(2)]
    r_ot = [Reg() for _ in range(2)]
    rc = [P.sb([128, 4], F32) for _ in range(2)]
    r_rc = [Reg() for _ in range(2)]
    r_att = Reg()
    blocks = [(qb * 512, 512, list(range(NT))) for qb in range(n_lat // 512)]
    blocks.append((n_lat, n_ctx, list(range(NTL, NT))))
    if stage < 2:
        blocks = []
    items = []
    groups = []
    for (q0, nq, kts) in blocks:
        for h in range(2):
            g = len(groups)
            groups.append((q0, nq, h))
            npair = len(kts) // 2
            for pi in range(npair):
                items.append((g, (kts[2 * pi], kts[2 * pi + 1]), pi == 0, pi == npair - 1))

    def emit_st(n):
        g, kt2, first, last = items[n]
        q0, nq, h = groups[g]
        sb_, r_sb = sps[n % NSB]
        for j in range(2):
            kt = kt2[j]
            P.op("pe", lambda e, j=j, kt=kt: e.matmul(sb_[:, j * 512:j * 512 + nq], KT[:, kt * 128:(kt + 1) * 128],
                                                      QT[:, h, q0:q0 + nq], start=True, stop=True),
                 reads=[r_KT, r_QT], writes=[r_sb], inc=(j == 1))

    def emit_rest(n):
        g, kt2, first, last = items[n]
        q0, nq, h = groups[g]
        sb_, r_sb = sps[n % NSB]
        p2 = n % NSB
        o2 = g % 2
        obank, r_ob = ops_[0]
        for j in range(2):
            P.op("act", lambda e, j=j: e.activation(out=pt[p2][:, j * 512:j * 512 + nq], in_=sb_[:, j * 512:j * 512 + nq],
                                                    func=AF.Exp, scale=0.125),
                 reads=[r_sb], writes=[r_pt[p2]])
        for j in range(2):
            kt = kt2[j]
            P.op("pe", lambda e, j=j, kt=kt: e.matmul(obank[0:65, 0:nq], VA[:, kt, :], pt[p2][:, j * 512:j * 512 + nq],
                                                      start=(first and j == 0), stop=(last and j == 1)),
                 reads=[r_VA, r_pt[p2]], writes=[r_ob], inc=(j == 1))
        if last:
            P.op("dve", lambda e: e.tensor_copy(out=osb[o2][:, 0:nq], in_=obank[0:65, 0:nq]), reads=[r_ob], writes=[r_osb[o2]])
            tf, r_tf = tpf
            nj = nq // 128
            for j in range(nj):
                P.op("pe", lambda e, j=j: e.transpose(tf[:, j, 0:65], osb[o2][0:65, j * 128:(j + 1) * 128], identf[0:65, 0:65]),
                     reads=[r_osb[o2], r_idf], writes=[r_tf], inc=(j == nj - 1))
            P.op("dve", lambda e: e.reciprocal(out=rc[o2][:, 0:nj], in_=tf[:, 0:nj, 64]), reads=[r_tf], writes=[r_rc[o2]])
            for j in range(nj):
                P.op("dve", lambda e, j=j: e.tensor_scalar(out=ot[o2][:, j, :], in0=tf[:, j, 0:64], scalar1=rc[o2][:, j:j + 1], scalar2=None,
                                                           op0=ALU.mult), reads=[r_tf, r_rc[o2]], writes=[r_ot[o2]])
            dst = att[q0:q0 + nq, h * 64:(h + 1) * 64].rearrange("(j p) d -> p j d", p=128)
            P.dma("sp", dst, ot[o2][:, 0:nj, :], reads=[r_ot[o2]], writes=[r_att])

    for n in range(min(NSB - 1, len(items))):
        emit_st(n)
    for n in range(len(items)):
        if n + NSB - 1 < len(items):
            emit_st(n + NSB - 1)
        emit_rest(n)
    P.finish([r_att])
    return P


GN_EPS = 64e-5
WSC = -0.6065306597126334


def rwkv_orders(n_lat, n_ctx, C=64):
    ncl, ncc = n_lat // C, n_ctx // C
    fwd = [n_lat + c * C for c in range(ncc)] + [c * C for c in range(ncl)]
    bwd = [n_lat + c * C for c in range(ncc - 1, -1, -1)] + [c * C for c in range(ncl - 1, -1, -1)]
    return fwd, bwd


def build_a2(n_lat=16384, n_ctx=256, stop=99, NBUF=4, REC=True):
    P = Prog()
    ntok = n_lat + n_ctx
    fwd, bwd = rwkv_orders(n_lat, n_ctx)
    NS = len(fwd)
    U3 = P.dram("U3", [NS * 128, 864], F32, "ExternalInput")
    coefmu = P.dram("coefmu", [128, 576], F32, "ExternalInput")
    rowp = P.dram("rowp", [128, 128], F32, "ExternalInput")
    hv = P.dram("hv", [128, 320], F32, "ExternalInput")
    Wl = P.dram("Wl", [96, 192], F32, "ExternalInput")
    cmask = P.dram("cmask", [128, 128 + 256 + 128 + 96], F32, "ExternalInput")
    rw = P.dram("rw", [ntok, 64], F32, "ExternalOutput")
    yfs = P.dram("yfs", [ntok, 64], F32, "Internal")
    ybs = P.dram("ybs", [ntok, 64], F32, "Internal")
    gs = P.dram("gs", [ntok, 64], F32, "Internal")
    r_yfs, r_ybs, r_gs, r_rw = Reg(), Reg(), Reg(), Reg()

    ident, r_id = make_ident(P, F32)
    cm = P.sb([128, 608], F32)
    r_cm = Reg()
    P.dma("sp", cm[:], cmask[:, :], writes=[r_cm])
    MIT = cm[:, 0:128]
    MM = cm[:, 128:384]
    MS = cm[:, 384:512]
    LM = cm[:, 512:608]
    coef = P.sb([128, 864], F32)
    r_coef = Reg()
    P.dma("sp", coef[:, 288:864], coefmu[:, :], writes=[r_coef])
    P.op("dve", lambda e: e.tensor_tensor(out=coef[:, 0:288], in0=coef[:, 288:576], in1=coef[:, 576:864], op=ALU.add), reads=[r_coef], writes=[r_coef])
    P.op("dve", lambda e: e.tensor_scalar(out=coef[:, 0:288], in0=coef[:, 0:288], scalar1=-1.0, scalar2=1.0, op0=ALU.mult, op1=ALU.add),
         reads=[r_coef], writes=[r_coef])
    rp = P.sb([128, 128], F32)
    hvs = P.sb([128, 320], F32)
    wl = P.sb([96, 192], F32)
    r_par = Reg()
    P.dma("sp", rp[:], rowp[:, :], writes=[r_par])
    P.dma("sp", hvs[:], hv[:, :], writes=[r_par])
    P.dma("sp", wl[:], Wl[:, :], writes=[r_par])
    KKW, KA, RK, GNG, GNB = (hvs[:, i * 64:(i + 1) * 64] for i in range(5))
    ones = P.sb([128, 128], F32)
    r_ones = Reg()
    P.op("pool", lambda e: e.memset(ones[:], 1.0), writes=[r_ones])

    banks = [P.ps([128, 512], F32) for _ in range(8)]
    if NBUF == 1 or not REC:
        bsets = [list(range(8))] * max(NBUF, 1)
    else:
        bsets = [[] for _ in range(NBUF)]
        for b_ in range(8):
            bsets[b_ * NBUF // 8].append(b_)
    bk = [0] * 8
    cur = [0]

    def nb():
        c = cur[0]
        b = banks[bsets[c][bk[c] % len(bsets[c])]]
        bk[c] += 1
        return b

    ev = [0]

    def evac(out, in_, reads, writes):
        ev[0] += 1
        if ev[0] % 2 == 0:
            P.op("act", lambda e: e.copy(out=out, in_=in_), reads=reads, writes=writes)
        else:
            P.op("dve", lambda e: e.tensor_copy(out=out, in_=in_), reads=reads, writes=writes)

    def T(shape=(128, 128), n=None):
        n = n or NBUF
        return [P.sb(list(shape), F32) for _ in range(n)], [Reg() for _ in range(n)]

    u3, r_u3 = T((128, 864))
    prod, r_prod = T((128, 864))
    us, r_us = T((128, 288))
    lo, r_lo = T((128, 96))
    loT, r_loT = T((96, 128))
    wa, r_wa = T((128, 128))
    gg, r_gg = T((128, 64))
    kk, r_kk = T((128, 64))
    sm, r_sm = T((128, 8))
    tmp, r_tmp = T((128, 64))
    tmp2, r_tmp2 = T((128, 64))
    kd, r_kd = T((128, 64))
    bdn = ["lw", "a", "b", "kd", "r", "v"]
    bd = {n: T() for n in bdn}
    for n in bdn:
        for i in range(NBUF):
            P.op("pool", lambda e, n=n, i=i: e.memset(bd[n][0][i][:], 0.0), writes=[bd[n][1][i]])
    ex, r_ex = T((128, 512))
    ee, r_ee = T((128, 256))
    At, r_At = T()
    BKt, r_BKt = T((128, 256))
    Rt, r_Rt = T()
    BKG, r_BKG = T((128, 256))
    gcc, r_gcc = T((128, 1))
    BKT, r_BKT = T((128, 256))
    ART, r_ART = T((128, 256))
    LA, r_LA = T((128, 256))
    LK, r_LK = T((128, 256))
    X, r_X = T()
    XT, r_XT = T()
    X2, r_X2 = T()
    XT2, r_XT2 = T()
    TT, r_TT = T()
    Pm, r_Pm = T()
    LV, r_LV = T()
    Q, r_Q = T()
    RpT, r_RpT = T()
    Mm, r_Mm = T()
    yo, r_yo = T()
    ST = [P.sb([128, 128], F32) for _ in range(2)]
    r_ST = [Reg(), Reg()]
    P.op("pool", lambda e: e.memset(ST[0][:], 0.0), writes=[r_ST[0]])

    lists = []
    for s in range(NS):
        i = s % NBUF
        cur[0] = i if REC else 0
        if REC:
            P.record()
        LW, r_LW = bd["lw"][0][i], bd["lw"][1][i]
        Ab, r_Ab = bd["a"][0][i], bd["a"][1][i]
        Bb, r_Bb = bd["b"][0][i], bd["b"][1][i]
        KDb, r_KDb = bd["kd"][0][i], bd["kd"][1][i]
        Rb, r_Rb = bd["r"][0][i], bd["r"][1][i]
        Vb, r_Vb = bd["v"][0][i], bd["v"][1][i]
        P.dma("sp", u3[i][:], U3[s * 128:(s + 1) * 128, :], writes=[r_u3[i]])
        P.op("dve", lambda e: e.tensor_tensor(out=prod[i][:], in0=u3[i][:], in1=coef[:], op=ALU.mult), reads=[r_u3[i], r_coef], writes=[r_prod[i]])
        P.op("pool", lambda e: e.tensor_tensor(out=us[i][:], in0=prod[i][:, 0:288], in1=prod[i][:, 288:576], op=ALU.add), reads=[r_prod[i]], writes=[r_us[i]])
        P.op("dve", lambda e: e.tensor_tensor(out=us[i][:], in0=us[i][:], in1=prod[i][:, 576:864], op=ALU.add), reads=[r_prod[i], r_us[i]], writes=[r_us[i]])
        r_ = us[i][:, 0:64]
        k_ = us[i][:, 64:128]
        v_ = us[i][:, 128:192]
        if stop <= 1:
            continue
        P.op("act", lambda e: e.activation(out=lo[i][:, 0:32], in_=us[i][:, 192:224], func=AF.Tanh), reads=[r_us[i]], writes=[r_lo[i]])
        P.op("act", lambda e: e.activation(out=lo[i][:, 64:96], in_=us[i][:, 256:288], func=AF.Sigmoid), reads=[r_us[i]], writes=[r_lo[i]])
        P.op("pool", lambda e: e.tensor_copy(out=lo[i][:, 32:64], in_=us[i][:, 224:256]), reads=[r_us[i]], writes=[r_lo[i]])
        P.op("dve", lambda e: e.tensor_tensor(out=lo[i][:], in0=lo[i][:], in1=LM, op=ALU.mult), reads=[r_lo[i], r_cm], writes=[r_lo[i]])
        b1, rb1 = nb()
        P.op("pe", lambda e: e.transpose(b1[0:96, 0:128], lo[i][:, :], ident[:, :]), reads=[r_lo[i], r_id], writes=[rb1])
        evac(loT[i][:, :], b1[0:96, 0:128], [rb1], [r_loT[i]])
        b2, rb2 = nb()
        P.op("pe", lambda e: e.matmul(b2[:, 0:192], loT[i][:, :], wl[:, :], start=True, stop=True), reads=[r_loT[i], r_par], writes=[rb2])
        P.op("dve", lambda e: e.tensor_tensor(out=wa[i][:], in0=b2[:, 0:128], in1=rp[:], op=ALU.add), reads=[rb2, r_par], writes=[r_wa[i]])
        P.op("act", lambda e: e.copy(out=gg[i][:], in_=b2[:, 128:192]), reads=[rb2], writes=[r_gg[i]])
        P.op("act", lambda e: e.activation(out=wa[i][:], in_=wa[i][:], func=AF.Sigmoid), reads=[r_wa[i]], writes=[r_wa[i]])
        sw = wa[i][:, 0:64]
        asg = wa[i][:, 64:128]
        if stop <= 2:
            continue
        P.op("dve", lambda e: e.tensor_tensor(out=kk[i][:], in0=k_, in1=KKW, op=ALU.mult), reads=[r_us[i], r_par], writes=[r_kk[i]])
        P.op("act", lambda e: e.activation(out=tmp[i][:], in_=kk[i][:], func=AF.Square, accum_out=sm[i][:, 0:1]), reads=[r_kk[i]], writes=[r_tmp[i], r_sm[i]])
        P.op("dve", lambda e: e.tensor_scalar_add(out=sm[i][:, 1:2], in0=sm[i][:, 0:1], scalar1=1e-12), reads=[r_sm[i]], writes=[r_sm[i]])
        P.op("act", lambda e: e.activation(out=sm[i][:, 1:2], in_=sm[i][:, 1:2], func=AF.Sqrt), reads=[r_sm[i]], writes=[r_sm[i]])
        P.op("dve", lambda e: e.reciprocal(out=sm[i][:, 2:3], in_=sm[i][:, 1:2]), reads=[r_sm[i]], writes=[r_sm[i]])
        P.op("dve", lambda e: e.tensor_scalar(out=kk[i][:], in0=kk[i][:], scalar1=sm[i][:, 2:3], scalar2=None, op0=ALU.mult), reads=[r_kk[i], r_sm[i]], writes=[r_kk[i]])
        P.op("dve", lambda e: e.scalar_tensor_tensor(out=tmp2[i][:], in0=asg, scalar=-1.0, in1=KA, op0=ALU.add, op1=ALU.mult), reads=[r_wa[i], r_par], writes=[r_tmp2[i]])
        P.op("dve", lambda e: e.scalar_tensor_tensor(out=kd[i][:], in0=tmp2[i][:], scalar=1.0, in1=k_, op0=ALU.add, op1=ALU.mult), reads=[r_tmp2[i], r_us[i]], writes=[r_kd[i]])
        P.op("dve", lambda e: e.tensor_tensor(out=tmp[i][:], in0=r_, in1=RK, op=ALU.mult), reads=[r_us[i], r_par], writes=[r_tmp[i]])
        P.op("dve", lambda e: e.tensor_tensor(out=tmp[i][:], in0=tmp[i][:], in1=kd[i][:], op=ALU.mult), reads=[r_tmp[i], r_kd[i]], writes=[r_tmp[i]])
        P.op("dve", lambda e: e.tensor_reduce(out=sm[i][:, 3:4], in_=tmp[i][:], axis=AX.X, op=ALU.add), reads=[r_tmp[i]], writes=[r_sm[i]])
        if stop <= 3:
            continue
        for h in range(2):
            ps_ = slice(h * 64, (h + 1) * 64)
            ce = "act" if h == 0 else "pool"
            P.op("dve", lambda e: e.tensor_scalar(out=LW[ps_, ps_], in0=wa[i][ps_, 0:64], scalar1=WSC, scalar2=None, op0=ALU.mult), reads=[r_wa[i]], writes=[r_LW])
            P.op("dve", lambda e: e.tensor_scalar(out=Ab[ps_, ps_], in0=kk[i][ps_, :], scalar1=-1.0, scalar2=None, op0=ALU.mult), reads=[r_kk[i]], writes=[r_Ab])
            P.op("pool", lambda e: e.tensor_tensor(out=Bb[ps_, ps_], in0=kk[i][ps_, :], in1=wa[i][ps_, 64:128], op=ALU.mult), reads=[r_kk[i], r_wa[i]], writes=[r_Bb])
            for (dst, r_dst, src, r_src) in ((KDb, r_KDb, kd[i][ps_, :], r_kd[i]), (Rb, r_Rb, us[i][ps_, 0:64], r_us[i]), (Vb, r_Vb, us[i][ps_, 128:192], r_us[i])):
                if ce == "act":
                    P.op("act", lambda e, dst=dst, src=src: e.copy(out=dst[ps_, ps_], in_=src), reads=[r_src], writes=[r_dst])
                else:
                    P.op("pool", lambda e, dst=dst, src=src: e.tensor_copy(out=dst[ps_, ps_], in_=src), reads=[r_src], writes=[r_dst])
        if stop <= 4:
            continue
        b3, rb3 = nb()
        P.op("pe", lambda e: e.matmul(b3[:, 0:128], MIT, LW[:, :], start=True, stop=True), reads=[r_cm, r_LW], writes=[rb3], inc=False)
        P.op("pe", lambda e: e.matmul(b3[:, 128:256], ones[:, :], LW[:, :], start=True, stop=True), reads=[r_ones, r_LW], writes=[rb3], inc=False)
        P.op("pe", lambda e: e.matmul(b3[:, 256:257], LW[:, :], ones[:, 0:1], start=True, stop=True), reads=[r_ones, r_LW], writes=[rb3])
        P.op("act", lambda e: e.activation(out=ex[i][:, 0:128], in_=b3[:, 0:128], func=AF.Exp), reads=[rb3], writes=[r_ex[i]])
        P.op("act", lambda e: e.activation(out=ex[i][:, 128:256], in_=b3[:, 0:128], func=AF.Exp, scale=-1.0), reads=[rb3], writes=[r_ex[i]])
        P.op("act", lambda e: e.activation(out=ex[i][:, 256:384], in_=b3[:, 128:256], func=AF.Exp), reads=[rb3], writes=[r_ex[i]])
        P.op("act", lambda e: e.activation(out=gcc[i][:, 0:1], in_=b3[:, 256:257], func=AF.Exp), reads=[rb3], writes=[r_gcc[i]])
        P.op("act", lambda e: e.activation(out=ex[i][:, 384:512], in_=LW[:, :], func=AF.Exp, scale=-1.0), reads=[r_LW], writes=[r_ex[i]])
        EI = ex[i][:, 0:128]
        EN = ex[i][:, 128:256]
        P.op("dve", lambda e: e.tensor_tensor(out=ee[i][:, 0:128], in0=EI, in1=ex[i][:, 384:512], op=ALU.mult), reads=[r_ex[i]], writes=[r_ee[i]])
        P.op("pool", lambda e: e.tensor_tensor(out=ee[i][:, 128:256], in0=ex[i][:, 256:384], in1=EN, op=ALU.mult), reads=[r_ex[i]], writes=[r_ee[i]])
        P.op("dve", lambda e: e.tensor_tensor(out=At[i][:], in0=Ab[:, :], in1=ee[i][:, 0:128], op=ALU.mult), reads=[r_Ab, r_ee[i]], writes=[r_At[i]])
        P.op("pool", lambda e: e.tensor_tensor(out=BKt[i][:, 0:128], in0=Bb[:, :], in1=EN, op=ALU.mult), reads=[r_Bb, r_ex[i]], writes=[r_BKt[i]])
        P.op("dve", lambda e: e.tensor_tensor(out=BKt[i][:, 128:256], in0=KDb[:, :], in1=EN, op=ALU.mult), reads=[r_KDb, r_ex[i]], writes=[r_BKt[i]])
        P.op("pool", lambda e: e.tensor_tensor(out=Rt[i][:], in0=Rb[:, :], in1=EI, op=ALU.mult), reads=[r_Rb, r_ex[i]], writes=[r_Rt[i]])
        P.op("dve", lambda e: e.tensor_tensor(out=BKG[i][:, 0:128], in0=Bb[:, :], in1=ee[i][:, 128:256], op=ALU.mult), reads=[r_Bb, r_ee[i]], writes=[r_BKG[i]])
        P.op("pool", lambda e: e.tensor_tensor(out=BKG[i][:, 128:256], in0=KDb[:, :], in1=ee[i][:, 128:256], op=ALU.mult), reads=[r_KDb, r_ee[i]], writes=[r_BKG[i]])
        if stop <= 5:
            continue
        b4, rb4 = nb()
        P.op("pe", lambda e: e.transpose(b4[:, 0:128], BKt[i][:, 0:128], ident[:, :]), reads=[r_BKt[i], r_id], writes=[rb4], inc=False)
        P.op("pe", lambda e: e.transpose(b4[:, 128:256], BKt[i][:, 128:256], ident[:, :]), reads=[r_BKt[i], r_id], writes=[rb4])
        evac(BKT[i][:, :], b4[:, 0:256], [rb4], [r_BKT[i]])
        b5, rb5 = nb()
        P.op("pe", lambda e: e.transpose(b5[:, 0:128], At[i][:, :], ident[:, :]), reads=[r_At[i], r_id], writes=[rb5], inc=False)
        P.op("pe", lambda e: e.transpose(b5[:, 128:256], Rt[i][:, :], ident[:, :]), reads=[r_Rt[i], r_id], writes=[rb5])
        evac(ART[i][:, :], b5[:, 0:256], [rb5], [r_ART[i]])
        b6, rb6 = nb()
        P.op("pe", lambda e: e.matmul(b6[:, 0:256], BKT[i][:, 0:128], ART[i][:, :], start=True, stop=True), reads=[r_BKT[i], r_ART[i]], writes=[rb6])
        P.op("dve", lambda e: e.tensor_tensor(out=LA[i][:], in0=b6[:, 0:256], in1=MM, op=ALU.mult), reads=[rb6, r_cm], writes=[r_LA[i]])
        b7, rb7 = nb()
        P.op("pe", lambda e: e.matmul(b7[:, 0:256], BKT[i][:, 128:256], ART[i][:, :], start=True, stop=True), reads=[r_BKT[i], r_ART[i]], writes=[rb7])
        P.op("dve", lambda e: e.tensor_tensor(out=LK[i][:], in0=b7[:, 0:256], in1=MM, op=ALU.mult), reads=[rb7, r_cm], writes=[r_LK[i]])
        b8, rb8 = nb()
        P.op("pe", lambda e: e.matmul(b8[:, 0:128], ART[i][:, 0:128], BKT[i][:, 0:128], start=True, stop=True), reads=[r_BKT[i], r_ART[i]], writes=[rb8])
        P.op("dve", lambda e: e.tensor_tensor(out=XT[i][:], in0=b8[:, 0:128], in1=MS, op=ALU.mult), reads=[rb8, r_cm], writes=[r_XT[i]])
        if stop <= 6:
            continue
        P.op("pool", lambda e: e.tensor_tensor(out=TT[i][:], in0=LA[i][:, 0:128], in1=ident[:, :], op=ALU.add), reads=[r_LA[i], r_id], writes=[r_TT[i]])
        cx, r_cx = LA[i][:, 0:128], r_LA[i]
        cxt, r_cxt = XT[i][:, :], r_XT[i]
        nxt = [(X2[i], r_X2[i], XT2[i], r_XT2[i]), (X[i], r_X[i], XT[i], r_XT[i])]
        for lv in range(5):
            nX, r_nX, nXT, r_nXT = nxt[lv % 2]
            if lv < 4:
                ba, rba = nb()
                P.op("pe", lambda e, cx=cx, cxt=cxt, ba=ba: e.matmul(ba[:, 0:128], cxt, cx, start=True, stop=True), reads=[r_cx, r_cxt], writes=[rba])
            bb, rbb = nb()
            P.op("pe", lambda e, cx=cx, cxt=cxt, bb=bb: e.matmul(bb[:, 0:128], cx, cxt, start=True, stop=True), reads=[r_cx, r_cxt], writes=[rbb])
            if lv < 4:
                P.op("act", lambda e, nX=nX, ba=ba: e.copy(out=nX[:, :], in_=ba[:, 0:128]), reads=[rba], writes=[r_nX])
            P.op("dve", lambda e, nXT=nXT, bb=bb: e.tensor_copy(out=nXT[:, :], in_=bb[:, 0:128]), reads=[rbb], writes=[r_nXT])
            bc, rbc = nb()
            P.op("pe", lambda e, nXT=nXT, bc=bc: e.matmul(bc[:, 0:128], nXT[:, :], TT[i][:, :], start=True, stop=True), reads=[r_nXT, r_TT[i]], writes=[rbc])
            P.op("dve", lambda e, bc=bc: e.tensor_tensor(out=TT[i][:], in0=bc[:, 0:128], in1=TT[i][:], op=ALU.add), reads=[rbc, r_TT[i]], writes=[r_TT[i]])
            cx, r_cx, cxt, r_cxt = nX[:, :], r_nX, nXT[:, :], r_nXT
        if stop <= 7:
            continue
        b9, rb9 = nb()
        P.op("pe", lambda e: e.matmul(b9[:, 0:128], TT[i][:, :], At[i][:, :], start=True, stop=True), reads=[r_TT[i], r_At[i]], writes=[rb9], inc=False)
        P.op("pe", lambda e: e.matmul(b9[:, 128:256], LK[i][:, 0:128], Vb[:, :], start=True, stop=True), reads=[r_LK[i], r_Vb], writes=[rb9])
        P.op("act", lambda e: e.copy(out=Pm[i][:, :], in_=b9[:, 0:128]), reads=[rb9], writes=[r_Pm[i]])
        P.op("dve", lambda e: e.tensor_copy(out=LV[i][:, :], in_=b9[:, 128:256]), reads=[rb9], writes=[r_LV[i]])
        b10, rb10 = nb()
        P.op("pe", lambda e: e.matmul(b10[:, 0:128], TT[i][:, :], LV[i][:, :], start=True, stop=True), reads=[r_TT[i], r_LV[i]], writes=[rb10], inc=False)
        P.op("pe", lambda e: e.matmul(b10[:, 128:256], Pm[i][:, :], LA[i][:, 128:256], start=True, stop=True), reads=[r_Pm[i], r_LA[i]], writes=[rb10], inc=False)
        P.op("pe", lambda e: e.matmul(b10[:, 256:384], Pm[i][:, :], BKG[i][:, 0:128], start=True, stop=True), reads=[r_Pm[i], r_BKG[i]], writes=[rb10])
        P.op("act", lambda e: e.copy(out=Q[i][:, :], in_=b10[:, 0:128]), reads=[rb10], writes=[r_Q[i]])
        P.op("dve", lambda e: e.tensor_tensor(out=RpT[i][:], in0=b10[:, 128:256], in1=ART[i][:, 128:256], op=ALU.add), reads=[rb10, r_ART[i]], writes=[r_RpT[i]])
        P.op("dve", lambda e: e.scalar_tensor_tensor(out=Mm[i][:], in0=ident[:, :], scalar=gcc[i][:, 0:1], in1=b10[:, 256:384], op0=ALU.mult, op1=ALU.add),
             reads=[rb10, r_id, r_gcc[i]], writes=[r_Mm[i]])
        if stop <= 8:
            continue
        sc, sn = s % 2, (s + 1) % 2
        b11, rb11 = nb()
        P.op("pe", lambda e: e.matmul(b11[:, 0:128], LA[i][:, 128:256], Q[i][:, :], start=True, stop=False), reads=[r_LA[i], r_Q[i]], writes=[rb11], inc=False)
        P.op("pe", lambda e: e.matmul(b11[:, 0:128], LK[i][:, 128:256], Vb[:, :], start=False, stop=False), reads=[r_LK[i], r_Vb], writes=[rb11], inc=False)
        P.op("pe", lambda e: e.matmul(b11[:, 0:128], RpT[i][:, :], ST[sc][:, :], start=False, stop=True), reads=[r_RpT[i], r_ST[sc]], writes=[rb11])
        b12, rb12 = nb()
        P.op("pe", lambda e: e.matmul(b12[:, 0:128], BKG[i][:, 0:128], Q[i][:, :], start=True, stop=False), reads=[r_BKG[i], r_Q[i]], writes=[rb12], inc=False)
        P.op("pe", lambda e: e.matmul(b12[:, 0:128], BKG[i][:, 128:256], Vb[:, :], start=False, stop=False), reads=[r_BKG[i], r_Vb], writes=[rb12], inc=False)
        P.op("pe", lambda e: e.matmul(b12[:, 0:128], Mm[i][:, :], ST[sc][:, :], start=False, stop=True), reads=[r_Mm[i], r_ST[sc]], writes=[rb12])
        P.op("act", lambda e: e.copy(out=ST[sn][:, :], in_=b12[:, 0:128]), reads=[rb12], writes=[r_ST[sn]])
        P.op("dve", lambda e: e.scalar_tensor_tensor(out=yo[i][:], in0=Vb[:, :], scalar=sm[i][:, 3:4], in1=b11[:, 0:128], op0=ALU.mult, op1=ALU.add),
             reads=[rb11, r_Vb, r_sm[i]], writes=[r_yo[i]])
        tf, tb_ = fwd[s], bwd[s]
        P.dma("sp", yfs[tf:tf + 64, :], yo[i][0:64, 0:64], reads=[r_yo[i]], writes=[r_yfs])
        P.dma("sp", ybs[tb_:tb_ + 64, :], yo[i][64:128, 64:128], reads=[r_yo[i]], writes=[r_ybs])
        P.dma("sp", gs[tf:tf + 64, :], gg[i][0:64, :], reads=[r_gg[i]], writes=[r_gs])
        if REC:
            lists.append(P.stop_record())
            if s == NS - 1:
                P.replay_skewed(lists, NBUF)
                lists = []

    if stop < 99:
        P.finish([])
        return P
    yf_, r_yf_ = T((128, 64), 2)
    yb_, r_yb_ = T((128, 64), 2)
    g_, r_g_ = T((128, 64), 2)
    st, r_st = T((128, 16), 2)
    yn, r_yn = T((128, 64), 2)
    for t in range(ntok // 128):
        i = t % 2
        t0 = t * 128
        P.dma("sp", yf_[i][:], yfs[t0:t0 + 128, :], reads=[r_yfs], writes=[r_yf_[i]])
        P.dma("sp", yb_[i][:], ybs[t0:t0 + 128, :], reads=[r_ybs], writes=[r_yb_[i]])
        P.dma("sp", g_[i][:], gs[t0:t0 + 128, :], reads=[r_gs], writes=[r_g_[i]])
        P.op("dve", lambda e: e.tensor_tensor(out=yf_[i][:], in0=yf_[i][:], in1=yb_[i][:], op=ALU.add), reads=[r_yf_[i], r_yb_[i]], writes=[r_yf_[i]])
        P.op("dve", lambda e: e.bn_stats(out=st[i][:, 0:6], in_=yf_[i][:]), reads=[r_yf_[i]], writes=[r_st[i]])
        P.op("dve", lambda e: e.bn_aggr(out=st[i][:, 6:8], in_=st[i][:, 0:6]), reads=[r_st[i]], writes=[r_st[i]])
        P.op("dve", lambda e: e.tensor_scalar_add(out=st[i][:, 8:9], in0=st[i][:, 7:8], scalar1=GN_EPS), reads=[r_st[i]], writes=[r_st[i]])
        P.op("act", lambda e: e.activation(out=st[i][:, 8:9], in_=st[i][:, 8:9], func=AF.Sqrt), reads=[r_st[i]], writes=[r_st[i]])
        P.op("dve", lambda e: e.reciprocal(out=st[i][:, 9:10], in_=st[i][:, 8:9]), reads=[r_st[i]], writes=[r_st[i]])
        P.op("dve", lambda e: e.tensor_scalar(out=yn[i][:], in0=yf_[i][:], scalar1=st[i][:, 6:7], scalar2=st[i][:, 9:10], op0=ALU.subtract, op1=ALU.mult),
             reads=[r_yf_[i], r_st[i]], writes=[r_yn[i]])
        P.op("pool", lambda e: e.tensor_tensor(out=yn[i][:], in0=yn[i][:], in1=GNG, op=ALU.mult), reads=[r_yn[i], r_par], writes=[r_yn[i]])
        P.op("pool", lambda e: e.tensor_tensor(out=yn[i][:], in0=yn[i][:], in1=GNB, op=ALU.add), reads=[r_yn[i], r_par], writes=[r_yn[i]])
        P.op("dve", lambda e: e.tensor_tensor(out=yn[i][:], in0=yn[i][:], in1=g_[i][:], op=ALU.mult), reads=[r_yn[i], r_g_[i]], writes=[r_yn[i]])
        P.dma("sp", rw[t0:t0 + 128, :], yn[i][:], reads=[r_yn[i]], writes=[r_rw])
    P.finish([r_rw])
    return P


PI = math.pi


def build_a3(n_lat=16384, n_ctx=256, stop=99):
    P = Prog()
    ntok = n_lat + n_ctx
    NT = ntok // 128
    H3 = P.dram("H3", [ntok, 576], F32, "ExternalInput")
    cw = P.dram("cw", [128, 768], F32, "ExternalInput")
    ztl = P.dram("ztl", [2, 33, n_lat], F32, "ExternalInput")
    ztc = P.dram("ztc", [2, 33, n_ctx], F32, "ExternalInput")
    w1d = P.dram("w1", [33, 64], F32, "ExternalInput")
    w2d = P.dram("w2", [64, 64], F32, "ExternalInput")
    w3d = P.dram("w3", [64, 256], F32, "ExternalInput")
    colp = P.dram("colp", [64, 4], F32, "ExternalInput")
    decd = P.dram("dec", [1, 256], F32, "ExternalInput")
    hbd = P.dram("hb", [128, 128], F32, "ExternalInput")
    hy = P.dram("hy", [ntok, 64], F32, "ExternalOutput")
    WL = 2 * n_lat - 1
    WC = 2 * n_ctx - 1
    KDl = P.dram("KDl", [128, WL + 1], BF16, "Internal")
    KDc = P.dram("KDc", [128, WC + 1], BF16, "Internal")
    r_KD = {0: Reg(), 1: Reg()}
    r_hy = Reg()

    identf, r_idf = make_ident(P, F32)
    J = P.sb([128, 128], BF16)
    r_J = Reg()
    P.op("pool", lambda e: e.memset(J[:], 0.0), writes=[r_J])
    P.op("pool", lambda e: e.affine_select(out=J[:], in_=J[:], pattern=[[1, 128]], compare_op=ALU.not_equal, fill=1.0, base=-127, channel_multiplier=1),
         reads=[r_J], writes=[r_J])
    ones = P.sb([128, 128], F32)
    r_ones = Reg()
    P.op("pool", lambda e: e.memset(ones[:], 1.0), writes=[r_ones])
    npi = P.sb([128, 1], F32)
    P.op("pool", lambda e: e.memset(npi[:], PI / 2), writes=[r_ones])

    cws = P.sb([128, 768], F32)
    w1s = P.sb([33, 64], F32)
    w2s = P.sb([64, 64], F32)
    w3s = P.sb([64, 256], F32)
    cps = P.sb([64, 4], F32)
    decs = P.sb([1, 256], F32)
    hbs = P.sb([128, 128], F32)
    r_par = Reg()
    for dst, src in ((cws, cw), (w1s, w1d), (w2s, w2d), (w3s, w3d), (cps, colp), (decs, decd), (hbs, hbd)):
        P.dma("sp", dst[:], src[:, :], writes=[r_par])
    P.op("dve", lambda e: e.tensor_scalar(out=decs[:], in0=decs[:], scalar1=-1.0, scalar2=None, op0=ALU.mult), reads=[r_par], writes=[r_par])

    banks = [P.ps([128, 512], F32) for _ in range(8)]
    bk = [0]

    def nb():
        b = banks[bk[0] % 6]
        bk[0] += 1
        return b
    ybanks = banks[6:8]

    SC = P.sb([128, 192, NT], F32)
    r_SC = Reg()
    RN = P.sb([128, 2, 128], F32)
    r_RN = Reg()
    P.push_scope()
    h3 = [P.sb([128, 576], F32) for _ in range(2)]
    r_h3 = [Reg(), Reg()]
    pr = [P.sb([128, 576], F32) for _ in range(2)]
    r_pr = [Reg(), Reg()]
    s1 = [P.sb([128, 192], F32) for _ in range(2)]
    r_s1 = [Reg(), Reg()]
    for t in range(NT):
        i = t % 2
        P.dma("sp", h3[i][:], H3[t * 128:(t + 1) * 128, :], writes=[r_h3[i]])
        P.op("dve", lambda e: e.tensor_tensor(out=pr[i][:], in0=h3[i][:], in1=cws[:, 0:576], op=ALU.mult), reads=[r_h3[i], r_par], writes=[r_pr[i]])
        P.op("pool", lambda e: e.tensor_tensor(out=s1[i][:], in0=pr[i][:, 0:192], in1=pr[i][:, 192:384], op=ALU.add), reads=[r_pr[i]], writes=[r_s1[i]])
        P.op("pool", lambda e: e.tensor_tensor(out=s1[i][:], in0=s1[i][:], in1=pr[i][:, 384:576], op=ALU.add), reads=[r_pr[i], r_s1[i]], writes=[r_s1[i]])
        P.op("dve", lambda e: e.tensor_tensor(out=SC[:, :, t], in0=s1[i][:], in1=cws[:, 576:768], op=ALU.add), reads=[r_s1[i], r_par], writes=[r_SC])

    nacc = 2 * max(n_lat // 512, 1) + 2
    acc = P.sb([128, 2, 2 * (n_lat // 512 + 1)], F32)
    r_acc = Reg()
    P.op("pool", lambda e: e.memset(acc[:], 0.0), writes=[r_acc])
    zt = [P.sb([33, 512], F32) for _ in range(2)]
    r_zt = [Reg(), Reg()]
    hA = [P.sb([64, 512], F32) for _ in range(2)]
    r_hA = [Reg(), Reg()]
    hB = [P.sb([64, 512], F32) for _ in range(2)]
    r_hB = [Reg(), Reg()]
    win = [P.sb([128, 512], F32) for _ in range(2)]
    r_win = [Reg(), Reg()]
    hw = [P.sb([128, 512], F32) for _ in range(2)]
    r_hw = [Reg(), Reg()]
    hwb = [P.sb([128, 512], BF16) for _ in range(2)]
    r_hwb = [Reg(), Reg()]
    junk = [P.sb([128, 512], F32) for _ in range(2)]
    r_junk = [Reg(), Reg()]
    sS = [P.sb([64, 512], F32) for _ in range(2)]
    sC = [P.sb([64, 512], F32) for _ in range(2)]
    sQ = [P.sb([64, 512], F32) for _ in range(2)]
    r_sS = [Reg(), Reg()]
    r_sC = [Reg(), Reg()]
    r_sQ = [Reg(), Reg()]

    def sin_big(h, r_h, N, i):
        S, C, Q = sS[i], sC[i], sQ[i]
        P.op("act", lambda e: e.activation(out=S[:, 0:N], in_=h[:, 0:N], func=AF.Sin, scale=0.125), reads=[r_h], writes=[r_sS[i]])
        P.op("act", lambda e: e.activation(out=C[:, 0:N], in_=h[:, 0:N], func=AF.Sin, scale=0.125, bias=npi[0:64, 0:1]), reads=[r_h, r_ones], writes=[r_sC[i]])
        for lv in range(3):
            dst = h if lv == 2 else S
            r_dst = r_h if lv == 2 else r_sS[i]
            if lv < 2:
                P.op("pool", lambda e: e.tensor_tensor(out=Q[:, 0:N], in0=S[:, 0:N], in1=S[:, 0:N], op=ALU.mult), reads=[r_sS[i]], writes=[r_sQ[i]])
            P.op("dve", lambda e, dst=dst: e.scalar_tensor_tensor(out=dst[:, 0:N], in0=S[:, 0:N], scalar=2.0, in1=C[:, 0:N], op0=ALU.mult, op1=ALU.mult),
                 reads=[r_sS[i], r_sC[i]], writes=[r_dst])
            if lv < 2:
                P.op("dve", lambda e: e.tensor_scalar(out=C[:, 0:N], in0=Q[:, 0:N], scalar1=-2.0, scalar2=1.0, op0=ALU.mult, op1=ALU.add),
                     reads=[r_sQ[i]], writes=[r_sC[i]])

    it = 0
    for seq, (L, ztd, KD) in enumerate(((n_lat, ztl, KDl), (n_ctx, ztc, KDc))):
        N = min(512, L)
        nblk = L // N
        for ps in range(2):
            for bl in range(nblk):
                i = it % 2
                it += 1
                P.dma("sp", zt[i][:, 0:N], ztd[ps, :, bl * N:(bl + 1) * N], writes=[r_zt[i]])
                b1, rb1 = nb()
                P.op("pe", lambda e: e.matmul(b1[0:64, 0:N], w1s[:, :], zt[i][:, 0:N], start=True, stop=True), reads=[r_par, r_zt[i]], writes=[rb1])
                P.op("dve", lambda e: e.tensor_scalar(out=hA[i][:, 0:N], in0=b1[0:64, 0:N], scalar1=cps[:, 0:1], scalar2=cps[:, 1:2], op0=ALU.add, op1=ALU.mult),
                     reads=[rb1, r_par], writes=[r_hA[i]])
                sin_big(hA[i], r_hA[i], N, i)
                b2, rb2 = nb()
                P.op("pe", lambda e: e.matmul(b2[0:64, 0:N], w2s[:, :], hA[i][:, 0:N], start=True, stop=True), reads=[r_par, r_hA[i]], writes=[rb2])
                P.op("dve", lambda e: e.tensor_scalar(out=hB[i][:, 0:N], in0=b2[0:64, 0:N], scalar1=cps[:, 2:3], scalar2=cps[:, 3:4], op0=ALU.add, op1=ALU.mult),
                     reads=[rb2, r_par], writes=[r_hB[i]])
                sin_big(hB[i], r_hB[i], N, i)
                b3, rb3 = nb()
                P.op("pe", lambda e: e.matmul(b3[:, 0:N], w3s[:, ps * 128:(ps + 1) * 128], hB[i][:, 0:N], start=True, stop=True), reads=[r_par, r_hB[i]], writes=[rb3])
                b4, rb4 = nb()
                P.op("pe", lambda e: e.matmul(b4[:, 0:N], decs[0:1, ps * 128:(ps + 1) * 128], zt[i][0:1, 0:N], start=True, stop=True), reads=[r_par, r_zt[i]], writes=[rb4])
                P.op("act", lambda e: e.activation(out=win[i][:, 0:N], in_=b4[:, 0:N], func=AF.Exp), reads=[rb4], writes=[r_win[i]])
                P.op("dve", lambda e: e.tensor_tensor(out=hw[i][:, 0:N], in0=b3[:, 0:N], in1=win[i][:, 0:N], op=ALU.mult), reads=[rb3, r_win[i]], writes=[r_hw[i]])
                if ps == 1 and bl == nblk - 1:
                    P.op("dve", lambda e: e.memset(hw[i][:, N - 1:N], 0.0), reads=[r_hw[i]], writes=[r_hw[i]])
                P.op("act", lambda e: e.activation(out=junk[i][:, 0:N], in_=hw[i][:, 0:N], func=AF.Abs, accum_out=acc[:, seq, ps * nblk + bl:ps * nblk + bl + 1]),
                     reads=[r_hw[i]], writes=[r_junk[i], r_acc])
                P.op("pool", lambda e: e.tensor_copy(out=hwb[i][:, 0:N], in_=hw[i][:, 0:N]), reads=[r_hw[i]], writes=[r_hwb[i]])
                if ps == 0:
                    q0 = L - 1 + bl * N
                    P.dma("sp", KD[:, q0:q0 + N], hwb[i][:, 0:N], reads=[r_hwb[i]], writes=[r_KD[seq]])
                else:
                    q0 = bl * N
                    nw = N - 1 if bl == nblk - 1 else N
                    P.dma("sp", KD[:, q0:q0 + nw], hwb[i][:, 0:nw], reads=[r_hwb[i]], writes=[r_KD[seq]])
    nrm = P.sb([128, 2], F32)
    r_nrm = Reg()
    dg = P.sb([128, 128], F32)
    r_dg = Reg()
    for seq in range(2):
        P.op("dve", lambda e: e.tensor_reduce(out=nrm[:, seq:seq + 1], in_=acc[:, seq, :], axis=AX.X, op=ALU.add), reads=[r_acc], writes=[r_nrm])
        P.op("dve", lambda e: e.tensor_scalar(out=dg[:], in0=identf[:], scalar1=nrm[:, seq:seq + 1], scalar2=None, op0=ALU.mult), reads=[r_nrm, r_idf], writes=[r_dg])
        b5, rb5 = nb()
        P.op("pe", lambda e: e.matmul(b5[:, 0:128], ones[:, :], dg[:, :], start=True, stop=True), reads=[r_ones, r_dg], writes=[rb5])
        P.op("dve", lambda e: e.reciprocal(out=RN[:, seq, :], in_=b5[:, 0:128]), reads=[rb5], writes=[r_RN])

    P.pop_scope([r_KD[0], r_KD[1], r_RN, r_SC])
    hsk = [P.sb([128, n_lat], BF16) for _ in range(2)]
    r_hsk = [Reg(), Reg()]
    zb = [P.sb([128, 128], BF16) for _ in range(2)]
    r_zb = [Reg(), Reg()]
    zr = [P.sb([128, 128], BF16) for _ in range(2)]
    r_zr = [Reg(), Reg()]
    tm = [P.sb([128, 128], F32) for _ in range(2)]
    r_tm = [Reg(), Reg()]
    hk = 0
    cv = 0
    for seq, (L, KD, W, j0) in enumerate(((n_lat, KDl, WL + 1, 0), (n_ctx, KDc, WC + 1, n_lat // 128))):
        NB = L // 128
        for ch in range(64):
            for o in range(2):
                c2 = cv % 2
                cv += 1
                row = o * 64 + ch
                src_c = (128 + ch) if o == 0 else ch
                Zs = SC[:, src_c, j0:j0 + NB]
                P.op("pool", lambda e: e.tensor_copy(out=zb[c2][:, 0:NB], in_=Zs), reads=[r_SC], writes=[r_zb[c2]])
                bz, rbz = nb()
                P.op("pe", lambda e: e.matmul(bz[:, 0:NB], J[:, :], zb[c2][:, 0:NB], start=True, stop=True), reads=[r_J, r_zb[c2]], writes=[rbz])
                P.op("act", lambda e: e.copy(out=zr[c2][:, 0:NB], in_=bz[:, 0:NB]), reads=[rbz], writes=[r_zr[c2]])
                yb_, r_yb = ybanks[c2]
                first = True
                for h in (1, 0):
                    hb_ = hk % 2
                    hk += 1
                    if h == 1:
                        x0, wd = L - 128, L
                        deltas = list(range(0, NB))
                    else:
                        x0, wd = 0, L - 128
                        deltas = list(range(-(NB - 1), 0))
                    if wd == 0:
                        continue
                    src = bass.AP(KD.tensor, row * W + x0, [[1, 128], [1, wd]])
                    P.dma("sp", hsk[hb_][:, 0:wd], src, reads=[r_KD[seq]], writes=[r_hsk[hb_]])
                    for di, d in enumerate(deltas):
                        xo = 128 * d + L - 128 - x0
                        lo_i, hi_i = max(0, d), NB + min(0, d)
                        lo_j, hi_j = max(0, -d), NB - max(0, d)
                        last = (h == 0 and di == len(deltas) - 1) or (NB == 1)
                        P.op("pe", lambda e, xo=xo, lo_i=lo_i, hi_i=hi_i, lo_j=lo_j, hi_j=hi_j, first=first, last=last: e.matmul(
                            yb_[:, lo_i:hi_i], hsk[hb_][:, xo:xo + 128], zr[c2][:, lo_j:hi_j], start=first, stop=last),
                            reads=[r_hsk[hb_], r_zr[c2]], writes=[r_yb], inc=(di == len(deltas) - 1))
                        first = False
                col = o * 64 + ch
                P.op("dve", lambda e: e.tensor_scalar(out=tm[c2][:, 0:NB], in0=yb_[:, 0:NB], scalar1=RN[:, seq, col:col + 1], scalar2=None, op0=ALU.mult),
                     reads=[r_yb, r_RN], writes=[r_tm[c2]])
                P.op("dve", lambda e: e.scalar_tensor_tensor(out=tm[c2][:, 0:NB], in0=Zs, scalar=hbs[:, col:col + 1], in1=tm[c2][:, 0:NB], op0=ALU.mult, op1=ALU.add),
                     reads=[r_SC, r_par, r_tm[c2]], writes=[r_tm[c2]])
                if o == 0:
                    P.op("dve", lambda e: e.tensor_tensor(out=SC[:, ch, j0:j0 + NB], in0=SC[:, ch, j0:j0 + NB], in1=tm[c2][:, 0:NB], op=ALU.mult),
                         reads=[r_SC, r_tm[c2]], writes=[r_SC])
                else:
                    P.op("dve", lambda e: e.tensor_tensor(out=SC[:, 64 + ch, j0:j0 + NB], in0=SC[:, 64 + ch, j0:j0 + NB], in1=tm[c2][:, 0:NB], op=ALU.mult),
                         reads=[r_SC, r_tm[c2]], writes=[r_SC])
    if stop <= 4:
        P.finish([])
        return P
    ot = [P.sb([128, 64], F32) for _ in range(2)]
    r_ot = [Reg(), Reg()]
    for t in range(NT):
        i = t % 2
        eng = "act" if t % 2 == 0 else "pool"
        if eng == "act":
            P.op("act", lambda e: e.copy(out=ot[i][:], in_=SC[:, 64:128, t]), reads=[r_SC], writes=[r_ot[i]])
        else:
            P.op("pool", lambda e: e.tensor_copy(out=ot[i][:], in_=SC[:, 64:128, t]), reads=[r_SC], writes=[r_ot[i]])
        P.dma("sp", hy[t * 128:(t + 1) * 128, :], ot[i][:], reads=[r_ot[i]], writes=[r_hy])
    P.finish([r_hy])
    return P


D = 1024
FF = 2816
ALPHA = float(4 ** 0.25)


def build_p2(n_lat=4096, n_ctx=64, moe=False):
    P = Prog()
    E = 8 if moe else 1
    ntok = n_lat + n_ctx
    mix = P.dram("mix", [ntok, D], F32, "ExternalInput")
    x = P.dram("x", [ntok, D], F32, "ExternalInput")
    cT = P.dram("cT", [128, 16], F32, "ExternalInput")
    adaw = P.dram("adaw", [D, 4096], F32, "ExternalInput")
    adab = P.dram("adab", [1, 4096], F32, "ExternalInput")
    wout = P.dram("wout", [D, D], F32, "ExternalInput")
    lnp = P.dram("lnp", [128, 4096], F32, "ExternalInput")
    w1 = P.dram("w1", [E, D, FF], F32, "ExternalInput")
    w3 = P.dram("w3", [E, D, FF], F32, "ExternalInput")
    w2 = P.dram("w2", [E, FF, D], F32, "ExternalInput")
    if moe:
        wr = P.dram("wr", [D, 8], F32, "ExternalInput")
    xo = P.dram("xo", [ntok, D], F32, "ExternalOutput")
    r_xo = Reg()
    w1b = P.dram("w1b", [E, D, FF], BF16, "Internal")
    w3b = P.dram("w3b", [E, D, FF], BF16, "Internal")
    w2b = P.dram("w2b", [E, FF, D], BF16, "Internal")
    r_wbf = [Reg() for _ in range(E)]
    for e_ in range(E):
        for k in range(8):
            P.dma("pool", w1b[e_, k * 128:(k + 1) * 128, :], w1[e_, k * 128:(k + 1) * 128, :], writes=[r_wbf[e_]])
            P.dma("pool", w3b[e_, k * 128:(k + 1) * 128, :], w3[e_, k * 128:(k + 1) * 128, :], writes=[r_wbf[e_]])
        for f in range(22):
            P.dma("pool", w2b[e_, f * 128:(f + 1) * 128, :], w2[e_, f * 128:(f + 1) * 128, :], writes=[r_wbf[e_]])

    fb = [P.ps([128, 512], F32) for _ in range(6)]
    tb = [P.ps([128, 8, 128], BF16) for _ in range(1)]
    tf = [P.ps([128, 4, 128], F32) for _ in range(1)]
    ones_row = P.sb([1, 128], F32)
    P.op("dve", lambda e: e.memset(ones_row[:], 1.0), writes=[Reg()])
    ident, r_id = make_ident(P)
    if moe:
        identf, r_idf = make_ident(P, F32)
        wrs = P.sb([128, 8, 8], F32)
        r_wrs = Reg()
        P.dma("sp", wrs[:], wr.rearrange("(k p) e -> p k e", p=128), writes=[r_wrs])
    wob = P.sb([128, 8, D], BF16)
    r_wob = Reg()
    for k in range(8):
        P.dma("pool", wob[:, k, :], wout[k * 128:(k + 1) * 128, :], writes=[r_wob])
    lns = P.sb([128, 4096], F32)
    r_lns = Reg()
    P.dma("sp", lns[:], lnp[:, :], writes=[r_lns])
    bcA, r_bcA = mods_block(P, cT, adaw[:, 0:2048], adab[:, 0:2048], 2048, ones_row, fb[:4], [])
    bcB, r_bcB = mods_block(P, cT, adaw[:, 2048:4096], adab[:, 2048:4096], 2048, ones_row, fb[:4], [(0, 1024)])

    tiles_all = [(i * 128, 128, 0) for i in range(n_lat // 128)]
    if n_ctx:
        tiles_all.append((n_lat, n_ctx, 1))
    SB = 4
    sblocks = [tiles_all[i:i + SB] for i in range(0, n_lat // 128, SB)]
    if n_ctx:
        sblocks[-1] = sblocks[-1] + [tiles_all[-1]]
    MT = SB + 1
    NTOK = SB * 128 + n_ctx

    def T(shape, dt=F32, n=2):
        return [P.sb(list(shape), dt) for _ in range(n)], [Reg() for _ in range(n)]
    xs, r_xs = T((128, D))
    ms, r_ms = T((128, D))
    mb, r_mb = T((128, D), BF16)
    mT, r_mT = T((128, 8, 128), BF16)
    yv, r_yv = T((128, D))
    tmp, r_tmp = T((128, D))
    stat, r_stat = T((128, 16))
    hb, r_hb = T((128, D), BF16)
    x1 = P.sb([128, MT, D], F32)
    r_x1 = [Reg() for _ in range(MT)]
    acc = P.sb([128, MT, D], F32)
    r_acc = [Reg() for _ in range(MT)]
    h2T = P.sb([128, 8, NTOK], BF16)
    r_h2T = Reg()
    if moe:
        hf, r_hf = T((128, D))
        hTf, r_hTf = T((128, 8, 128))
        lg, r_lg = T((128, 32))
        gates = P.sb([128, MT, 8], F32)
        r_gates = [Reg() for _ in range(MT)]
    UF = 2
    units = [(f0, min(UF, 22 - f0)) for f0 in range(0, 22, UF)]
    w1u, r_w1u = T((128, 8, UF * 128), BF16)
    w3u, r_w3u = T((128, 8, UF * 128), BF16)
    w2u, r_w2u = T((128, UF, D), BF16)
    GT, r_GT = T((128, UF, NTOK), BF16)
    sa, r_sa = T((128, 512))
    wq = [0]
    it = [0]

    for sbk in sblocks:
        ntk = sum(r for (_, r, _) in sbk)
        col = 0
        for ti, (t0, rows, s) in enumerate(sbk):
            i = it[0] % 2
            it[0] += 1
            P.dma("sp", ms[i][:rows, :], mix[t0:t0 + rows, :], writes=[r_ms[i]])
            P.dma("sp", xs[i][:rows, :], x[t0:t0 + rows, :], writes=[r_xs[i]])
            P.op("pool", lambda e: e.tensor_copy(out=mb[i][:rows, :], in_=ms[i][:rows, :]), reads=[r_ms[i]], writes=[r_mb[i]])
            tp, r_tp = tb[0]
            for k in range(8):
                P.op("pe", lambda e, k=k: e.transpose(tp[:, k, :rows], mb[i][:rows, k * 128:(k + 1) * 128], ident[:rows, :rows]),
                     reads=[r_mb[i], r_id], writes=[r_tp], inc=(k == 7))
            P.op("act", lambda e: e.copy(out=mT[i][:, :, :rows], in_=tp[:, :, :rows]), reads=[r_tp], writes=[r_mT[i]])
            P.op("act", lambda e: e.mul(out=yv[i][:rows, :], in_=xs[i][:rows, :], mul=ALPHA), reads=[r_xs[i]], writes=[r_yv[i]])
            for cb in range(2):
                bank, rb = fb[4 + cb]
                for k in range(8):
                    P.op("pe", lambda e, k=k, cb=cb, bank=bank: e.matmul(bank[:rows, :], mT[i][:, k, :rows], wob[:, k, cb * 512:(cb + 1) * 512],
                                                                   start=(k == 0), stop=(k == 7)), reads=[r_mT[i], r_wob], writes=[rb], inc=(k == 7))
                P.op("dve", lambda e, cb=cb, bank=bank: e.tensor_tensor(out=tmp[i][:rows, cb * 512:(cb + 1) * 512], in0=bank[:rows, :],
                                                                in1=bcA[s][:rows, cb * 512:(cb + 1) * 512], op=ALU.mult),
                     reads=[rb, r_bcA[s]], writes=[r_tmp[i]])
            P.op("pool", lambda e: e.tensor_tensor(out=yv[i][:rows, :], in0=yv[i][:rows, :], in1=tmp[i][:rows, :], op=ALU.add), reads=[r_yv[i], r_tmp[i]], writes=[r_yv[i]])
            ln_tile(P, rows, yv[i][:rows, :], r_yv[i], tmp[i], r_tmp[i], stat[i], r_stat[i])
            P.op("pool", lambda e: e.tensor_tensor(out=tmp[i][:rows, :], in0=tmp[i][:rows, :], in1=lns[:rows, 0:1024], op=ALU.mult), reads=[r_tmp[i], r_lns], writes=[r_tmp[i]])
            P.op("dve", lambda e: e.tensor_tensor(out=x1[:rows, ti, :], in0=tmp[i][:rows, :], in1=lns[:rows, 1024:2048], op=ALU.add), reads=[r_tmp[i], r_lns], writes=[r_x1[ti]])
            ln_tile(P, rows, x1[:rows, ti, :], r_x1[ti], tmp[i], r_tmp[i], stat[i], r_stat[i])
            P.op("pool", lambda e: e.tensor_tensor(out=tmp[i][:rows, :], in0=tmp[i][:rows, :], in1=bcB[s][:rows, 0:1024], op=ALU.mult), reads=[r_tmp[i], r_bcB[s]], writes=[r_tmp[i]])
            if moe:
                P.op("dve", lambda e: e.tensor_tensor(out=hf[i][:rows, :], in0=tmp[i][:rows, :], in1=bcA[s][:rows, 1024:2048], op=ALU.add), reads=[r_tmp[i], r_bcA[s]], writes=[r_hf[i]])
                P.op("pool", lambda e: e.tensor_copy(out=hb[i][:rows, :], in_=hf[i][:rows, :]), reads=[r_hf[i]], writes=[r_hb[i]])
            else:
                P.op("dve", lambda e: e.tensor_tensor(out=hb[i][:rows, :], in0=tmp[i][:rows, :], in1=bcA[s][:rows, 1024:2048], op=ALU.add), reads=[r_tmp[i], r_bcA[s]], writes=[r_hb[i]])
            for k in range(8):
                P.op("pe", lambda e, k=k: e.transpose(tp[:, k, :rows], hb[i][:rows, k * 128:(k + 1) * 128], ident[:rows, :rows]),
                     reads=[r_hb[i], r_id], writes=[r_tp], inc=(k == 7))
            P.op("act", lambda e, col=col: e.copy(out=h2T[:, :, col:col + rows], in_=tp[:, :, :rows]), reads=[r_tp], writes=[r_h2T])
            if moe:
                tq, r_tq = tf[0]
                for half in range(2):
                    for k in range(4):
                        kk = half * 4 + k
                        P.op("pe", lambda e, k=k, kk=kk: e.transpose(tq[:, k, :rows], hf[i][:rows, kk * 128:(kk + 1) * 128], identf[:rows, :rows]),
                             reads=[r_hf[i], r_idf], writes=[r_tq], inc=(k == 3))
                    P.op("dve", lambda e, half=half: e.tensor_copy(out=hTf[i][:, half * 4:half * 4 + 4, :rows], in_=tq[:, :, :rows]), reads=[r_tq], writes=[r_hTf[i]])
                bank, rb = fb[4]
                for k in range(8):
                    P.op("pe", lambda e, k=k, bank=bank: e.matmul(bank[:rows, 0:8], hTf[i][:, k, :rows], wrs[:, k, :], start=(k == 0), stop=(k == 7)),
                         reads=[r_hTf[i], r_wrs], writes=[rb], inc=(k == 7))
                L = lg[i]
                rl = r_lg[i]
                P.op("dve", lambda e, bank=bank: e.tensor_copy(out=L[:rows, 0:8], in_=bank[:rows, 0:8]), reads=[rb], writes=[rl])
                P.op("dve", lambda e: e.tensor_reduce(out=L[:rows, 24:25], in_=L[:rows, 0:8], axis=AX.X, op=ALU.max), reads=[rl], writes=[rl])
                P.op("dve", lambda e: e.tensor_scalar(out=L[:rows, 8:16], in0=L[:rows, 0:8], scalar1=L[:rows, 24:25], scalar2=None, op0=ALU.is_equal), reads=[rl], writes=[rl])
                P.op("dve", lambda e: e.scalar_tensor_tensor(out=L[:rows, 16:24], in0=L[:rows, 8:16], scalar=-1e30, in1=L[:rows, 0:8], op0=ALU.mult, op1=ALU.add), reads=[rl], writes=[rl])
                P.op("dve", lambda e: e.tensor_reduce(out=L[:rows, 25:26], in_=L[:rows, 16:24], axis=AX.X, op=ALU.max), reads=[rl], writes=[rl])
                P.op("dve", lambda e: e.tensor_scalar(out=L[:rows, 16:24], in0=L[:rows, 16:24], scalar1=L[:rows, 25:26], scalar2=None, op0=ALU.is_equal), reads=[rl], writes=[rl])
                P.op("dve", lambda e: e.tensor_tensor(out=L[:rows, 26:27], in0=L[:rows, 25:26], in1=L[:rows, 24:25], op=ALU.subtract), reads=[rl], writes=[rl])
                P.op("act", lambda e: e.activation(out=L[:rows, 26:27], in_=L[:rows, 26:27], func=AF.Exp), reads=[rl], writes=[rl])
                P.op("dve", lambda e: e.tensor_scalar_add(out=L[:rows, 27:28], in0=L[:rows, 26:27], scalar1=1.0), reads=[rl], writes=[rl])
                P.op("dve", lambda e: e.reciprocal(out=L[:rows, 27:28], in_=L[:rows, 27:28]), reads=[rl], writes=[rl])
                P.op("dve", lambda e: e.tensor_tensor(out=L[:rows, 28:29], in0=L[:rows, 26:27], in1=L[:rows, 27:28], op=ALU.mult), reads=[rl], writes=[rl])
                P.op("dve", lambda e: e.tensor_scalar(out=L[:rows, 8:16], in0=L[:rows, 8:16], scalar1=L[:rows, 27:28], scalar2=None, op0=ALU.mult), reads=[rl], writes=[rl])
                P.op("dve", lambda e, ti=ti: e.scalar_tensor_tensor(out=gates[:rows, ti, :], in0=L[:rows, 16:24], scalar=L[:rows, 28:29], in1=L[:rows, 8:16],
                                                                    op0=ALU.mult, op1=ALU.add), reads=[rl], writes=[r_gates[ti]])
            col += rows
        tblocks = []
        c0 = 0
        while c0 < ntk:
            n = min(512, ntk - c0)
            tblocks.append((c0, n))
            c0 += n
        for e_ in range(E):
            for (f0, nf) in units:
                q = wq[0] % 2
                wq[0] += 1
                P.dma("sp", w1u[q][:, :, 0:nf * 128], w1b[e_, :, f0 * 128:(f0 + nf) * 128].rearrange("(k p) f -> p k f", p=128),
                      reads=[r_wbf[e_]], writes=[r_w1u[q]])
                P.dma("act", w3u[q][:, :, 0:nf * 128], w3b[e_, :, f0 * 128:(f0 + nf) * 128].rearrange("(k p) f -> p k f", p=128),
                      reads=[r_wbf[e_]], writes=[r_w3u[q]])
                P.dma("sp", w2u[q][:, 0:nf, :], w2b[e_, f0 * 128:(f0 + nf) * 128, :].rearrange("(f p) d -> p f d", p=128),
                      reads=[r_wbf[e_]], writes=[r_w2u[q]])
                for (c0, n) in tblocks:
                    for f in range(nf):
                        ba, rba = fb[0 + (f % 2) * 2]
                        bb, rbb = fb[1 + (f % 2) * 2]
                        for k in range(8):
                            P.op("pe", lambda e, k=k, f=f, ba=ba: e.matmul(ba[:, 0:n], w1u[q][:, k, f * 128:(f + 1) * 128], h2T[:, k, c0:c0 + n],
                                                                     start=(k == 0), stop=(k == 7)), reads=[r_w1u[q], r_h2T], writes=[rba], inc=(k == 7))
                        for k in range(8):
                            P.op("pe", lambda e, k=k, f=f, bb=bb: e.matmul(bb[:, 0:n], w3u[q][:, k, f * 128:(f + 1) * 128], h2T[:, k, c0:c0 + n],
                                                                     start=(k == 0), stop=(k == 7)), reads=[r_w3u[q], r_h2T], writes=[rbb], inc=(k == 7))
                        j = f % 2
                        P.op("act", lambda e, ba=ba, j=j: e.activation(out=sa[j][:, 0:n], in_=ba[:, 0:n], func=AF.Silu), reads=[rba], writes=[r_sa[j]])
                        P.op("dve", lambda e, bb=bb, j=j, f=f: e.tensor_tensor(out=GT[q][:, f, c0:c0 + n], in0=bb[:, 0:n], in1=sa[j][:, 0:n], op=ALU.mult),
                             reads=[rbb, r_sa[j]], writes=[r_GT[q]])
                col = 0
                for ti, (t0, rows, s) in enumerate(sbk):
                    for cb in range(2):
                        bank, rb = fb[4 + cb]
                        for f in range(nf):
                            P.op("pe", lambda e, f=f, cb=cb, bank=bank, col=col: e.matmul(bank[:rows, :], GT[q][:, f, col:col + rows], w2u[q][:, f, cb * 512:(cb + 1) * 512],
                                                                                  start=(f == 0), stop=(f == nf - 1)),
                                 reads=[r_GT[q], r_w2u[q]], writes=[rb], inc=(f == nf - 1))
                        first = (e_ == 0 and f0 == 0)
                        dst = acc[:rows, ti, cb * 512:(cb + 1) * 512]
                        if moe:
                            gsc = gates[:rows, ti, e_:e_ + 1]
                            if first:
                                P.op("dve", lambda e, bank=bank, dst=dst, gsc=gsc: e.tensor_scalar(out=dst, in0=bank[:rows, :], scalar1=gsc, scalar2=None, op0=ALU.mult),
                                     reads=[rb, r_gates[ti]], writes=[r_acc[ti]])
                            else:
                                P.op("dve", lambda e, bank=bank, dst=dst, gsc=gsc: e.scalar_tensor_tensor(out=dst, in0=bank[:rows, :], scalar=gsc, in1=dst, op0=ALU.mult, op1=ALU.add),
                                     reads=[rb, r_gates[ti], r_acc[ti]], writes=[r_acc[ti]])
                        else:
                            if first:
                                P.op("act", lambda e, bank=bank, dst=dst: e.copy(out=dst, in_=bank[:rows, :]), reads=[rb], writes=[r_acc[ti]])
                            else:
                                P.op("dve", lambda e, bank=bank, dst=dst: e.tensor_tensor(out=dst, in0=bank[:rows, :], in1=dst, op=ALU.add),
                                     reads=[rb, r_acc[ti]], writes=[r_acc[ti]])
                    col += rows
        for ti, (t0, rows, s) in enumerate(sbk):
            i = it[0] % 2
            it[0] += 1
            P.op("pool", lambda e: e.tensor_tensor(out=acc[:rows, ti, :], in0=acc[:rows, ti, :], in1=bcB[s][:rows, 1024:2048], op=ALU.mult), reads=[r_acc[ti], r_bcB[s]], writes=[r_acc[ti]])
            P.op("dve", lambda e: e.scalar_tensor_tensor(out=yv[i][:rows, :], in0=x1[:rows, ti, :], scalar=ALPHA, in1=acc[:rows, ti, :], op0=ALU.mult, op1=ALU.add),
                 reads=[r_x1[ti], r_acc[ti]], writes=[r_yv[i]])
            ln_tile(P, rows, yv[i][:rows, :], r_yv[i], tmp[i], r_tmp[i], stat[i], r_stat[i])
            P.op("pool", lambda e: e.tensor_tensor(out=tmp[i][:rows, :], in0=tmp[i][:rows, :], in1=lns[:rows, 2048:3072], op=ALU.mult), reads=[r_tmp[i], r_lns], writes=[r_tmp[i]])
            P.op("dve", lambda e: e.tensor_tensor(out=yv[i][:rows, :], in0=tmp[i][:rows, :], in1=lns[:rows, 3072:4096], op=ALU.add), reads=[r_tmp[i], r_lns], writes=[r_yv[i]])
            P.dma("sp", xo[t0:t0 + rows, :], yv[i][:rows, :], reads=[r_yv[i]], writes=[r_xo])
    P.finish([r_xo])
    return P


def a2_consts():
    i = np.arange(128); half = i // 64
    same = half[:, None] == half[None, :]
    MST = (same & np.where(half[:, None] == 0, i[:, None] < i[None, :], i[:, None] > i[None, :])).astype(np.float32)
    MIT = MST + np.eye(128, dtype=np.float32)
    MS = np.ascontiguousarray(MST.T)
    LM = np.zeros((128, 96), np.float32)
    LM[:64, 0:16] = 1; LM[64:, 16:32] = 1; LM[:64, 32:48] = 1; LM[64:, 48:64] = 1; LM[:, 64:96] = 1
    return np.concatenate([MIT, MST, MIT, MS, LM], 1).astype(np.float32)

def a2_inputs(ur, n_lat, n_ctx, hd, mu, w0, wB, a0, aB, gB, kkw, ka, rk, gng, gnb):
    C = 256
    cols = np.concatenate([np.arange(hd * 64, hd * 64 + 64), C + np.arange(hd * 64, hd * 64 + 64), 2 * C + np.arange(hd * 64, hd * 64 + 64),
                           np.arange(768, 864)])
    u = ur[:, cols]
    def shifted(x, d):
        o = np.zeros_like(x)
        if d == 1: o[1:] = x[:-1]
        else: o[:-1] = x[1:]
        return o
    prev = np.concatenate([shifted(u[:n_lat], 1), shifted(u[n_lat:], 1)], 0)
    nxt = np.concatenate([shifted(u[:n_lat], -1), shifted(u[n_lat:], -1)], 0)
    u3 = np.concatenate([u, prev, nxt], 1)
    fwd, bwd = rwkv_orders(n_lat, n_ctx)
    idx = np.concatenate([np.concatenate([np.arange(f, f + 64), np.arange(b, b + 64)]) for f, b in zip(fwd, bwd)])
    U3 = np.ascontiguousarray(u3[idx])
    coefmu = np.tile(np.concatenate([mu[0][cols], mu[1][cols]])[None], (128, 1)).astype(np.float32)
    hs = slice(hd * 64, hd * 64 + 64)
    rowp = np.zeros((128, 128), np.float32)
    rowp[:64, :64] = w0[0][hs]; rowp[64:, :64] = w0[1][hs]; rowp[:64, 64:] = a0[0][hs]; rowp[64:, 64:] = a0[1][hs]
    hv = np.tile(np.concatenate([kkw[hs], ka[hs], rk[hd], gng[hs], gnb[hs]])[None], (128, 1)).astype(np.float32)
    Wl = np.zeros((96, 192), np.float32)
    Wl[0:16, 0:64] = wB[0][:, hs]; Wl[16:32, 0:64] = wB[1][:, hs]; Wl[32:48, 64:128] = aB[0][:, hs]; Wl[48:64, 64:128] = aB[1][:, hs]
    Wl[64:96, 128:192] = gB[:, hs]
    return dict(U3=U3, coefmu=coefmu, rowp=rowp, hv=hv, Wl=Wl, cmask=a2_consts())


def hy_ztab(L):
    bands = 16
    t = np.linspace(0.0, 1.0, L, dtype=np.float32)[:, None]
    f = np.linspace(1e-4, bands - 1, bands, dtype=np.float32)[None, :]
    wt = (np.float32(2.0 * math.pi) * np.arange(L, dtype=np.float32)[:, None] / np.float32(L)).astype(np.float32)
    z = np.concatenate([t, np.cos(f * wt), -np.sin(f * wt)], -1).astype(np.float32)
    return np.ascontiguousarray(np.stack([z.T, z[::-1].T], 0))

def a3_inputs(uh, n_lat, n_ctx, j, sw, sb, w1, b1, f1, w2, b2, f2, w3, dec, hbias):
    cs = slice(j * 64, j * 64 + 64)
    cols = np.concatenate([np.arange(256)[cs], 256 + np.arange(256)[cs], 512 + np.arange(256)[cs]])
    u = uh[:, cols]
    def shifted(x, d):
        o = np.zeros_like(x)
        if d == 1: o[1:] = x[:-1]
        else: o[:-1] = x[1:]
        return o
    prev = np.concatenate([shifted(u[:n_lat], 1), shifted(u[n_lat:], 1)], 0)
    nxt = np.concatenate([shifted(u[:n_lat], -1), shifted(u[n_lat:], -1)], 0)
    H3 = np.ascontiguousarray(np.concatenate([u, prev, nxt], 1), dtype=np.float32)
    cw = np.tile(np.concatenate([sw[1][cols], sw[0][cols], sw[2][cols], sb[cols]])[None], (128, 1)).astype(np.float32)
    fc = np.array([o * 512 + d * 256 + j * 64 + c for d in range(2) for o in range(2) for c in range(64)])
    colp = np.stack([b1, f1, b2, f2], 1).astype(np.float32)
    hb = np.tile(np.concatenate([hbias[0][cs], hbias[1][cs]])[None], (128, 1)).astype(np.float32)
    return dict(H3=H3, cw=cw, ztl=hy_ztab(n_lat), ztc=hy_ztab(n_ctx), w1=np.ascontiguousarray(w1, dtype=np.float32),
                w2=np.ascontiguousarray(w2, dtype=np.float32), w3=np.ascontiguousarray(w3[:, fc], dtype=np.float32), colp=colp,
                dec=np.ascontiguousarray(dec[fc][None], dtype=np.float32), hb=hb)


N_LAT = 16384
N_CTX = 256
_PROGS = {}


def _prog(name):
    if name not in _PROGS:
        if name == "p1":
            P = build_p1(4096, 64)
        elif name == "a1":
            P = build_a1(N_LAT, N_CTX)
        elif name == "a2":
            P = build_a2(N_LAT, N_CTX)
        elif name == "a3":
            P = build_a3(N_LAT, N_CTX)
        elif name == "p2d":
            P = build_p2(4096, 64, False)
        elif name == "p2m":
            P = build_p2(4096, 64, True)
        _PROGS[name] = P.close()
    return _PROGS[name]


def _run(name, in_maps, out_name):
    nc = _prog(name)
    in_maps = [{k: np.ascontiguousarray(v, dtype=np.float32) for k, v in m.items()} for m in in_maps]
    res = run_bass_kernel_spmd(nc, in_maps, core_ids=list(range(8)))
    return [np.asarray(r[out_name]) for r in res.results]


def _rope_cs(L):
    rows = L // 64
    row = np.repeat(np.arange(rows, dtype=np.float32), 64)
    col = np.tile(np.arange(64, dtype=np.float32), rows)
    inv = (np.float32(10000.0) ** (-np.arange(16, dtype=np.float32) / np.float32(16))).astype(np.float32)
    ang = np.concatenate([row[:, None] * inv, col[:, None] * inv], -1).astype(np.float32)
    cos, sin = np.cos(ang).astype(np.float32), np.sin(ang).astype(np.float32)
    return np.concatenate([cos, cos, cos, sin, sin, sin], -1).astype(np.float32)


def _cT(cb, cctx):
    c2 = np.stack([cb, cctx], 0).astype(np.float32)
    return np.ascontiguousarray(c2.reshape(2, 8, 128).transpose(2, 0, 1).reshape(128, 16))


def kernel(x, c, ctx, c_ctx, ada_w, ada_b, w_in, w_out, q_gain, k_gain,
           rwkv_mu, rwkv_w0, rwkv_wB, rwkv_a0, rwkv_aB, rwkv_gB, rwkv_kk, rwkv_ka, rwkv_rk,
           rwkv_gn_g, rwkv_gn_b, hy_short_w, hy_short_b, hy_w1, hy_b1, hy_freq1, hy_w2, hy_b2,
           hy_freq2, hy_w3, hy_decay, hy_bias, ln1_g, ln1_b, ln2_g, ln2_b,
           ffn_w1, ffn_w3, ffn_w2, moe_router, moe_w1, moe_w3, moe_w2):
    f32 = lambda a: np.asarray(a, dtype=np.float32)
    x = f32(x).copy()
    xc = f32(ctx).copy()
    c, c_ctx = f32(c), f32(c_ctx)
    cs = _rope_cs(N_LAT)
    depth = 2
    for l in range(depth):
        aw, ab = f32(ada_w[l]), f32(ada_b[l])
        cores = [(b, q) for b in range(2) for q in range(4)]
        ins = []
        for (b, q) in cores:
            xt = np.concatenate([x[b, q * 4096:(q + 1) * 4096], xc[b, q * 64:(q + 1) * 64]], 0)
            ins.append(dict(x=xt, cT=_cT(c[b], c_ctx), adaw=aw[:, 0:2048], adab=ab[None, 0:2048], win=f32(w_in[l])))
        us = _run("p1", ins, "u")
        u = np.empty((2, N_LAT + N_CTX, 2400), np.float32)
        for (b, q), uu in zip(cores, us):
            u[b, q * 4096:(q + 1) * 4096] = uu[:4096]
            u[b, N_LAT + q * 64:N_LAT + (q + 1) * 64] = uu[4096:]
        mixo = np.empty((2, N_LAT + N_CTX, 1024), np.float32)
        gains = np.tile(np.concatenate([f32(q_gain[l]), f32(q_gain[l]), f32(k_gain[l])])[None], (128, 1))
        ins = []
        for (b, j) in cores:
            g = j // 2
            qk = np.concatenate([u[b][:, 128 * j:128 * j + 128], u[b][:, 512 + 64 * g:512 + 64 * g + 64]], 1)
            ins.append(dict(qk=qk, v=u[b][:, 640 + 64 * g:640 + 64 * g + 64], gains=gains, cs=cs))
        for (b, j), o in zip(cores, _run("a1", ins, "att")):
            mixo[b][:, 128 * j:128 * j + 128] = o
        ins = []
        for (b, j) in cores:
            ins.append(a2_inputs(u[b][:, 768:1632], N_LAT, N_CTX, j, f32(rwkv_mu[l]), f32(rwkv_w0[l]), f32(rwkv_wB[l]), f32(rwkv_a0[l]),
                                 f32(rwkv_aB[l]), f32(rwkv_gB[l]), f32(rwkv_kk[l]), f32(rwkv_ka[l]), f32(rwkv_rk[l]),
                                 f32(rwkv_gn_g[l]), f32(rwkv_gn_b[l])))
        for (b, j), o in zip(cores, _run("a2", ins, "rw")):
            mixo[b][:, 512 + 64 * j:512 + 64 * j + 64] = o
        ins = []
        for (b, j) in cores:
            ins.append(a3_inputs(u[b][:, 1632:2400], N_LAT, N_CTX, j, f32(hy_short_w[l]), f32(hy_short_b[l]), f32(hy_w1[l]), f32(hy_b1[l]),
                                 f32(hy_freq1[l]), f32(hy_w2[l]), f32(hy_b2[l]), f32(hy_freq2[l]), f32(hy_w3[l]), f32(hy_decay[l]),
                                 f32(hy_bias[l])))
        for (b, j), o in zip(cores, _run("a3", ins, "hy")):
            mixo[b][:, 768 + 64 * j:768 + 64 * j + 64] = o
        lnp = np.tile(np.concatenate([f32(ln1_g[l]), f32(ln1_b[l]), f32(ln2_g[l]), f32(ln2_b[l])])[None], (128, 1))
        jj = l // 2
        ins = []
        for (b, q) in cores:
            mt = np.concatenate([mixo[b, q * 4096:(q + 1) * 4096], mixo[b, N_LAT + q * 64:N_LAT + (q + 1) * 64]], 0)
            xt = np.concatenate([x[b, q * 4096:(q + 1) * 4096], xc[b, q * 64:(q + 1) * 64]], 0)
            d = dict(mix=mt, x=xt, cT=_cT(c[b], c_ctx), adaw=aw[:, 2048:6144], adab=ab[None, 2048:6144], wout=f32(w_out[l]), lnp=lnp)
            if l % 2 == 0:
                d.update(w1=f32(ffn_w1[jj])[None], w3=f32(ffn_w3[jj])[None], w2=f32(ffn_w2[jj])[None])
            else:
                d.update(w1=f32(moe_w1[jj]), w3=f32(moe_w3[jj]), w2=f32(moe_w2[jj]), wr=f32(moe_router[jj]))
            ins.append(d)
        outs = _run("p2d" if l % 2 == 0 else "p2m", ins, "xo")
        xn = np.empty_like(x)
        xcn = np.empty_like(xc)
        for (b, q), o in zip(cores, outs):
            xn[b, q * 4096:(q + 1) * 4096] = o[:4096]
            xcn[b, q * 64:(q + 1) * 64] = o[4096:]
        x, xc = xn, xcn
    return x
```

```python
import math


import numpy as np
from contextlib import ExitStack
import concourse.bass as bass
import concourse.mybir as mybir
from concourse.bass_utils import run_bass_kernel_spmd

F32 = mybir.dt.float32
BF16 = mybir.dt.bfloat16
AF = mybir.ActivationFunctionType
ALU = mybir.AluOpType
AX = mybir.AxisListType
NDS = 24


class Reg:
    __slots__ = ("w", "r", "name", "excl")

    def __init__(self, name="", excl=False):
        self.w = []
        self.r = {}
        self.name = name
        self.excl = excl


class _EngRec:
    def __init__(self):
        self.call = None

    def __getattr__(self, name):
        def f(*a, **k):
            self.call = (name, a, k)
        return f


class Prog:
    def __init__(self):
        self.nc = bass.Bass("TRN2", target_bir_lowering=False)
        self.es = ExitStack()
        nc = self.nc
        self.eng = {"pe": nc.tensor, "act": nc.scalar, "dve": nc.vector, "pool": nc.gpsimd, "sp": nc.sync}
        self.sem = {k: self.es.enter_context(nc.semaphore("s_" + k)) for k in self.eng}
        self.seq = {k: 0 for k in self.eng}
        self.known = {k: {} for k in self.eng}
        self.pend = {k: ([], []) for k in self.eng}
        self.dsem = [self.es.enter_context(nc.semaphore("d%d" % i)) for i in range(NDS)]
        self.dval = [0] * NDS
        self.dnext = 0
        self.ninst = 0
        self._n = 0
        self.scopes = []
        self.ccsem = None
        self.rec = None

    def sb(self, shape, dt, name=None):
        self._n += 1
        es = self.scopes[-1] if self.scopes else self.es
        return es.enter_context(self.nc.sbuf_tensor(name or "t%d" % self._n, list(shape), dt))

    def push_scope(self):
        self.scopes.append(ExitStack())

    def pop_scope(self, regs):
        for r in regs:
            for e in self.eng:
                for t in r.w:
                    self._wait1(e, t)
        self.scopes.pop().close()

    def ps(self, shape, dt, name=None):
        self._n += 1
        nbytes = int(np.prod(shape[1:])) * (4 if dt == F32 else 2)
        assert nbytes % 2048 == 0, "PSUM tensors must be whole banks"
        es = self.scopes[-1] if self.scopes else self.es
        return es.enter_context(self.nc.psum_tensor(name or "p%d" % self._n, list(shape), dt)), Reg(excl=True)

    def dram(self, name, shape, dt, kind):
        return self.nc.dram_tensor(name, list(shape), dt, kind=kind).ap()

    def _wait1(self, e, tok):
        key, sem, val = tok
        if self.known[e].get(key, 0) < val:
            self.eng[e].wait_ge(sem, val)
            self.known[e][key] = val
            self.ninst += 1

    def _deps(self, e, reads, writes, is_dma):
        for r in reads:
            for t in r.w:
                if t[0] == e and e == "pe" and not is_dma:
                    continue
                self._wait1(e, t)
            if r.excl:
                for k, t in r.r.items():
                    if k != e or is_dma:
                        self._wait1(e, t)
        for w in writes:
            for t in w.w:
                if not (t[0] == e and not is_dma):
                    self._wait1(e, t)
            for k, t in w.r.items():
                if k == e and not is_dma:
                    continue
                self._wait1(e, t)

    def op(self, e, fn, reads=(), writes=(), inc=True):
        if self.rec is not None:
            prox = _EngRec()
            fn(prox)
            name, a, k = prox.call
            self.rec.append(("op", e, (lambda eng, name=name, a=a, k=k: getattr(eng, name)(*a, **k)), tuple(reads), tuple(writes), inc))
            return None
        self._deps(e, reads, writes, False)
        inst = fn(self.eng[e])
        self.ninst += 1
        pr, pw = self.pend[e]
        pr.extend(reads)
        pw.extend(writes)
        if inc:
            self.seq[e] += 1
            inst.then_inc(self.sem[e], 1)
            tok = (e, self.sem[e], self.seq[e])
            for r in pr:
                r.r[e] = tok
            for w in pw:
                w.w = [tok]
                w.r = {}
            self.pend[e] = ([], [])
        return inst

    def dma(self, q, out, in_, reads=(), writes=(), **kw):
        if self.rec is not None:
            self.rec.append(("dma", q, out, in_, tuple(reads), tuple(writes), kw))
            return None
        self._deps(q, reads, writes, True)
        slot = self.dnext
        self.dnext = (slot + 1) % NDS
        key = ("d", slot)
        if self.dval[slot] > 0:
            self._wait1(q, (key, self.dsem[slot], self.dval[slot]))
        inst = self.eng[q].dma_start(out=out, in_=in_, **kw)
        self.ninst += 1
        self.dval[slot] += 16
        inst.then_inc(self.dsem[slot], 16)
        tok = (key, self.dsem[slot], self.dval[slot])
        for r in reads:
            r.r[key] = tok
        for w in writes:
            w.w = [t for t in w.w if isinstance(t[0], tuple) and t[0] != key] + [tok]
            w.r = {}
        return inst

    def allgather(self, out, in_, groups, reads=(), writes=()):
        q = "pool"
        self._deps(q, reads, writes, True)
        if self.ccsem is None:
            self.ccsem = self.es.enter_context(self.nc.semaphore("ccsem"))
            self.ccval = 0
        inst = self.eng[q].collective_compute("AllGather", ALU.bypass, replica_groups=groups, ins=[in_], outs=[out])
        self.ninst += 1
        self.ccval += 1
        inst.then_inc(self.ccsem, 1)
        tok = ("cc", self.ccsem, self.ccval)
        for r in reads:
            r.r["cc"] = tok
        for w in writes:
            w.w = [tok]
            w.r = {}
        return inst

    def record(self):
        self.rec = []

    def stop_record(self):
        r, self.rec = self.rec, None
        return r

    def replay_interleaved(self, lists):
        n = max(len(l) for l in lists)
        for i in range(n):
            for l in lists:
                if i < len(l):
                    it = l[i]
                    if it[0] == "op":
                        self.op(it[1], it[2], it[3], it[4], it[5])
                    else:
                        self.dma(it[1], it[2], it[3], it[4], it[5], **it[6])

    def replay_skewed(self, lists, nact):
        L = max(len(l) for l in lists)
        D = -(-L // nact)
        T = (len(lists) - 1) * D + L
        for t in range(T):
            c0 = max(0, (t - L) // D)
            for c in range(c0, min(len(lists), t // D + 1)):
                k = t - c * D
                l = lists[c]
                if 0 <= k < len(l):
                    it = l[k]
                    if it[0] == "op":
                        self.op(it[1], it[2], it[3], it[4], it[5])
                    else:
                        self.dma(it[1], it[2], it[3], it[4], it[5], **it[6])

    def finish(self, regs):
        for r in regs:
            for t in r.w:
                self._wait1("sp", t)
        for slot in range(NDS):
            if self.dval[slot] > 0:
                self._wait1("sp", (("d", slot), self.dsem[slot], self.dval[slot]))

    def close(self):
        self.es.close()
        return self.nc


D = 1024
INW = 2400
LN_EPS = 1e-6


def mods_block(P, cT, adaw, adab, ncol, ones_row, psum_banks, plus_one_ranges):
    ncb = ncol // 512
    assert ncb <= len(psum_banks)
    bc = [P.sb([128, ncol], F32) for _ in range(2)]
    r_bc = [Reg(), Reg()]
    P.push_scope()
    c_sb = P.sb([128, 16], F32)
    cs_sb = P.sb([128, 16], F32)
    r_c = Reg()
    r_cs = Reg()
    P.dma("sp", c_sb[:], cT[:, :], writes=[r_c])
    P.op("act", lambda e: e.activation(out=cs_sb[:], in_=c_sb[:], func=AF.Silu), reads=[r_c], writes=[r_cs])
    ab_sb = P.sb([1, ncol], F32)
    r_ab = Reg()
    P.dma("sp", ab_sb[:], adab[:, :], writes=[r_ab])
    aw = [P.sb([128, ncol], F32) for _ in range(2)]
    r_aw = [Reg(), Reg()]
    modrow = P.sb([1, ncol], F32)
    r_mr = Reg()
    it = 0
    for s in range(2):
        for k in range(8):
            b = it % 2
            it += 1
            P.dma("sp", aw[b][:], adaw[k * 128:(k + 1) * 128, :], writes=[r_aw[b]])
            for cb in range(ncb):
                bank, rb = psum_banks[cb]
                P.op("pe", lambda e, s=s, cb=cb, bank=bank, k=k, b=b: e.matmul(
                    bank[0:1, 0:512], cs_sb[:, s * 8 + k:s * 8 + k + 1], aw[b][:, cb * 512:(cb + 1) * 512],
                    start=(k == 0), stop=(k == 7)),
                    reads=[r_cs, r_aw[b]], writes=[rb], inc=(cb == ncb - 1))
        for cb in range(ncb):
            bank, rb = psum_banks[cb]
            P.op("dve", lambda e, cb=cb, bank=bank: e.tensor_tensor(
                out=modrow[0:1, cb * 512:(cb + 1) * 512], in0=bank[0:1, 0:512],
                in1=ab_sb[0:1, cb * 512:(cb + 1) * 512], op=ALU.add),
                reads=[rb, r_ab], writes=[r_mr])
        for (a, b2) in plus_one_ranges:
            P.op("dve", lambda e, a=a, b2=b2: e.tensor_scalar_add(out=modrow[0:1, a:b2], in0=modrow[0:1, a:b2], scalar1=1.0),
                 reads=[r_mr], writes=[r_mr])
        for cb in range(ncb):
            bank, rb = psum_banks[cb]
            P.op("pe", lambda e, cb=cb, bank=bank: e.matmul(
                bank[:, 0:512], ones_row[0:1, 0:128], modrow[0:1, cb * 512:(cb + 1) * 512], start=True, stop=True),
                reads=[r_mr], writes=[rb])
            P.op("act", lambda e, s=s, cb=cb, bank=bank: e.copy(out=bc[s][:, cb * 512:(cb + 1) * 512], in_=bank[:, 0:512]),
                 reads=[rb], writes=[r_bc[s]])
    P.pop_scope([r_bc[1]])
    return bc, r_bc


def ln_tile(P, rows, x_ap, r_x, tmp, r_tmp, stat, r_stat):
    st = stat
    P.op("dve", lambda e: e.bn_stats(out=st[:rows, 0:6], in_=x_ap[:, 0:512]), reads=[r_x], writes=[r_stat])
    P.op("dve", lambda e: e.bn_stats(out=st[:rows, 6:12], in_=x_ap[:, 512:1024]), reads=[r_x], writes=[r_stat])
    P.op("dve", lambda e: e.bn_aggr(out=st[:rows, 12:14], in_=st[:rows, 0:12]), reads=[r_stat], writes=[r_stat])
    P.op("dve", lambda e: e.tensor_scalar_add(out=st[:rows, 15:16], in0=st[:rows, 13:14], scalar1=LN_EPS), reads=[r_stat], writes=[r_stat])
    P.op("act", lambda e: e.activation(out=st[:rows, 14:15], in_=st[:rows, 15:16], func=AF.Sqrt), reads=[r_stat], writes=[r_stat])
    P.op("dve", lambda e: e.reciprocal(out=st[:rows, 14:15], in_=st[:rows, 14:15]), reads=[r_stat], writes=[r_stat])
    P.op("dve", lambda e: e.tensor_scalar(out=tmp[:rows, :], in0=x_ap, scalar1=st[:rows, 12:13], scalar2=st[:rows, 14:15],
                                          op0=ALU.subtract, op1=ALU.mult), reads=[r_x, r_stat], writes=[r_tmp])


def make_ident(P, dt=BF16):
    ident = P.sb([128, 128], dt)
    r_id = Reg()
    P.op("pool", lambda e: e.memset(ident[:], 0.0), writes=[r_id])
    P.op("pool", lambda e: e.affine_select(out=ident[:], in_=ident[:], pattern=[[-1, 128]], compare_op=ALU.not_equal,
                                           fill=1.0, base=0, channel_multiplier=1), reads=[r_id], writes=[r_id])
    return ident, r_id


def build_p1(n_lat=4096, n_ctx=64):
    P = Prog()
    ntok = n_lat + n_ctx
    x = P.dram("x", [ntok, D], F32, "ExternalInput")
    cT = P.dram("cT", [128, 16], F32, "ExternalInput")
    adaw = P.dram("adaw", [D, 2048], F32, "ExternalInput")
    adab = P.dram("adab", [1, 2048], F32, "ExternalInput")
    win = P.dram("win", [D, INW], F32, "ExternalInput")
    u = P.dram("u", [ntok, INW], F32, "ExternalOutput")

    fb = [P.ps([128, 512], F32) for i in range(6)]
    tb = [P.ps([128, 8, 128], BF16) for i in range(2)]
    ones_row = P.sb([1, 128], F32)
    r_ones = Reg()
    P.op("dve", lambda e: e.memset(ones_row[:], 1.0), writes=[r_ones])
    ident, r_id = make_ident(P)
    wb = P.sb([128, 8, INW], BF16)
    r_wb = Reg()
    for k in range(8):
        P.dma("pool", wb[:, k, :], win[k * 128:(k + 1) * 128, :], writes=[r_wb])
    bc, r_bc = mods_block(P, cT, adaw, adab, 2048, ones_row, fb[:4], [(1024, 2048)])

    NX = 3
    xs = [P.sb([128, D], F32) for _ in range(NX)]
    r_xs = [Reg() for _ in range(NX)]
    tmp = [P.sb([128, D], F32) for _ in range(2)]
    r_tmp = [Reg() for _ in range(2)]
    tmp2 = [P.sb([128, D], F32) for _ in range(2)]
    r_tmp2 = [Reg() for _ in range(2)]
    hb = [P.sb([128, D], BF16) for _ in range(2)]
    r_hb = [Reg() for _ in range(2)]
    stat = [P.sb([128, 16], F32) for _ in range(2)]
    r_stat = [Reg() for _ in range(2)]
    hT = [P.sb([128, 8, 128], BF16) for _ in range(2)]
    r_hT = [Reg() for _ in range(2)]
    uo = [P.sb([128, INW], F32) for _ in range(2)]
    r_uo = [Reg() for _ in range(2)]
    r_u_out = Reg()

    tiles = [(i * 128, 128, 0) for i in range(n_lat // 128)]
    t0 = n_lat
    while t0 < ntok:
        rows = min(128, ntok - t0)
        tiles.append((t0, rows, 1))
        t0 += rows
    for i, (t0, rows, s) in enumerate(tiles):
        a = i % NX
        b = i % 2
        P.dma("sp", xs[a][:rows, :], x[t0:t0 + rows, :], writes=[r_xs[a]])
        ln_tile(P, rows, xs[a][:rows, :], r_xs[a], tmp[b], r_tmp[b], stat[b], r_stat[b])
        P.op("pool", lambda e: e.tensor_tensor(out=tmp2[b][:rows, :], in0=tmp[b][:rows, :], in1=bc[s][:rows, 1024:2048], op=ALU.mult),
             reads=[r_tmp[b], r_bc[s]], writes=[r_tmp2[b]])
        P.op("dve", lambda e: e.tensor_tensor(out=hb[b][:rows, :], in0=tmp2[b][:rows, :], in1=bc[s][:rows, 0:1024], op=ALU.add),
             reads=[r_tmp2[b], r_bc[s]], writes=[r_hb[b]])
        tp, r_tp = tb[b]
        for k in range(8):
            P.op("pe", lambda e, k=k: e.transpose(tp[:, k, :rows], hb[b][:rows, k * 128:(k + 1) * 128], ident[:rows, :rows]),
                 reads=[r_hb[b], r_id], writes=[r_tp], inc=(k == 7))
        P.op("act", lambda e: e.copy(out=hT[b][:, :, :rows], in_=tp[:, :, :rows]), reads=[r_tp], writes=[r_hT[b]])
        for cb in range(5):
            bank, rb = fb[1 + cb]
            for k in range(8):
                P.op("pe", lambda e, k=k, cb=cb, bank=bank: e.matmul(bank[:rows, 0:480], hT[b][:, k, :rows], wb[:, k, cb * 480:(cb + 1) * 480],
                                                       start=(k == 0), stop=(k == 7)),
                     reads=[r_hT[b], r_wb], writes=[rb], inc=(k == 7))
            eng = "act" if cb % 2 == 0 else "dve"
            if eng == "act":
                P.op("act", lambda e, cb=cb, bank=bank: e.copy(out=uo[b][:rows, cb * 480:(cb + 1) * 480], in_=bank[:rows, 0:480]),
                     reads=[rb], writes=[r_uo[b]])
            else:
                P.op("dve", lambda e, cb=cb, bank=bank: e.tensor_copy(out=uo[b][:rows, cb * 480:(cb + 1) * 480], in_=bank[:rows, 0:480]),
                     reads=[rb], writes=[r_uo[b]])
        P.dma("sp", u[t0:t0 + rows, :], uo[b][:rows, :], reads=[r_uo[b]], writes=[r_u_out])
    P.finish([r_u_out])
    return P


HD = 64
QK_EPS = 1e-6


def build_a1(n_lat=16384, n_ctx=256, stage=2):
    P = Prog()
    ntok = n_lat + n_ctx
    NT = ntok // 128
    NTL = n_lat // 128
    qk = P.dram("qk", [ntok, 192], F32, "ExternalInput")
    v = P.dram("v", [ntok, 64], F32, "ExternalInput")
    gains = P.dram("gains", [128, 192], F32, "ExternalInput")
    cs = P.dram("cs", [n_lat, 192], F32, "ExternalInput")
    att = P.dram("att", [ntok, 128], F32, "ExternalOutput")

    identb, r_idb = make_ident(P, BF16)
    identf, r_idf = make_ident(P, F32)
    g_sb = P.sb([128, 192], F32)
    r_g = Reg()
    P.dma("sp", g_sb[:], gains[:, :], writes=[r_g])

    QT = P.sb([64, 2, ntok], BF16)
    KT = P.sb([64, ntok], BF16)
    VA = P.sb([128, NT, 65], BF16)
    r_QT = Reg()
    r_KT = Reg()
    r_VA = Reg()
    P.op("pool", lambda e: e.memset(VA[:, :, 64:65], 1.0), writes=[r_VA])

    NSB = 3

    P.push_scope()
    tpb = P.ps([128, 8, 128], BF16)
    NB = 2
    qk_sb = [P.sb([128, 192], F32) for _ in range(NB)]
    r_qk = [Reg() for _ in range(NB)]
    v_sb = [P.sb([128, 64], F32) for _ in range(NB)]
    r_v = [Reg() for _ in range(NB)]
    cs_sb = [P.sb([128, 192], F32) for _ in range(NB)]
    r_cs = [Reg() for _ in range(NB)]
    junk = [P.sb([128, 64], F32) for _ in range(NB)]
    r_junk = [Reg() for _ in range(NB)]
    ss = [P.sb([128, 8], F32) for _ in range(NB)]
    r_ss = [Reg() for _ in range(NB)]
    qn = [P.sb([128, 192], F32) for _ in range(NB)]
    r_qn = [Reg() for _ in range(NB)]
    ra = [P.sb([128, 96], F32) for _ in range(NB)]
    rb_ = [P.sb([128, 96], F32) for _ in range(NB)]
    r_ra = [Reg() for _ in range(NB)]
    r_rb = [Reg() for _ in range(NB)]
    qr = [P.sb([128, 192], BF16) for _ in range(NB)]
    r_qr = [Reg() for _ in range(NB)]

    for t in range(NT):
        b = t % NB
        t0 = t * 128
        lat = t < NTL
        P.dma("sp", qk_sb[b][:], qk[t0:t0 + 128, :], writes=[r_qk[b]])
        P.dma("sp", v_sb[b][:], v[t0:t0 + 128, :], writes=[r_v[b]])
        if lat:
            P.dma("sp", cs_sb[b][:], cs[t0:t0 + 128, :], writes=[r_cs[b]])
        for h in range(3):
            P.op("act", lambda e, h=h: e.activation(out=junk[b][:], in_=qk_sb[b][:, h * 64:(h + 1) * 64], func=AF.Square,
                                                    accum_out=ss[b][:, h:h + 1]), reads=[r_qk[b]], writes=[r_junk[b], r_ss[b]])
        P.op("dve", lambda e: e.tensor_scalar(out=ss[b][:, 3:6], in0=ss[b][:, 0:3], scalar1=1.0 / 64, scalar2=QK_EPS,
                                              op0=ALU.mult, op1=ALU.add), reads=[r_ss[b]], writes=[r_ss[b]])
        P.op("act", lambda e: e.activation(out=ss[b][:, 3:6], in_=ss[b][:, 3:6], func=AF.Sqrt), reads=[r_ss[b]], writes=[r_ss[b]])
        P.op("dve", lambda e: e.reciprocal(out=ss[b][:, 3:6], in_=ss[b][:, 3:6]), reads=[r_ss[b]], writes=[r_ss[b]])
        for h in range(3):
            P.op("dve", lambda e, h=h: e.scalar_tensor_tensor(out=qn[b][:, h * 64:(h + 1) * 64], in0=qk_sb[b][:, h * 64:(h + 1) * 64],
                                                              scalar=ss[b][:, 3 + h:4 + h], in1=g_sb[:, h * 64:(h + 1) * 64],
                                                              op0=ALU.mult, op1=ALU.mult),
                 reads=[r_qk[b], r_ss[b], r_g], writes=[r_qn[b]])
        if lat:
            x0 = qn[b][:].rearrange("p (i two) -> p i two", two=2)[:, :, 0]
            x1 = qn[b][:].rearrange("p (i two) -> p i two", two=2)[:, :, 1]
            o0 = qr[b][:].rearrange("p (i two) -> p i two", two=2)[:, :, 0]
            o1 = qr[b][:].rearrange("p (i two) -> p i two", two=2)[:, :, 1]
            c_ = cs_sb[b][:, 0:96]
            s_ = cs_sb[b][:, 96:192]
            P.op("dve", lambda e: e.tensor_tensor(out=ra[b][:], in0=x0, in1=c_, op=ALU.mult), reads=[r_qn[b], r_cs[b]], writes=[r_ra[b]])
            P.op("pool", lambda e: e.tensor_tensor(out=rb_[b][:], in0=x1, in1=s_, op=ALU.mult), reads=[r_qn[b], r_cs[b]], writes=[r_rb[b]])
            P.op("dve", lambda e: e.tensor_tensor(out=o0, in0=ra[b][:], in1=rb_[b][:], op=ALU.subtract), reads=[r_ra[b], r_rb[b]], writes=[r_qr[b]])
            P.op("dve", lambda e: e.tensor_tensor(out=ra[b][:], in0=x0, in1=s_, op=ALU.mult), reads=[r_qn[b], r_cs[b]], writes=[r_ra[b]])
            P.op("pool", lambda e: e.tensor_tensor(out=rb_[b][:], in0=x1, in1=c_, op=ALU.mult), reads=[r_qn[b], r_cs[b]], writes=[r_rb[b]])
            P.op("dve", lambda e: e.tensor_tensor(out=o1, in0=ra[b][:], in1=rb_[b][:], op=ALU.add), reads=[r_ra[b], r_rb[b]], writes=[r_qr[b]])
        else:
            P.op("dve", lambda e: e.tensor_copy(out=qr[b][:], in_=qn[b][:]), reads=[r_qn[b]], writes=[r_qr[b]])
        P.op("pool", lambda e: e.tensor_copy(out=VA[:, t, 0:64], in_=v_sb[b][:]), reads=[r_v[b]], writes=[r_VA])
        tp, r_tp = tpb
        for h in range(3):
            P.op("pe", lambda e, h=h: e.transpose(tp[0:64, h, :], qr[b][:, h * 64:(h + 1) * 64], identb[:, :]),
                 reads=[r_qr[b], r_idb], writes=[r_tp], inc=(h == 2))
        P.op("act", lambda e: e.copy(out=QT[:, :, t0:t0 + 128], in_=tp[0:64, 0:2, :]), reads=[r_tp], writes=[r_QT])
        P.op("dve", lambda e: e.tensor_copy(out=KT[:, t0:t0 + 128], in_=tp[0:64, 2, :]), reads=[r_tp], writes=[r_KT])

    P.pop_scope([r_QT, r_KT, r_VA])
    sps = [P.ps([128, 1024], F32) for _ in range(NSB)]
    ops_ = [P.ps([128, 512], F32) for _ in range(1)]
    tpf = P.ps([128, 4, 128], F32)
    pt = [P.sb([128, 1024], BF16) for _ in range(NSB)]
    r_pt = [Reg() for _ in range(NSB)]
    osb = [P.sb([65, 512], F32) for _ in range(2)]
    r_osb = [Reg() for _ in range(2)]
    ot = [P.sb([128, 4, 64], F32) for _ in range(2)]
    r_ot = [Reg() for _ in range(2)]
    rc = [P.sb([128, 4], F32) for _ in range(2)]
    r_rc = [Reg() for _ in range(2)]
    r_att = Reg()
    blocks = [(qb * 512, 512, list(range(NT))) for qb in range(n_lat // 512)]
    blocks.append((n_lat, n_ctx, list(range(NTL, NT))))
    if stage < 2:
        blocks = []
    items = []
    groups = []
    for (q0, nq, kts) in blocks:
        for h in range(2):
            g = len(groups)
            groups.append((q0, nq, h))
            npair = len(kts) // 2
            for pi in range(npair):
                items.append((g, (kts[2 * pi], kts[2 * pi + 1]), pi == 0, pi == npair - 1))

    def emit_st(n):
        g, kt2, first, last = items[n]
        q0, nq, h = groups[g]
        sb_, r_sb = sps[n % NSB]
        for j in range(2):
            kt = kt2[j]
            P.op("pe", lambda e, j=j, kt=kt: e.matmul(sb_[:, j * 512:j * 512 + nq], KT[:, kt * 128:(kt + 1) * 128],
                                                      QT[:, h, q0:q0 + nq], start=True, stop=True),
                 reads=[r_KT, r_QT], writes=[r_sb], inc=(j == 1))

    def emit_rest(n):
        g, kt2, first, last = items[n]
        q0, nq, h = groups[g]
        sb_, r_sb = sps[n % NSB]
        p2 = n % NSB
        o2 = g % 2
        obank, r_ob = ops_[0]
        for j in range(2):
            P.op("act", lambda e, j=j: e.activation(out=pt[p2][:, j * 512:j * 512 + nq], in_=sb_[:, j * 512:j * 512 + nq],
                                                    func=AF.Exp, scale=0.125),
                 reads=[r_sb], writes=[r_pt[p2]])
        for j in range(2):
            kt = kt2[j]
            P.op("pe", lambda e, j=j, kt=kt: e.matmul(obank[0:65, 0:nq], VA[:, kt, :], pt[p2][:, j * 512:j * 512 + nq],
                                                      start=(first and j == 0), stop=(last and j == 1)),
                 reads=[r_VA, r_pt[p2]], writes=[r_ob], inc=(j == 1))
        if last:
            P.op("dve", lambda e: e.tensor_copy(out=osb[o2][:, 0:nq], in_=obank[0:65, 0:nq]), reads=[r_ob], writes=[r_osb[o2]])
            tf, r_tf = tpf
            nj = nq // 128
            for j in range(nj):
                P.op("pe", lambda e, j=j: e.transpose(tf[:, j, 0:65], osb[o2][0:65, j * 128:(j + 1) * 128], identf[0:65, 0:65]),
                     reads=[r_osb[o2], r_idf], writes=[r_tf], inc=(j == nj - 1))
            P.op("dve", lambda e: e.reciprocal(out=rc[o2][:, 0:nj], in_=tf[:, 0:nj, 64]), reads=[r_tf], writes=[r_rc[o2]])
            for j in range(nj):
                P.op("dve", lambda e, j=j: e.tensor_scalar(out=ot[o2][:, j, :], in0=tf[:, j, 0:64], scalar1=rc[o2][:, j:j + 1], scalar2=None,
                                                           op0=ALU.mult), reads=[r_tf, r_rc[o2]], writes=[r_ot[o2]])
            dst = att[q0:q0 + nq, h * 64:(h + 1) * 64].rearrange("(j p) d -> p j d", p=128)
            P.dma("sp", dst, ot[o2][:, 0:nj, :], reads=[r_ot[o2]], writes=[r_att])

    for n in range(min(NSB - 1, len(items))):
        emit_st(n)
    for n in range(len(items)):
        if n + NSB - 1 < len(items):
            emit_st(n + NSB - 1)
        emit_rest(n)
    P.finish([r_att])
    return P


GN_EPS = 64e-5
WSC = -0.6065306597126334


def rwkv_orders(n_lat, n_ctx, C=64):
    ncl, ncc = n_lat // C, n_ctx // C
    fwd = [n_lat + c * C for c in range(ncc)] + [c * C for c in range(ncl)]
    bwd = [n_lat + c * C for c in range(ncc - 1, -1, -1)] + [c * C for c in range(ncl - 1, -1, -1)]
    return fwd, bwd


def build_a2(n_lat=16384, n_ctx=256, stop=99, NBUF=4, REC=True):
    P = Prog()
    ntok = n_lat + n_ctx
    fwd, bwd = rwkv_orders(n_lat, n_ctx)
    NS = len(fwd)
    U3 = P.dram("U3", [NS * 128, 864], F32, "ExternalInput")
    coefmu = P.dram("coefmu", [128, 576], F32, "ExternalInput")
    rowp = P.dram("rowp", [128, 128], F32, "ExternalInput")
    hv = P.dram("hv", [128, 320], F32, "ExternalInput")
    Wl = P.dram("Wl", [96, 192], F32, "ExternalInput")
    cmask = P.dram("cmask", [128, 128 + 256 + 128 + 96], F32, "ExternalInput")
    rw = P.dram("rw", [ntok, 64], F32, "ExternalOutput")
    yfs = P.dram("yfs", [ntok, 64], F32, "Internal")
    ybs = P.dram("ybs", [ntok, 64], F32, "Internal")
    gs = P.dram("gs", [ntok, 64], F32, "Internal")
    r_yfs, r_ybs, r_gs, r_rw = Reg(), Reg(), Reg(), Reg()

    ident, r_id = make_ident(P, F32)
    cm = P.sb([128, 608], F32)
    r_cm = Reg()
    P.dma("sp", cm[:], cmask[:, :], writes=[r_cm])
    MIT = cm[:, 0:128]
    MM = cm[:, 128:384]
    MS = cm[:, 384:512]
    LM = cm[:, 512:608]
    coef = P.sb([128, 864], F32)
    r_coef = Reg()
    P.dma("sp", coef[:, 288:864], coefmu[:, :], writes=[r_coef])
    P.op("dve", lambda e: e.tensor_tensor(out=coef[:, 0:288], in0=coef[:, 288:576], in1=coef[:, 576:864], op=ALU.add), reads=[r_coef], writes=[r_coef])
    P.op("dve", lambda e: e.tensor_scalar(out=coef[:, 0:288], in0=coef[:, 0:288], scalar1=-1.0, scalar2=1.0, op0=ALU.mult, op1=ALU.add),
         reads=[r_coef], writes=[r_coef])
    rp = P.sb([128, 128], F32)
    hvs = P.sb([128, 320], F32)
    wl = P.sb([96, 192], F32)
    r_par = Reg()
    P.dma("sp", rp[:], rowp[:, :], writes=[r_par])
    P.dma("sp", hvs[:], hv[:, :], writes=[r_par])
    P.dma("sp", wl[:], Wl[:, :], writes=[r_par])
    KKW, KA, RK, GNG, GNB = (hvs[:, i * 64:(i + 1) * 64] for i in range(5))
    ones = P.sb([128, 128], F32)
    r_ones = Reg()
    P.op("pool", lambda e: e.memset(ones[:], 1.0), writes=[r_ones])

    banks = [P.ps([128, 512], F32) for _ in range(8)]
    if NBUF == 1 or not REC:
        bsets = [list(range(8))] * max(NBUF, 1)
    else:
        bsets = [[] for _ in range(NBUF)]
        for b_ in range(8):
            bsets[b_ * NBUF // 8].append(b_)
    bk = [0] * 8
    cur = [0]

    def nb():
        c = cur[0]
        b = banks[bsets[c][bk[c] % len(bsets[c])]]
        bk[c] += 1
        return b

    ev = [0]

    def evac(out, in_, reads, writes):
        ev[0] += 1
        if ev[0] % 2 == 0:
            P.op("act", lambda e: e.copy(out=out, in_=in_), reads=reads, writes=writes)
        else:
            P.op("dve", lambda e: e.tensor_copy(out=out, in_=in_), reads=reads, writes=writes)

    def T(shape=(128, 128), n=None):
        n = n or NBUF
        return [P.sb(list(shape), F32) for _ in range(n)], [Reg() for _ in range(n)]

    u3, r_u3 = T((128, 864))
    prod, r_prod = T((128, 864))
    us, r_us = T((128, 288))
    lo, r_lo = T((128, 96))
    loT, r_loT = T((96, 128))
    wa, r_wa = T((128, 128))
    gg, r_gg = T((128, 64))
    kk, r_kk = T((128, 64))
    sm, r_sm = T((128, 8))
    tmp, r_tmp = T((128, 64))
    tmp2, r_tmp2 = T((128, 64))
    kd, r_kd = T((128, 64))
    bdn = ["lw", "a", "b", "kd", "r", "v"]
    bd = {n: T() for n in bdn}
    for n in bdn:
        for i in range(NBUF):
            P.op("pool", lambda e, n=n, i=i: e.memset(bd[n][0][i][:], 0.0), writes=[bd[n][1][i]])
    ex, r_ex = T((128, 512))
    ee, r_ee = T((128, 256))
    At, r_At = T()
    BKt, r_BKt = T((128, 256))
    Rt, r_Rt = T()
    BKG, r_BKG = T((128, 256))
    gcc, r_gcc = T((128, 1))
    BKT, r_BKT = T((128, 256))
    ART, r_ART = T((128, 256))
    LA, r_LA = T((128, 256))
    LK, r_LK = T((128, 256))
    X, r_X = T()
    XT, r_XT = T()
    X2, r_X2 = T()
    XT2, r_XT2 = T()
    TT, r_TT = T()
    Pm, r_Pm = T()
    LV, r_LV = T()
    Q, r_Q = T()
    RpT, r_RpT = T()
    Mm, r_Mm = T()
    yo, r_yo = T()
    ST = [P.sb([128, 128], F32) for _ in range(2)]
    r_ST = [Reg(), Reg()]
    P.op("pool", lambda e: e.memset(ST[0][:], 0.0), writes=[r_ST[0]])

    lists = []
    for s in range(NS):
        i = s % NBUF
        cur[0] = i if REC else 0
        if REC:
            P.record()
        LW, r_LW = bd["lw"][0][i], bd["lw"][1][i]
        Ab, r_Ab = bd["a"][0][i], bd["a"][1][i]
        Bb, r_Bb = bd["b"][0][i], bd["b"][1][i]
        KDb, r_KDb = bd["kd"][0][i], bd["kd"][1][i]
        Rb, r_Rb = bd["r"][0][i], bd["r"][1][i]
        Vb, r_Vb = bd["v"][0][i], bd["v"][1][i]
        P.dma("sp", u3[i][:], U3[s * 128:(s + 1) * 128, :], writes=[r_u3[i]])
        P.op("dve", lambda e: e.tensor_tensor(out=prod[i][:], in0=u3[i][:], in1=coef[:], op=ALU.mult), reads=[r_u3[i], r_coef], writes=[r_prod[i]])
        P.op("pool", lambda e: e.tensor_tensor(out=us[i][:], in0=prod[i][:, 0:288], in1=prod[i][:, 288:576], op=ALU.add), reads=[r_prod[i]], writes=[r_us[i]])
        P.op("dve", lambda e: e.tensor_tensor(out=us[i][:], in0=us[i][:], in1=prod[i][:, 576:864], op=ALU.add), reads=[r_prod[i], r_us[i]], writes=[r_us[i]])
        r_ = us[i][:, 0:64]
        k_ = us[i][:, 64:128]
        v_ = us[i][:, 128:192]
        if stop <= 1:
            continue
        P.op("act", lambda e: e.activation(out=lo[i][:, 0:32], in_=us[i][:, 192:224], func=AF.Tanh), reads=[r_us[i]], writes=[r_lo[i]])
        P.op("act", lambda e: e.activation(out=lo[i][:, 64:96], in_=us[i][:, 256:288], func=AF.Sigmoid), reads=[r_us[i]], writes=[r_lo[i]])
        P.op("pool", lambda e: e.tensor_copy(out=lo[i][:, 32:64], in_=us[i][:, 224:256]), reads=[r_us[i]], writes=[r_lo[i]])
        P.op("dve", lambda e: e.tensor_tensor(out=lo[i][:], in0=lo[i][:], in1=LM, op=ALU.mult), reads=[r_lo[i], r_cm], writes=[r_lo[i]])
        b1, rb1 = nb()
        P.op("pe", lambda e: e.transpose(b1[0:96, 0:128], lo[i][:, :], ident[:, :]), reads=[r_lo[i], r_id], writes=[rb1])
        evac(loT[i][:, :], b1[0:96, 0:128], [rb1], [r_loT[i]])
        b2, rb2 = nb()
        P.op("pe", lambda e: e.matmul(b2[:, 0:192], loT[i][:, :], wl[:, :], start=True, stop=True), reads=[r_loT[i], r_par], writes=[rb2])
        P.op("dve", lambda e: e.tensor_tensor(out=wa[i][:], in0=b2[:, 0:128], in1=rp[:], op=ALU.add), reads=[rb2, r_par], writes=[r_wa[i]])
        P.op("act", lambda e: e.copy(out=gg[i][:], in_=b2[:, 128:192]), reads=[rb2], writes=[r_gg[i]])
        P.op("act", lambda e: e.activation(out=wa[i][:], in_=wa[i][:], func=AF.Sigmoid), reads=[r_wa[i]], writes=[r_wa[i]])
        sw = wa[i][:, 0:64]
        asg = wa[i][:, 64:128]
        if stop <= 2:
            continue
        P.op("dve", lambda e: e.tensor_tensor(out=kk[i][:], in0=k_, in1=KKW, op=ALU.mult), reads=[r_us[i], r_par], writes=[r_kk[i]])
        P.op("act", lambda e: e.activation(out=tmp[i][:], in_=kk[i][:], func=AF.Square, accum_out=sm[i][:, 0:1]), reads=[r_kk[i]], writes=[r_tmp[i], r_sm[i]])
        P.op("dve", lambda e: e.tensor_scalar_add(out=sm[i][:, 1:2], in0=sm[i][:, 0:1], scalar1=1e-12), reads=[r_sm[i]], writes=[r_sm[i]])
        P.op("act", lambda e: e.activation(out=sm[i][:, 1:2], in_=sm[i][:, 1:2], func=AF.Sqrt), reads=[r_sm[i]], writes=[r_sm[i]])
        P.op("dve", lambda e: e.reciprocal(out=sm[i][:, 2:3], in_=sm[i][:, 1:2]), reads=[r_sm[i]], writes=[r_sm[i]])
        P.op("dve", lambda e: e.tensor_scalar(out=kk[i][:], in0=kk[i][:], scalar1=sm[i][:, 2:3], scalar2=None, op0=ALU.mult), reads=[r_kk[i], r_sm[i]], writes=[r_kk[i]])
        P.op("dve", lambda e: e.scalar_tensor_tensor(out=tmp2[i][:], in0=asg, scalar=-1.0, in1=KA, op0=ALU.add, op1=ALU.mult), reads=[r_wa[i], r_par], writes=[r_tmp2[i]])
        P.op("dve", lambda e: e.scalar_tensor_tensor(out=kd[i][:], in0=tmp2[i][:], scalar=1.0, in1=k_, op0=ALU.add, op1=ALU.mult), reads=[r_tmp2[i], r_us[i]], writes=[r_kd[i]])
        P.op("dve", lambda e: e.tensor_tensor(out=tmp[i][:], in0=r_, in1=RK, op=ALU.mult), reads=[r_us[i], r_par], writes=[r_tmp[i]])
        P.op("dve", lambda e: e.tensor_tensor(out=tmp[i][:], in0=tmp[i][:], in1=kd[i][:], op=ALU.mult), reads=[r_tmp[i], r_kd[i]], writes=[r_tmp[i]])
        P.op("dve", lambda e: e.tensor_reduce(out=sm[i][:, 3:4], in_=tmp[i][:], axis=AX.X, op=ALU.add), reads=[r_tmp[i]], writes=[r_sm[i]])
        if stop <= 3:
            continue
        for h in range(2):
            ps_ = slice(h * 64, (h + 1) * 64)
            ce = "act" if h == 0 else "pool"
            P.op("dve", lambda e: e.tensor_scalar(out=LW[ps_, ps_], in0=wa[i][ps_, 0:64], scalar1=WSC, scalar2=None, op0=ALU.mult), reads=[r_wa[i]], writes=[r_LW])
            P.op("dve", lambda e: e.tensor_scalar(out=Ab[ps_, ps_], in0=kk[i][ps_, :], scalar1=-1.0, scalar2=None, op0=ALU.mult), reads=[r_kk[i]], writes=[r_Ab])
            P.op("pool", lambda e: e.tensor_tensor(out=Bb[ps_, ps_], in0=kk[i][ps_, :], in1=wa[i][ps_, 64:128], op=ALU.mult), reads=[r_kk[i], r_wa[i]], writes=[r_Bb])
            for (dst, r_dst, src, r_src) in ((KDb, r_KDb, kd[i][ps_, :], r_kd[i]), (Rb, r_Rb, us[i][ps_, 0:64], r_us[i]), (Vb, r_Vb, us[i][ps_, 128:192], r_us[i])):
                if ce == "act":
                    P.op("act", lambda e, dst=dst, src=src: e.copy(out=dst[ps_, ps_], in_=src), reads=[r_src], writes=[r_dst])
                else:
                    P.op("pool", lambda e, dst=dst, src=src: e.tensor_copy(out=dst[ps_, ps_], in_=src), reads=[r_src], writes=[r_dst])
        if stop <= 4:
            continue
        b3, rb3 = nb()
        P.op("pe", lambda e: e.matmul(b3[:, 0:128], MIT, LW[:, :], start=True, stop=True), reads=[r_cm, r_LW], writes=[rb3], inc=False)
        P.op("pe", lambda e: e.matmul(b3[:, 128:256], ones[:, :], LW[:, :], start=True, stop=True), reads=[r_ones, r_LW], writes=[rb3], inc=False)
        P.op("pe", lambda e: e.matmul(b3[:, 256:257], LW[:, :], ones[:, 0:1], start=True, stop=True), reads=[r_ones, r_LW], writes=[rb3])
        P.op("act", lambda e: e.activation(out=ex[i][:, 0:128], in_=b3[:, 0:128], func=AF.Exp), reads=[rb3], writes=[r_ex[i]])
        P.op("act", lambda e: e.activation(out=ex[i][:, 128:256], in_=b3[:, 0:128], func=AF.Exp, scale=-1.0), reads=[rb3], writes=[r_ex[i]])
        P.op("act", lambda e: e.activation(out=ex[i][:, 256:384], in_=b3[:, 128:256], func=AF.Exp), reads=[rb3], writes=[r_ex[i]])
        P.op("act", lambda e: e.activation(out=gcc[i][:, 0:1], in_=b3[:, 256:257], func=AF.Exp), reads=[rb3], writes=[r_gcc[i]])
        P.op("act", lambda e: e.activation(out=ex[i][:, 384:512], in_=LW[:, :], func=AF.Exp, scale=-1.0), reads=[r_LW], writes=[r_ex[i]])
        EI = ex[i][:, 0:128]
        EN = ex[i][:, 128:256]
        P.op("dve", lambda e: e.tensor_tensor(out=ee[i][:, 0:128], in0=EI, in1=ex[i][:, 384:512], op=ALU.mult), reads=[r_ex[i]], writes=[r_ee[i]])
        P.op("pool", lambda e: e.tensor_tensor(out=ee[i][:, 128:256], in0=ex[i][:, 256:384], in1=EN, op=ALU.mult), reads=[r_ex[i]], writes=[r_ee[i]])
        P.op("dve", lambda e: e.tensor_tensor(out=At[i][:], in0=Ab[:, :], in1=ee[i][:, 0:128], op=ALU.mult), reads=[r_Ab, r_ee[i]], writes=[r_At[i]])
        P.op("pool", lambda e: e.tensor_tensor(out=BKt[i][:, 0:128], in0=Bb[:, :], in1=EN, op=ALU.mult), reads=[r_Bb, r_ex[i]], writes=[r_BKt[i]])
        P.op("dve", lambda e: e.tensor_tensor(out=BKt[i][:, 128:256], in0=KDb[:, :], in1=EN, op=ALU.mult), reads=[r_KDb, r_ex[i]], writes=[r_BKt[i]])
        P.op("pool", lambda e: e.tensor_tensor(out=Rt[i][:], in0=Rb[:, :], in1=EI, op=ALU.mult), reads=[r_Rb, r_ex[i]], writes=[r_Rt[i]])
        P.op("dve", lambda e: e.tensor_tensor(out=BKG[i][:, 0:128], in0=Bb[:, :], in1=ee[i][:, 128:256], op=ALU.mult), reads=[r_Bb, r_ee[i]], writes=[r_BKG[i]])
        P.op("pool", lambda e: e.tensor_tensor(out=BKG[i][:, 128:256], in0=KDb[:, :], in1=ee[i][:, 128:256], op=ALU.mult), reads=[r_KDb, r_ee[i]], writes=[r_BKG[i]])
        if stop <= 5:
            continue
        b4, rb4 = nb()
        P.op("pe", lambda e: e.transpose(b4[:, 0:128], BKt[i][:, 0:128], ident[:, :]), reads=[r_BKt[i], r_id], writes=[rb4], inc=False)
        P.op("pe", lambda e: e.transpose(b4[:, 128:256], BKt[i][:, 128:256], ident[:, :]), reads=[r_BKt[i], r_id], writes=[rb4])
        evac(BKT[i][:, :], b4[:, 0:256], [rb4], [r_BKT[i]])
        b5, rb5 = nb()
        P.op("pe", lambda e: e.transpose(b5[:, 0:128], At[i][:, :], ident[:, :]), reads=[r_At[i], r_id], writes=[rb5], inc=False)
        P.op("pe", lambda e: e.transpose(b5[:, 128:256], Rt[i][:, :], ident[:, :]), reads=[r_Rt[i], r_id], writes=[rb5])
        evac(ART[i][:, :], b5[:, 0:256], [rb5], [r_ART[i]])
        b6, rb6 = nb()
        P.op("pe", lambda e: e.matmul(b6[:, 0:256], BKT[i][:, 0:128], ART[i][:, :], start=True, stop=True), reads=[r_BKT[i], r_ART[i]], writes=[rb6])
        P.op("dve", lambda e: e.tensor_tensor(out=LA[i][:], in0=b6[:, 0:256], in1=MM, op=ALU.mult), reads=[rb6, r_cm], writes=[r_LA[i]])
        b7, rb7 = nb()
        P.op("pe", lambda e: e.matmul(b7[:, 0:256], BKT[i][:, 128:256], ART[i][:, :], start=True, stop=True), reads=[r_BKT[i], r_ART[i]], writes=[rb7])
        P.op("dve", lambda e: e.tensor_tensor(out=LK[i][:], in0=b7[:, 0:256], in1=MM, op=ALU.mult), reads=[rb7, r_cm], writes=[r_LK[i]])
        b8, rb8 = nb()
        P.op("pe", lambda e: e.matmul(b8[:, 0:128], ART[i][:, 0:128], BKT[i][:, 0:128], start=True, stop=True), reads=[r_BKT[i], r_ART[i]], writes=[rb8])
        P.op("dve", lambda e: e.tensor_tensor(out=XT[i][:], in0=b8[:, 0:128], in1=MS, op=ALU.mult), reads=[rb8, r_cm], writes=[r_XT[i]])
        if stop <= 6:
            continue
        P.op("pool", lambda e: e.tensor_tensor(out=TT[i][:], in0=LA[i][:, 0:128], in1=ident[:, :], op=ALU.add), reads=[r_LA[i], r_id], writes=[r_TT[i]])
        cx, r_cx = LA[i][:, 0:128], r_LA[i]
        cxt, r_cxt = XT[i][:, :], r_XT[i]
        nxt = [(X2[i], r_X2[i], XT2[i], r_XT2[i]), (X[i], r_X[i], XT[i], r_XT[i])]
        for lv in range(5):
            nX, r_nX, nXT, r_nXT = nxt[lv % 2]
            if lv < 4:
                ba, rba = nb()
                P.op("pe", lambda e, cx=cx, cxt=cxt, ba=ba: e.matmul(ba[:, 0:128], cxt, cx, start=True, stop=True), reads=[r_cx, r_cxt], writes=[rba])
            bb, rbb = nb()
            P.op("pe", lambda e, cx=cx, cxt=cxt, bb=bb: e.matmul(bb[:, 0:128], cx, cxt, start=True, stop=True), reads=[r_cx, r_cxt], writes=[rbb])
            if lv < 4:
                P.op("act", lambda e, nX=nX, ba=ba: e.copy(out=nX[:, :], in_=ba[:, 0:128]), reads=[rba], writes=[r_nX])
            P.op("dve", lambda e, nXT=nXT, bb=bb: e.tensor_copy(out=nXT[:, :], in_=bb[:, 0:128]), reads=[rbb], writes=[r_nXT])
            bc, rbc = nb()
            P.op("pe", lambda e, nXT=nXT, bc=bc: e.matmul(bc[:, 0:128], nXT[:, :], TT[i][:, :], start=True, stop=True), reads=[r_nXT, r_TT[i]], writes=[rbc])
            P.op("dve", lambda e, bc=bc: e.tensor_tensor(out=TT[i][:], in0=bc[:, 0:128], in1=TT[i][:], op=ALU.add), reads=[rbc, r_TT[i]], writes=[r_TT[i]])
            cx, r_cx, cxt, r_cxt = nX[:, :], r_nX, nXT[:, :], r_nXT
        if stop <= 7:
            continue
        b9, rb9 = nb()
        P.op("pe", lambda e: e.matmul(b9[:, 0:128], TT[i][:, :], At[i][:, :], start=True, stop=True), reads=[r_TT[i], r_At[i]], writes=[rb9], inc=False)
        P.op("pe", lambda e: e.matmul(b9[:, 128:256], LK[i][:, 0:128], Vb[:, :], start=True, stop=True), reads=[r_LK[i], r_Vb], writes=[rb9])
        P.op("act", lambda e: e.copy(out=Pm[i][:, :], in_=b9[:, 0:128]), reads=[rb9], writes=[r_Pm[i]])
        P.op("dve", lambda e: e.tensor_copy(out=LV[i][:, :], in_=b9[:, 128:256]), reads=[rb9], writes=[r_LV[i]])
        b10, rb10 = nb()
        P.op("pe", lambda e: e.matmul(b10[:, 0:128], TT[i][:, :], LV[i][:, :], start=True, stop=True), reads=[r_TT[i], r_LV[i]], writes=[rb10], inc=False)
        P.op("pe", lambda e: e.matmul(b10[:, 128:256], Pm[i][:, :], LA[i][:, 128:256], start=True, stop=True), reads=[r_Pm[i], r_LA[i]], writes=[rb10], inc=False)
        P.op("pe", lambda e: e.matmul(b10[:, 256:384], Pm[i][:, :], BKG[i][:, 0:128], start=True, stop=True), reads=[r_Pm[i], r_BKG[i]], writes=[rb10])
        P.op("act", lambda e: e.copy(out=Q[i][:, :], in_=b10[:, 0:128]), reads=[rb10], writes=[r_Q[i]])
        P.op("dve", lambda e: e.tensor_tensor(out=RpT[i][:], in0=b10[:, 128:256], in1=ART[i][:, 128:256], op=ALU.add), reads=[rb10, r_ART[i]], writes=[r_RpT[i]])
        P.op("dve", lambda e: e.scalar_tensor_tensor(out=Mm[i][:], in0=ident[:, :], scalar=gcc[i][:, 0:1], in1=b10[:, 256:384], op0=ALU.mult, op1=ALU.add),
             reads=[rb10, r_id, r_gcc[i]], writes=[r_Mm[i]])
        if stop <= 8:
            continue
        sc, sn = s % 2, (s + 1) % 2
        b11, rb11 = nb()
        P.op("pe", lambda e: e.matmul(b11[:, 0:128], LA[i][:, 128:256], Q[i][:, :], start=True, stop=False), reads=[r_LA[i], r_Q[i]], writes=[rb11], inc=False)
        P.op("pe", lambda e: e.matmul(b11[:, 0:128], LK[i][:, 128:256], Vb[:, :], start=False, stop=False), reads=[r_LK[i], r_Vb], writes=[rb11], inc=False)
        P.op("pe", lambda e: e.matmul(b11[:, 0:128], RpT[i][:, :], ST[sc][:, :], start=False, stop=True), reads=[r_RpT[i], r_ST[sc]], writes=[rb11])
        b12, rb12 = nb()
        P.op("pe", lambda e: e.matmul(b12[:, 0:128], BKG[i][:, 0:128], Q[i][:, :], start=True, stop=False), reads=[r_BKG[i], r_Q[i]], writes=[rb12], inc=False)
        P.op("pe", lambda e: e.matmul(b12[:, 0:128], BKG[i][:, 128:256], Vb[:, :], start=False, stop=False), reads=[r_BKG[i], r_Vb], writes=[rb12], inc=False)
        P.op("pe", lambda e: e.matmul(b12[:, 0:128], Mm[i][:, :], ST[sc][:, :], start=False, stop=True), reads=[r_Mm[i], r_ST[sc]], writes=[rb12])
        P.op("act", lambda e: e.copy(out=ST[sn][:, :], in_=b12[:, 0:128]), reads=[rb12], writes=[r_ST[sn]])
        P.op("dve", lambda e: e.scalar_tensor_tensor(out=yo[i][:], in0=Vb[:, :], scalar=sm[i][:, 3:4], in1=b11[:, 0:128], op0=ALU.mult, op1=ALU.add),
             reads=[rb11, r_Vb, r_sm[i]], writes=[r_yo[i]])
        tf, tb_ = fwd[s], bwd[s]
        P.dma("sp", yfs[tf:tf + 64, :], yo[i][0:64, 0:64], reads=[r_yo[i]], writes=[r_yfs])
        P.dma("sp", ybs[tb_:tb_ + 64, :], yo[i][64:128, 64:128], reads=[r_yo[i]], writes=[r_ybs])
        P.dma("sp", gs[tf:tf + 64, :], gg[i][0:64, :], reads=[r_gg[i]], writes=[r_gs])
        if REC:
            lists.append(P.stop_record())
            if s == NS - 1:
                P.replay_skewed(lists, NBUF)
                lists = []

    if stop < 99:
        P.finish([])
        return P
    yf_, r_yf_ = T((128, 64), 2)
    yb_, r_yb_ = T((128, 64), 2)
    g_, r_g_ = T((128, 64), 2)
    st, r_st = T((128, 16), 2)
    yn, r_yn = T((128, 64), 2)
    for t in range(ntok // 128):
        i = t % 2
        t0 = t * 128
        P.dma("sp", yf_[i][:], yfs[t0:t0 + 128, :], reads=[r_yfs], writes=[r_yf_[i]])
        P.dma("sp", yb_[i][:], ybs[t0:t0 + 128, :], reads=[r_ybs], writes=[r_yb_[i]])
        P.dma("sp", g_[i][:], gs[t0:t0 + 128, :], reads=[r_gs], writes=[r_g_[i]])
        P.op("dve", lambda e: e.tensor_tensor(out=yf_[i][:], in0=yf_[i][:], in1=yb_[i][:], op=ALU.add), reads=[r_yf_[i], r_yb_[i]], writes=[r_yf_[i]])
        P.op("dve", lambda e: e.bn_stats(out=st[i][:, 0:6], in_=yf_[i][:]), reads=[r_yf_[i]], writes=[r_st[i]])
        P.op("dve", lambda e: e.bn_aggr(out=st[i][:, 6:8], in_=st[i][:, 0:6]), reads=[r_st[i]], writes=[r_st[i]])
        P.op("dve", lambda e: e.tensor_scalar_add(out=st[i][:, 8:9], in0=st[i][:, 7:8], scalar1=GN_EPS), reads=[r_st[i]], writes=[r_st[i]])
        P.op("act", lambda e: e.activation(out=st[i][:, 8:9], in_=st[i][:, 8:9], func=AF.Sqrt), reads=[r_st[i]], writes=[r_st[i]])
        P.op("dve", lambda e: e.reciprocal(out=st[i][:, 9:10], in_=st[i][:, 8:9]), reads=[r_st[i]], writes=[r_st[i]])
        P.op("dve", lambda e: e.tensor_scalar(out=yn[i][:], in0=yf_[i][:], scalar1=st[i][:, 6:7], scalar2=st[i][:, 9:10], op0=ALU.subtract, op1=ALU.mult),
             reads=[r_yf_[i], r_st[i]], writes=[r_yn[i]])
        P.op("pool", lambda e: e.tensor_tensor(out=yn[i][:], in0=yn[i][:], in1=GNG, op=ALU.mult), reads=[r_yn[i], r_par], writes=[r_yn[i]])
        P.op("pool", lambda e: e.tensor_tensor(out=yn[i][:], in0=yn[i][:], in1=GNB, op=ALU.add), reads=[r_yn[i], r_par], writes=[r_yn[i]])
        P.op("dve", lambda e: e.tensor_tensor(out=yn[i][:], in0=yn[i][:], in1=g_[i][:], op=ALU.mult), reads=[r_yn[i], r_g_[i]], writes=[r_yn[i]])
        P.dma("sp", rw[t0:t0 + 128, :], yn[i][:], reads=[r_yn[i]], writes=[r_rw])
    P.finish([r_rw])
    return P


PI = math.pi


def build_a3(n_lat=16384, n_ctx=256, stop=99):
    P = Prog()
    ntok = n_lat + n_ctx
    NT = ntok // 128
    H3 = P.dram("H3", [ntok, 576], F32, "ExternalInput")
    cw = P.dram("cw", [128, 768], F32, "ExternalInput")
    ztl = P.dram("ztl", [2, 33, n_lat], F32, "ExternalInput")
    ztc = P.dram("ztc", [2, 33, n_ctx], F32, "ExternalInput")
    w1d = P.dram("w1", [33, 64], F32, "ExternalInput")
    w2d = P.dram("w2", [64, 64], F32, "ExternalInput")
    w3d = P.dram("w3", [64, 256], F32, "ExternalInput")
    colp = P.dram("colp", [64, 4], F32, "ExternalInput")
    decd = P.dram("dec", [1, 256], F32, "ExternalInput")
    hbd = P.dram("hb", [128, 128], F32, "ExternalInput")
    hy = P.dram("hy", [ntok, 64], F32, "ExternalOutput")
    WL = 2 * n_lat - 1
    WC = 2 * n_ctx - 1
    KDl = P.dram("KDl", [128, WL + 1], BF16, "Internal")
    KDc = P.dram("KDc", [128, WC + 1], BF16, "Internal")
    r_KD = {0: Reg(), 1: Reg()}
    r_hy = Reg()

    identf, r_idf = make_ident(P, F32)
    J = P.sb([128, 128], BF16)
    r_J = Reg()
    P.op("pool", lambda e: e.memset(J[:], 0.0), writes=[r_J])
    P.op("pool", lambda e: e.affine_select(out=J[:], in_=J[:], pattern=[[1, 128]], compare_op=ALU.not_equal, fill=1.0, base=-127, channel_multiplier=1),
         reads=[r_J], writes=[r_J])
    ones = P.sb([128, 128], F32)
    r_ones = Reg()
    P.op("pool", lambda e: e.memset(ones[:], 1.0), writes=[r_ones])
    npi = P.sb([128, 1], F32)
    P.op("pool", lambda e: e.memset(npi[:], PI / 2), writes=[r_ones])

    cws = P.sb([128, 768], F32)
    w1s = P.sb([33, 64], F32)
    w2s = P.sb([64, 64], F32)
    w3s = P.sb([64, 256], F32)
    cps = P.sb([64, 4], F32)
    decs = P.sb([1, 256], F32)
    hbs = P.sb([128, 128], F32)
    r_par = Reg()
    for dst, src in ((cws, cw), (w1s, w1d), (w2s, w2d), (w3s, w3d), (cps, colp), (decs, decd), (hbs, hbd)):
        P.dma("sp", dst[:], src[:, :], writes=[r_par])
    P.op("dve", lambda e: e.tensor_scalar(out=decs[:], in0=decs[:], scalar1=-1.0, scalar2=None, op0=ALU.mult), reads=[r_par], writes=[r_par])

    banks = [P.ps([128, 512], F32) for _ in range(8)]
    bk = [0]

    def nb():
        b = banks[bk[0] % 6]
        bk[0] += 1
        return b
    ybanks = banks[6:8]

    SC = P.sb([128, 192, NT], F32)
    r_SC = Reg()
    RN = P.sb([128, 2, 128], F32)
    r_RN = Reg()
    P.push_scope()
    h3 = [P.sb([128, 576], F32) for _ in range(2)]
    r_h3 = [Reg(), Reg()]
    pr = [P.sb([128, 576], F32) for _ in range(2)]
    r_pr = [Reg(), Reg()]
    s1 = [P.sb([128, 192], F32) for _ in range(2)]
    r_s1 = [Reg(), Reg()]
    for t in range(NT):
        i = t % 2
        P.dma("sp", h3[i][:], H3[t * 128:(t + 1) * 128, :], writes=[r_h3[i]])
        P.op("dve", lambda e: e.tensor_tensor(out=pr[i][:], in0=h3[i][:], in1=cws[:, 0:576], op=ALU.mult), reads=[r_h3[i], r_par], writes=[r_pr[i]])
        P.op("pool", lambda e: e.tensor_tensor(out=s1[i][:], in0=pr[i][:, 0:192], in1=pr[i][:, 192:384], op=ALU.add), reads=[r_pr[i]], writes=[r_s1[i]])
        P.op("pool", lambda e: e.tensor_tensor(out=s1[i][:], in0=s1[i][:], in1=pr[i][:, 384:576], op=ALU.add), reads=[r_pr[i], r_s1[i]], writes=[r_s1[i]])
        P.op("dve", lambda e: e.tensor_tensor(out=SC[:, :, t], in0=s1[i][:], in1=cws[:, 576:768], op=ALU.add), reads=[r_s1[i], r_par], writes=[r_SC])

    nacc = 2 * max(n_lat // 512, 1) + 2
    acc = P.sb([128, 2, 2 * (n_lat // 512 + 1)], F32)
    r_acc = Reg()
    P.op("pool", lambda e: e.memset(acc[:], 0.0), writes=[r_acc])
    zt = [P.sb([33, 512], F32) for _ in range(2)]
    r_zt = [Reg(), Reg()]
    hA = [P.sb([64, 512], F32) for _ in range(2)]
    r_hA = [Reg(), Reg()]
    hB = [P.sb([64, 512], F32) for _ in range(2)]
    r_hB = [Reg(), Reg()]
    win = [P.sb([128, 512], F32) for _ in range(2)]
    r_win = [Reg(), Reg()]
    hw = [P.sb([128, 512], F32) for _ in range(2)]
    r_hw = [Reg(), Reg()]
    hwb = [P.sb([128, 512], BF16) for _ in range(2)]
    r_hwb = [Reg(), Reg()]
    junk = [P.sb([128, 512], F32) for _ in range(2)]
    r_junk = [Reg(), Reg()]
    sS = [P.sb([64, 512], F32) for _ in range(2)]
    sC = [P.sb([64, 512], F32) for _ in range(2)]
    sQ = [P.sb([64, 512], F32) for _ in range(2)]
    r_sS = [Reg(), Reg()]
    r_sC = [Reg(), Reg()]
    r_sQ = [Reg(), Reg()]

    def sin_big(h, r_h, N, i):
        S, C, Q = sS[i], sC[i], sQ[i]
        P.op("act", lambda e: e.activation(out=S[:, 0:N], in_=h[:, 0:N], func=AF.Sin, scale=0.125), reads=[r_h], writes=[r_sS[i]])
        P.op("act", lambda e: e.activation(out=C[:, 0:N], in_=h[:, 0:N], func=AF.Sin, scale=0.125, bias=npi[0:64, 0:1]), reads=[r_h, r_ones], writes=[r_sC[i]])
        for lv in range(3):
            dst = h if lv == 2 else S
            r_dst = r_h if lv == 2 else r_sS[i]
            if lv < 2:
                P.op("pool", lambda e: e.tensor_tensor(out=Q[:, 0:N], in0=S[:, 0:N], in1=S[:, 0:N], op=ALU.mult), reads=[r_sS[i]], writes=[r_sQ[i]])
            P.op("dve", lambda e, dst=dst: e.scalar_tensor_tensor(out=dst[:, 0:N], in0=S[:, 0:N], scalar=2.0, in1=C[:, 0:N], op0=ALU.mult, op1=ALU.mult),
                 reads=[r_sS[i], r_sC[i]], writes=[r_dst])
            if lv < 2:
                P.op("dve", lambda e: e.tensor_scalar(out=C[:, 0:N], in0=Q[:, 0:N], scalar1=-2.0, scalar2=1.0, op0=ALU.mult, op1=ALU.add),
                     reads=[r_sQ[i]], writes=[r_sC[i]])

    it = 0
    for seq, (L, ztd, KD) in enumerate(((n_lat, ztl, KDl), (n_ctx, ztc, KDc))):
        N = min(512, L)
        nblk = L // N
        for ps in range(2):
            for bl in range(nblk):
                i = it % 2
                it += 1
                P.dma("sp", zt[i][:, 0:N], ztd[ps, :, bl * N:(bl + 1) * N], writes=[r_zt[i]])
                b1, rb1 = nb()
                P.op("pe", lambda e: e.matmul(b1[0:64, 0:N], w1s[:, :], zt[i][:, 0:N], start=True, stop=True), reads=[r_par, r_zt[i]], writes=[rb1])
                P.op("dve", lambda e: e.tensor_scalar(out=hA[i][:, 0:N], in0=b1[0:64, 0:N], scalar1=cps[:, 0:1], scalar2=cps[:, 1:2], op0=ALU.add, op1=ALU.mult),
                     reads=[rb1, r_par], writes=[r_hA[i]])
                sin_big(hA[i], r_hA[i], N, i)
                b2, rb2 = nb()
                P.op("pe", lambda e: e.matmul(b2[0:64, 0:N], w2s[:, :], hA[i][:, 0:N], start=True, stop=True), reads=[r_par, r_hA[i]], writes=[rb2])
                P.op("dve", lambda e: e.tensor_scalar(out=hB[i][:, 0:N], in0=b2[0:64, 0:N], scalar1=cps[:, 2:3], scalar2=cps[:, 3:4], op0=ALU.add, op1=ALU.mult),
                     reads=[rb2, r_par], writes=[r_hB[i]])
                sin_big(hB[i], r_hB[i], N, i)
                b3, rb3 = nb()
                P.op("pe", lambda e: e.matmul(b3[:, 0:N], w3s[:, ps * 128:(ps + 1) * 128], hB[i][:, 0:N], start=True, stop=True), reads=[r_par, r_hB[i]], writes=[rb3])
                b4, rb4 = nb()
                P.op("pe", lambda e: e.matmul(b4[:, 0:N], decs[0:1, ps * 128:(ps + 1) * 128], zt[i][0:1, 0:N], start=True, stop=True), reads=[r_par, r_zt[i]], writes=[rb4])
                P.op("act", lambda e: e.activation(out=win[i][:, 0:N], in_=b4[:, 0:N], func=AF.Exp), reads=[rb4], writes=[r_win[i]])
                P.op("dve", lambda e: e.tensor_tensor(out=hw[i][:, 0:N], in0=b3[:, 0:N], in1=win[i][:, 0:N], op=ALU.mult), reads=[rb3, r_win[i]], writes=[r_hw[i]])
                if ps == 1 and bl == nblk - 1:
                    P.op("dve", lambda e: e.memset(hw[i][:, N - 1:N], 0.0), reads=[r_hw[i]], writes=[r_hw[i]])
                P.op("act", lambda e: e.activation(out=junk[i][:, 0:N], in_=hw[i][:, 0:N], func=AF.Abs, accum_out=acc[:, seq, ps * nblk + bl:ps * nblk + bl + 1]),
                     reads=[r_hw[i]], writes=[r_junk[i], r_acc])
                P.op("pool", lambda e: e.tensor_copy(out=hwb[i][:, 0:N], in_=hw[i][:, 0:N]), reads=[r_hw[i]], writes=[r_hwb[i]])
                if ps == 0:
                    q0 = L - 1 + bl * N
                    P.dma("sp", KD[:, q0:q0 + N], hwb[i][:, 0:N], reads=[r_hwb[i]], writes=[r_KD[seq]])
                else:
                    q0 = bl * N
                    nw = N - 1 if bl == nblk - 1 else N
                    P.dma("sp", KD[:, q0:q0 + nw], hwb[i][:, 0:nw], reads=[r_hwb[i]], writes=[r_KD[seq]])
    nrm = P.sb([128, 2], F32)
    r_nrm = Reg()
    dg = P.sb([128, 128], F32)
    r_dg = Reg()
    for seq in range(2):
        P.op("dve", lambda e: e.tensor_reduce(out=nrm[:, seq:seq + 1], in_=acc[:, seq, :], axis=AX.X, op=ALU.add), reads=[r_acc], writes=[r_nrm])
        P.op("dve", lambda e: e.tensor_scalar(out=dg[:], in0=identf[:], scalar1=nrm[:, seq:seq + 1], scalar2=None, op0=ALU.mult), reads=[r_nrm, r_idf], writes=[r_dg])
        b5, rb5 = nb()
        P.op("pe", lambda e: e.matmul(b5[:, 0:128], ones[:, :], dg[:, :], start=True, stop=True), reads=[r_ones, r_dg], writes=[rb5])
        P.op("dve", lambda e: e.reciprocal(out=RN[:, seq, :], in_=b5[:, 0:128]), reads=[rb5], writes=[r_RN])

    P.pop_scope([r_KD[0], r_KD[1], r_RN, r_SC])
    hsk = [P.sb([128, n_lat], BF16) for _ in range(2)]
    r_hsk = [Reg(), Reg()]
    zb = [P.sb([128, 128], BF16) for _ in range(2)]
    r_zb = [Reg(), Reg()]
    zr = [P.sb([128, 128], BF16) for _ in range(2)]
    r_zr = [Reg(), Reg()]
    tm = [P.sb([128, 128], F32) for _ in range(2)]
    r_tm = [Reg(), Reg()]
    hk = 0
    cv = 0
    for seq, (L, KD, W, j0) in enumerate(((n_lat, KDl, WL + 1, 0), (n_ctx, KDc, WC + 1, n_lat // 128))):
        NB = L // 128
        for ch in range(64):
            for o in range(2):
                c2 = cv % 2
                cv += 1
                row = o * 64 + ch
                src_c = (128 + ch) if o == 0 else ch
                Zs = SC[:, src_c, j0:j0 + NB]
                P.op("pool", lambda e: e.tensor_copy(out=zb[c2][:, 0:NB], in_=Zs), reads=[r_SC], writes=[r_zb[c2]])
                bz, rbz = nb()
                P.op("pe", lambda e: e.matmul(bz[:, 0:NB], J[:, :], zb[c2][:, 0:NB], start=True, stop=True), reads=[r_J, r_zb[c2]], writes=[rbz])
                P.op("act", lambda e: e.copy(out=zr[c2][:, 0:NB], in_=bz[:, 0:NB]), reads=[rbz], writes=[r_zr[c2]])
                yb_, r_yb = ybanks[c2]
                first = True
                for h in (1, 0):
                    hb_ = hk % 2
                    hk += 1
                    if h == 1:
                        x0, wd = L - 128, L
                        deltas = list(range(0, NB))
                    else:
                        x0, wd = 0, L - 128
                        deltas = list(range(-(NB - 1), 0))
                    if wd == 0:
                        continue
                    src = bass.AP(KD.tensor, row * W + x0, [[1, 128], [1, wd]])
                    P.dma("sp", hsk[hb_][:, 0:wd], src, reads=[r_KD[seq]], writes=[r_hsk[hb_]])
                    for di, d in enumerate(deltas):
                        xo = 128 * d + L - 128 - x0
                        lo_i, hi_i = max(0, d), NB + min(0, d)
                        lo_j, hi_j = max(0, -d), NB - max(0, d)
                        last = (h == 0 and di == len(deltas) - 1) or (NB == 1)
                        P.op("pe", lambda e, xo=xo, lo_i=lo_i, hi_i=hi_i, lo_j=lo_j, hi_j=hi_j, first=first, last=last: e.matmul(
                            yb_[:, lo_i:hi_i], hsk[hb_][:, xo:xo + 128], zr[c2][:, lo_j:hi_j], start=first, stop=last),
                            reads=[r_hsk[hb_], r_zr[c2]], writes=[r_yb], inc=(di == len(deltas) - 1))
                        first = False
                col = o * 64 + ch
                P.op("dve", lambda e: e.tensor_scalar(out=tm[c2][:, 0:NB], in0=yb_[:, 0:NB], scalar1=RN[:, seq, col:col + 1], scalar2=None, op0=ALU.mult),
                     reads=[r_yb, r_RN], writes=[r_tm[c2]])
                P.op("dve", lambda e: e.scalar_tensor_tensor(out=tm[c2][:, 0:NB], in0=Zs, scalar=hbs[:, col:col + 1], in1=tm[c2][:, 0:NB], op0=ALU.mult, op1=ALU.add),
                     reads=[r_SC, r_par, r_tm[c2]], writes=[r_tm[c2]])
                if o == 0:
                    P.op("dve", lambda e: e.tensor_tensor(out=SC[:, ch, j0:j0 + NB], in0=SC[:, ch, j0:j0 + NB], in1=tm[c2][:, 0:NB], op=ALU.mult),
                         reads=[r_SC, r_tm[c2]], writes=[r_SC])
                else:
                    P.op("dve", lambda e: e.tensor_tensor(out=SC[:, 64 + ch, j0:j0 + NB], in0=SC[:, 64 + ch, j0:j0 + NB], in1=tm[c2][:, 0:NB], op=ALU.mult),
                         reads=[r_SC, r_tm[c2]], writes=[r_SC])
    if stop <= 4:
        P.finish([])
        return P
    ot = [P.sb([128, 64], F32) for _ in range(2)]
    r_ot = [Reg(), Reg()]
    for t in range(NT):
        i = t % 2
        eng = "act" if t % 2 == 0 else "pool"
        if eng == "act":
            P.op("act", lambda e: e.copy(out=ot[i][:], in_=SC[:, 64:128, t]), reads=[r_SC], writes=[r_ot[i]])
        else:
            P.op("pool", lambda e: e.tensor_copy(out=ot[i][:], in_=SC[:, 64:128, t]), reads=[r_SC], writes=[r_ot[i]])
        P.dma("sp", hy[t * 128:(t + 1) * 128, :], ot[i][:], reads=[r_ot[i]], writes=[r_hy])
    P.finish([r_hy])
    return P


D = 1024
FF = 2816
ALPHA = float(4 ** 0.25)


def build_p2(n_lat=4096, n_ctx=64, moe=False, SB=None):
    P = Prog()
    E = 8 if moe else 1
    ntok = n_lat + n_ctx
    mix = P.dram("mix", [ntok, D], F32, "ExternalInput")
    x = P.dram("x", [ntok, D], F32, "ExternalInput")
    cT = P.dram("cT", [128, 16], F32, "ExternalInput")
    adaw = P.dram("adaw", [D, 4096], F32, "ExternalInput")
    adab = P.dram("adab", [1, 4096], F32, "ExternalInput")
    wout = P.dram("wout", [D, D], F32, "ExternalInput")
    lnp = P.dram("lnp", [128, 4096], F32, "ExternalInput")
    w1 = P.dram("w1", [E, D, FF], F32, "ExternalInput")
    w3 = P.dram("w3", [E, D, FF], F32, "ExternalInput")
    w2 = P.dram("w2", [E, FF, D], F32, "ExternalInput")
    if moe:
        wr = P.dram("wr", [D, 8], F32, "ExternalInput")
    xo = P.dram("xo", [ntok, D], F32, "ExternalOutput")
    r_xo = Reg()
    w1b = P.dram("w1b", [E, D, FF], BF16, "Internal")
    w3b = P.dram("w3b", [E, D, FF], BF16, "Internal")
    w2b = P.dram("w2b", [E, FF, D], BF16, "Internal")
    r_wbf = [Reg() for _ in range(E)]
    for e_ in range(E):
        for k in range(8):
            P.dma("pool", w1b[e_, k * 128:(k + 1) * 128, :], w1[e_, k * 128:(k + 1) * 128, :], writes=[r_wbf[e_]])
            P.dma("pool", w3b[e_, k * 128:(k + 1) * 128, :], w3[e_, k * 128:(k + 1) * 128, :], writes=[r_wbf[e_]])
        for f in range(22):
            P.dma("pool", w2b[e_, f * 128:(f + 1) * 128, :], w2[e_, f * 128:(f + 1) * 128, :], writes=[r_wbf[e_]])

    fb = [P.ps([128, 512], F32) for _ in range(8)]
    tb = [(fb[6][0][:, :].bitcast(BF16).rearrange("p (k c) -> p k c", k=8), fb[6][1])]
    tf = [(fb[7][0][:, :].rearrange("p (k c) -> p k c", k=4), fb[7][1])]
    ob_rr = [0]
    ones_row = P.sb([1, 128], F32)
    P.op("dve", lambda e: e.memset(ones_row[:], 1.0), writes=[Reg()])
    ident, r_id = make_ident(P)
    if moe:
        identf, r_idf = make_ident(P, F32)
        wrs = P.sb([128, 8, 8], F32)
        r_wrs = Reg()
        P.dma("sp", wrs[:], wr.rearrange("(k p) e -> p k e", p=128), writes=[r_wrs])
    wob = P.sb([128, 8, D], BF16)
    r_wob = Reg()
    for k in range(8):
        P.dma("pool", wob[:, k, :], wout[k * 128:(k + 1) * 128, :], writes=[r_wob])
    lns = P.sb([128, 4096], F32)
    r_lns = Reg()
    P.dma("sp", lns[:], lnp[:, :], writes=[r_lns])
    bcA, r_bcA = mods_block(P, cT, adaw[:, 0:2048], adab[:, 0:2048], 2048, ones_row, fb[:4], [])
    bcB, r_bcB = mods_block(P, cT, adaw[:, 2048:4096], adab[:, 2048:4096], 2048, ones_row, fb[:4], [(0, 1024)])

    tiles_all = [(i * 128, 128, 0) for i in range(n_lat // 128)]
    if n_ctx:
        tiles_all.append((n_lat, n_ctx, 1))
    SB = SB or (7 if moe else 4)
    sblocks = [tiles_all[i:i + SB] for i in range(0, n_lat // 128, SB)]
    if n_ctx:
        sblocks[-1] = sblocks[-1] + [tiles_all[-1]]
    MT = SB + 1
    NTOK = SB * 128 + n_ctx

    def T(shape, dt=F32, n=2):
        return [P.sb(list(shape), dt) for _ in range(n)], [Reg() for _ in range(n)]
    xs, r_xs = T((128, D))
    ms, r_ms = T((128, D), n=1)
    ms, r_ms = ms * 2, r_ms * 2
    mb, r_mb = T((128, D), BF16, n=1)
    mb, r_mb = mb * 2, r_mb * 2
    mT, r_mT = T((128, 8, 128), BF16)
    yv, r_yv = T((128, D))
    tmp, r_tmp = T((128, D))
    stat, r_stat = T((128, 16))
    hb, r_hb = T((128, D), BF16, n=1)
    hb, r_hb = hb * 2, r_hb * 2
    x1d = P.dram("x1d", [ntok, D], F32, "Internal")
    r_x1d = Reg()
    x1t, r_x1t = T((128, D))
    acc = P.sb([128, MT, D], F32)
    r_acc = [Reg() for _ in range(MT)]
    r_acc2 = r_acc
    evt, r_evt = [None, None], [Reg(), Reg()]
    evq = [0]
    h2T = P.sb([128, 8, NTOK], BF16)
    r_h2T = Reg()
    if moe:
        hf, r_hf = T((128, D), n=1)
        hf, r_hf = hf * 2, r_hf * 2
        hTf, r_hTf = T((128, 8, 128), n=1)
        hTf, r_hTf = hTf * 2, r_hTf * 2
        lg, r_lg = T((128, 32))
        gates = P.sb([128, MT, 8], F32)
        r_gates = [Reg() for _ in range(MT)]
    UF = 2
    units = [(f0, min(UF, 22 - f0)) for f0 in range(0, 22, UF)]
    w1u, r_w1u = T((128, 8, UF * 128), BF16)
    w3u, r_w3u = T((128, 8, UF * 128), BF16)
    w2u, r_w2u = T((128, UF, D), BF16)
    GT, r_GT = T((128, UF, NTOK), BF16)
    sa, r_sa = T((128, 512))
    wq = [0]
    it = [0]

    for sbk in sblocks:
        ntk = sum(r for (_, r, _) in sbk)
        col = 0
        for ti, (t0, rows, s) in enumerate(sbk):
            i = it[0] % 2
            it[0] += 1
            P.dma("sp", ms[i][:rows, :], mix[t0:t0 + rows, :], writes=[r_ms[i]])
            P.dma("sp", xs[i][:rows, :], x[t0:t0 + rows, :], writes=[r_xs[i]])
            P.op("pool", lambda e: e.tensor_copy(out=mb[i][:rows, :], in_=ms[i][:rows, :]), reads=[r_ms[i]], writes=[r_mb[i]])
            tp, r_tp = tb[0]
            for k in range(8):
                P.op("pe", lambda e, k=k: e.transpose(tp[:, k, :rows], mb[i][:rows, k * 128:(k + 1) * 128], ident[:rows, :rows]),
                     reads=[r_mb[i], r_id], writes=[r_tp], inc=(k == 7))
            P.op("act", lambda e: e.copy(out=mT[i][:, :, :rows], in_=tp[:, :, :rows]), reads=[r_tp], writes=[r_mT[i]])
            P.op("act", lambda e: e.mul(out=yv[i][:rows, :], in_=xs[i][:rows, :], mul=ALPHA), reads=[r_xs[i]], writes=[r_yv[i]])
            for cb in range(2):
                bank, rb = fb[4 + cb]
                for k in range(8):
                    P.op("pe", lambda e, k=k, cb=cb, bank=bank: e.matmul(bank[:rows, :], mT[i][:, k, :rows], wob[:, k, cb * 512:(cb + 1) * 512],
                                                                   start=(k == 0), stop=(k == 7)), reads=[r_mT[i], r_wob], writes=[rb], inc=(k == 7))
                P.op("dve", lambda e, cb=cb, bank=bank: e.tensor_tensor(out=tmp[i][:rows, cb * 512:(cb + 1) * 512], in0=bank[:rows, :],
                                                                in1=bcA[s][:rows, cb * 512:(cb + 1) * 512], op=ALU.mult),
                     reads=[rb, r_bcA[s]], writes=[r_tmp[i]])
            P.op("pool", lambda e: e.tensor_tensor(out=yv[i][:rows, :], in0=yv[i][:rows, :], in1=tmp[i][:rows, :], op=ALU.add), reads=[r_yv[i], r_tmp[i]], writes=[r_yv[i]])
            ln_tile(P, rows, yv[i][:rows, :], r_yv[i], tmp[i], r_tmp[i], stat[i], r_stat[i])
            P.op("pool", lambda e: e.tensor_tensor(out=tmp[i][:rows, :], in0=tmp[i][:rows, :], in1=lns[:rows, 0:1024], op=ALU.mult), reads=[r_tmp[i], r_lns], writes=[r_tmp[i]])
            P.op("dve", lambda e: e.tensor_tensor(out=x1t[i][:rows, :], in0=tmp[i][:rows, :], in1=lns[:rows, 1024:2048], op=ALU.add), reads=[r_tmp[i], r_lns], writes=[r_x1t[i]])
            P.dma("sp", x1d[t0:t0 + rows, :], x1t[i][:rows, :], reads=[r_x1t[i]], writes=[r_x1d])
            ln_tile(P, rows, x1t[i][:rows, :], r_x1t[i], tmp[i], r_tmp[i], stat[i], r_stat[i])
            P.op("pool", lambda e: e.tensor_tensor(out=tmp[i][:rows, :], in0=tmp[i][:rows, :], in1=bcB[s][:rows, 0:1024], op=ALU.mult), reads=[r_tmp[i], r_bcB[s]], writes=[r_tmp[i]])
            if moe:
                P.op("dve", lambda e: e.tensor_tensor(out=hf[i][:rows, :], in0=tmp[i][:rows, :], in1=bcA[s][:rows, 1024:2048], op=ALU.add), reads=[r_tmp[i], r_bcA[s]], writes=[r_hf[i]])
                P.op("pool", lambda e: e.tensor_copy(out=hb[i][:rows, :], in_=hf[i][:rows, :]), reads=[r_hf[i]], writes=[r_hb[i]])
            else:
                P.op("dve", lambda e: e.tensor_tensor(out=hb[i][:rows, :], in0=tmp[i][:rows, :], in1=bcA[s][:rows, 1024:2048], op=ALU.add), reads=[r_tmp[i], r_bcA[s]], writes=[r_hb[i]])
            for k in range(8):
                P.op("pe", lambda e, k=k: e.transpose(tp[:, k, :rows], hb[i][:rows, k * 128:(k + 1) * 128], ident[:rows, :rows]),
                     reads=[r_hb[i], r_id], writes=[r_tp], inc=(k == 7))
            P.op("act", lambda e, col=col: e.copy(out=h2T[:, :, col:col + rows], in_=tp[:, :, :rows]), reads=[r_tp], writes=[r_h2T])
            if moe:
                tq, r_tq = tf[0]
                for half in range(2):
                    for k in range(4):
                        kk = half * 4 + k
                        P.op("pe", lambda e, k=k, kk=kk: e.transpose(tq[:, k, :rows], hf[i][:rows, kk * 128:(kk + 1) * 128], identf[:rows, :rows]),
                             reads=[r_hf[i], r_idf], writes=[r_tq], inc=(k == 3))
                    P.op("dve", lambda e, half=half: e.tensor_copy(out=hTf[i][:, half * 4:half * 4 + 4, :rows], in_=tq[:, :, :rows]), reads=[r_tq], writes=[r_hTf[i]])
                bank, rb = fb[4]
                for k in range(8):
                    P.op("pe", lambda e, k=k, bank=bank: e.matmul(bank[:rows, 0:8], hTf[i][:, k, :rows], wrs[:, k, :], start=(k == 0), stop=(k == 7)),
                         reads=[r_hTf[i], r_wrs], writes=[rb], inc=(k == 7))
                L = lg[i]
                rl = r_lg[i]
                P.op("dve", lambda e, bank=bank: e.tensor_copy(out=L[:rows, 0:8], in_=bank[:rows, 0:8]), reads=[rb], writes=[rl])
                P.op("dve", lambda e: e.tensor_reduce(out=L[:rows, 24:25], in_=L[:rows, 0:8], axis=AX.X, op=ALU.max), reads=[rl], writes=[rl])
                P.op("dve", lambda e: e.tensor_scalar(out=L[:rows, 8:16], in0=L[:rows, 0:8], scalar1=L[:rows, 24:25], scalar2=None, op0=ALU.is_equal), reads=[rl], writes=[rl])
                P.op("dve", lambda e: e.scalar_tensor_tensor(out=L[:rows, 16:24], in0=L[:rows, 8:16], scalar=-1e30, in1=L[:rows, 0:8], op0=ALU.mult, op1=ALU.add), reads=[rl], writes=[rl])
                P.op("dve", lambda e: e.tensor_reduce(out=L[:rows, 25:26], in_=L[:rows, 16:24], axis=AX.X, op=ALU.max), reads=[rl], writes=[rl])
                P.op("dve", lambda e: e.tensor_scalar(out=L[:rows, 16:24], in0=L[:rows, 16:24], scalar1=L[:rows, 25:26], scalar2=None, op0=ALU.is_equal), reads=[rl], writes=[rl])
                P.op("dve", lambda e: e.tensor_tensor(out=L[:rows, 26:27], in0=L[:rows, 25:26], in1=L[:rows, 24:25], op=ALU.subtract), reads=[rl], writes=[rl])
                P.op("act", lambda e: e.activation(out=L[:rows, 26:27], in_=L[:rows, 26:27], func=AF.Exp), reads=[rl], writes=[rl])
                P.op("dve", lambda e: e.tensor_scalar_add(out=L[:rows, 27:28], in0=L[:rows, 26:27], scalar1=1.0), reads=[rl], writes=[rl])
                P.op("dve", lambda e: e.reciprocal(out=L[:rows, 27:28], in_=L[:rows, 27:28]), reads=[rl], writes=[rl])
                P.op("dve", lambda e: e.tensor_tensor(out=L[:rows, 28:29], in0=L[:rows, 26:27], in1=L[:rows, 27:28], op=ALU.mult), reads=[rl], writes=[rl])
                P.op("dve", lambda e: e.tensor_scalar(out=L[:rows, 8:16], in0=L[:rows, 8:16], scalar1=L[:rows, 27:28], scalar2=None, op0=ALU.mult), reads=[rl], writes=[rl])
                P.op("dve", lambda e, ti=ti: e.scalar_tensor_tensor(out=gates[:rows, ti, :], in0=L[:rows, 16:24], scalar=L[:rows, 28:29], in1=L[:rows, 8:16],
                                                                    op0=ALU.mult, op1=ALU.add), reads=[rl], writes=[r_gates[ti]])
            col += rows
        tblocks = []
        c0 = 0
        while c0 < ntk:
            n = min(512, ntk - c0)
            tblocks.append((c0, n))
            c0 += n
        for e_ in range(E):
            for (f0, nf) in units:
                q = wq[0] % 2
                wq[0] += 1
                P.dma("sp", w1u[q][:, :, 0:nf * 128], w1b[e_, :, f0 * 128:(f0 + nf) * 128].rearrange("(k p) f -> p k f", p=128),
                      reads=[r_wbf[e_]], writes=[r_w1u[q]])
                P.dma("act", w3u[q][:, :, 0:nf * 128], w3b[e_, :, f0 * 128:(f0 + nf) * 128].rearrange("(k p) f -> p k f", p=128),
                      reads=[r_wbf[e_]], writes=[r_w3u[q]])
                P.dma("sp", w2u[q][:, 0:nf, :], w2b[e_, f0 * 128:(f0 + nf) * 128, :].rearrange("(f p) d -> p f d", p=128),
                      reads=[r_wbf[e_]], writes=[r_w2u[q]])
                for (c0, n) in tblocks:
                    for f in range(nf):
                        ba, rba = fb[0 + (f % 2) * 2]
                        bb, rbb = fb[1 + (f % 2) * 2]
                        for k in range(8):
                            P.op("pe", lambda e, k=k, f=f, ba=ba: e.matmul(ba[:, 0:n], w1u[q][:, k, f * 128:(f + 1) * 128], h2T[:, k, c0:c0 + n],
                                                                     start=(k == 0), stop=(k == 7)), reads=[r_w1u[q], r_h2T], writes=[rba], inc=(k == 7))
                        for k in range(8):
                            P.op("pe", lambda e, k=k, f=f, bb=bb: e.matmul(bb[:, 0:n], w3u[q][:, k, f * 128:(f + 1) * 128], h2T[:, k, c0:c0 + n],
                                                                     start=(k == 0), stop=(k == 7)), reads=[r_w3u[q], r_h2T], writes=[rbb], inc=(k == 7))
                        j = f % 2
                        P.op("act", lambda e, ba=ba, j=j: e.activation(out=sa[j][:, 0:n], in_=ba[:, 0:n], func=AF.Silu), reads=[rba], writes=[r_sa[j]])
                        P.op("dve", lambda e, bb=bb, j=j, f=f: e.tensor_tensor(out=GT[q][:, f, c0:c0 + n], in0=bb[:, 0:n], in1=sa[j][:, 0:n], op=ALU.mult),
                             reads=[rbb, r_sa[j]], writes=[r_GT[q]])
                col = 0
                for ti, (t0, rows, s) in enumerate(sbk):
                    for cb in range(2):
                        bank, rb = fb[4 + ob_rr[0] % 4]
                        ob_rr[0] += 1
                        for f in range(nf):
                            P.op("pe", lambda e, f=f, cb=cb, bank=bank, col=col: e.matmul(bank[:rows, :], GT[q][:, f, col:col + rows], w2u[q][:, f, cb * 512:(cb + 1) * 512],
                                                                                  start=(f == 0), stop=(f == nf - 1)),
                                 reads=[r_GT[q], r_w2u[q]], writes=[rb], inc=(f == nf - 1))
                        first = (e_ == 0 and f0 == 0)
                        dst = acc[:rows, ti, cb * 512:(cb + 1) * 512]
                        if moe:
                            gsc = gates[:rows, ti, e_:e_ + 1]
                            if first:
                                P.op("dve", lambda e, bank=bank, dst=dst, gsc=gsc: e.tensor_scalar(out=dst, in0=bank[:rows, :], scalar1=gsc, scalar2=None, op0=ALU.mult),
                                     reads=[rb, r_gates[ti]], writes=[r_acc[ti]])
                            elif True:
                                P.op("dve", lambda e, bank=bank, dst=dst, gsc=gsc: e.scalar_tensor_tensor(out=dst, in0=bank[:rows, :], scalar=gsc, in1=dst, op0=ALU.mult, op1=ALU.add),
                                     reads=[rb, r_gates[ti], r_acc[ti]], writes=[r_acc[ti]])
                            else:
                                ev_i = evq[0] % 2
                                evq[0] += 1
                                P.op("act", lambda e, bank=bank, gsc=gsc, ev_i=ev_i: e.activation(out=evt[ev_i][:rows, :], in_=bank[:rows, :], func=AF.Copy, scale=gsc),
                                     reads=[rb, r_gates[ti]], writes=[r_evt[ev_i]])
                                P.op("pool", lambda e, dst=dst, ev_i=ev_i: e.tensor_tensor(out=dst, in0=dst, in1=evt[ev_i][:rows, :], op=ALU.add),
                                     reads=[r_evt[ev_i], r_acc2[ti]], writes=[r_acc2[ti]])
                        else:
                            if first:
                                P.op("act", lambda e, bank=bank, dst=dst: e.copy(out=dst, in_=bank[:rows, :]), reads=[rb], writes=[r_acc[ti]])
                            else:
                                P.op("dve", lambda e, bank=bank, dst=dst: e.tensor_tensor(out=dst, in0=bank[:rows, :], in1=dst, op=ALU.add),
                                     reads=[rb, r_acc[ti]], writes=[r_acc[ti]])
                    col += rows
        for ti, (t0, rows, s) in enumerate(sbk):
            i = it[0] % 2
            it[0] += 1
            P.op("pool", lambda e: e.tensor_tensor(out=acc[:rows, ti, :], in0=acc[:rows, ti, :], in1=bcB[s][:rows, 1024:2048], op=ALU.mult), reads=[r_acc[ti], r_bcB[s]], writes=[r_acc[ti]])
            P.dma("sp", x1t[i][:rows, :], x1d[t0:t0 + rows, :], reads=[r_x1d], writes=[r_x1t[i]])
            P.op("dve", lambda e: e.scalar_tensor_tensor(out=yv[i][:rows, :], in0=x1t[i][:rows, :], scalar=ALPHA, in1=acc[:rows, ti, :], op0=ALU.mult, op1=ALU.add),
                 reads=[r_x1t[i], r_acc[ti]], writes=[r_yv[i]])
            ln_tile(P, rows, yv[i][:rows, :], r_yv[i], tmp[i], r_tmp[i], stat[i], r_stat[i])
            P.op("pool", lambda e: e.tensor_tensor(out=tmp[i][:rows, :], in0=tmp[i][:rows, :], in1=lns[:rows, 2048:3072], op=ALU.mult), reads=[r_tmp[i], r_lns], writes=[r_tmp[i]])
            P.op("dve", lambda e: e.tensor_tensor(out=yv[i][:rows, :], in0=tmp[i][:rows, :], in1=lns[:rows, 3072:4096], op=ALU.add), reads=[r_tmp[i], r_lns], writes=[r_yv[i]])
            P.dma("sp", xo[t0:t0 + rows, :], yv[i][:rows, :], reads=[r_yv[i]], writes=[r_xo])
    P.finish([r_xo])
    return P


def a2_consts():
    i = np.arange(128); half = i // 64
    same = half[:, None] == half[None, :]
    MST = (same & np.where(half[:, None] == 0, i[:, None] < i[None, :], i[:, None] > i[None, :])).astype(np.float32)
    MIT = MST + np.eye(128, dtype=np.float32)
    MS = np.ascontiguousarray(MST.T)
    LM = np.zeros((128, 96), np.float32)
    LM[:64, 0:16] = 1; LM[64:, 16:32] = 1; LM[:64, 32:48] = 1; LM[64:, 48:64] = 1; LM[:, 64:96] = 1
    return np.concatenate([MIT, MST, MIT, MS, LM], 1).astype(np.float32)

def a2_inputs(ur, n_lat, n_ctx, hd, mu, w0, wB, a0, aB, gB, kkw, ka, rk, gng, gnb):
    C = 256
    cols = np.concatenate([np.arange(hd * 64, hd * 64 + 64), C + np.arange(hd * 64, hd * 64 + 64), 2 * C + np.arange(hd * 64, hd * 64 + 64),
                           np.arange(768, 864)])
    u = ur[:, cols]
    def shifted(x, d):
        o = np.zeros_like(x)
        if d == 1: o[1:] = x[:-1]
        else: o[:-1] = x[1:]
        return o
    prev = np.concatenate([shifted(u[:n_lat], 1), shifted(u[n_lat:], 1)], 0)
    nxt = np.concatenate([shifted(u[:n_lat], -1), shifted(u[n_lat:], -1)], 0)
    u3 = np.concatenate([u, prev, nxt], 1)
    fwd, bwd = rwkv_orders(n_lat, n_ctx)
    idx = np.concatenate([np.concatenate([np.arange(f, f + 64), np.arange(b, b + 64)]) for f, b in zip(fwd, bwd)])
    U3 = np.ascontiguousarray(u3[idx])
    coefmu = np.tile(np.concatenate([mu[0][cols], mu[1][cols]])[None], (128, 1)).astype(np.float32)
    hs = slice(hd * 64, hd * 64 + 64)
    rowp = np.zeros((128, 128), np.float32)
    rowp[:64, :64] = w0[0][hs]; rowp[64:, :64] = w0[1][hs]; rowp[:64, 64:] = a0[0][hs]; rowp[64:, 64:] = a0[1][hs]
    hv = np.tile(np.concatenate([kkw[hs], ka[hs], rk[hd], gng[hs], gnb[hs]])[None], (128, 1)).astype(np.float32)
    Wl = np.zeros((96, 192), np.float32)
    Wl[0:16, 0:64] = wB[0][:, hs]; Wl[16:32, 0:64] = wB[1][:, hs]; Wl[32:48, 64:128] = aB[0][:, hs]; Wl[48:64, 64:128] = aB[1][:, hs]
    Wl[64:96, 128:192] = gB[:, hs]
    return dict(U3=U3, coefmu=coefmu, rowp=rowp, hv=hv, Wl=Wl, cmask=a2_consts())


def hy_ztab(L):
    bands = 16
    t = np.linspace(0.0, 1.0, L, dtype=np.float32)[:, None]
    f = np.linspace(1e-4, bands - 1, bands, dtype=np.float32)[None, :]
    wt = (np.float32(2.0 * math.pi) * np.arange(L, dtype=np.float32)[:, None] / np.float32(L)).astype(np.float32)
    z = np.concatenate([t, np.cos(f * wt), -np.sin(f * wt)], -1).astype(np.float32)
    return np.ascontiguousarray(np.stack([z.T, z[::-1].T], 0))

def a3_inputs(uh, n_lat, n_ctx, j, sw, sb, w1, b1, f1, w2, b2, f2, w3, dec, hbias):
    cs = slice(j * 64, j * 64 + 64)
    cols = np.concatenate([np.arange(256)[cs], 256 + np.arange(256)[cs], 512 + np.arange(256)[cs]])
    u = uh[:, cols]
    def shifted(x, d):
        o = np.zeros_like(x)
        if d == 1: o[1:] = x[:-1]
        else: o[:-1] = x[1:]
        return o
    prev = np.concatenate([shifted(u[:n_lat], 1), shifted(u[n_lat:], 1)], 0)
    nxt = np.concatenate([shifted(u[:n_lat], -1), shifted(u[n_lat:], -1)], 0)
    H3 = np.ascontiguousarray(np.concatenate([u, prev, nxt], 1), dtype=np.float32)
    cw = np.tile(np.concatenate([sw[1][cols], sw[0][cols], sw[2][cols], sb[cols]])[None], (128, 1)).astype(np.float32)
    fc = np.array([o * 512 + d * 256 + j * 64 + c for d in range(2) for o in range(2) for c in range(64)])
    colp = np.stack([b1, f1, b2, f2], 1).astype(np.float32)
    hb = np.tile(np.concatenate([hbias[0][cs], hbias[1][cs]])[None], (128, 1)).astype(np.float32)
    return dict(H3=H3, cw=cw, ztl=hy_ztab(n_lat), ztc=hy_ztab(n_ctx), w1=np.ascontiguousarray(w1, dtype=np.float32),
                w2=np.ascontiguousarray(w2, dtype=np.float32), w3=np.ascontiguousarray(w3[:, fc], dtype=np.float32), colp=colp,
                dec=np.ascontiguousarray(dec[fc][None], dtype=np.float32), hb=hb)


N_LAT = 16384
N_CTX = 256
_PROGS = {}


def _prog(name):
    if name not in _PROGS:
        if name == "p1":
            P = build_p1(4096, 64)
        elif name == "a1":
            P = build_a1(N_LAT, N_CTX)
        elif name == "a2":
            P = build_a2(N_LAT, N_CTX)
        elif name == "a3":
            P = build_a3(N_LAT, N_CTX)
        elif name == "p2d":
            P = build_p2(4096, 64, False)
        elif name == "p2m":
            P = build_p2(4096, 64, True)
        _PROGS[name] = P.close()
    return _PROGS[name]


def _run(name, in_maps, out_name):
    nc = _prog(name)
    in_maps = [{k: np.ascontiguousarray(v, dtype=np.float32) for k, v in m.items()} for m in in_maps]
    res = run_bass_kernel_spmd(nc, in_maps, core_ids=list(range(8)))
    return [np.asarray(r[out_name]) for r in res.results]


def _rope_cs(L):
    rows = L // 64
    row = np.repeat(np.arange(rows, dtype=np.float32), 64)
    col = np.tile(np.arange(64, dtype=np.float32), rows)
    inv = (np.float32(10000.0) ** (-np.arange(16, dtype=np.float32) / np.float32(16))).astype(np.float32)
    ang = np.concatenate([row[:, None] * inv, col[:, None] * inv], -1).astype(np.float32)
    cos, sin = np.cos(ang).astype(np.float32), np.sin(ang).astype(np.float32)
    return np.concatenate([cos, cos, cos, sin, sin, sin], -1).astype(np.float32)


def _cT(cb, cctx):
    c2 = np.stack([cb, cctx], 0).astype(np.float32)
    return np.ascontiguousarray(c2.reshape(2, 8, 128).transpose(2, 0, 1).reshape(128, 16))


def kernel(x, c, ctx, c_ctx, ada_w, ada_b, w_in, w_out, q_gain, k_gain,
           rwkv_mu, rwkv_w0, rwkv_wB, rwkv_a0, rwkv_aB, rwkv_gB, rwkv_kk, rwkv_ka, rwkv_rk,
           rwkv_gn_g, rwkv_gn_b, hy_short_w, hy_short_b, hy_w1, hy_b1, hy_freq1, hy_w2, hy_b2,
           hy_freq2, hy_w3, hy_decay, hy_bias, ln1_g, ln1_b, ln2_g, ln2_b,
           ffn_w1, ffn_w3, ffn_w2, moe_router, moe_w1, moe_w3, moe_w2):
    f32 = lambda a: np.asarray(a, dtype=np.float32)
    x = f32(x).copy()
    xc = f32(ctx).copy()
    c, c_ctx = f32(c), f32(c_ctx)
    cs = _rope_cs(N_LAT)
    depth = 2
    for l in range(depth):
        aw, ab = f32(ada_w[l]), f32(ada_b[l])
        cores = [(b, q) for b in range(2) for q in range(4)]
        ins = []
        for (b, q) in cores:
            xt = np.concatenate([x[b, q * 4096:(q + 1) * 4096], xc[b, q * 64:(q + 1) * 64]], 0)
            ins.append(dict(x=xt, cT=_cT(c[b], c_ctx), adaw=aw[:, 0:2048], adab=ab[None, 0:2048], win=f32(w_in[l])))
        us = _run("p1", ins, "u")
        u = np.empty((2, N_LAT + N_CTX, 2400), np.float32)
        for (b, q), uu in zip(cores, us):
            u[b, q * 4096:(q + 1) * 4096] = uu[:4096]
            u[b, N_LAT + q * 64:N_LAT + (q + 1) * 64] = uu[4096:]
        mixo = np.empty((2, N_LAT + N_CTX, 1024), np.float32)
        gains = np.tile(np.concatenate([f32(q_gain[l]), f32(q_gain[l]), f32(k_gain[l])])[None], (128, 1))
        ins = []
        for (b, j) in cores:
            g = j // 2
            qk = np.concatenate([u[b][:, 128 * j:128 * j + 128], u[b][:, 512 + 64 * g:512 + 64 * g + 64]], 1)
            ins.append(dict(qk=qk, v=u[b][:, 640 + 64 * g:640 + 64 * g + 64], gains=gains, cs=cs))
        for (b, j), o in zip(cores, _run("a1", ins, "att")):
            mixo[b][:, 128 * j:128 * j + 128] = o
        ins = []
        for (b, j) in cores:
            ins.append(a2_inputs(u[b][:, 768:1632], N_LAT, N_CTX, j, f32(rwkv_mu[l]), f32(rwkv_w0[l]), f32(rwkv_wB[l]), f32(rwkv_a0[l]),
                                 f32(rwkv_aB[l]), f32(rwkv_gB[l]), f32(rwkv_kk[l]), f32(rwkv_ka[l]), f32(rwkv_rk[l]),
                                 f32(rwkv_gn_g[l]), f32(rwkv_gn_b[l])))
        for (b, j), o in zip(cores, _run("a2", ins, "rw")):
            mixo[b][:, 512 + 64 * j:512 + 64 * j + 64] = o
        ins = []
        for (b, j) in cores:
            ins.append(a3_inputs(u[b][:, 1632:2400], N_LAT, N_CTX, j, f32(hy_short_w[l]), f32(hy_short_b[l]), f32(hy_w1[l]), f32(hy_b1[l]),
                                 f32(hy_freq1[l]), f32(hy_w2[l]), f32(hy_b2[l]), f32(hy_freq2[l]), f32(hy_w3[l]), f32(hy_decay[l]),
                                 f32(hy_bias[l])))
        for (b, j), o in zip(cores, _run("a3", ins, "hy")):
            mixo[b][:, 768 + 64 * j:768 + 64 * j + 64] = o
        lnp = np.tile(np.concatenate([f32(ln1_g[l]), f32(ln1_b[l]), f32(ln2_g[l]), f32(ln2_b[l])])[None], (128, 1))
        jj = l // 2
        ins = []
        for (b, q) in cores:
            mt = np.concatenate([mixo[b, q * 4096:(q + 1) * 4096], mixo[b, N_LAT + q * 64:N_LAT + (q + 1) * 64]], 0)
            xt = np.concatenate([x[b, q * 4096:(q + 1) * 4096], xc[b, q * 64:(q + 1) * 64]], 0)
            d = dict(mix=mt, x=xt, cT=_cT(c[b], c_ctx), adaw=aw[:, 2048:6144], adab=ab[None, 2048:6144], wout=f32(w_out[l]), lnp=lnp)
            if l % 2 == 0:
                d.update(w1=f32(ffn_w1[jj])[None], w3=f32(ffn_w3[jj])[None], w2=f32(ffn_w2[jj])[None])
            else:
                d.update(w1=f32(moe_w1[jj]), w3=f32(moe_w3[jj]), w2=f32(moe_w2[jj]), wr=f32(moe_router[jj]))
            ins.append(d)
        outs = _run("p2d" if l % 2 == 0 else "p2m", ins, "xo")
        xn = np.empty_like(x)
        xcn = np.empty_like(xc)
        for (b, q), o in zip(cores, outs):
            xn[b, q * 4096:(q + 1) * 4096] = o[:4096]
            xcn[b, q * 64:(q + 1) * 64] = o[4096:]
        x, xc = xn, xcn
    return x
```

```python
import math


import numpy as np
from contextlib import ExitStack
import concourse.bass as bass
import concourse.mybir as mybir
from concourse.bass_utils import run_bass_kernel_spmd

F32 = mybir.dt.float32
BF16 = mybir.dt.bfloat16
AF = mybir.ActivationFunctionType
ALU = mybir.AluOpType
AX = mybir.AxisListType
NDS = 24


class Reg:
    __slots__ = ("w", "r", "name", "excl")

    def __init__(self, name="", excl=False):
        self.w = []
        self.r = {}
        self.name = name
        self.excl = excl


class _EngRec:
    def __init__(self):
        self.call = None

    def __getattr__(self, name):
        def f(*a, **k):
            self.call = (name, a, k)
        return f


class Prog:
    def __init__(self):
        self.nc = bass.Bass("TRN2", target_bir_lowering=False)
        self.es = ExitStack()
        nc = self.nc
        self.eng = {"pe": nc.tensor, "act": nc.scalar, "dve": nc.vector, "pool": nc.gpsimd, "sp": nc.sync}
        self.sem = {k: self.es.enter_context(nc.semaphore("s_" + k)) for k in self.eng}
        self.seq = {k: 0 for k in self.eng}
        self.known = {k: {} for k in self.eng}
        self.pend = {k: ([], []) for k in self.eng}
        self.dsem = [self.es.enter_context(nc.semaphore("d%d" % i)) for i in range(NDS)]
        self.dval = [0] * NDS
        self.dnext = 0
        self.ninst = 0
        self._n = 0
        self.scopes = []
        self.ccsem = None
        self.rec = None

    def sb(self, shape, dt, name=None):
        self._n += 1
        es = self.scopes[-1] if self.scopes else self.es
        return es.enter_context(self.nc.sbuf_tensor(name or "t%d" % self._n, list(shape), dt))

    def push_scope(self):
        self.scopes.append(ExitStack())

    def pop_scope(self, regs):
        for r in regs:
            for e in self.eng:
                for t in r.w:
                    self._wait1(e, t)
        self.scopes.pop().close()

    def ps(self, shape, dt, name=None):
        self._n += 1
        nbytes = int(np.prod(shape[1:])) * (4 if dt == F32 else 2)
        assert nbytes % 2048 == 0, "PSUM tensors must be whole banks"
        es = self.scopes[-1] if self.scopes else self.es
        return es.enter_context(self.nc.psum_tensor(name or "p%d" % self._n, list(shape), dt)), Reg(excl=True)

    def dram(self, name, shape, dt, kind):
        return self.nc.dram_tensor(name, list(shape), dt, kind=kind).ap()

    def _wait1(self, e, tok):
        key, sem, val = tok
        if self.known[e].get(key, 0) < val:
            self.eng[e].wait_ge(sem, val)
            self.known[e][key] = val
            self.ninst += 1

    def _deps(self, e, reads, writes, is_dma):
        for r in reads:
            for t in r.w:
                if t[0] == e and e == "pe" and not is_dma:
                    continue
                self._wait1(e, t)
            if r.excl:
                for k, t in r.r.items():
                    if k != e or is_dma:
                        self._wait1(e, t)
        for w in writes:
            for t in w.w:
                if not (t[0] == e and not is_dma):
                    self._wait1(e, t)
            for k, t in w.r.items():
                if k == e and not is_dma:
                    continue
                self._wait1(e, t)

    def op(self, e, fn, reads=(), writes=(), inc=True):
        if self.rec is not None:
            prox = _EngRec()
            fn(prox)
            name, a, k = prox.call
            self.rec.append(("op", e, (lambda eng, name=name, a=a, k=k: getattr(eng, name)(*a, **k)), tuple(reads), tuple(writes), inc))
            return None
        self._deps(e, reads, writes, False)
        inst = fn(self.eng[e])
        self.ninst += 1
        pr, pw = self.pend[e]
        pr.extend(reads)
        pw.extend(writes)
        if inc:
            self.seq[e] += 1
            inst.then_inc(self.sem[e], 1)
            tok = (e, self.sem[e], self.seq[e])
            for r in pr:
                r.r[e] = tok
            for w in pw:
                w.w = [tok]
                w.r = {}
            self.pend[e] = ([], [])
        return inst

    def dma(self, q, out, in_, reads=(), writes=(), **kw):
        if self.rec is not None:
            self.rec.append(("dma", q, out, in_, tuple(reads), tuple(writes), kw))
            return None
        self._deps(q, reads, writes, True)
        slot = self.dnext
        self.dnext = (slot + 1) % NDS
        key = ("d", slot)
        if self.dval[slot] > 0:
            self._wait1(q, (key, self.dsem[slot], self.dval[slot]))
        inst = self.eng[q].dma_start(out=out, in_=in_, **kw)
        self.ninst += 1
        self.dval[slot] += 16
        inst.then_inc(self.dsem[slot], 16)
        tok = (key, self.dsem[slot], self.dval[slot])
        for r in reads:
            r.r[key] = tok
        for w in writes:
            w.w = [t for t in w.w if isinstance(t[0], tuple) and t[0] != key] + [tok]
            w.r = {}
        return inst

    def allgather(self, out, in_, groups, reads=(), writes=()):
        q = "pool"
        self._deps(q, reads, writes, True)
        if self.ccsem is None:
            self.ccsem = self.es.enter_context(self.nc.semaphore("ccsem"))
            self.ccval = 0
        inst = self.eng[q].collective_compute("AllGather", ALU.bypass, replica_groups=groups, ins=[in_], outs=[out])
        self.ninst += 1
        self.ccval += 1
        inst.then_inc(self.ccsem, 1)
        tok = ("cc", self.ccsem, self.ccval)
        for r in reads:
            r.r["cc"] = tok
        for w in writes:
            w.w = [tok]
            w.r = {}
        return inst

    def record(self):
        self.rec = []

    def stop_record(self):
        r, self.rec = self.rec, None
        return r

    def replay_interleaved(self, lists):
        n = max(len(l) for l in lists)
        for i in range(n):
            for l in lists:
                if i < len(l):
                    it = l[i]
                    if it[0] == "op":
                        self.op(it[1], it[2], it[3], it[4], it[5])
                    else:
                        self.dma(it[1], it[2], it[3], it[4], it[5], **it[6])

    def replay_skewed(self, lists, nact):
        L = max(len(l) for l in lists)
        D = -(-L // nact)
        T = (len(lists) - 1) * D + L
        for t in range(T):
            c0 = max(0, (t - L) // D)
            for c in range(c0, min(len(lists), t // D + 1)):
                k = t - c * D
                l = lists[c]
                if 0 <= k < len(l):
                    it = l[k]
                    if it[0] == "op":
                        self.op(it[1], it[2], it[3], it[4], it[5])
                    else:
                        self.dma(it[1], it[2], it[3], it[4], it[5], **it[6])

    def finish(self, regs):
        for r in regs:
            for t in r.w:
                self._wait1("sp", t)
        for slot in range(NDS):
            if self.dval[slot] > 0:
                self._wait1("sp", (("d", slot), self.dsem[slot], self.dval[slot]))

    def close(self):
        self.es.close()
        return self.nc


D = 1024
INW = 2400
LN_EPS = 1e-6


def mods_block(P, cT, adaw, adab, ncol, ones_row, psum_banks, plus_one_ranges):
    ncb = ncol // 512
    assert ncb <= len(psum_banks)
    bc = [P.sb([128, ncol], F32) for _ in range(2)]
    r_bc = [Reg(), Reg()]
    P.push_scope()
    c_sb = P.sb([128, 16], F32)
    cs_sb = P.sb([128, 16], F32)
    r_c = Reg()
    r_cs = Reg()
    P.dma("sp", c_sb[:], cT[:, :], writes=[r_c])
    P.op("act", lambda e: e.activation(out=cs_sb[:], in_=c_sb[:], func=AF.Silu), reads=[r_c], writes=[r_cs])
    ab_sb = P.sb([1, ncol], F32)
    r_ab = Reg()
    P.dma("sp", ab_sb[:], adab[:, :], writes=[r_ab])
    aw = [P.sb([128, ncol], F32) for _ in range(2)]
    r_aw = [Reg(), Reg()]
    modrow = P.sb([1, ncol], F32)
    r_mr = Reg()
    it = 0
    for s in range(2):
        for k in range(8):
            b = it % 2
            it += 1
            P.dma("sp", aw[b][:], adaw[k * 128:(k + 1) * 128, :], writes=[r_aw[b]])
            for cb in range(ncb):
                bank, rb = psum_banks[cb]
                P.op("pe", lambda e, s=s, cb=cb, bank=bank, k=k, b=b: e.matmul(
                    bank[0:1, 0:512], cs_sb[:, s * 8 + k:s * 8 + k + 1], aw[b][:, cb * 512:(cb + 1) * 512],
                    start=(k == 0), stop=(k == 7)),
                    reads=[r_cs, r_aw[b]], writes=[rb], inc=(cb == ncb - 1))
        for cb in range(ncb):
            bank, rb = psum_banks[cb]
            P.op("dve", lambda e, cb=cb, bank=bank: e.tensor_tensor(
                out=modrow[0:1, cb * 512:(cb + 1) * 512], in0=bank[0:1, 0:512],
                in1=ab_sb[0:1, cb * 512:(cb + 1) * 512], op=ALU.add),
                reads=[rb, r_ab], writes=[r_mr])
        for (a, b2) in plus_one_ranges:
            P.op("dve", lambda e, a=a, b2=b2: e.tensor_scalar_add(out=modrow[0:1, a:b2], in0=modrow[0:1, a:b2], scalar1=1.0),
                 reads=[r_mr], writes=[r_mr])
        for cb in range(ncb):
            bank, rb = psum_banks[cb]
            P.op("pe", lambda e, cb=cb, bank=bank: e.matmul(
                bank[:, 0:512], ones_row[0:1, 0:128], modrow[0:1, cb * 512:(cb + 1) * 512], start=True, stop=True),
                reads=[r_mr], writes=[rb])
            P.op("act", lambda e, s=s, cb=cb, bank=bank: e.copy(out=bc[s][:, cb * 512:(cb + 1) * 512], in_=bank[:, 0:512]),
                 reads=[rb], writes=[r_bc[s]])
    P.pop_scope([r_bc[1]])
    return bc, r_bc


def ln_tile(P, rows, x_ap, r_x, tmp, r_tmp, stat, r_stat):
    st = stat
    P.op("dve", lambda e: e.bn_stats(out=st[:rows, 0:6], in_=x_ap[:, 0:512]), reads=[r_x], writes=[r_stat])
    P.op("dve", lambda e: e.bn_stats(out=st[:rows, 6:12], in_=x_ap[:, 512:1024]), reads=[r_x], writes=[r_stat])
    P.op("dve", lambda e: e.bn_aggr(out=st[:rows, 12:14], in_=st[:rows, 0:12]), reads=[r_stat], writes=[r_stat])
    P.op("dve", lambda e: e.tensor_scalar_add(out=st[:rows, 15:16], in0=st[:rows, 13:14], scalar1=LN_EPS), reads=[r_stat], writes=[r_stat])
    P.op("act", lambda e: e.activation(out=st[:rows, 14:15], in_=st[:rows, 15:16], func=AF.Sqrt), reads=[r_stat], writes=[r_stat])
    P.op("dve", lambda e: e.reciprocal(out=st[:rows, 14:15], in_=st[:rows, 14:15]), reads=[r_stat], writes=[r_stat])
    P.op("dve", lambda e: e.tensor_scalar(out=tmp[:rows, :], in0=x_ap, scalar1=st[:rows, 12:13], scalar2=st[:rows, 14:15],
                                          op0=ALU.subtract, op1=ALU.mult), reads=[r_x, r_stat], writes=[r_tmp])


def make_ident(P, dt=BF16):
    ident = P.sb([128, 128], dt)
    r_id = Reg()
    P.op("pool", lambda e: e.memset(ident[:], 0.0), writes=[r_id])
    P.op("pool", lambda e: e.affine_select(out=ident[:], in_=ident[:], pattern=[[-1, 128]], compare_op=ALU.not_equal,
                                           fill=1.0, base=0, channel_multiplier=1), reads=[r_id], writes=[r_id])
    return ident, r_id


def build_p1(n_lat=4096, n_ctx=64):
    P = Prog()
    ntok = n_lat + n_ctx
    x = P.dram("x", [ntok, D], F32, "ExternalInput")
    cT = P.dram("cT", [128, 16], F32, "ExternalInput")
    adaw = P.dram("adaw", [D, 2048], F32, "ExternalInput")
    adab = P.dram("adab", [1, 2048], F32, "ExternalInput")
    win = P.dram("win", [D, INW], F32, "ExternalInput")
    u = P.dram("u", [ntok, INW], F32, "ExternalOutput")

    fb = [P.ps([128, 512], F32) for i in range(6)]
    tb = [P.ps([128, 8, 128], BF16) for i in range(2)]
    ones_row = P.sb([1, 128], F32)
    r_ones = Reg()
    P.op("dve", lambda e: e.memset(ones_row[:], 1.0), writes=[r_ones])
    ident, r_id = make_ident(P)
    wb = P.sb([128, 8, INW], BF16)
    r_wb = Reg()
    for k in range(8):
        P.dma("pool", wb[:, k, :], win[k * 128:(k + 1) * 128, :], writes=[r_wb])
    bc, r_bc = mods_block(P, cT, adaw, adab, 2048, ones_row, fb[:4], [(1024, 2048)])

    NX = 3
    xs = [P.sb([128, D], F32) for _ in range(NX)]
    r_xs = [Reg() for _ in range(NX)]
    tmp = [P.sb([128, D], F32) for _ in range(2)]
    r_tmp = [Reg() for _ in range(2)]
    tmp2 = [P.sb([128, D], F32) for _ in range(2)]
    r_tmp2 = [Reg() for _ in range(2)]
    hb = [P.sb([128, D], BF16) for _ in range(2)]
    r_hb = [Reg() for _ in range(2)]
    stat = [P.sb([128, 16], F32) for _ in range(2)]
    r_stat = [Reg() for _ in range(2)]
    hT = [P.sb([128, 8, 128], BF16) for _ in range(2)]
    r_hT = [Reg() for _ in range(2)]
    uo = [P.sb([128, INW], F32) for _ in range(2)]
    r_uo = [Reg() for _ in range(2)]
    r_u_out = Reg()

    tiles = [(i * 128, 128, 0) for i in range(n_lat // 128)]
    t0 = n_lat
    while t0 < ntok:
        rows = min(128, ntok - t0)
        tiles.append((t0, rows, 1))
        t0 += rows
    for i, (t0, rows, s) in enumerate(tiles):
        a = i % NX
        b = i % 2
        P.dma("sp", xs[a][:rows, :], x[t0:t0 + rows, :], writes=[r_xs[a]])
        ln_tile(P, rows, xs[a][:rows, :], r_xs[a], tmp[b], r_tmp[b], stat[b], r_stat[b])
        P.op("pool", lambda e: e.tensor_tensor(out=tmp2[b][:rows, :], in0=tmp[b][:rows, :], in1=bc[s][:rows, 1024:2048], op=ALU.mult),
             reads=[r_tmp[b], r_bc[s]], writes=[r_tmp2[b]])
        P.op("dve", lambda e: e.tensor_tensor(out=hb[b][:rows, :], in0=tmp2[b][:rows, :], in1=bc[s][:rows, 0:1024], op=ALU.add),
             reads=[r_tmp2[b], r_bc[s]], writes=[r_hb[b]])
        tp, r_tp = tb[b]
        for k in range(8):
            P.op("pe", lambda e, k=k: e.transpose(tp[:, k, :rows], hb[b][:rows, k * 128:(k + 1) * 128], ident[:rows, :rows]),
                 reads=[r_hb[b], r_id], writes=[r_tp], inc=(k == 7))
        P.op("act", lambda e: e.copy(out=hT[b][:, :, :rows], in_=tp[:, :, :rows]), reads=[r_tp], writes=[r_hT[b]])
        for cb in range(5):
            bank, rb = fb[1 + cb]
            for k in range(8):
                P.op("pe", lambda e, k=k, cb=cb, bank=bank: e.matmul(bank[:rows, 0:480], hT[b][:, k, :rows], wb[:, k, cb * 480:(cb + 1) * 480],
                                                       start=(k == 0), stop=(k == 7)),
                     reads=[r_hT[b], r_wb], writes=[rb], inc=(k == 7))
            eng = "act" if cb % 2 == 0 else "dve"
            if eng == "act":
                P.op("act", lambda e, cb=cb, bank=bank: e.copy(out=uo[b][:rows, cb * 480:(cb + 1) * 480], in_=bank[:rows, 0:480]),
                     reads=[rb], writes=[r_uo[b]])
            else:
                P.op("dve", lambda e, cb=cb, bank=bank: e.tensor_copy(out=uo[b][:rows, cb * 480:(cb + 1) * 480], in_=bank[:rows, 0:480]),
                     reads=[rb], writes=[r_uo[b]])
        P.dma("sp", u[t0:t0 + rows, :], uo[b][:rows, :], reads=[r_uo[b]], writes=[r_u_out])
    P.finish([r_u_out])
    return P


HD = 64
QK_EPS = 1e-6


def build_a1(n_lat=16384, n_ctx=256, stage=2):
    P = Prog()
    ntok = n_lat + n_ctx
    NT = ntok // 128
    NTL = n_lat // 128
    qk = P.dram("qk", [ntok, 192], F32, "ExternalInput")
    v = P.dram("v", [ntok, 64], F32, "ExternalInput")
    gains = P.dram("gains", [128, 192], F32, "ExternalInput")
    cs = P.dram("cs", [n_lat, 192], F32, "ExternalInput")
    att = P.dram("att", [ntok, 128], F32, "ExternalOutput")

    identb, r_idb = make_ident(P, BF16)
    identf, r_idf = make_ident(P, F32)
    g_sb = P.sb([128, 192], F32)
    r_g = Reg()
    P.dma("sp", g_sb[:], gains[:, :], writes=[r_g])

    QT = P.sb([128, ntok], BF16)
    KT = P.sb([128, 2, ntok], BF16)
    VA = P.sb([128, NT, 65], BF16)
    r_QT = Reg()
    r_KT = Reg()
    r_VA = Reg()
    P.op("pool", lambda e: e.memset(VA[:, :, 64:65], 1.0), writes=[r_VA])

    NSB = 2
    NP = 3

    P.push_scope()
    tpb = P.ps([128, 8, 128], BF16)
    NB = 2
    qk_sb = [P.sb([128, 192], F32) for _ in range(NB)]
    r_qk = [Reg() for _ in range(NB)]
    v_sb = [P.sb([128, 64], F32) for _ in range(NB)]
    r_v = [Reg() for _ in range(NB)]
    cs_sb = [P.sb([128, 192], F32) for _ in range(NB)]
    r_cs = [Reg() for _ in range(NB)]
    junk = [P.sb([128, 64], F32) for _ in range(NB)]
    r_junk = [Reg() for _ in range(NB)]
    ss = [P.sb([128, 8], F32) for _ in range(NB)]
    r_ss = [Reg() for _ in range(NB)]
    qn = [P.sb([128, 192], F32) for _ in range(NB)]
    r_qn = [Reg() for _ in range(NB)]
    ra = [P.sb([128, 96], F32) for _ in range(NB)]
    rb_ = [P.sb([128, 96], F32) for _ in range(NB)]
    r_ra = [Reg() for _ in range(NB)]
    r_rb = [Reg() for _ in range(NB)]
    qr = [P.sb([128, 384], BF16) for _ in range(NB)]
    r_qr = [Reg() for _ in range(NB)]
    for b in range(NB):
        P.op("dve", lambda e, b=b: e.memset(qr[b][:], 0.0), writes=[r_qr[b]])

    for t in range(NT):
        b = t % NB
        t0 = t * 128
        lat = t < NTL
        P.dma("sp", qk_sb[b][:], qk[t0:t0 + 128, :], writes=[r_qk[b]])
        P.dma("sp", v_sb[b][:], v[t0:t0 + 128, :], writes=[r_v[b]])
        if lat:
            P.dma("sp", cs_sb[b][:], cs[t0:t0 + 128, :], writes=[r_cs[b]])
        for h in range(3):
            P.op("act", lambda e, h=h: e.activation(out=junk[b][:], in_=qk_sb[b][:, h * 64:(h + 1) * 64], func=AF.Square,
                                                    accum_out=ss[b][:, h:h + 1]), reads=[r_qk[b]], writes=[r_junk[b], r_ss[b]])
        P.op("dve", lambda e: e.tensor_scalar(out=ss[b][:, 3:6], in0=ss[b][:, 0:3], scalar1=1.0 / 64, scalar2=QK_EPS,
                                              op0=ALU.mult, op1=ALU.add), reads=[r_ss[b]], writes=[r_ss[b]])
        P.op("act", lambda e: e.activation(out=ss[b][:, 3:6], in_=ss[b][:, 3:6], func=AF.Sqrt), reads=[r_ss[b]], writes=[r_ss[b]])
        P.op("dve", lambda e: e.reciprocal(out=ss[b][:, 3:6], in_=ss[b][:, 3:6]), reads=[r_ss[b]], writes=[r_ss[b]])
        for h in range(3):
            P.op("dve", lambda e, h=h: e.scalar_tensor_tensor(out=qn[b][:, h * 64:(h + 1) * 64], in0=qk_sb[b][:, h * 64:(h + 1) * 64],
                                                              scalar=ss[b][:, 3 + h:4 + h], in1=g_sb[:, h * 64:(h + 1) * 64],
                                                              op0=ALU.mult, op1=ALU.mult),
                 reads=[r_qk[b], r_ss[b], r_g], writes=[r_qn[b]])
        if lat:
            x0 = qn[b][:].rearrange("p (i two) -> p i two", two=2)[:, :, 0]
            x1 = qn[b][:].rearrange("p (i two) -> p i two", two=2)[:, :, 1]
            o0 = qr[b][:, 0:192].rearrange("p (i two) -> p i two", two=2)[:, :, 0]
            o1 = qr[b][:, 0:192].rearrange("p (i two) -> p i two", two=2)[:, :, 1]
            c_ = cs_sb[b][:, 0:96]
            s_ = cs_sb[b][:, 96:192]
            P.op("dve", lambda e: e.tensor_tensor(out=ra[b][:], in0=x0, in1=c_, op=ALU.mult), reads=[r_qn[b], r_cs[b]], writes=[r_ra[b]])
            P.op("pool", lambda e: e.tensor_tensor(out=rb_[b][:], in0=x1, in1=s_, op=ALU.mult), reads=[r_qn[b], r_cs[b]], writes=[r_rb[b]])
            P.op("dve", lambda e: e.tensor_tensor(out=o0, in0=ra[b][:], in1=rb_[b][:], op=ALU.subtract), reads=[r_ra[b], r_rb[b]], writes=[r_qr[b]])
            P.op("dve", lambda e: e.tensor_tensor(out=ra[b][:], in0=x0, in1=s_, op=ALU.mult), reads=[r_qn[b], r_cs[b]], writes=[r_ra[b]])
            P.op("pool", lambda e: e.tensor_tensor(out=rb_[b][:], in0=x1, in1=c_, op=ALU.mult), reads=[r_qn[b], r_cs[b]], writes=[r_rb[b]])
            P.op("dve", lambda e: e.tensor_tensor(out=o1, in0=ra[b][:], in1=rb_[b][:], op=ALU.add), reads=[r_ra[b], r_rb[b]], writes=[r_qr[b]])
        else:
            P.op("dve", lambda e: e.tensor_copy(out=qr[b][:, 0:192], in_=qn[b][:]), reads=[r_qn[b]], writes=[r_qr[b]])
        P.op("pool", lambda e: e.tensor_copy(out=qr[b][:, 320:384], in_=qr[b][:, 128:192]), reads=[r_qr[b]], writes=[r_qr[b]])
        P.op("pool", lambda e: e.tensor_copy(out=VA[:, t, 0:64], in_=v_sb[b][:]), reads=[r_v[b]], writes=[r_VA])
        tp, r_tp = tpb
        for h in range(3):
            P.op("pe", lambda e, h=h: e.transpose(tp[:, h, :], qr[b][:, h * 128:(h + 1) * 128], identb[:, :]),
                 reads=[r_qr[b], r_idb], writes=[r_tp], inc=(h == 2))
        P.op("act", lambda e: e.copy(out=QT[:, t0:t0 + 128], in_=tp[:, 0, :]), reads=[r_tp], writes=[r_QT])
        P.op("dve", lambda e: e.tensor_copy(out=KT[:, :, t0:t0 + 128], in_=tp[:, 1:3, :]), reads=[r_tp], writes=[r_KT])

    P.pop_scope([r_QT, r_KT, r_VA])
    sps = [P.ps([128, 1024], F32) for _ in range(NSB)]
    ops_ = [P.ps([128, 512], F32) for _ in range(2)]
    tpf = P.ps([128, 4, 128], F32)
    pt = [P.sb([128, 1024], BF16) for _ in range(NP)]
    r_pt = [Reg() for _ in range(NP)]
    osb = [P.sb([65, 512], F32) for _ in range(2)]
    r_osb = [Reg() for _ in range(2)]
    ot = [P.sb([128, 4, 64], F32) for _ in range(2)]
    r_ot = [Reg() for _ in range(2)]
    rc = [P.sb([128, 4], F32) for _ in range(2)]
    r_rc = [Reg() for _ in range(2)]
    r_att = Reg()
    blocks = [(qb * 512, 512, list(range(NT))) for qb in range(n_lat // 512)]
    blocks.append((n_lat, n_ctx, list(range(NTL, NT))))
    if stage < 2:
        blocks = []
    items = []
    groups = []
    for (q0, nq, kts) in blocks:
        g = len(groups)
        groups.append((q0, nq))
        for ki, kt in enumerate(kts):
            items.append((g, kt, ki == 0, ki == len(kts) - 1))

    def emit_st(n):
        g, kt, first, last = items[n]
        q0, nq = groups[g]
        sb_, r_sb = sps[n % NSB]
        for h in range(2):
            P.op("pe", lambda e, h=h: e.matmul(sb_[:, h * 512:h * 512 + nq], KT[:, h, kt * 128:(kt + 1) * 128],
                                               QT[:, q0:q0 + nq], start=True, stop=True),
                 reads=[r_KT, r_QT], writes=[r_sb], inc=(h == 1))

    def emit_rest(n):
        g, kt, first, last = items[n]
        q0, nq = groups[g]
        sb_, r_sb = sps[n % NSB]
        p2 = n % NP
        for h in range(2):
            P.op("act", lambda e, h=h: e.activation(out=pt[p2][:, h * 512:h * 512 + nq], in_=sb_[:, h * 512:h * 512 + nq],
                                                    func=AF.Exp, scale=0.125),
                 reads=[r_sb], writes=[r_pt[p2]])
        for h in range(2):
            obank, r_ob = ops_[h]
            P.op("pe", lambda e, h=h, obank=obank: e.matmul(obank[0:65, 0:nq], VA[:, kt, :], pt[p2][:, h * 512:h * 512 + nq],
                                                            start=first, stop=last),
                 reads=[r_VA, r_pt[p2]], writes=[r_ob], inc=(h == 1))
        if last:
            for h in range(2):
                o2 = h
                obank, r_ob = ops_[h]
                P.op("dve", lambda e, obank=obank: e.tensor_copy(out=osb[o2][:, 0:nq], in_=obank[0:65, 0:nq]), reads=[r_ob], writes=[r_osb[o2]])
                tf, r_tf = tpf
                nj = nq // 128
                for j in range(nj):
                    P.op("pe", lambda e, j=j: e.transpose(tf[:, j, 0:65], osb[o2][0:65, j * 128:(j + 1) * 128], identf[0:65, 0:65]),
                         reads=[r_osb[o2], r_idf], writes=[r_tf], inc=(j == nj - 1))
                P.op("dve", lambda e: e.reciprocal(out=rc[o2][:, 0:nj], in_=tf[:, 0:nj, 64]), reads=[r_tf], writes=[r_rc[o2]])
                for j in range(nj):
                    P.op("dve", lambda e, j=j: e.tensor_scalar(out=ot[o2][:, j, :], in0=tf[:, j, 0:64], scalar1=rc[o2][:, j:j + 1], scalar2=None,
                                                               op0=ALU.mult), reads=[r_tf, r_rc[o2]], writes=[r_ot[o2]])
                dst = att[q0:q0 + nq, h * 64:(h + 1) * 64].rearrange("(j p) d -> p j d", p=128)
                P.dma("sp", dst, ot[o2][:, 0:nj, :], reads=[r_ot[o2]], writes=[r_att])

    for n in range(min(NSB - 1, len(items))):
        emit_st(n)
    for n in range(len(items)):
        if n + NSB - 1 < len(items):
            emit_st(n + NSB - 1)
        emit_rest(n)
    P.finish([r_att])
    return P


GN_EPS = 64e-5
WSC = -0.6065306597126334


def rwkv_orders(n_lat, n_ctx, C=64):
    ncl, ncc = n_lat // C, n_ctx // C
    fwd = [n_lat + c * C for c in range(ncc)] + [c * C for c in range(ncl)]
    bwd = [n_lat + c * C for c in range(ncc - 1, -1, -1)] + [c * C for c in range(ncl - 1, -1, -1)]
    return fwd, bwd


def build_a2(n_lat=16384, n_ctx=256, stop=99, NBUF=4, REC=True):
    P = Prog()
    ntok = n_lat + n_ctx
    fwd, bwd = rwkv_orders(n_lat, n_ctx)
    NS = len(fwd)
    U3 = P.dram("U3", [NS * 128, 864], F32, "ExternalInput")
    coefmu = P.dram("coefmu", [128, 576], F32, "ExternalInput")
    rowp = P.dram("rowp", [128, 128], F32, "ExternalInput")
    hv = P.dram("hv", [128, 320], F32, "ExternalInput")
    Wl = P.dram("Wl", [96, 192], F32, "ExternalInput")
    cmask = P.dram("cmask", [128, 128 + 256 + 128 + 96], F32, "ExternalInput")
    rw = P.dram("rw", [ntok, 64], F32, "ExternalOutput")
    yfs = P.dram("yfs", [ntok, 64], F32, "Internal")
    ybs = P.dram("ybs", [ntok, 64], F32, "Internal")
    gs = P.dram("gs", [ntok, 64], F32, "Internal")
    r_yfs, r_ybs, r_gs, r_rw = Reg(), Reg(), Reg(), Reg()

    ident, r_id = make_ident(P, F32)
    cm = P.sb([128, 608], F32)
    r_cm = Reg()
    P.dma("sp", cm[:], cmask[:, :], writes=[r_cm])
    MIT = cm[:, 0:128]
    MM = cm[:, 128:384]
    MS = cm[:, 384:512]
    LM = cm[:, 512:608]
    coef = P.sb([128, 864], F32)
    r_coef = Reg()
    P.dma("sp", coef[:, 288:864], coefmu[:, :], writes=[r_coef])
    P.op("dve", lambda e: e.tensor_tensor(out=coef[:, 0:288], in0=coef[:, 288:576], in1=coef[:, 576:864], op=ALU.add), reads=[r_coef], writes=[r_coef])
    P.op("dve", lambda e: e.tensor_scalar(out=coef[:, 0:288], in0=coef[:, 0:288], scalar1=-1.0, scalar2=1.0, op0=ALU.mult, op1=ALU.add),
         reads=[r_coef], writes=[r_coef])
    rp = P.sb([128, 128], F32)
    hvs = P.sb([128, 320], F32)
    wl = P.sb([96, 192], F32)
    r_par = Reg()
    P.dma("sp", rp[:], rowp[:, :], writes=[r_par])
    P.dma("sp", hvs[:], hv[:, :], writes=[r_par])
    P.dma("sp", wl[:], Wl[:, :], writes=[r_par])
    KKW, KA, RK, GNG, GNB = (hvs[:, i * 64:(i + 1) * 64] for i in range(5))
    ones = P.sb([128, 128], F32)
    r_ones = Reg()
    P.op("pool", lambda e: e.memset(ones[:], 1.0), writes=[r_ones])

    banks = [P.ps([128, 512], F32) for _ in range(8)]
    if NBUF == 1 or not REC:
        bsets = [list(range(8))] * max(NBUF, 1)
    else:
        bsets = [[] for _ in range(NBUF)]
        for b_ in range(8):
            bsets[b_ * NBUF // 8].append(b_)
    bk = [0] * 8
    cur = [0]

    def nb():
        c = cur[0]
        b = banks[bsets[c][bk[c] % len(bsets[c])]]
        bk[c] += 1
        return b

    ev = [0]

    def evac(out, in_, reads, writes):
        ev[0] += 1
        if ev[0] % 2 == 0:
            P.op("act", lambda e: e.copy(out=out, in_=in_), reads=reads, writes=writes)
        else:
            P.op("dve", lambda e: e.tensor_copy(out=out, in_=in_), reads=reads, writes=writes)

    def T(shape=(128, 128), n=None):
        n = n or NBUF
        return [P.sb(list(shape), F32) for _ in range(n)], [Reg() for _ in range(n)]

    u3, r_u3 = T((128, 864))
    prod, r_prod = T((128, 864))
    us, r_us = T((128, 288))
    lo, r_lo = T((128, 96))
    loT, r_loT = T((96, 128))
    wa, r_wa = T((128, 128))
    gg, r_gg = T((128, 64))
    kk, r_kk = T((128, 64))
    sm, r_sm = T((128, 8))
    tmp, r_tmp = T((128, 64))
    tmp2, r_tmp2 = T((128, 64))
    kd, r_kd = T((128, 64))
    bdn = ["lw", "a", "b", "kd", "r", "v"]
    bd = {n: T() for n in bdn}
    for n in bdn:
        for i in range(NBUF):
            P.op("pool", lambda e, n=n, i=i: e.memset(bd[n][0][i][:], 0.0), writes=[bd[n][1][i]])
    ex, r_ex = T((128, 512))
    ee, r_ee = T((128, 256))
    At, r_At = T()
    BKt, r_BKt = T((128, 256))
    Rt, r_Rt = T()
    BKG, r_BKG = T((128, 256))
    gcc, r_gcc = T((128, 1))
    BKT, r_BKT = T((128, 256))
    ART, r_ART = T((128, 256))
    LA, r_LA = T((128, 256))
    LK, r_LK = T((128, 256))
    X, r_X = T()
    XT, r_XT = T()
    X2, r_X2 = T()
    XT2, r_XT2 = T()
    TT, r_TT = T()
    Pm, r_Pm = T()
    LV, r_LV = T()
    Q, r_Q = T()
    RpT, r_RpT = T()
    Mm, r_Mm = T()
    yo, r_yo = T()
    ST = [P.sb([128, 128], F32) for _ in range(2)]
    r_ST = [Reg(), Reg()]
    P.op("pool", lambda e: e.memset(ST[0][:], 0.0), writes=[r_ST[0]])

    lists = []
    for s in range(NS):
        i = s % NBUF
        cur[0] = i if REC else 0
        if REC:
            P.record()
        LW, r_LW = bd["lw"][0][i], bd["lw"][1][i]
        Ab, r_Ab = bd["a"][0][i], bd["a"][1][i]
        Bb, r_Bb = bd["b"][0][i], bd["b"][1][i]
        KDb, r_KDb = bd["kd"][0][i], bd["kd"][1][i]
        Rb, r_Rb = bd["r"][0][i], bd["r"][1][i]
        Vb, r_Vb = bd["v"][0][i], bd["v"][1][i]
        P.dma("sp", u3[i][:], U3[s * 128:(s + 1) * 128, :], writes=[r_u3[i]])
        P.op("dve", lambda e: e.tensor_tensor(out=prod[i][:], in0=u3[i][:], in1=coef[:], op=ALU.mult), reads=[r_u3[i], r_coef], writes=[r_prod[i]])
        P.op("pool", lambda e: e.tensor_tensor(out=us[i][:], in0=prod[i][:, 0:288], in1=prod[i][:, 288:576], op=ALU.add), reads=[r_prod[i]], writes=[r_us[i]])
        P.op("dve", lambda e: e.tensor_tensor(out=us[i][:], in0=us[i][:], in1=prod[i][:, 576:864], op=ALU.add), reads=[r_prod[i], r_us[i]], writes=[r_us[i]])
        r_ = us[i][:, 0:64]
        k_ = us[i][:, 64:128]
        v_ = us[i][:, 128:192]
        if stop <= 1:
            continue
        P.op("act", lambda e: e.activation(out=lo[i][:, 0:32], in_=us[i][:, 192:224], func=AF.Tanh), reads=[r_us[i]], writes=[r_lo[i]])
        P.op("act", lambda e: e.activation(out=lo[i][:, 64:96], in_=us[i][:, 256:288], func=AF.Sigmoid), reads=[r_us[i]], writes=[r_lo[i]])
        P.op("pool", lambda e: e.tensor_copy(out=lo[i][:, 32:64], in_=us[i][:, 224:256]), reads=[r_us[i]], writes=[r_lo[i]])
        P.op("dve", lambda e: e.tensor_tensor(out=lo[i][:], in0=lo[i][:], in1=LM, op=ALU.mult), reads=[r_lo[i], r_cm], writes=[r_lo[i]])
        b1, rb1 = nb()
        P.op("pe", lambda e: e.transpose(b1[0:96, 0:128], lo[i][:, :], ident[:, :]), reads=[r_lo[i], r_id], writes=[rb1])
        evac(loT[i][:, :], b1[0:96, 0:128], [rb1], [r_loT[i]])
        b2, rb2 = nb()
        P.op("pe", lambda e: e.matmul(b2[:, 0:192], loT[i][:, :], wl[:, :], start=True, stop=True), reads=[r_loT[i], r_par], writes=[rb2])
        P.op("dve", lambda e: e.tensor_tensor(out=wa[i][:], in0=b2[:, 0:128], in1=rp[:], op=ALU.add), reads=[rb2, r_par], writes=[r_wa[i]])
        P.op("act", lambda e: e.copy(out=gg[i][:], in_=b2[:, 128:192]), reads=[rb2], writes=[r_gg[i]])
        P.op("act", lambda e: e.activation(out=wa[i][:], in_=wa[i][:], func=AF.Sigmoid), reads=[r_wa[i]], writes=[r_wa[i]])
        sw = wa[i][:, 0:64]
        asg = wa[i][:, 64:128]
        if stop <= 2:
            continue
        P.op("dve", lambda e: e.tensor_tensor(out=kk[i][:], in0=k_, in1=KKW, op=ALU.mult), reads=[r_us[i], r_par], writes=[r_kk[i]])
        P.op("act", lambda e: e.activation(out=tmp[i][:], in_=kk[i][:], func=AF.Square, accum_out=sm[i][:, 0:1]), reads=[r_kk[i]], writes=[r_tmp[i], r_sm[i]])
        P.op("dve", lambda e: e.tensor_scalar_add(out=sm[i][:, 1:2], in0=sm[i][:, 0:1], scalar1=1e-12), reads=[r_sm[i]], writes=[r_sm[i]])
        P.op("act", lambda e: e.activation(out=sm[i][:, 1:2], in_=sm[i][:, 1:2], func=AF.Sqrt), reads=[r_sm[i]], writes=[r_sm[i]])
        P.op("dve", lambda e: e.reciprocal(out=sm[i][:, 2:3], in_=sm[i][:, 1:2]), reads=[r_sm[i]], writes=[r_sm[i]])
        P.op("dve", lambda e: e.tensor_scalar(out=kk[i][:], in0=kk[i][:], scalar1=sm[i][:, 2:3], scalar2=None, op0=ALU.mult), reads=[r_kk[i], r_sm[i]], writes=[r_kk[i]])
        P.op("dve", lambda e: e.scalar_tensor_tensor(out=tmp2[i][:], in0=asg, scalar=-1.0, in1=KA, op0=ALU.add, op1=ALU.mult), reads=[r_wa[i], r_par], writes=[r_tmp2[i]])
        P.op("dve", lambda e: e.scalar_tensor_tensor(out=kd[i][:], in0=tmp2[i][:], scalar=1.0, in1=k_, op0=ALU.add, op1=ALU.mult), reads=[r_tmp2[i], r_us[i]], writes=[r_kd[i]])
        P.op("dve", lambda e: e.tensor_tensor(out=tmp[i][:], in0=r_, in1=RK, op=ALU.mult), reads=[r_us[i], r_par], writes=[r_tmp[i]])
        P.op("dve", lambda e: e.tensor_tensor(out=tmp[i][:], in0=tmp[i][:], in1=kd[i][:], op=ALU.mult), reads=[r_tmp[i], r_kd[i]], writes=[r_tmp[i]])
        P.op("dve", lambda e: e.tensor_reduce(out=sm[i][:, 3:4], in_=tmp[i][:], axis=AX.X, op=ALU.add), reads=[r_tmp[i]], writes=[r_sm[i]])
        if stop <= 3:
            continue
        for h in range(2):
            ps_ = slice(h * 64, (h + 1) * 64)
            ce = "act" if h == 0 else "pool"
            P.op("dve", lambda e: e.tensor_scalar(out=LW[ps_, ps_], in0=wa[i][ps_, 0:64], scalar1=WSC, scalar2=None, op0=ALU.mult), reads=[r_wa[i]], writes=[r_LW])
            P.op("dve", lambda e: e.tensor_scalar(out=Ab[ps_, ps_], in0=kk[i][ps_, :], scalar1=-1.0, scalar2=None, op0=ALU.mult), reads=[r_kk[i]], writes=[r_Ab])
            P.op("pool", lambda e: e.tensor_tensor(out=Bb[ps_, ps_], in0=kk[i][ps_, :], in1=wa[i][ps_, 64:128], op=ALU.mult), reads=[r_kk[i], r_wa[i]], writes=[r_Bb])
            for (dst, r_dst, src, r_src) in ((KDb, r_KDb, kd[i][ps_, :], r_kd[i]), (Rb, r_Rb, us[i][ps_, 0:64], r_us[i]), (Vb, r_Vb, us[i][ps_, 128:192], r_us[i])):
                if ce == "act":
                    P.op("act", lambda e, dst=dst, src=src: e.copy(out=dst[ps_, ps_], in_=src), reads=[r_src], writes=[r_dst])
                else:
                    P.op("pool", lambda e, dst=dst, src=src: e.tensor_copy(out=dst[ps_, ps_], in_=src), reads=[r_src], writes=[r_dst])
        if stop <= 4:
            continue
        b3, rb3 = nb()
        P.op("pe", lambda e: e.matmul(b3[:, 0:128], MIT, LW[:, :], start=True, stop=True), reads=[r_cm, r_LW], writes=[rb3], inc=False)
        P.op("pe", lambda e: e.matmul(b3[:, 128:256], ones[:, :], LW[:, :], start=True, stop=True), reads=[r_ones, r_LW], writes=[rb3], inc=False)
        P.op("pe", lambda e: e.matmul(b3[:, 256:257], LW[:, :], ones[:, 0:1], start=True, stop=True), reads=[r_ones, r_LW], writes=[rb3])
        P.op("act", lambda e: e.activation(out=ex[i][:, 0:128], in_=b3[:, 0:128], func=AF.Exp), reads=[rb3], writes=[r_ex[i]])
        P.op("act", lambda e: e.activation(out=ex[i][:, 128:256], in_=b3[:, 0:128], func=AF.Exp, scale=-1.0), reads=[rb3], writes=[r_ex[i]])
        P.op("act", lambda e: e.activation(out=ex[i][:, 256:384], in_=b3[:, 128:256], func=AF.Exp), reads=[rb3], writes=[r_ex[i]])
        P.op("act", lambda e: e.activation(out=gcc[i][:, 0:1], in_=b3[:, 256:257], func=AF.Exp), reads=[rb3], writes=[r_gcc[i]])
        P.op("act", lambda e: e.activation(out=ex[i][:, 384:512], in_=LW[:, :], func=AF.Exp, scale=-1.0), reads=[r_LW], writes=[r_ex[i]])
        EI = ex[i][:, 0:128]
        EN = ex[i][:, 128:256]
        P.op("dve", lambda e: e.tensor_tensor(out=ee[i][:, 0:128], in0=EI, in1=ex[i][:, 384:512], op=ALU.mult), reads=[r_ex[i]], writes=[r_ee[i]])
        P.op("pool", lambda e: e.tensor_tensor(out=ee[i][:, 128:256], in0=ex[i][:, 256:384], in1=EN, op=ALU.mult), reads=[r_ex[i]], writes=[r_ee[i]])
        P.op("dve", lambda e: e.tensor_tensor(out=At[i][:], in0=Ab[:, :], in1=ee[i][:, 0:128], op=ALU.mult), reads=[r_Ab, r_ee[i]], writes=[r_At[i]])
        P.op("pool", lambda e: e.tensor_tensor(out=BKt[i][:, 0:128], in0=Bb[:, :], in1=EN, op=ALU.mult), reads=[r_Bb, r_ex[i]], writes=[r_BKt[i]])
        P.op("dve", lambda e: e.tensor_tensor(out=BKt[i][:, 128:256], in0=KDb[:, :], in1=EN, op=ALU.mult), reads=[r_KDb, r_ex[i]], writes=[r_BKt[i]])
        P.op("pool", lambda e: e.tensor_tensor(out=Rt[i][:], in0=Rb[:, :], in1=EI, op=ALU.mult), reads=[r_Rb, r_ex[i]], writes=[r_Rt[i]])
        P.op("dve", lambda e: e.tensor_tensor(out=BKG[i][:, 0:128], in0=Bb[:, :], in1=ee[i][:, 128:256], op=ALU.mult), reads=[r_Bb, r_ee[i]], writes=[r_BKG[i]])
        P.op("pool", lambda e: e.tensor_tensor(out=BKG[i][:, 128:256], in0=KDb[:, :], in1=ee[i][:, 128:256], op=ALU.mult), reads=[r_KDb, r_ee[i]], writes=[r_BKG[i]])
        if stop <= 5:
            continue
        b4, rb4 = nb()
        P.op("pe", lambda e: e.transpose(b4[:, 0:128], BKt[i][:, 0:128], ident[:, :]), reads=[r_BKt[i], r_id], writes=[rb4], inc=False)
        P.op("pe", lambda e: e.transpose(b4[:, 128:256], BKt[i][:, 128:256], ident[:, :]), reads=[r_BKt[i], r_id], writes=[rb4])
        evac(BKT[i][:, :], b4[:, 0:256], [rb4], [r_BKT[i]])
        b5, rb5 = nb()
        P.op("pe", lambda e: e.transpose(b5[:, 0:128], At[i][:, :], ident[:, :]), reads=[r_At[i], r_id], writes=[rb5], inc=False)
        P.op("pe", lambda e: e.transpose(b5[:, 128:256], Rt[i][:, :], ident[:, :]), reads=[r_Rt[i], r_id], writes=[rb5])
        evac(ART[i][:, :], b5[:, 0:256], [rb5], [r_ART[i]])
        b6, rb6 = nb()
        P.op("pe", lambda e: e.matmul(b6[:, 0:256], BKT[i][:, 0:128], ART[i][:, :], start=True, stop=True), reads=[r_BKT[i], r_ART[i]], writes=[rb6])
        P.op("dve", lambda e: e.tensor_tensor(out=LA[i][:], in0=b6[:, 0:256], in1=MM, op=ALU.mult), reads=[rb6, r_cm], writes=[r_LA[i]])
        b7, rb7 = nb()
        P.op("pe", lambda e: e.matmul(b7[:, 0:256], BKT[i][:, 128:256], ART[i][:, :], start=True, stop=True), reads=[r_BKT[i], r_ART[i]], writes=[rb7])
        P.op("dve", lambda e: e.tensor_tensor(out=LK[i][:], in0=b7[:, 0:256], in1=MM, op=ALU.mult), reads=[rb7, r_cm], writes=[r_LK[i]])
        b8, rb8 = nb()
        P.op("pe", lambda e: e.matmul(b8[:, 0:128], ART[i][:, 0:128], BKT[i][:, 0:128], start=True, stop=True), reads=[r_BKT[i], r_ART[i]], writes=[rb8])
        P.op("dve", lambda e: e.tensor_tensor(out=XT[i][:], in0=b8[:, 0:128], in1=MS, op=ALU.mult), reads=[rb8, r_cm], writes=[r_XT[i]])
        if stop <= 6:
            continue
        P.op("pool", lambda e: e.tensor_tensor(out=TT[i][:], in0=LA[i][:, 0:128], in1=ident[:, :], op=ALU.add), reads=[r_LA[i], r_id], writes=[r_TT[i]])
        cx, r_cx = LA[i][:, 0:128], r_LA[i]
        cxt, r_cxt = XT[i][:, :], r_XT[i]
        nxt = [(X2[i], r_X2[i], XT2[i], r_XT2[i]), (X[i], r_X[i], XT[i], r_XT[i])]
        for lv in range(5):
            nX, r_nX, nXT, r_nXT = nxt[lv % 2]
            if lv < 4:
                ba, rba = nb()
                P.op("pe", lambda e, cx=cx, cxt=cxt, ba=ba: e.matmul(ba[:, 0:128], cxt, cx, start=True, stop=True), reads=[r_cx, r_cxt], writes=[rba])
            bb, rbb = nb()
            P.op("pe", lambda e, cx=cx, cxt=cxt, bb=bb: e.matmul(bb[:, 0:128], cx, cxt, start=True, stop=True), reads=[r_cx, r_cxt], writes=[rbb])
            if lv < 4:
                P.op("act", lambda e, nX=nX, ba=ba: e.copy(out=nX[:, :], in_=ba[:, 0:128]), reads=[rba], writes=[r_nX])
            P.op("dve", lambda e, nXT=nXT, bb=bb: e.tensor_copy(out=nXT[:, :], in_=bb[:, 0:128]), reads=[rbb], writes=[r_nXT])
            bc, rbc = nb()
            P.op("pe", lambda e, nXT=nXT, bc=bc: e.matmul(bc[:, 0:128], nXT[:, :], TT[i][:, :], start=True, stop=True), reads=[r_nXT, r_TT[i]], writes=[rbc])
            P.op("dve", lambda e, bc=bc: e.tensor_tensor(out=TT[i][:], in0=bc[:, 0:128], in1=TT[i][:], op=ALU.add), reads=[rbc, r_TT[i]], writes=[r_TT[i]])
            cx, r_cx, cxt, r_cxt = nX[:, :], r_nX, nXT[:, :], r_nXT
        if stop <= 7:
            continue
        b9, rb9 = nb()
        P.op("pe", lambda e: e.matmul(b9[:, 0:128], TT[i][:, :], At[i][:, :], start=True, stop=True), reads=[r_TT[i], r_At[i]], writes=[rb9], inc=False)
        P.op("pe", lambda e: e.matmul(b9[:, 128:256], LK[i][:, 0:128], Vb[:, :], start=True, stop=True), reads=[r_LK[i], r_Vb], writes=[rb9])
        P.op("act", lambda e: e.copy(out=Pm[i][:, :], in_=b9[:, 0:128]), reads=[rb9], writes=[r_Pm[i]])
        P.op("dve", lambda e: e.tensor_copy(out=LV[i][:, :], in_=b9[:, 128:256]), reads=[rb9], writes=[r_LV[i]])
        b10, rb10 = nb()
        P.op("pe", lambda e: e.matmul(b10[:, 0:128], TT[i][:, :], LV[i][:, :], start=True, stop=True), reads=[r_TT[i], r_LV[i]], writes=[rb10], inc=False)
        P.op("pe", lambda e: e.matmul(b10[:, 128:256], Pm[i][:, :], LA[i][:, 128:256], start=True, stop=True), reads=[r_Pm[i], r_LA[i]], writes=[rb10], inc=False)
        P.op("pe", lambda e: e.matmul(b10[:, 256:384], Pm[i][:, :], BKG[i][:, 0:128], start=True, stop=True), reads=[r_Pm[i], r_BKG[i]], writes=[rb10])
        P.op("act", lambda e: e.copy(out=Q[i][:, :], in_=b10[:, 0:128]), reads=[rb10], writes=[r_Q[i]])
        P.op("dve", lambda e: e.tensor_tensor(out=RpT[i][:], in0=b10[:, 128:256], in1=ART[i][:, 128:256], op=ALU.add), reads=[rb10, r_ART[i]], writes=[r_RpT[i]])
        P.op("dve", lambda e: e.scalar_tensor_tensor(out=Mm[i][:], in0=ident[:, :], scalar=gcc[i][:, 0:1], in1=b10[:, 256:384], op0=ALU.mult, op1=ALU.add),
             reads=[rb10, r_id, r_gcc[i]], writes=[r_Mm[i]])
        if stop <= 8:
            continue
        sc, sn = s % 2, (s + 1) % 2
        b11, rb11 = nb()
        P.op("pe", lambda e: e.matmul(b11[:, 0:128], LA[i][:, 128:256], Q[i][:, :], start=True, stop=False), reads=[r_LA[i], r_Q[i]], writes=[rb11], inc=False)
        P.op("pe", lambda e: e.matmul(b11[:, 0:128], LK[i][:, 128:256], Vb[:, :], start=False, stop=False), reads=[r_LK[i], r_Vb], writes=[rb11], inc=False)
        P.op("pe", lambda e: e.matmul(b11[:, 0:128], RpT[i][:, :], ST[sc][:, :], start=False, stop=True), reads=[r_RpT[i], r_ST[sc]], writes=[rb11])
        b12, rb12 = nb()
        P.op("pe", lambda e: e.matmul(b12[:, 0:128], BKG[i][:, 0:128], Q[i][:, :], start=True, stop=False), reads=[r_BKG[i], r_Q[i]], writes=[rb12], inc=False)
        P.op("pe", lambda e: e.matmul(b12[:, 0:128], BKG[i][:, 128:256], Vb[:, :], start=False, stop=False), reads=[r_BKG[i], r_Vb], writes=[rb12], inc=False)
        P.op("pe", lambda e: e.matmul(b12[:, 0:128], Mm[i][:, :], ST[sc][:, :], start=False, stop=True), reads=[r_Mm[i], r_ST[sc]], writes=[rb12])
        P.op("act", lambda e: e.copy(out=ST[sn][:, :], in_=b12[:, 0:128]), reads=[rb12], writes=[r_ST[sn]])
        P.op("dve", lambda e: e.scalar_tensor_tensor(out=yo[i][:], in0=Vb[:, :], scalar=sm[i][:, 3:4], in1=b11[:, 0:128], op0=ALU.mult, op1=ALU.add),
             reads=[rb11, r_Vb, r_sm[i]], writes=[r_yo[i]])
        tf, tb_ = fwd[s], bwd[s]
        P.dma("sp", yfs[tf:tf + 64, :], yo[i][0:64, 0:64], reads=[r_yo[i]], writes=[r_yfs])
        P.dma("sp", ybs[tb_:tb_ + 64, :], yo[i][64:128, 64:128], reads=[r_yo[i]], writes=[r_ybs])
        P.dma("sp", gs[tf:tf + 64, :], gg[i][0:64, :], reads=[r_gg[i]], writes=[r_gs])
        if REC:
            lists.append(P.stop_record())
            if s == NS - 1:
                P.replay_skewed(lists, NBUF)
                lists = []

    if stop < 99:
        P.finish([])
        return P
    yf_, r_yf_ = T((128, 64), 2)
    yb_, r_yb_ = T((128, 64), 2)
    g_, r_g_ = T((128, 64), 2)
    st, r_st = T((128, 16), 2)
    yn, r_yn = T((128, 64), 2)
    for t in range(ntok // 128):
        i = t % 2
        t0 = t * 128
        P.dma("sp", yf_[i][:], yfs[t0:t0 + 128, :], reads=[r_yfs], writes=[r_yf_[i]])
        P.dma("sp", yb_[i][:], ybs[t0:t0 + 128, :], reads=[r_ybs], writes=[r_yb_[i]])
        P.dma("sp", g_[i][:], gs[t0:t0 + 128, :], reads=[r_gs], writes=[r_g_[i]])
        P.op("dve", lambda e: e.tensor_tensor(out=yf_[i][:], in0=yf_[i][:], in1=yb_[i][:], op=ALU.add), reads=[r_yf_[i], r_yb_[i]], writes=[r_yf_[i]])
        P.op("dve", lambda e: e.bn_stats(out=st[i][:, 0:6], in_=yf_[i][:]), reads=[r_yf_[i]], writes=[r_st[i]])
        P.op("dve", lambda e: e.bn_aggr(out=st[i][:, 6:8], in_=st[i][:, 0:6]), reads=[r_st[i]], writes=[r_st[i]])
        P.op("dve", lambda e: e.tensor_scalar_add(out=st[i][:, 8:9], in0=st[i][:, 7:8], scalar1=GN_EPS), reads=[r_st[i]], writes=[r_st[i]])
        P.op("act", lambda e: e.activation(out=st[i][:, 8:9], in_=st[i][:, 8:9], func=AF.Sqrt), reads=[r_st[i]], writes=[r_st[i]])
        P.op("dve", lambda e: e.reciprocal(out=st[i][:, 9:10], in_=st[i][:, 8:9]), reads=[r_st[i]], writes=[r_st[i]])
        P.op("dve", lambda e: e.tensor_scalar(out=yn[i][:], in0=yf_[i][:], scalar1=st[i][:, 6:7], scalar2=st[i][:, 9:10], op0=ALU.subtract, op1=ALU.mult),
             reads=[r_yf_[i], r_st[i]], writes=[r_yn[i]])
        P.op("pool", lambda e: e.tensor_tensor(out=yn[i][:], in0=yn[i][:], in1=GNG, op=ALU.mult), reads=[r_yn[i], r_par], writes=[r_yn[i]])
        P.op("pool", lambda e: e.tensor_tensor(out=yn[i][:], in0=yn[i][:], in1=GNB, op=ALU.add), reads=[r_yn[i], r_par], writes=[r_yn[i]])
        P.op("dve", lambda e: e.tensor_tensor(out=yn[i][:], in0=yn[i][:], in1=g_[i][:], op=ALU.mult), reads=[r_yn[i], r_g_[i]], writes=[r_yn[i]])
        P.dma("sp", rw[t0:t0 + 128, :], yn[i][:], reads=[r_yn[i]], writes=[r_rw])
    P.finish([r_rw])
    return P


PI = math.pi


def build_a3(n_lat=16384, n_ctx=256, stop=99):
    P = Prog()
    ntok = n_lat + n_ctx
    NT = ntok // 128
    H3 = P.dram("H3", [ntok, 576], F32, "ExternalInput")
    cw = P.dram("cw", [128, 768], F32, "ExternalInput")
    ztl = P.dram("ztl", [2, 33, n_lat], F32, "ExternalInput")
    ztc = P.dram("ztc", [2, 33, n_ctx], F32, "ExternalInput")
    w1d = P.dram("w1", [33, 64], F32, "ExternalInput")
    w2d = P.dram("w2", [64, 64], F32, "ExternalInput")
    w3d = P.dram("w3", [64, 256], F32, "ExternalInput")
    colp = P.dram("colp", [64, 4], F32, "ExternalInput")
    decd = P.dram("dec", [1, 256], F32, "ExternalInput")
    hbd = P.dram("hb", [128, 128], F32, "ExternalInput")
    hy = P.dram("hy", [ntok, 64], F32, "ExternalOutput")
    WL = 2 * n_lat - 1
    WC = 2 * n_ctx - 1
    KDl = P.dram("KDl", [128, WL + 1], BF16, "Internal")
    KDc = P.dram("KDc", [128, WC + 1], BF16, "Internal")
    r_KD = {0: Reg(), 1: Reg()}
    r_hy = Reg()

    identf, r_idf = make_ident(P, F32)
    J = P.sb([128, 128], BF16)
    r_J = Reg()
    P.op("pool", lambda e: e.memset(J[:], 0.0), writes=[r_J])
    P.op("pool", lambda e: e.affine_select(out=J[:], in_=J[:], pattern=[[1, 128]], compare_op=ALU.not_equal, fill=1.0, base=-127, channel_multiplier=1),
         reads=[r_J], writes=[r_J])
    ones = P.sb([128, 128], F32)
    r_ones = Reg()
    P.op("pool", lambda e: e.memset(ones[:], 1.0), writes=[r_ones])
    npi = P.sb([128, 1], F32)
    P.op("pool", lambda e: e.memset(npi[:], PI / 2), writes=[r_ones])

    cws = P.sb([128, 768], F32)
    w1s = P.sb([33, 64], F32)
    w2s = P.sb([64, 64], F32)
    w3s = P.sb([64, 256], F32)
    cps = P.sb([64, 4], F32)
    decs = P.sb([1, 256], F32)
    hbs = P.sb([128, 128], F32)
    r_par = Reg()
    for dst, src in ((cws, cw), (w1s, w1d), (w2s, w2d), (w3s, w3d), (cps, colp), (decs, decd), (hbs, hbd)):
        P.dma("sp", dst[:], src[:, :], writes=[r_par])
    P.op("dve", lambda e: e.tensor_scalar(out=decs[:], in0=decs[:], scalar1=-1.0, scalar2=None, op0=ALU.mult), reads=[r_par], writes=[r_par])

    banks = [P.ps([128, 512], F32) for _ in range(8)]
    bk = [0]

    def nb():
        b = banks[bk[0] % 6]
        bk[0] += 1
        return b
    ybanks = banks[6:8]

    SC = P.sb([128, 192, NT], F32)
    r_SC = Reg()
    RN = P.sb([128, 2, 128], F32)
    r_RN = Reg()
    P.push_scope()
    h3 = [P.sb([128, 576], F32) for _ in range(2)]
    r_h3 = [Reg(), Reg()]
    pr = [P.sb([128, 576], F32) for _ in range(2)]
    r_pr = [Reg(), Reg()]
    s1 = [P.sb([128, 192], F32) for _ in range(2)]
    r_s1 = [Reg(), Reg()]
    for t in range(NT):
        i = t % 2
        P.dma("sp", h3[i][:], H3[t * 128:(t + 1) * 128, :], writes=[r_h3[i]])
        P.op("dve", lambda e: e.tensor_tensor(out=pr[i][:], in0=h3[i][:], in1=cws[:, 0:576], op=ALU.mult), reads=[r_h3[i], r_par], writes=[r_pr[i]])
        P.op("pool", lambda e: e.tensor_tensor(out=s1[i][:], in0=pr[i][:, 0:192], in1=pr[i][:, 192:384], op=ALU.add), reads=[r_pr[i]], writes=[r_s1[i]])
        P.op("pool", lambda e: e.tensor_tensor(out=s1[i][:], in0=s1[i][:], in1=pr[i][:, 384:576], op=ALU.add), reads=[r_pr[i], r_s1[i]], writes=[r_s1[i]])
        P.op("dve", lambda e: e.tensor_tensor(out=SC[:, :, t], in0=s1[i][:], in1=cws[:, 576:768], op=ALU.add), reads=[r_s1[i], r_par], writes=[r_SC])

    nacc = 2 * max(n_lat // 512, 1) + 2
    acc = P.sb([128, 2, 2 * (n_lat // 512 + 1)], F32)
    r_acc = Reg()
    P.op("pool", lambda e: e.memset(acc[:], 0.0), writes=[r_acc])
    zt = [P.sb([33, 512], F32) for _ in range(2)]
    r_zt = [Reg(), Reg()]
    hA = [P.sb([64, 512], F32) for _ in range(2)]
    r_hA = [Reg(), Reg()]
    hB = [P.sb([64, 512], F32) for _ in range(2)]
    r_hB = [Reg(), Reg()]
    win = [P.sb([128, 512], F32) for _ in range(2)]
    r_win = [Reg(), Reg()]
    hw = [P.sb([128, 512], F32) for _ in range(2)]
    r_hw = [Reg(), Reg()]
    hwb = [P.sb([128, 512], BF16) for _ in range(2)]
    r_hwb = [Reg(), Reg()]
    junk = [P.sb([128, 512], F32) for _ in range(2)]
    r_junk = [Reg(), Reg()]
    sS = [P.sb([64, 512], F32) for _ in range(2)]
    sC = [P.sb([64, 512], F32) for _ in range(2)]
    sQ = [P.sb([64, 512], F32) for _ in range(2)]
    r_sS = [Reg(), Reg()]
    r_sC = [Reg(), Reg()]
    r_sQ = [Reg(), Reg()]

    def sin_big(h, r_h, N, i):
        S, C, Q = sS[i], sC[i], sQ[i]
        P.op("act", lambda e: e.activation(out=S[:, 0:N], in_=h[:, 0:N], func=AF.Sin, scale=0.125), reads=[r_h], writes=[r_sS[i]])
        P.op("act", lambda e: e.activation(out=C[:, 0:N], in_=h[:, 0:N], func=AF.Sin, scale=0.125, bias=npi[0:64, 0:1]), reads=[r_h, r_ones], writes=[r_sC[i]])
        for lv in range(3):
            dst = h if lv == 2 else S
            r_dst = r_h if lv == 2 else r_sS[i]
            if lv < 2:
                P.op("pool", lambda e: e.tensor_tensor(out=Q[:, 0:N], in0=S[:, 0:N], in1=S[:, 0:N], op=ALU.mult), reads=[r_sS[i]], writes=[r_sQ[i]])
            P.op("dve", lambda e, dst=dst: e.scalar_tensor_tensor(out=dst[:, 0:N], in0=S[:, 0:N], scalar=2.0, in1=C[:, 0:N], op0=ALU.mult, op1=ALU.mult),
                 reads=[r_sS[i], r_sC[i]], writes=[r_dst])
            if lv < 2:
                P.op("dve", lambda e: e.tensor_scalar(out=C[:, 0:N], in0=Q[:, 0:N], scalar1=-2.0, scalar2=1.0, op0=ALU.mult, op1=ALU.add),
                     reads=[r_sQ[i]], writes=[r_sC[i]])

    it = 0
    for seq, (L, ztd, KD) in enumerate(((n_lat, ztl, KDl), (n_ctx, ztc, KDc))):
        N = min(512, L)
        nblk = L // N
        for ps in range(2):
            for bl in range(nblk):
                i = it % 2
                it += 1
                P.dma("sp", zt[i][:, 0:N], ztd[ps, :, bl * N:(bl + 1) * N], writes=[r_zt[i]])
                b1, rb1 = nb()
                P.op("pe", lambda e: e.matmul(b1[0:64, 0:N], w1s[:, :], zt[i][:, 0:N], start=True, stop=True), reads=[r_par, r_zt[i]], writes=[rb1])
                P.op("dve", lambda e: e.tensor_scalar(out=hA[i][:, 0:N], in0=b1[0:64, 0:N], scalar1=cps[:, 0:1], scalar2=cps[:, 1:2], op0=ALU.add, op1=ALU.mult),
                     reads=[rb1, r_par], writes=[r_hA[i]])
                sin_big(hA[i], r_hA[i], N, i)
                b2, rb2 = nb()
                P.op("pe", lambda e: e.matmul(b2[0:64, 0:N], w2s[:, :], hA[i][:, 0:N], start=True, stop=True), reads=[r_par, r_hA[i]], writes=[rb2])
                P.op("dve", lambda e: e.tensor_scalar(out=hB[i][:, 0:N], in0=b2[0:64, 0:N], scalar1=cps[:, 2:3], scalar2=cps[:, 3:4], op0=ALU.add, op1=ALU.mult),
                     reads=[rb2, r_par], writes=[r_hB[i]])
                sin_big(hB[i], r_hB[i], N, i)
                b3, rb3 = nb()
                P.op("pe", lambda e: e.matmul(b3[:, 0:N], w3s[:, ps * 128:(ps + 1) * 128], hB[i][:, 0:N], start=True, stop=True), reads=[r_par, r_hB[i]], writes=[rb3])
                b4, rb4 = nb()
                P.op("pe", lambda e: e.matmul(b4[:, 0:N], decs[0:1, ps * 128:(ps + 1) * 128], zt[i][0:1, 0:N], start=True, stop=True), reads=[r_par, r_zt[i]], writes=[rb4])
                P.op("act", lambda e: e.activation(out=win[i][:, 0:N], in_=b4[:, 0:N], func=AF.Exp), reads=[rb4], writes=[r_win[i]])
                P.op("dve", lambda e: e.tensor_tensor(out=hw[i][:, 0:N], in0=b3[:, 0:N], in1=win[i][:, 0:N], op=ALU.mult), reads=[rb3, r_win[i]], writes=[r_hw[i]])
                if ps == 1 and bl == nblk - 1:
                    P.op("dve", lambda e: e.memset(hw[i][:, N - 1:N], 0.0), reads=[r_hw[i]], writes=[r_hw[i]])
                P.op("act", lambda e: e.activation(out=junk[i][:, 0:N], in_=hw[i][:, 0:N], func=AF.Abs, accum_out=acc[:, seq, ps * nblk + bl:ps * nblk + bl + 1]),
                     reads=[r_hw[i]], writes=[r_junk[i], r_acc])
                P.op("pool", lambda e: e.tensor_copy(out=hwb[i][:, 0:N], in_=hw[i][:, 0:N]), reads=[r_hw[i]], writes=[r_hwb[i]])
                if ps == 0:
                    q0 = L - 1 + bl * N
                    P.dma("sp", KD[:, q0:q0 + N], hwb[i][:, 0:N], reads=[r_hwb[i]], writes=[r_KD[seq]])
                else:
                    q0 = bl * N
                    nw = N - 1 if bl == nblk - 1 else N
                    P.dma("sp", KD[:, q0:q0 + nw], hwb[i][:, 0:nw], reads=[r_hwb[i]], writes=[r_KD[seq]])
    nrm = P.sb([128, 2], F32)
    r_nrm = Reg()
    dg = P.sb([128, 128], F32)
    r_dg = Reg()
    for seq in range(2):
        P.op("dve", lambda e: e.tensor_reduce(out=nrm[:, seq:seq + 1], in_=acc[:, seq, :], axis=AX.X, op=ALU.add), reads=[r_acc], writes=[r_nrm])
        P.op("dve", lambda e: e.tensor_scalar(out=dg[:], in0=identf[:], scalar1=nrm[:, seq:seq + 1], scalar2=None, op0=ALU.mult), reads=[r_nrm, r_idf], writes=[r_dg])
        b5, rb5 = nb()
        P.op("pe", lambda e: e.matmul(b5[:, 0:128], ones[:, :], dg[:, :], start=True, stop=True), reads=[r_ones, r_dg], writes=[rb5])
        P.op("dve", lambda e: e.reciprocal(out=RN[:, seq, :], in_=b5[:, 0:128]), reads=[rb5], writes=[r_RN])

    P.pop_scope([r_KD[0], r_KD[1], r_RN, r_SC])
    hsk = [P.sb([128, n_lat], BF16) for _ in range(2)]
    r_hsk = [Reg(), Reg()]
    zb = [P.sb([128, 128], BF16) for _ in range(2)]
    r_zb = [Reg(), Reg()]
    zr = [P.sb([128, 128], BF16) for _ in range(2)]
    r_zr = [Reg(), Reg()]
    tm = [P.sb([128, 128], F32) for _ in range(2)]
    r_tm = [Reg(), Reg()]
    hk = 0
    cv = 0
    for seq, (L, KD, W, j0) in enumerate(((n_lat, KDl, WL + 1, 0), (n_ctx, KDc, WC + 1, n_lat // 128))):
        NB = L // 128
        for ch in range(64):
            for o in range(2):
                c2 = cv % 2
                cv += 1
                row = o * 64 + ch
                src_c = (128 + ch) if o == 0 else ch
                Zs = SC[:, src_c, j0:j0 + NB]
                P.op("pool", lambda e: e.tensor_copy(out=zb[c2][:, 0:NB], in_=Zs), reads=[r_SC], writes=[r_zb[c2]])
                bz, rbz = nb()
                P.op("pe", lambda e: e.matmul(bz[:, 0:NB], J[:, :], zb[c2][:, 0:NB], start=True, stop=True), reads=[r_J, r_zb[c2]], writes=[rbz])
                P.op("act", lambda e: e.copy(out=zr[c2][:, 0:NB], in_=bz[:, 0:NB]), reads=[rbz], writes=[r_zr[c2]])
                yb_, r_yb = ybanks[c2]
                first = True
                for h in (1, 0):
                    hb_ = hk % 2
                    hk += 1
                    if h == 1:
                        x0, wd = L - 128, L
                        deltas = list(range(0, NB))
                    else:
                        x0, wd = 0, L - 128
                        deltas = list(range(-(NB - 1), 0))
                    if wd == 0:
                        continue
                    src = bass.AP(KD.tensor, row * W + x0, [[1, 128], [1, wd]])
                    P.dma("sp", hsk[hb_][:, 0:wd], src, reads=[r_KD[seq]], writes=[r_hsk[hb_]])
                    for di, d in enumerate(deltas):
                        xo = 128 * d + L - 128 - x0
                        lo_i, hi_i = max(0, d), NB + min(0, d)
                        lo_j, hi_j = max(0, -d), NB - max(0, d)
                        last = (h == 0 and di == len(deltas) - 1) or (NB == 1)
                        P.op("pe", lambda e, xo=xo, lo_i=lo_i, hi_i=hi_i, lo_j=lo_j, hi_j=hi_j, first=first, last=last: e.matmul(
                            yb_[:, lo_i:hi_i], hsk[hb_][:, xo:xo + 128], zr[c2][:, lo_j:hi_j], start=first, stop=last),
                            reads=[r_hsk[hb_], r_zr[c2]], writes=[r_yb], inc=(di == len(deltas) - 1))
                        first = False
                col = o * 64 + ch
                P.op("dve", lambda e: e.tensor_scalar(out=tm[c2][:, 0:NB], in0=yb_[:, 0:NB], scalar1=RN[:, seq, col:col + 1], scalar2=None, op0=ALU.mult),
                     reads=[r_yb, r_RN], writes=[r_tm[c2]])
                P.op("dve", lambda e: e.scalar_tensor_tensor(out=tm[c2][:, 0:NB], in0=Zs, scalar=hbs[:, col:col + 1], in1=tm[c2][:, 0:NB], op0=ALU.mult, op1=ALU.add),
                     reads=[r_SC, r_par, r_tm[c2]], writes=[r_tm[c2]])
                if o == 0:
                    P.op("dve", lambda e: e.tensor_tensor(out=SC[:, ch, j0:j0 + NB], in0=SC[:, ch, j0:j0 + NB], in1=tm[c2][:, 0:NB], op=ALU.mult),
                         reads=[r_SC, r_tm[c2]], writes=[r_SC])
                else:
                    P.op("dve", lambda e: e.tensor_tensor(out=SC[:, 64 + ch, j0:j0 + NB], in0=SC[:, 64 + ch, j0:j0 + NB], in1=tm[c2][:, 0:NB], op=ALU.mult),
                         reads=[r_SC, r_tm[c2]], writes=[r_SC])
    if stop <= 4:
        P.finish([])
        return P
    ot = [P.sb([128, 64], F32) for _ in range(2)]
    r_ot = [Reg(), Reg()]
    for t in range(NT):
        i = t % 2
        eng = "act" if t % 2 == 0 else "pool"
        if eng == "act":
            P.op("act", lambda e: e.copy(out=ot[i][:], in_=SC[:, 64:128, t]), reads=[r_SC], writes=[r_ot[i]])
        else:
            P.op("pool", lambda e: e.tensor_copy(out=ot[i][:], in_=SC[:, 64:128, t]), reads=[r_SC], writes=[r_ot[i]])
        P.dma("sp", hy[t * 128:(t + 1) * 128, :], ot[i][:], reads=[r_ot[i]], writes=[r_hy])
    P.finish([r_hy])
    return P


D = 1024
FF = 2816
ALPHA = float(4 ** 0.25)


def build_p2(n_lat=4096, n_ctx=64, moe=False, SB=None):
    P = Prog()
    E = 8 if moe else 1
    ntok = n_lat + n_ctx
    mix = P.dram("mix", [ntok, D], F32, "ExternalInput")
    x = P.dram("x", [ntok, D], F32, "ExternalInput")
    cT = P.dram("cT", [128, 16], F32, "ExternalInput")
    adaw = P.dram("adaw", [D, 4096], F32, "ExternalInput")
    adab = P.dram("adab", [1, 4096], F32, "ExternalInput")
    wout = P.dram("wout", [D, D], F32, "ExternalInput")
    lnp = P.dram("lnp", [128, 4096], F32, "ExternalInput")
    w1 = P.dram("w1", [E, D, FF], F32, "ExternalInput")
    w3 = P.dram("w3", [E, D, FF], F32, "ExternalInput")
    w2 = P.dram("w2", [E, FF, D], F32, "ExternalInput")
    if moe:
        wr = P.dram("wr", [D, 8], F32, "ExternalInput")
    xo = P.dram("xo", [ntok, D], F32, "ExternalOutput")
    r_xo = Reg()
    w1b = P.dram("w1b", [E, D, FF], BF16, "Internal")
    w3b = P.dram("w3b", [E, D, FF], BF16, "Internal")
    w2b = P.dram("w2b", [E, FF, D], BF16, "Internal")
    r_wbf = [Reg() for _ in range(E)]
    for e_ in range(E):
        for k in range(8):
            P.dma("pool", w1b[e_, k * 128:(k + 1) * 128, :], w1[e_, k * 128:(k + 1) * 128, :], writes=[r_wbf[e_]])
            P.dma("pool", w3b[e_, k * 128:(k + 1) * 128, :], w3[e_, k * 128:(k + 1) * 128, :], writes=[r_wbf[e_]])
        for f in range(22):
            P.dma("pool", w2b[e_, f * 128:(f + 1) * 128, :], w2[e_, f * 128:(f + 1) * 128, :], writes=[r_wbf[e_]])

    fb = [P.ps([128, 512], F32) for _ in range(8)]
    tb = [(fb[6][0][:, :].bitcast(BF16).rearrange("p (k c) -> p k c", k=8), fb[6][1])]
    tf = [(fb[7][0][:, :].rearrange("p (k c) -> p k c", k=4), fb[7][1])]
    ob_rr = [0]
    ones_row = P.sb([1, 128], F32)
    P.op("dve", lambda e: e.memset(ones_row[:], 1.0), writes=[Reg()])
    ident, r_id = make_ident(P)
    if moe:
        identf, r_idf = make_ident(P, F32)
        wrs = P.sb([128, 8, 8], F32)
        r_wrs = Reg()
        P.dma("sp", wrs[:], wr.rearrange("(k p) e -> p k e", p=128), writes=[r_wrs])
    wob = P.sb([128, 8, D], BF16)
    r_wob = Reg()
    for k in range(8):
        P.dma("pool", wob[:, k, :], wout[k * 128:(k + 1) * 128, :], writes=[r_wob])
    lns = P.sb([128, 4096], F32)
    r_lns = Reg()
    P.dma("sp", lns[:], lnp[:, :], writes=[r_lns])
    bcA, r_bcA = mods_block(P, cT, adaw[:, 0:2048], adab[:, 0:2048], 2048, ones_row, fb[:4], [])
    bcB, r_bcB = mods_block(P, cT, adaw[:, 2048:4096], adab[:, 2048:4096], 2048, ones_row, fb[:4], [(0, 1024)])

    tiles_all = [(i * 128, 128, 0) for i in range(n_lat // 128)]
    if n_ctx:
        tiles_all.append((n_lat, n_ctx, 1))
    SB = SB or (7 if moe else 4)
    sblocks = [tiles_all[i:i + SB] for i in range(0, n_lat // 128, SB)]
    if n_ctx:
        sblocks[-1] = sblocks[-1] + [tiles_all[-1]]
    MT = SB + 1
    NTOK = SB * 128 + n_ctx

    def T(shape, dt=F32, n=2):
        return [P.sb(list(shape), dt) for _ in range(n)], [Reg() for _ in range(n)]
    xs, r_xs = T((128, D))
    ms, r_ms = T((128, D), n=1)
    ms, r_ms = ms * 2, r_ms * 2
    mb, r_mb = T((128, D), BF16, n=1)
    mb, r_mb = mb * 2, r_mb * 2
    mT, r_mT = T((128, 8, 128), BF16)
    yv, r_yv = T((128, D))
    tmp, r_tmp = T((128, D))
    stat, r_stat = T((128, 16))
    hb, r_hb = T((128, D), BF16, n=1)
    hb, r_hb = hb * 2, r_hb * 2
    x1d = P.dram("x1d", [ntok, D], F32, "Internal")
    r_x1d = Reg()
    x1t, r_x1t = T((128, D))
    acc = P.sb([128, MT, D], F32)
    r_acc = [Reg() for _ in range(MT)]
    r_acc2 = r_acc
    evt, r_evt = [None, None], [Reg(), Reg()]
    evq = [0]
    h2T = P.sb([128, 8, NTOK], BF16)
    r_h2T = Reg()
    if moe:
        hf, r_hf = T((128, D), n=1)
        hf, r_hf = hf * 2, r_hf * 2
        hTf, r_hTf = T((128, 8, 128), n=1)
        hTf, r_hTf = hTf * 2, r_hTf * 2
        lg, r_lg = T((128, 32))
        gates = P.sb([128, MT, 8], F32)
        r_gates = [Reg() for _ in range(MT)]
    UF = 2
    units = [(f0, min(UF, 22 - f0)) for f0 in range(0, 22, UF)]
    w1u, r_w1u = T((128, 8, UF * 128), BF16)
    w3u, r_w3u = T((128, 8, UF * 128), BF16)
    w2u, r_w2u = T((128, UF, D), BF16)
    GT, r_GT = T((128, UF, NTOK), BF16)
    sa, r_sa = T((128, 512))
    wq = [0]
    it = [0]

    for sbk in sblocks:
        ntk = sum(r for (_, r, _) in sbk)
        col = 0
        for ti, (t0, rows, s) in enumerate(sbk):
            i = it[0] % 2
            it[0] += 1
            P.dma("sp", ms[i][:rows, :], mix[t0:t0 + rows, :], writes=[r_ms[i]])
            P.dma("sp", xs[i][:rows, :], x[t0:t0 + rows, :], writes=[r_xs[i]])
            P.op("pool", lambda e: e.tensor_copy(out=mb[i][:rows, :], in_=ms[i][:rows, :]), reads=[r_ms[i]], writes=[r_mb[i]])
            tp, r_tp = tb[0]
            for k in range(8):
                P.op("pe", lambda e, k=k: e.transpose(tp[:, k, :rows], mb[i][:rows, k * 128:(k + 1) * 128], ident[:rows, :rows]),
                     reads=[r_mb[i], r_id], writes=[r_tp], inc=(k == 7))
            P.op("act", lambda e: e.copy(out=mT[i][:, :, :rows], in_=tp[:, :, :rows]), reads=[r_tp], writes=[r_mT[i]])
            P.op("act", lambda e: e.mul(out=yv[i][:rows, :], in_=xs[i][:rows, :], mul=ALPHA), reads=[r_xs[i]], writes=[r_yv[i]])
            for cb in range(2):
                bank, rb = fb[4 + cb]
                for k in range(8):
                    P.op("pe", lambda e, k=k, cb=cb, bank=bank: e.matmul(bank[:rows, :], mT[i][:, k, :rows], wob[:, k, cb * 512:(cb + 1) * 512],
                                                                   start=(k == 0), stop=(k == 7)), reads=[r_mT[i], r_wob], writes=[rb], inc=(k == 7))
                P.op("dve", lambda e, cb=cb, bank=bank: e.tensor_tensor(out=tmp[i][:rows, cb * 512:(cb + 1) * 512], in0=bank[:rows, :],
                                                                in1=bcA[s][:rows, cb * 512:(cb + 1) * 512], op=ALU.mult),
                     reads=[rb, r_bcA[s]], writes=[r_tmp[i]])
            P.op("pool", lambda e: e.tensor_tensor(out=yv[i][:rows, :], in0=yv[i][:rows, :], in1=tmp[i][:rows, :], op=ALU.add), reads=[r_yv[i], r_tmp[i]], writes=[r_yv[i]])
            ln_tile(P, rows, yv[i][:rows, :], r_yv[i], tmp[i], r_tmp[i], stat[i], r_stat[i])
            P.op("pool", lambda e: e.tensor_tensor(out=tmp[i][:rows, :], in0=tmp[i][:rows, :], in1=lns[:rows, 0:1024], op=ALU.mult), reads=[r_tmp[i], r_lns], writes=[r_tmp[i]])
            P.op("dve", lambda e: e.tensor_tensor(out=x1t[i][:rows, :], in0=tmp[i][:rows, :], in1=lns[:rows, 1024:2048], op=ALU.add), reads=[r_tmp[i], r_lns], writes=[r_x1t[i]])
            P.dma("sp", x1d[t0:t0 + rows, :], x1t[i][:rows, :], reads=[r_x1t[i]], writes=[r_x1d])
            ln_tile(P, rows, x1t[i][:rows, :], r_x1t[i], tmp[i], r_tmp[i], stat[i], r_stat[i])
            P.op("pool", lambda e: e.tensor_tensor(out=tmp[i][:rows, :], in0=tmp[i][:rows, :], in1=bcB[s][:rows, 0:1024], op=ALU.mult), reads=[r_tmp[i], r_bcB[s]], writes=[r_tmp[i]])
            if moe:
                P.op("dve", lambda e: e.tensor_tensor(out=hf[i][:rows, :], in0=tmp[i][:rows, :], in1=bcA[s][:rows, 1024:2048], op=ALU.add), reads=[r_tmp[i], r_bcA[s]], writes=[r_hf[i]])
                P.op("pool", lambda e: e.tensor_copy(out=hb[i][:rows, :], in_=hf[i][:rows, :]), reads=[r_hf[i]], writes=[r_hb[i]])
            else:
                P.op("dve", lambda e: e.tensor_tensor(out=hb[i][:rows, :], in0=tmp[i][:rows, :], in1=bcA[s][:rows, 1024:2048], op=ALU.add), reads=[r_tmp[i], r_bcA[s]], writes=[r_hb[i]])
            for k in range(8):
                P.op("pe", lambda e, k=k: e.transpose(tp[:, k, :rows], hb[i][:rows, k * 128:(k + 1) * 128], ident[:rows, :rows]),
                     reads=[r_hb[i], r_id], writes=[r_tp], inc=(k == 7))
            P.op("act", lambda e, col=col: e.copy(out=h2T[:, :, col:col + rows], in_=tp[:, :, :rows]), reads=[r_tp], writes=[r_h2T])
            if moe:
                tq, r_tq = tf[0]
                for half in range(2):
                    for k in range(4):
                        kk = half * 4 + k
                        P.op("pe", lambda e, k=k, kk=kk: e.transpose(tq[:, k, :rows], hf[i][:rows, kk * 128:(kk + 1) * 128], identf[:rows, :rows]),
                             reads=[r_hf[i], r_idf], writes=[r_tq], inc=(k == 3))
                    P.op("dve", lambda e, half=half: e.tensor_copy(out=hTf[i][:, half * 4:half * 4 + 4, :rows], in_=tq[:, :, :rows]), reads=[r_tq], writes=[r_hTf[i]])
                bank, rb = fb[4]
                for k in range(8):
                    P.op("pe", lambda e, k=k, bank=bank: e.matmul(bank[:rows, 0:8], hTf[i][:, k, :rows], wrs[:, k, :], start=(k == 0), stop=(k == 7)),
                         reads=[r_hTf[i], r_wrs], writes=[rb], inc=(k == 7))
                L = lg[i]
                rl = r_lg[i]
                P.op("dve", lambda e, bank=bank: e.tensor_copy(out=L[:rows, 0:8], in_=bank[:rows, 0:8]), reads=[rb], writes=[rl])
                P.op("dve", lambda e: e.tensor_reduce(out=L[:rows, 24:25], in_=L[:rows, 0:8], axis=AX.X, op=ALU.max), reads=[rl], writes=[rl])
                P.op("dve", lambda e: e.tensor_scalar(out=L[:rows, 8:16], in0=L[:rows, 0:8], scalar1=L[:rows, 24:25], scalar2=None, op0=ALU.is_equal), reads=[rl], writes=[rl])
                P.op("dve", lambda e: e.scalar_tensor_tensor(out=L[:rows, 16:24], in0=L[:rows, 8:16], scalar=-1e30, in1=L[:rows, 0:8], op0=ALU.mult, op1=ALU.add), reads=[rl], writes=[rl])
                P.op("dve", lambda e: e.tensor_reduce(out=L[:rows, 25:26], in_=L[:rows, 16:24], axis=AX.X, op=ALU.max), reads=[rl], writes=[rl])
                P.op("dve", lambda e: e.tensor_scalar(out=L[:rows, 16:24], in0=L[:rows, 16:24], scalar1=L[:rows, 25:26], scalar2=None, op0=ALU.is_equal), reads=[rl], writes=[rl])
                P.op("dve", lambda e: e.tensor_tensor(out=L[:rows, 26:27], in0=L[:rows, 25:26], in1=L[:rows, 24:25], op=ALU.subtract), reads=[rl], writes=[rl])
                P.op("act", lambda e: e.activation(out=L[:rows, 26:27], in_=L[:rows, 26:27], func=AF.Exp), reads=[rl], writes=[rl])
                P.op("dve", lambda e: e.tensor_scalar_add(out=L[:rows, 27:28], in0=L[:rows, 26:27], scalar1=1.0), reads=[rl], writes=[rl])
                P.op("dve", lambda e: e.reciprocal(out=L[:rows, 27:28], in_=L[:rows, 27:28]), reads=[rl], writes=[rl])
                P.op("dve", lambda e: e.tensor_tensor(out=L[:rows, 28:29], in0=L[:rows, 26:27], in1=L[:rows, 27:28], op=ALU.mult), reads=[rl], writes=[rl])
                P.op("dve", lambda e: e.tensor_scalar(out=L[:rows, 8:16], in0=L[:rows, 8:16], scalar1=L[:rows, 27:28], scalar2=None, op0=ALU.mult), reads=[rl], writes=[rl])
                P.op("dve", lambda e, ti=ti: e.scalar_tensor_tensor(out=gates[:rows, ti, :], in0=L[:rows, 16:24], scalar=L[:rows, 28:29], in1=L[:rows, 8:16],
                                                                    op0=ALU.mult, op1=ALU.add), reads=[rl], writes=[r_gates[ti]])
            col += rows
        tblocks = []
        c0 = 0
        while c0 < ntk:
            n = min(512, ntk - c0)
            tblocks.append((c0, n))
            c0 += n
        for e_ in range(E):
            for (f0, nf) in units:
                q = wq[0] % 2
                wq[0] += 1
                P.dma("sp", w1u[q][:, :, 0:nf * 128], w1b[e_, :, f0 * 128:(f0 + nf) * 128].rearrange("(k p) f -> p k f", p=128),
                      reads=[r_wbf[e_]], writes=[r_w1u[q]])
                P.dma("act", w3u[q][:, :, 0:nf * 128], w3b[e_, :, f0 * 128:(f0 + nf) * 128].rearrange("(k p) f -> p k f", p=128),
                      reads=[r_wbf[e_]], writes=[r_w3u[q]])
                P.dma("sp", w2u[q][:, 0:nf, :], w2b[e_, f0 * 128:(f0 + nf) * 128, :].rearrange("(f p) d -> p f d", p=128),
                      reads=[r_wbf[e_]], writes=[r_w2u[q]])
                for (c0, n) in tblocks:
                    for f in range(nf):
                        ba, rba = fb[0 + (f % 2) * 2]
                        bb, rbb = fb[1 + (f % 2) * 2]
                        for k in range(8):
                            P.op("pe", lambda e, k=k, f=f, ba=ba: e.matmul(ba[:, 0:n], w1u[q][:, k, f * 128:(f + 1) * 128], h2T[:, k, c0:c0 + n],
                                                                     start=(k == 0), stop=(k == 7)), reads=[r_w1u[q], r_h2T], writes=[rba], inc=(k == 7))
                        for k in range(8):
                            P.op("pe", lambda e, k=k, f=f, bb=bb: e.matmul(bb[:, 0:n], w3u[q][:, k, f * 128:(f + 1) * 128], h2T[:, k, c0:c0 + n],
                                                                     start=(k == 0), stop=(k == 7)), reads=[r_w3u[q], r_h2T], writes=[rbb], inc=(k == 7))
                        j = f % 2
                        P.op("act", lambda e, ba=ba, j=j: e.activation(out=sa[j][:, 0:n], in_=ba[:, 0:n], func=AF.Silu), reads=[rba], writes=[r_sa[j]])
                        P.op("dve", lambda e, bb=bb, j=j, f=f: e.tensor_tensor(out=GT[q][:, f, c0:c0 + n], in0=bb[:, 0:n], in1=sa[j][:, 0:n], op=ALU.mult),
                             reads=[rbb, r_sa[j]], writes=[r_GT[q]])
                col = 0
                for ti, (t0, rows, s) in enumerate(sbk):
                    for cb in range(2):
                        bank, rb = fb[4 + ob_rr[0] % 4]
                        ob_rr[0] += 1
                        for f in range(nf):
                            P.op("pe", lambda e, f=f, cb=cb, bank=bank, col=col: e.matmul(bank[:rows, :], GT[q][:, f, col:col + rows], w2u[q][:, f, cb * 512:(cb + 1) * 512],
                                                                                  start=(f == 0), stop=(f == nf - 1)),
                                 reads=[r_GT[q], r_w2u[q]], writes=[rb], inc=(f == nf - 1))
                        first = (e_ == 0 and f0 == 0)
                        dst = acc[:rows, ti, cb * 512:(cb + 1) * 512]
                        if moe:
                            gsc = gates[:rows, ti, e_:e_ + 1]
                            if first:
                                P.op("dve", lambda e, bank=bank, dst=dst, gsc=gsc: e.tensor_scalar(out=dst, in0=bank[:rows, :], scalar1=gsc, scalar2=None, op0=ALU.mult),
                                     reads=[rb, r_gates[ti]], writes=[r_acc[ti]])
                            elif True:
                                P.op("dve", lambda e, bank=bank, dst=dst, gsc=gsc: e.scalar_tensor_tensor(out=dst, in0=bank[:rows, :], scalar=gsc, in1=dst, op0=ALU.mult, op1=ALU.add),
                                     reads=[rb, r_gates[ti], r_acc[ti]], writes=[r_acc[ti]])
                            else:
                                ev_i = evq[0] % 2
                                evq[0] += 1
                                P.op("act", lambda e, bank=bank, gsc=gsc, ev_i=ev_i: e.activation(out=evt[ev_i][:rows, :], in_=bank[:rows, :], func=AF.Copy, scale=gsc),
                                     reads=[rb, r_gates[ti]], writes=[r_evt[ev_i]])
                                P.op("pool", lambda e, dst=dst, ev_i=ev_i: e.tensor_tensor(out=dst, in0=dst, in1=evt[ev_i][:rows, :], op=ALU.add),
                                     reads=[r_evt[ev_i], r_acc2[ti]], writes=[r_acc2[ti]])
                        else:
                            if first:
                                P.op("act", lambda e, bank=bank, dst=dst: e.copy(out=dst, in_=bank[:rows, :]), reads=[rb], writes=[r_acc[ti]])
                            else:
                                P.op("dve", lambda e, bank=bank, dst=dst: e.tensor_tensor(out=dst, in0=bank[:rows, :], in1=dst, op=ALU.add),
                                     reads=[rb, r_acc[ti]], writes=[r_acc[ti]])
                    col += rows
        for ti, (t0, rows, s) in enumerate(sbk):
            i = it[0] % 2
            it[0] += 1
            P.op("pool", lambda e: e.tensor_tensor(out=acc[:rows, ti, :], in0=acc[:rows, ti, :], in1=bcB[s][:rows, 1024:2048], op=ALU.mult), reads=[r_acc[ti], r_bcB[s]], writes=[r_acc[ti]])
            P.dma("sp", x1t[i][:rows, :], x1d[t0:t0 + rows, :], reads=[r_x1d], writes=[r_x1t[i]])
            P.op("dve", lambda e: e.scalar_tensor_tensor(out=yv[i][:rows, :], in0=x1t[i][:rows, :], scalar=ALPHA, in1=acc[:rows, ti, :], op0=ALU.mult, op1=ALU.add),
                 reads=[r_x1t[i], r_acc[ti]], writes=[r_yv[i]])
            ln_tile(P, rows, yv[i][:rows, :], r_yv[i], tmp[i], r_tmp[i], stat[i], r_stat[i])
            P.op("pool", lambda e: e.tensor_tensor(out=tmp[i][:rows, :], in0=tmp[i][:rows, :], in1=lns[:rows, 2048:3072], op=ALU.mult), reads=[r_tmp[i], r_lns], writes=[r_tmp[i]])
            P.op("dve", lambda e: e.tensor_tensor(out=yv[i][:rows, :], in0=tmp[i][:rows, :], in1=lns[:rows, 3072:4096], op=ALU.add), reads=[r_tmp[i], r_lns], writes=[r_yv[i]])
            P.dma("sp", xo[t0:t0 + rows, :], yv[i][:rows, :], reads=[r_yv[i]], writes=[r_xo])
    P.finish([r_xo])
    return P


def a2_consts():
    i = np.arange(128); half = i // 64
    same = half[:, None] == half[None, :]
    MST = (same & np.where(half[:, None] == 0, i[:, None] < i[None, :], i[:, None] > i[None, :])).astype(np.float32)
    MIT = MST + np.eye(128, dtype=np.float32)
    MS = np.ascontiguousarray(MST.T)
    LM = np.zeros((128, 96), np.float32)
    LM[:64, 0:16] = 1; LM[64:, 16:32] = 1; LM[:64, 32:48] = 1; LM[64:, 48:64] = 1; LM[:, 64:96] = 1
    return np.concatenate([MIT, MST, MIT, MS, LM], 1).astype(np.float32)

def a2_inputs(ur, n_lat, n_ctx, hd, mu, w0, wB, a0, aB, gB, kkw, ka, rk, gng, gnb):
    C = 256
    cols = np.concatenate([np.arange(hd * 64, hd * 64 + 64), C + np.arange(hd * 64, hd * 64 + 64), 2 * C + np.arange(hd * 64, hd * 64 + 64),
                           np.arange(768, 864)])
    u = ur[:, cols]
    def shifted(x, d):
        o = np.zeros_like(x)
        if d == 1: o[1:] = x[:-1]
        else: o[:-1] = x[1:]
        return o
    prev = np.concatenate([shifted(u[:n_lat], 1), shifted(u[n_lat:], 1)], 0)
    nxt = np.concatenate([shifted(u[:n_lat], -1), shifted(u[n_lat:], -1)], 0)
    u3 = np.concatenate([u, prev, nxt], 1)
    fwd, bwd = rwkv_orders(n_lat, n_ctx)
    idx = np.concatenate([np.concatenate([np.arange(f, f + 64), np.arange(b, b + 64)]) for f, b in zip(fwd, bwd)])
    U3 = np.ascontiguousarray(u3[idx])
    coefmu = np.tile(np.concatenate([mu[0][cols], mu[1][cols]])[None], (128, 1)).astype(np.float32)
    hs = slice(hd * 64, hd * 64 + 64)
    rowp = np.zeros((128, 128), np.float32)
    rowp[:64, :64] = w0[0][hs]; rowp[64:, :64] = w0[1][hs]; rowp[:64, 64:] = a0[0][hs]; rowp[64:, 64:] = a0[1][hs]
    hv = np.tile(np.concatenate([kkw[hs], ka[hs], rk[hd], gng[hs], gnb[hs]])[None], (128, 1)).astype(np.float32)
    Wl = np.zeros((96, 192), np.float32)
    Wl[0:16, 0:64] = wB[0][:, hs]; Wl[16:32, 0:64] = wB[1][:, hs]; Wl[32:48, 64:128] = aB[0][:, hs]; Wl[48:64, 64:128] = aB[1][:, hs]
    Wl[64:96, 128:192] = gB[:, hs]
    return dict(U3=U3, coefmu=coefmu, rowp=rowp, hv=hv, Wl=Wl, cmask=a2_consts())


def hy_ztab(L):
    bands = 16
    t = np.linspace(0.0, 1.0, L, dtype=np.float32)[:, None]
    f = np.linspace(1e-4, bands - 1, bands, dtype=np.float32)[None, :]
    wt = (np.float32(2.0 * math.pi) * np.arange(L, dtype=np.float32)[:, None] / np.float32(L)).astype(np.float32)
    z = np.concatenate([t, np.cos(f * wt), -np.sin(f * wt)], -1).astype(np.float32)
    return np.ascontiguousarray(np.stack([z.T, z[::-1].T], 0))

def a3_inputs(uh, n_lat, n_ctx, j, sw, sb, w1, b1, f1, w2, b2, f2, w3, dec, hbias):
    cs = slice(j * 64, j * 64 + 64)
    cols = np.concatenate([np.arange(256)[cs], 256 + np.arange(256)[cs], 512 + np.arange(256)[cs]])
    u = uh[:, cols]
    def shifted(x, d):
        o = np.zeros_like(x)
        if d == 1: o[1:] = x[:-1]
        else: o[:-1] = x[1:]
        return o
    prev = np.concatenate([shifted(u[:n_lat], 1), shifted(u[n_lat:], 1)], 0)
    nxt = np.concatenate([shifted(u[:n_lat], -1), shifted(u[n_lat:], -1)], 0)
    H3 = np.ascontiguousarray(np.concatenate([u, prev, nxt], 1), dtype=np.float32)
    cw = np.tile(np.concatenate([sw[1][cols], sw[0][cols], sw[2][cols], sb[cols]])[None], (128, 1)).astype(np.float32)
    fc = np.array([o * 512 + d * 256 + j * 64 + c for d in range(2) for o in range(2) for c in range(64)])
    colp = np.stack([b1, f1, b2, f2], 1).astype(np.float32)
    hb = np.tile(np.concatenate([hbias[0][cs], hbias[1][cs]])[None], (128, 1)).astype(np.float32)
    return dict(H3=H3, cw=cw, ztl=hy_ztab(n_lat), ztc=hy_ztab(n_ctx), w1=np.ascontiguousarray(w1, dtype=np.float32),
                w2=np.ascontiguousarray(w2, dtype=np.float32), w3=np.ascontiguousarray(w3[:, fc], dtype=np.float32), colp=colp,
                dec=np.ascontiguousarray(dec[fc][None], dtype=np.float32), hb=hb)


N_LAT = 16384
N_CTX = 256
_PROGS = {}


def _prog(name):
    if name not in _PROGS:
        if name == "p1":
            P = build_p1(4096, 64)
        elif name == "a1":
            P = build_a1(N_LAT, N_CTX)
        elif name == "a2":
            P = build_a2(N_LAT, N_CTX)
        elif name == "a3":
            P = build_a3(N_LAT, N_CTX)
        elif name == "p2d":
            P = build_p2(4096, 64, False)
        elif name == "p2m":
            P = build_p2(4096, 64, True)
        _PROGS[name] = P.close()
    return _PROGS[name]


def _run(name, in_maps, out_name):
    nc = _prog(name)
    in_maps = [{k: np.ascontiguousarray(v, dtype=np.float32) for k, v in m.items()} for m in in_maps]
    res = run_bass_kernel_spmd(nc, in_maps, core_ids=list(range(8)))
    return [np.asarray(r[out_name]) for r in res.results]


def _rope_cs(L):
    rows = L // 64
    row = np.repeat(np.arange(rows, dtype=np.float32), 64)
    col = np.tile(np.arange(64, dtype=np.float32), rows)
    inv = (np.float32(10000.0) ** (-np.arange(16, dtype=np.float32) / np.float32(16))).astype(np.float32)
    ang = np.concatenate([row[:, None] * inv, col[:, None] * inv], -1).astype(np.float32)
    cos, sin = np.cos(ang).astype(np.float32), np.sin(ang).astype(np.float32)
    return np.concatenate([cos, cos, cos, sin, sin, sin], -1).astype(np.float32)


def _cT(cb, cctx):
    c2 = np.stack([cb, cctx], 0).astype(np.float32)
    return np.ascontiguousarray(c2.reshape(2, 8, 128).transpose(2, 0, 1).reshape(128, 16))


def kernel(x, c, ctx, c_ctx, ada_w, ada_b, w_in, w_out, q_gain, k_gain,
           rwkv_mu, rwkv_w0, rwkv_wB, rwkv_a0, rwkv_aB, rwkv_gB, rwkv_kk, rwkv_ka, rwkv_rk,
           rwkv_gn_g, rwkv_gn_b, hy_short_w, hy_short_b, hy_w1, hy_b1, hy_freq1, hy_w2, hy_b2,
           hy_freq2, hy_w3, hy_decay, hy_bias, ln1_g, ln1_b, ln2_g, ln2_b,
           ffn_w1, ffn_w3, ffn_w2, moe_router, moe_w1, moe_w3, moe_w2):
    f32 = lambda a: np.asarray(a, dtype=np.float32)
    x = f32(x).copy()
    xc = f32(ctx).copy()
    c, c_ctx = f32(c), f32(c_ctx)
    cs = _rope_cs(N_LAT)
    depth = 2
    for l in range(depth):
        aw, ab = f32(ada_w[l]), f32(ada_b[l])
        cores = [(b, q) for b in range(2) for q in range(4)]
        ins = []
        for (b, q) in cores:
            xt = np.concatenate([x[b, q * 4096:(q + 1) * 4096], xc[b, q * 64:(q + 1) * 64]], 0)
            ins.append(dict(x=xt, cT=_cT(c[b], c_ctx), adaw=aw[:, 0:2048], adab=ab[None, 0:2048], win=f32(w_in[l])))
        us = _run("p1", ins, "u")
        u = np.empty((2, N_LAT + N_CTX, 2400), np.float32)
        for (b, q), uu in zip(cores, us):
            u[b, q * 4096:(q + 1) * 4096] = uu[:4096]
            u[b, N_LAT + q * 64:N_LAT + (q + 1) * 64] = uu[4096:]
        mixo = np.empty((2, N_LAT + N_CTX, 1024), np.float32)
        gains = np.tile(np.concatenate([f32(q_gain[l]), f32(q_gain[l]), f32(k_gain[l])])[None], (128, 1))
        ins = []
        for (b, j) in cores:
            g = j // 2
            qk = np.concatenate([u[b][:, 128 * j:128 * j + 128], u[b][:, 512 + 64 * g:512 + 64 * g + 64]], 1)
            ins.append(dict(qk=qk, v=u[b][:, 640 + 64 * g:640 + 64 * g + 64], gains=gains, cs=cs))
        for (b, j), o in zip(cores, _run("a1", ins, "att")):
            mixo[b][:, 128 * j:128 * j + 128] = o
        ins = []
        for (b, j) in cores:
            ins.append(a2_inputs(u[b][:, 768:1632], N_LAT, N_CTX, j, f32(rwkv_mu[l]), f32(rwkv_w0[l]), f32(rwkv_wB[l]), f32(rwkv_a0[l]),
                                 f32(rwkv_aB[l]), f32(rwkv_gB[l]), f32(rwkv_kk[l]), f32(rwkv_ka[l]), f32(rwkv_rk[l]),
                                 f32(rwkv_gn_g[l]), f32(rwkv_gn_b[l])))
        for (b, j), o in zip(cores, _run("a2", ins, "rw")):
            mixo[b][:, 512 + 64 * j:512 + 64 * j + 64] = o
        ins = []
        for (b, j) in cores:
            ins.append(a3_inputs(u[b][:, 1632:2400], N_LAT, N_CTX, j, f32(hy_short_w[l]), f32(hy_short_b[l]), f32(hy_w1[l]), f32(hy_b1[l]),
                                 f32(hy_freq1[l]), f32(hy_w2[l]), f32(hy_b2[l]), f32(hy_freq2[l]), f32(hy_w3[l]), f32(hy_decay[l]),
                                 f32(hy_bias[l])))
        for (b, j), o in zip(cores, _run("a3", ins, "hy")):
            mixo[b][:, 768 + 64 * j:768 + 64 * j + 64] = o
        lnp = np.tile(np.concatenate([f32(ln1_g[l]), f32(ln1_b[l]), f32(ln2_g[l]), f32(ln2_b[l])])[None], (128, 1))
        jj = l // 2
        ins = []
        for (b, q) in cores:
            mt = np.concatenate([mixo[b, q * 4096:(q + 1) * 4096], mixo[b, N_LAT + q * 64:N_LAT + (q + 1) * 64]], 0)
            xt = np.concatenate([x[b, q * 4096:(q + 1) * 4096], xc[b, q * 64:(q + 1) * 64]], 0)
            d = dict(mix=mt, x=xt, cT=_cT(c[b], c_ctx), adaw=aw[:, 2048:6144], adab=ab[None, 2048:6144], wout=f32(w_out[l]), lnp=lnp)
            if l % 2 == 0:
                d.update(w1=f32(ffn_w1[jj])[None], w3=f32(ffn_w3[jj])[None], w2=f32(ffn_w2[jj])[None])
            else:
                d.update(w1=f32(moe_w1[jj]), w3=f32(moe_w3[jj]), w2=f32(moe_w2[jj]), wr=f32(moe_router[jj]))
            ins.append(d)
        outs = _run("p2d" if l % 2 == 0 else "p2m", ins, "xo")
        xn = np.empty_like(x)
        xcn = np.empty_like(xc)
        for (b, q), o in zip(cores, outs):
            xn[b, q * 4096:(q + 1) * 4096] = o[:4096]
            xcn[b, q * 64:(q + 1) * 64] = o[4096:]
        x, xc = xn, xcn
    return x
```

```python
import math


import numpy as np
from contextlib import ExitStack
import concourse.bass as bass
import concourse.mybir as mybir
from concourse.bass_utils import run_bass_kernel_spmd

F32 = mybir.dt.float32
BF16 = mybir.dt.bfloat16
AF = mybir.ActivationFunctionType
ALU = mybir.AluOpType
AX = mybir.AxisListType
NDS = 24


class Reg:
    __slots__ = ("w", "r", "name", "excl")

    def __init__(self, name="", excl=False):
        self.w = []
        self.r = {}
        self.name = name
        self.excl = excl


class _EngRec:
    def __init__(self):
        self.call = None

    def __getattr__(self, name):
        def f(*a, **k):
            self.call = (name, a, k)
        return f


class Prog:
    def __init__(self):
        self.nc = bass.Bass("TRN2", target_bir_lowering=False)
        self.es = ExitStack()
        nc = self.nc
        self.eng = {"pe": nc.tensor, "act": nc.scalar, "dve": nc.vector, "pool": nc.gpsimd, "sp": nc.sync}
        self.sem = {k: self.es.enter_context(nc.semaphore("s_" + k)) for k in self.eng}
        self.seq = {k: 0 for k in self.eng}
        self.known = {k: {} for k in self.eng}
        self.pend = {k: ([], []) for k in self.eng}
        self.dsem = [self.es.enter_context(nc.semaphore("d%d" % i)) for i in range(NDS)]
        self.dval = [0] * NDS
        self.dnext = 0
        self.ninst = 0
        self._n = 0
        self.scopes = []
        self.ccsem = None
        self.rec = None

    def sb(self, shape, dt, name=None):
        self._n += 1
        es = self.scopes[-1] if self.scopes else self.es
        return es.enter_context(self.nc.sbuf_tensor(name or "t%d" % self._n, list(shape), dt))

    def push_scope(self):
        self.scopes.append(ExitStack())

    def pop_scope(self, regs):
        for r in regs:
            for e in self.eng:
                for t in r.w:
                    self._wait1(e, t)
        self.scopes.pop().close()

    def ps(self, shape, dt, name=None):
        self._n += 1
        nbytes = int(np.prod(shape[1:])) * (4 if dt == F32 else 2)
        assert nbytes % 2048 == 0, "PSUM tensors must be whole banks"
        es = self.scopes[-1] if self.scopes else self.es
        return es.enter_context(self.nc.psum_tensor(name or "p%d" % self._n, list(shape), dt)), Reg(excl=True)

    def dram(self, name, shape, dt, kind):
        return self.nc.dram_tensor(name, list(shape), dt, kind=kind).ap()

    def _wait1(self, e, tok):
        key, sem, val = tok
        if self.known[e].get(key, 0) < val:
            self.eng[e].wait_ge(sem, val)
            self.known[e][key] = val
            self.ninst += 1

    def _deps(self, e, reads, writes, is_dma):
        for r in reads:
            for t in r.w:
                if t[0] == e and e == "pe" and not is_dma:
                    continue
                self._wait1(e, t)
            if r.excl:
                for k, t in r.r.items():
                    if k != e or is_dma:
                        self._wait1(e, t)
        for w in writes:
            for t in w.w:
                if not (t[0] == e and not is_dma):
                    self._wait1(e, t)
            for k, t in w.r.items():
                if k == e and not is_dma:
                    continue
                self._wait1(e, t)

    def op(self, e, fn, reads=(), writes=(), inc=True):
        if self.rec is not None:
            prox = _EngRec()
            fn(prox)
            name, a, k = prox.call
            self.rec.append(("op", e, (lambda eng, name=name, a=a, k=k: getattr(eng, name)(*a, **k)), tuple(reads), tuple(writes), inc))
            return None
        self._deps(e, reads, writes, False)
        inst = fn(self.eng[e])
        self.ninst += 1
        pr, pw = self.pend[e]
        pr.extend(reads)
        pw.extend(writes)
        if inc:
            self.seq[e] += 1
            inst.then_inc(self.sem[e], 1)
            tok = (e, self.sem[e], self.seq[e])
            for r in pr:
                r.r[e] = tok
            for w in pw:
                w.w = [tok]
                w.r = {}
            self.pend[e] = ([], [])
        return inst

    def dma(self, q, out, in_, reads=(), writes=(), **kw):
        if self.rec is not None:
            self.rec.append(("dma", q, out, in_, tuple(reads), tuple(writes), kw))
            return None
        self._deps(q, reads, writes, True)
        slot = self.dnext
        self.dnext = (slot + 1) % NDS
        key = ("d", slot)
        if self.dval[slot] > 0:
            self._wait1(q, (key, self.dsem[slot], self.dval[slot]))
        inst = self.eng[q].dma_start(out=out, in_=in_, **kw)
        self.ninst += 1
        self.dval[slot] += 16
        inst.then_inc(self.dsem[slot], 16)
        tok = (key, self.dsem[slot], self.dval[slot])
        for r in reads:
            r.r[key] = tok
        for w in writes:
            w.w = [t for t in w.w if isinstance(t[0], tuple) and t[0] != key] + [tok]
            w.r = {}
        return inst

    def allgather(self, out, in_, groups, reads=(), writes=()):
        q = "pool"
        self._deps(q, reads, writes, True)
        if self.ccsem is None:
            self.ccsem = self.es.enter_context(self.nc.semaphore("ccsem"))
            self.ccval = 0
        inst = self.eng[q].collective_compute("AllGather", ALU.bypass, replica_groups=groups, ins=[in_], outs=[out])
        self.ninst += 1
        self.ccval += 1
        inst.then_inc(self.ccsem, 1)
        tok = ("cc", self.ccsem, self.ccval)
        for r in reads:
            r.r["cc"] = tok
        for w in writes:
            w.w = [tok]
            w.r = {}
        return inst

    def record(self):
        self.rec = []

    def stop_record(self):
        r, self.rec = self.rec, None
        return r

    def replay_interleaved(self, lists):
        n = max(len(l) for l in lists)
        for i in range(n):
            for l in lists:
                if i < len(l):
                    it = l[i]
                    if it[0] == "op":
                        self.op(it[1], it[2], it[3], it[4], it[5])
                    else:
                        self.dma(it[1], it[2], it[3], it[4], it[5], **it[6])

    def replay_skewed(self, lists, nact):
        L = max(len(l) for l in lists)
        D = -(-L // nact)
        T = (len(lists) - 1) * D + L
        for t in range(T):
            c0 = max(0, (t - L) // D)
            for c in range(c0, min(len(lists), t // D + 1)):
                k = t - c * D
                l = lists[c]
                if 0 <= k < len(l):
                    it = l[k]
                    if it[0] == "op":
                        self.op(it[1], it[2], it[3], it[4], it[5])
                    else:
                        self.dma(it[1], it[2], it[3], it[4], it[5], **it[6])

    def finish(self, regs):
        for r in regs:
            for t in r.w:
                self._wait1("sp", t)
        for slot in range(NDS):
            if self.dval[slot] > 0:
                self._wait1("sp", (("d", slot), self.dsem[slot], self.dval[slot]))

    def close(self):
        self.es.close()
        return self.nc


D = 1024
INW = 2400
LN_EPS = 1e-6


def mods_block(P, cT, adaw, adab, ncol, ones_row, psum_banks, plus_one_ranges):
    ncb = ncol // 512
    assert ncb <= len(psum_banks)
    bc = [P.sb([128, ncol], F32) for _ in range(2)]
    r_bc = [Reg(), Reg()]
    P.push_scope()
    c_sb = P.sb([128, 16], F32)
    cs_sb = P.sb([128, 16], F32)
    r_c = Reg()
    r_cs = Reg()
    P.dma("sp", c_sb[:], cT[:, :], writes=[r_c])
    P.op("act", lambda e: e.activation(out=cs_sb[:], in_=c_sb[:], func=AF.Silu), reads=[r_c], writes=[r_cs])
    ab_sb = P.sb([1, ncol], F32)
    r_ab = Reg()
    P.dma("sp", ab_sb[:], adab[:, :], writes=[r_ab])
    aw = [P.sb([128, ncol], F32) for _ in range(2)]
    r_aw = [Reg(), Reg()]
    modrow = P.sb([1, ncol], F32)
    r_mr = Reg()
    it = 0
    for s in range(2):
        for k in range(8):
            b = it % 2
            it += 1
            P.dma("sp", aw[b][:], adaw[k * 128:(k + 1) * 128, :], writes=[r_aw[b]])
            for cb in range(ncb):
                bank, rb = psum_banks[cb]
                P.op("pe", lambda e, s=s, cb=cb, bank=bank, k=k, b=b: e.matmul(
                    bank[0:1, 0:512], cs_sb[:, s * 8 + k:s * 8 + k + 1], aw[b][:, cb * 512:(cb + 1) * 512],
                    start=(k == 0), stop=(k == 7)),
                    reads=[r_cs, r_aw[b]], writes=[rb], inc=(cb == ncb - 1))
        for cb in range(ncb):
            bank, rb = psum_banks[cb]
            P.op("dve", lambda e, cb=cb, bank=bank: e.tensor_tensor(
                out=modrow[0:1, cb * 512:(cb + 1) * 512], in0=bank[0:1, 0:512],
                in1=ab_sb[0:1, cb * 512:(cb + 1) * 512], op=ALU.add),
                reads=[rb, r_ab], writes=[r_mr])
        for (a, b2) in plus_one_ranges:
            P.op("dve", lambda e, a=a, b2=b2: e.tensor_scalar_add(out=modrow[0:1, a:b2], in0=modrow[0:1, a:b2], scalar1=1.0),
                 reads=[r_mr], writes=[r_mr])
        for cb in range(ncb):
            bank, rb = psum_banks[cb]
            P.op("pe", lambda e, cb=cb, bank=bank: e.matmul(
                bank[:, 0:512], ones_row[0:1, 0:128], modrow[0:1, cb * 512:(cb + 1) * 512], start=True, stop=True),
                reads=[r_mr], writes=[rb])
            P.op("act", lambda e, s=s, cb=cb, bank=bank: e.copy(out=bc[s][:, cb * 512:(cb + 1) * 512], in_=bank[:, 0:512]),
                 reads=[rb], writes=[r_bc[s]])
    P.pop_scope([r_bc[1]])
    return bc, r_bc


def ln_tile(P, rows, x_ap, r_x, tmp, r_tmp, stat, r_stat):
    st = stat
    P.op("dve", lambda e: e.bn_stats(out=st[:rows, 0:6], in_=x_ap[:, 0:512]), reads=[r_x], writes=[r_stat])
    P.op("dve", lambda e: e.bn_stats(out=st[:rows, 6:12], in_=x_ap[:, 512:1024]), reads=[r_x], writes=[r_stat])
    P.op("dve", lambda e: e.bn_aggr(out=st[:rows, 12:14], in_=st[:rows, 0:12]), reads=[r_stat], writes=[r_stat])
    P.op("dve", lambda e: e.tensor_scalar_add(out=st[:rows, 15:16], in0=st[:rows, 13:14], scalar1=LN_EPS), reads=[r_stat], writes=[r_stat])
    P.op("act", lambda e: e.activation(out=st[:rows, 14:15], in_=st[:rows, 15:16], func=AF.Sqrt), reads=[r_stat], writes=[r_stat])
    P.op("dve", lambda e: e.reciprocal(out=st[:rows, 14:15], in_=st[:rows, 14:15]), reads=[r_stat], writes=[r_stat])
    P.op("dve", lambda e: e.tensor_scalar(out=tmp[:rows, :], in0=x_ap, scalar1=st[:rows, 12:13], scalar2=st[:rows, 14:15],
                                          op0=ALU.subtract, op1=ALU.mult), reads=[r_x, r_stat], writes=[r_tmp])


def make_ident(P, dt=BF16):
    ident = P.sb([128, 128], dt)
    r_id = Reg()
    P.op("pool", lambda e: e.memset(ident[:], 0.0), writes=[r_id])
    P.op("pool", lambda e: e.affine_select(out=ident[:], in_=ident[:], pattern=[[-1, 128]], compare_op=ALU.not_equal,
                                           fill=1.0, base=0, channel_multiplier=1), reads=[r_id], writes=[r_id])
    return ident, r_id


def build_p1(n_lat=4096, n_ctx=64):
    P = Prog()
    ntok = n_lat + n_ctx
    x = P.dram("x", [ntok, D], F32, "ExternalInput")
    cT = P.dram("cT", [128, 16], F32, "ExternalInput")
    adaw = P.dram("adaw", [D, 2048], F32, "ExternalInput")
    adab = P.dram("adab", [1, 2048], F32, "ExternalInput")
    win = P.dram("win", [D, INW], F32, "ExternalInput")
    u = P.dram("u", [ntok, INW], F32, "ExternalOutput")

    fb = [P.ps([128, 512], F32) for i in range(6)]
    tb = [P.ps([128, 8, 128], BF16) for i in range(2)]
    ones_row = P.sb([1, 128], F32)
    r_ones = Reg()
    P.op("dve", lambda e: e.memset(ones_row[:], 1.0), writes=[r_ones])
    ident, r_id = make_ident(P)
    wb = P.sb([128, 8, INW], BF16)
    r_wb = Reg()
    for k in range(8):
        P.dma("pool", wb[:, k, :], win[k * 128:(k + 1) * 128, :], writes=[r_wb])
    bc, r_bc = mods_block(P, cT, adaw, adab, 2048, ones_row, fb[:4], [(1024, 2048)])

    NX = 3
    xs = [P.sb([128, D], F32) for _ in range(NX)]
    r_xs = [Reg() for _ in range(NX)]
    tmp = [P.sb([128, D], F32) for _ in range(2)]
    r_tmp = [Reg() for _ in range(2)]
    tmp2 = [P.sb([128, D], F32) for _ in range(2)]
    r_tmp2 = [Reg() for _ in range(2)]
    hb = [P.sb([128, D], BF16) for _ in range(2)]
    r_hb = [Reg() for _ in range(2)]
    stat = [P.sb([128, 16], F32) for _ in range(2)]
    r_stat = [Reg() for _ in range(2)]
    hT = [P.sb([128, 8, 128], BF16) for _ in range(2)]
    r_hT = [Reg() for _ in range(2)]
    uo = [P.sb([128, INW], F32) for _ in range(2)]
    r_uo = [Reg() for _ in range(2)]
    r_u_out = Reg()

    tiles = [(i * 128, 128, 0) for i in range(n_lat // 128)]
    t0 = n_lat
    while t0 < ntok:
        rows = min(128, ntok - t0)
        tiles.append((t0, rows, 1))
        t0 += rows
    for i, (t0, rows, s) in enumerate(tiles):
        a = i % NX
        b = i % 2
        P.dma("sp", xs[a][:rows, :], x[t0:t0 + rows, :], writes=[r_xs[a]])
        ln_tile(P, rows, xs[a][:rows, :], r_xs[a], tmp[b], r_tmp[b], stat[b], r_stat[b])
        P.op("pool", lambda e: e.tensor_tensor(out=tmp2[b][:rows, :], in0=tmp[b][:rows, :], in1=bc[s][:rows, 1024:2048], op=ALU.mult),
             reads=[r_tmp[b], r_bc[s]], writes=[r_tmp2[b]])
        P.op("dve", lambda e: e.tensor_tensor(out=hb[b][:rows, :], in0=tmp2[b][:rows, :], in1=bc[s][:rows, 0:1024], op=ALU.add),
             reads=[r_tmp2[b], r_bc[s]], writes=[r_hb[b]])
        tp, r_tp = tb[b]
        for k in range(8):
            P.op("pe", lambda e, k=k: e.transpose(tp[:, k, :rows], hb[b][:rows, k * 128:(k + 1) * 128], ident[:rows, :rows]),
                 reads=[r_hb[b], r_id], writes=[r_tp], inc=(k == 7))
        P.op("act", lambda e: e.copy(out=hT[b][:, :, :rows], in_=tp[:, :, :rows]), reads=[r_tp], writes=[r_hT[b]])
        for cb in range(5):
            bank, rb = fb[1 + cb]
            for k in range(8):
                P.op("pe", lambda e, k=k, cb=cb, bank=bank: e.matmul(bank[:rows, 0:480], hT[b][:, k, :rows], wb[:, k, cb * 480:(cb + 1) * 480],
                                                       start=(k == 0), stop=(k == 7)),
                     reads=[r_hT[b], r_wb], writes=[rb], inc=(k == 7))
            eng = "act" if cb % 2 == 0 else "dve"
            if eng == "act":
                P.op("act", lambda e, cb=cb, bank=bank: e.copy(out=uo[b][:rows, cb * 480:(cb + 1) * 480], in_=bank[:rows, 0:480]),
                     reads=[rb], writes=[r_uo[b]])
            else:
                P.op("dve", lambda e, cb=cb, bank=bank: e.tensor_copy(out=uo[b][:rows, cb * 480:(cb + 1) * 480], in_=bank[:rows, 0:480]),
                     reads=[rb], writes=[r_uo[b]])
        P.dma("sp", u[t0:t0 + rows, :], uo[b][:rows, :], reads=[r_uo[b]], writes=[r_u_out])
    P.finish([r_u_out])
    return P


HD = 64
QK_EPS = 1e-6


def build_a1(n_lat=16384, n_ctx=256, stage=2):
    P = Prog()
    ntok = n_lat + n_ctx
    NT = ntok // 128
    NTL = n_lat // 128
    qk = P.dram("qk", [ntok, 192], F32, "ExternalInput")
    v = P.dram("v", [ntok, 64], F32, "ExternalInput")
    gains = P.dram("gains", [128, 192], F32, "ExternalInput")
    cs = P.dram("cs", [n_lat, 192], F32, "ExternalInput")
    att = P.dram("att", [ntok, 128], F32, "ExternalOutput")

    identb, r_idb = make_ident(P, BF16)
    identf, r_idf = make_ident(P, F32)
    g_sb = P.sb([128, 192], F32)
    r_g = Reg()
    P.dma("sp", g_sb[:], gains[:, :], writes=[r_g])

    QT = P.sb([128, ntok], BF16)
    KT = P.sb([128, 2, ntok], BF16)
    VA = P.sb([128, NT, 66], BF16)
    r_QT = Reg()
    r_KT = Reg()
    r_VA = Reg()
    P.op("pool", lambda e: e.memset(VA[:, :, 64:65], 1.0), writes=[r_VA])

    NSB = 2
    NP = 3

    P.push_scope()
    tpb = P.ps([128, 8, 128], BF16)
    NB = 2
    qk_sb = [P.sb([128, 192], F32) for _ in range(NB)]
    r_qk = [Reg() for _ in range(NB)]
    v_sb = [P.sb([128, 64], F32) for _ in range(NB)]
    r_v = [Reg() for _ in range(NB)]
    cs_sb = [P.sb([128, 192], F32) for _ in range(NB)]
    r_cs = [Reg() for _ in range(NB)]
    junk = [P.sb([128, 64], F32) for _ in range(NB)]
    r_junk = [Reg() for _ in range(NB)]
    ss = [P.sb([128, 8], F32) for _ in range(NB)]
    r_ss = [Reg() for _ in range(NB)]
    qn = [P.sb([128, 192], F32) for _ in range(NB)]
    r_qn = [Reg() for _ in range(NB)]
    ra = [P.sb([128, 96], F32) for _ in range(NB)]
    rb_ = [P.sb([128, 96], F32) for _ in range(NB)]
    r_ra = [Reg() for _ in range(NB)]
    r_rb = [Reg() for _ in range(NB)]
    qr = [P.sb([128, 384], BF16) for _ in range(NB)]
    r_qr = [Reg() for _ in range(NB)]
    for b in range(NB):
        P.op("dve", lambda e, b=b: e.memset(qr[b][:], 0.0), writes=[r_qr[b]])

    for t in range(NT):
        b = t % NB
        t0 = t * 128
        lat = t < NTL
        P.dma("sp", qk_sb[b][:], qk[t0:t0 + 128, :], writes=[r_qk[b]])
        P.dma("sp", v_sb[b][:], v[t0:t0 + 128, :], writes=[r_v[b]])
        if lat:
            P.dma("sp", cs_sb[b][:], cs[t0:t0 + 128, :], writes=[r_cs[b]])
        for h in range(3):
            P.op("act", lambda e, h=h: e.activation(out=junk[b][:], in_=qk_sb[b][:, h * 64:(h + 1) * 64], func=AF.Square,
                                                    accum_out=ss[b][:, h:h + 1]), reads=[r_qk[b]], writes=[r_junk[b], r_ss[b]])
        P.op("dve", lambda e: e.tensor_scalar(out=ss[b][:, 3:6], in0=ss[b][:, 0:3], scalar1=1.0 / 64, scalar2=QK_EPS,
                                              op0=ALU.mult, op1=ALU.add), reads=[r_ss[b]], writes=[r_ss[b]])
        P.op("act", lambda e: e.activation(out=ss[b][:, 3:6], in_=ss[b][:, 3:6], func=AF.Sqrt), reads=[r_ss[b]], writes=[r_ss[b]])
        P.op("dve", lambda e: e.reciprocal(out=ss[b][:, 3:6], in_=ss[b][:, 3:6]), reads=[r_ss[b]], writes=[r_ss[b]])
        for h in range(3):
            P.op("dve", lambda e, h=h: e.scalar_tensor_tensor(out=qn[b][:, h * 64:(h + 1) * 64], in0=qk_sb[b][:, h * 64:(h + 1) * 64],
                                                              scalar=ss[b][:, 3 + h:4 + h], in1=g_sb[:, h * 64:(h + 1) * 64],
                                                              op0=ALU.mult, op1=ALU.mult),
                 reads=[r_qk[b], r_ss[b], r_g], writes=[r_qn[b]])
        if lat:
            x0 = qn[b][:].rearrange("p (i two) -> p i two", two=2)[:, :, 0]
            x1 = qn[b][:].rearrange("p (i two) -> p i two", two=2)[:, :, 1]
            o0 = qr[b][:, 0:192].rearrange("p (i two) -> p i two", two=2)[:, :, 0]
            o1 = qr[b][:, 0:192].rearrange("p (i two) -> p i two", two=2)[:, :, 1]
            c_ = cs_sb[b][:, 0:96]
            s_ = cs_sb[b][:, 96:192]
            P.op("dve", lambda e: e.tensor_tensor(out=ra[b][:], in0=x0, in1=c_, op=ALU.mult), reads=[r_qn[b], r_cs[b]], writes=[r_ra[b]])
            P.op("pool", lambda e: e.tensor_tensor(out=rb_[b][:], in0=x1, in1=s_, op=ALU.mult), reads=[r_qn[b], r_cs[b]], writes=[r_rb[b]])
            P.op("dve", lambda e: e.tensor_tensor(out=o0, in0=ra[b][:], in1=rb_[b][:], op=ALU.subtract), reads=[r_ra[b], r_rb[b]], writes=[r_qr[b]])
            P.op("dve", lambda e: e.tensor_tensor(out=ra[b][:], in0=x0, in1=s_, op=ALU.mult), reads=[r_qn[b], r_cs[b]], writes=[r_ra[b]])
            P.op("pool", lambda e: e.tensor_tensor(out=rb_[b][:], in0=x1, in1=c_, op=ALU.mult), reads=[r_qn[b], r_cs[b]], writes=[r_rb[b]])
            P.op("dve", lambda e: e.tensor_tensor(out=o1, in0=ra[b][:], in1=rb_[b][:], op=ALU.add), reads=[r_ra[b], r_rb[b]], writes=[r_qr[b]])
        else:
            P.op("dve", lambda e: e.tensor_copy(out=qr[b][:, 0:192], in_=qn[b][:]), reads=[r_qn[b]], writes=[r_qr[b]])
        P.op("pool", lambda e: e.tensor_copy(out=qr[b][:, 320:384], in_=qr[b][:, 128:192]), reads=[r_qr[b]], writes=[r_qr[b]])
        P.op("pool", lambda e: e.tensor_copy(out=VA[:, t, 0:64], in_=v_sb[b][:]), reads=[r_v[b]], writes=[r_VA])
        tp, r_tp = tpb
        for h in range(3):
            P.op("pe", lambda e, h=h: e.transpose(tp[:, h, :], qr[b][:, h * 128:(h + 1) * 128], identb[:, :]),
                 reads=[r_qr[b], r_idb], writes=[r_tp], inc=(h == 2))
        P.op("act", lambda e: e.copy(out=QT[:, t0:t0 + 128], in_=tp[:, 0, :]), reads=[r_tp], writes=[r_QT])
        P.op("dve", lambda e: e.tensor_copy(out=KT[:, :, t0:t0 + 128], in_=tp[:, 1:3, :]), reads=[r_tp], writes=[r_KT])

    P.pop_scope([r_QT, r_KT, r_VA])
    sps = [P.ps([128, 1024], F32) for _ in range(NSB)]
    accs = [[P.ps([128, 512], F32) for _ in range(2)] for _ in range(2)]
    pt = [P.sb([128, 1024], BF16) for _ in range(NP)]
    r_pt = [Reg() for _ in range(NP)]
    ot = [[P.sb([128, 4, 64], F32) for _ in range(2)] for _ in range(2)]
    r_ot = [[Reg() for _ in range(2)] for _ in range(2)]
    rc = [P.sb([128, 4], F32) for _ in range(2)]
    r_rc = [Reg() for _ in range(2)]
    r_att = Reg()
    qtiles = [(qt * 128, list(range(NT)), qt % 4, 4, (qt // 4) * 512) for qt in range(NTL)]
    ncq = n_ctx // 128
    qtiles += [(n_lat + j * 128, list(range(NTL, NT)), j, ncq, n_lat) for j in range(ncq)]
    if stage < 2:
        qtiles = []
    items = []
    groups = []
    for (q0, kts, jq, nj, qb0) in qtiles:
        g = len(groups)
        groups.append((q0, jq, nj, qb0))
        kbs = [kts[i:i + 4] for i in range(0, len(kts), 4)]
        for bi, kb in enumerate(kbs):
            items.append((g, kb, bi == 0, bi == len(kbs) - 1))

    def emit_st(n):
        g, kb, first, last = items[n]
        q0 = groups[g][0]
        sb_, r_sb = sps[n % NSB]
        nk = len(kb)
        for h in range(2):
            for j, kt in enumerate(kb):
                P.op("pe", lambda e, h=h, j=j, kt=kt: e.matmul(sb_[:, h * 512 + j * 128:h * 512 + (j + 1) * 128], KT[:, h, kt * 128:(kt + 1) * 128],
                                                               QT[:, q0:q0 + 128], start=True, stop=True),
                     reads=[r_KT, r_QT], writes=[r_sb], inc=(h == 1 and j == nk - 1))

    def emit_rest(n):
        g, kb, first, last = items[n]
        q0, jq, nj, qb0 = groups[g]
        sb_, r_sb = sps[n % NSB]
        p2 = n % NP
        a2 = g % 2
        nk = len(kb)
        w = nk * 128
        P.op("act", lambda e: e.activation(out=pt[p2][:, :].rearrange("p (h c) -> p h c", h=2)[:, :, 0:w],
                                           in_=sb_[:, :].rearrange("p (h c) -> p h c", h=2)[:, :, 0:w], func=AF.Exp, scale=0.125),
             reads=[r_sb], writes=[r_pt[p2]])
        for h in range(2):
            A, r_A = accs[h][a2]
            for j, kt in enumerate(kb):
                P.op("pe", lambda e, h=h, j=j, kt=kt, A=A: e.matmul(A[:, 0:65], pt[p2][:, h * 512 + j * 128:h * 512 + (j + 1) * 128], VA[:, kt, 0:65],
                                                                    start=(first and j == 0), stop=(last and j == nk - 1)),
                     reads=[r_VA, r_pt[p2]], writes=[r_A], inc=(h == 1 and j == nk - 1))
        if last:
            o2 = (g // 4) % 2
            for h in range(2):
                A, r_A = accs[h][a2]
                P.op("dve", lambda e, A=A: e.reciprocal(out=rc[h][:, 0:1], in_=A[:, 64:65]), reads=[r_A], writes=[r_rc[h]])
                P.op("dve", lambda e, A=A: e.tensor_scalar(out=ot[h][o2][:, jq, :], in0=A[:, 0:64], scalar1=rc[h][:, 0:1], scalar2=None,
                                                           op0=ALU.mult), reads=[r_A, r_rc[h]], writes=[r_ot[h][o2]])
                if jq == nj - 1:
                    dst = att[qb0:qb0 + nj * 128, h * 64:(h + 1) * 64].rearrange("(j p) d -> p j d", p=128)
                    P.dma("sp", dst, ot[h][o2][:, 0:nj, :], reads=[r_ot[h][o2]], writes=[r_att])

    for n in range(min(NSB - 1, len(items))):
        emit_st(n)
    for n in range(len(items)):
        if n + NSB - 1 < len(items):
            emit_st(n + NSB - 1)
        emit_rest(n)
    P.finish([r_att])
    return P


GN_EPS = 64e-5
WSC = -0.6065306597126334


def rwkv_orders(n_lat, n_ctx, C=64):
    ncl, ncc = n_lat // C, n_ctx // C
    fwd = [n_lat + c * C for c in range(ncc)] + [c * C for c in range(ncl)]
    bwd = [n_lat + c * C for c in range(ncc - 1, -1, -1)] + [c * C for c in range(ncl - 1, -1, -1)]
    return fwd, bwd


def build_a2(n_lat=16384, n_ctx=256, stop=99, NBUF=4, REC=True):
    P = Prog()
    ntok = n_lat + n_ctx
    fwd, bwd = rwkv_orders(n_lat, n_ctx)
    NS = len(fwd)
    U3 = P.dram("U3", [NS * 128, 864], F32, "ExternalInput")
    coefmu = P.dram("coefmu", [128, 576], F32, "ExternalInput")
    rowp = P.dram("rowp", [128, 128], F32, "ExternalInput")
    hv = P.dram("hv", [128, 320], F32, "ExternalInput")
    Wl = P.dram("Wl", [96, 192], F32, "ExternalInput")
    cmask = P.dram("cmask", [128, 128 + 256 + 128 + 96], F32, "ExternalInput")
    rw = P.dram("rw", [ntok, 64], F32, "ExternalOutput")
    yfs = P.dram("yfs", [ntok, 64], F32, "Internal")
    ybs = P.dram("ybs", [ntok, 64], F32, "Internal")
    gs = P.dram("gs", [ntok, 64], F32, "Internal")
    r_yfs, r_ybs, r_gs, r_rw = Reg(), Reg(), Reg(), Reg()

    ident, r_id = make_ident(P, F32)
    cm = P.sb([128, 608], F32)
    r_cm = Reg()
    P.dma("sp", cm[:], cmask[:, :], writes=[r_cm])
    MIT = cm[:, 0:128]
    MM = cm[:, 128:384]
    MS = cm[:, 384:512]
    LM = cm[:, 512:608]
    coef = P.sb([128, 864], F32)
    r_coef = Reg()
    P.dma("sp", coef[:, 288:864], coefmu[:, :], writes=[r_coef])
    P.op("dve", lambda e: e.tensor_tensor(out=coef[:, 0:288], in0=coef[:, 288:576], in1=coef[:, 576:864], op=ALU.add), reads=[r_coef], writes=[r_coef])
    P.op("dve", lambda e: e.tensor_scalar(out=coef[:, 0:288], in0=coef[:, 0:288], scalar1=-1.0, scalar2=1.0, op0=ALU.mult, op1=ALU.add),
         reads=[r_coef], writes=[r_coef])
    rp = P.sb([128, 128], F32)
    hvs = P.sb([128, 320], F32)
    wl = P.sb([96, 192], F32)
    r_par = Reg()
    P.dma("sp", rp[:], rowp[:, :], writes=[r_par])
    P.dma("sp", hvs[:], hv[:, :], writes=[r_par])
    P.dma("sp", wl[:], Wl[:, :], writes=[r_par])
    KKW, KA, RK, GNG, GNB = (hvs[:, i * 64:(i + 1) * 64] for i in range(5))
    ones = P.sb([128, 128], F32)
    r_ones = Reg()
    P.op("pool", lambda e: e.memset(ones[:], 1.0), writes=[r_ones])

    banks = [P.ps([128, 512], F32) for _ in range(8)]
    if NBUF == 1 or not REC:
        bsets = [list(range(8))] * max(NBUF, 1)
    else:
        bsets = [[] for _ in range(NBUF)]
        for b_ in range(8):
            bsets[b_ * NBUF // 8].append(b_)
    bk = [0] * 8
    cur = [0]

    def nb():
        c = cur[0]
        b = banks[bsets[c][bk[c] % len(bsets[c])]]
        bk[c] += 1
        return b

    ev = [0]

    def evac(out, in_, reads, writes):
        ev[0] += 1
        if ev[0] % 2 == 0:
            P.op("act", lambda e: e.copy(out=out, in_=in_), reads=reads, writes=writes)
        else:
            P.op("dve", lambda e: e.tensor_copy(out=out, in_=in_), reads=reads, writes=writes)

    def T(shape=(128, 128), n=None):
        n = n or NBUF
        return [P.sb(list(shape), F32) for _ in range(n)], [Reg() for _ in range(n)]

    u3, r_u3 = T((128, 864))
    prod, r_prod = T((128, 864))
    us, r_us = T((128, 288))
    lo, r_lo = T((128, 96))
    loT, r_loT = T((96, 128))
    wa, r_wa = T((128, 128))
    gg, r_gg = T((128, 64))
    kk, r_kk = T((128, 64))
    sm, r_sm = T((128, 8))
    tmp, r_tmp = T((128, 64))
    tmp2, r_tmp2 = T((128, 64))
    kd, r_kd = T((128, 64))
    bdn = ["lw", "a", "b", "kd", "r", "v"]
    bd = {n: T() for n in bdn}
    for n in bdn:
        for i in range(NBUF):
            P.op("pool", lambda e, n=n, i=i: e.memset(bd[n][0][i][:], 0.0), writes=[bd[n][1][i]])
    ex, r_ex = T((128, 512))
    ee, r_ee = T((128, 256))
    At, r_At = T()
    BKt, r_BKt = T((128, 256))
    Rt, r_Rt = T()
    BKG, r_BKG = T((128, 256))
    gcc, r_gcc = T((128, 1))
    BKT, r_BKT = T((128, 256))
    ART, r_ART = T((128, 256))
    LA, r_LA = T((128, 256))
    LK, r_LK = T((128, 256))
    X, r_X = T()
    XT, r_XT = T()
    X2, r_X2 = T()
    XT2, r_XT2 = T()
    TT, r_TT = T()
    Pm, r_Pm = T()
    LV, r_LV = T()
    Q, r_Q = T()
    RpT, r_RpT = T()
    Mm, r_Mm = T()
    yo, r_yo = T()
    ST = [P.sb([128, 128], F32) for _ in range(2)]
    r_ST = [Reg(), Reg()]
    P.op("pool", lambda e: e.memset(ST[0][:], 0.0), writes=[r_ST[0]])

    lists = []
    for s in range(NS):
        i = s % NBUF
        cur[0] = i if REC else 0
        if REC:
            P.record()
        LW, r_LW = bd["lw"][0][i], bd["lw"][1][i]
        Ab, r_Ab = bd["a"][0][i], bd["a"][1][i]
        Bb, r_Bb = bd["b"][0][i], bd["b"][1][i]
        KDb, r_KDb = bd["kd"][0][i], bd["kd"][1][i]
        Rb, r_Rb = bd["r"][0][i], bd["r"][1][i]
        Vb, r_Vb = bd["v"][0][i], bd["v"][1][i]
        P.dma("sp", u3[i][:], U3[s * 128:(s + 1) * 128, :], writes=[r_u3[i]])
        P.op("dve", lambda e: e.tensor_tensor(out=prod[i][:], in0=u3[i][:], in1=coef[:], op=ALU.mult), reads=[r_u3[i], r_coef], writes=[r_prod[i]])
        P.op("pool", lambda e: e.tensor_tensor(out=us[i][:], in0=prod[i][:, 0:288], in1=prod[i][:, 288:576], op=ALU.add), reads=[r_prod[i]], writes=[r_us[i]])
        P.op("dve", lambda e: e.tensor_tensor(out=us[i][:], in0=us[i][:], in1=prod[i][:, 576:864], op=ALU.add), reads=[r_prod[i], r_us[i]], writes=[r_us[i]])
        r_ = us[i][:, 0:64]
        k_ = us[i][:, 64:128]
        v_ = us[i][:, 128:192]
        if stop <= 1:
            continue
        P.op("act", lambda e: e.activation(out=lo[i][:, 0:32], in_=us[i][:, 192:224], func=AF.Tanh), reads=[r_us[i]], writes=[r_lo[i]])
        P.op("act", lambda e: e.activation(out=lo[i][:, 64:96], in_=us[i][:, 256:288], func=AF.Sigmoid), reads=[r_us[i]], writes=[r_lo[i]])
        P.op("pool", lambda e: e.tensor_copy(out=lo[i][:, 32:64], in_=us[i][:, 224:256]), reads=[r_us[i]], writes=[r_lo[i]])
        P.op("dve", lambda e: e.tensor_tensor(out=lo[i][:], in0=lo[i][:], in1=LM, op=ALU.mult), reads=[r_lo[i], r_cm], writes=[r_lo[i]])
        b1, rb1 = nb()
        P.op("pe", lambda e: e.transpose(b1[0:96, 0:128], lo[i][:, :], ident[:, :]), reads=[r_lo[i], r_id], writes=[rb1])
        evac(loT[i][:, :], b1[0:96, 0:128], [rb1], [r_loT[i]])
        b2, rb2 = nb()
        P.op("pe", lambda e: e.matmul(b2[:, 0:192], loT[i][:, :], wl[:, :], start=True, stop=True), reads=[r_loT[i], r_par], writes=[rb2])
        P.op("dve", lambda e: e.tensor_tensor(out=wa[i][:], in0=b2[:, 0:128], in1=rp[:], op=ALU.add), reads=[rb2, r_par], writes=[r_wa[i]])
        P.op("act", lambda e: e.copy(out=gg[i][:], in_=b2[:, 128:192]), reads=[rb2], writes=[r_gg[i]])
        P.op("act", lambda e: e.activation(out=wa[i][:], in_=wa[i][:], func=AF.Sigmoid), reads=[r_wa[i]], writes=[r_wa[i]])
        sw = wa[i][:, 0:64]
        asg = wa[i][:, 64:128]
        if stop <= 2:
            continue
        P.op("dve", lambda e: e.tensor_tensor(out=kk[i][:], in0=k_, in1=KKW, op=ALU.mult), reads=[r_us[i], r_par], writes=[r_kk[i]])
        P.op("act", lambda e: e.activation(out=tmp[i][:], in_=kk[i][:], func=AF.Square, accum_out=sm[i][:, 0:1]), reads=[r_kk[i]], writes=[r_tmp[i], r_sm[i]])
        P.op("dve", lambda e: e.tensor_scalar_add(out=sm[i][:, 1:2], in0=sm[i][:, 0:1], scalar1=1e-12), reads=[r_sm[i]], writes=[r_sm[i]])
        P.op("act", lambda e: e.activation(out=sm[i][:, 1:2], in_=sm[i][:, 1:2], func=AF.Sqrt), reads=[r_sm[i]], writes=[r_sm[i]])
        P.op("dve", lambda e: e.reciprocal(out=sm[i][:, 2:3], in_=sm[i][:, 1:2]), reads=[r_sm[i]], writes=[r_sm[i]])
        P.op("dve", lambda e: e.tensor_scalar(out=kk[i][:], in0=kk[i][:], scalar1=sm[i][:, 2:3], scalar2=None, op0=ALU.mult), reads=[r_kk[i], r_sm[i]], writes=[r_kk[i]])
        P.op("dve", lambda e: e.scalar_tensor_tensor(out=tmp2[i][:], in0=asg, scalar=-1.0, in1=KA, op0=ALU.add, op1=ALU.mult), reads=[r_wa[i], r_par], writes=[r_tmp2[i]])
        P.op("dve", lambda e: e.scalar_tensor_tensor(out=kd[i][:], in0=tmp2[i][:], scalar=1.0, in1=k_, op0=ALU.add, op1=ALU.mult), reads=[r_tmp2[i], r_us[i]], writes=[r_kd[i]])
        P.op("dve", lambda e: e.tensor_tensor(out=tmp[i][:], in0=r_, in1=RK, op=ALU.mult), reads=[r_us[i], r_par], writes=[r_tmp[i]])
        P.op("dve", lambda e: e.tensor_tensor(out=tmp[i][:], in0=tmp[i][:], in1=kd[i][:], op=ALU.mult), reads=[r_tmp[i], r_kd[i]], writes=[r_tmp[i]])
        P.op("dve", lambda e: e.tensor_reduce(out=sm[i][:, 3:4], in_=tmp[i][:], axis=AX.X, op=ALU.add), reads=[r_tmp[i]], writes=[r_sm[i]])
        if stop <= 3:
            continue
        for h in range(2):
            ps_ = slice(h * 64, (h + 1) * 64)
            ce = "act" if h == 0 else "pool"
            P.op("dve", lambda e: e.tensor_scalar(out=LW[ps_, ps_], in0=wa[i][ps_, 0:64], scalar1=WSC, scalar2=None, op0=ALU.mult), reads=[r_wa[i]], writes=[r_LW])
            P.op("dve", lambda e: e.tensor_scalar(out=Ab[ps_, ps_], in0=kk[i][ps_, :], scalar1=-1.0, scalar2=None, op0=ALU.mult), reads=[r_kk[i]], writes=[r_Ab])
            P.op("pool", lambda e: e.tensor_tensor(out=Bb[ps_, ps_], in0=kk[i][ps_, :], in1=wa[i][ps_, 64:128], op=ALU.mult), reads=[r_kk[i], r_wa[i]], writes=[r_Bb])
            for (dst, r_dst, src, r_src) in ((KDb, r_KDb, kd[i][ps_, :], r_kd[i]), (Rb, r_Rb, us[i][ps_, 0:64], r_us[i]), (Vb, r_Vb, us[i][ps_, 128:192], r_us[i])):
                if ce == "act":
                    P.op("act", lambda e, dst=dst, src=src: e.copy(out=dst[ps_, ps_], in_=src), reads=[r_src], writes=[r_dst])
                else:
                    P.op("pool", lambda e, dst=dst, src=src: e.tensor_copy(out=dst[ps_, ps_], in_=src), reads=[r_src], writes=[r_dst])
        if stop <= 4:
            continue
        b3, rb3 = nb()
        P.op("pe", lambda e: e.matmul(b3[:, 0:128], MIT, LW[:, :], start=True, stop=True), reads=[r_cm, r_LW], writes=[rb3], inc=False)
        P.op("pe", lambda e: e.matmul(b3[:, 128:256], ones[:, :], LW[:, :], start=True, stop=True), reads=[r_ones, r_LW], writes=[rb3], inc=False)
        P.op("pe", lambda e: e.matmul(b3[:, 256:257], LW[:, :], ones[:, 0:1], start=True, stop=True), reads=[r_ones, r_LW], writes=[rb3])
        P.op("act", lambda e: e.activation(out=ex[i][:, 0:128], in_=b3[:, 0:128], func=AF.Exp), reads=[rb3], writes=[r_ex[i]])
        P.op("act", lambda e: e.activation(out=ex[i][:, 128:256], in_=b3[:, 0:128], func=AF.Exp, scale=-1.0), reads=[rb3], writes=[r_ex[i]])
        P.op("act", lambda e: e.activation(out=ex[i][:, 256:384], in_=b3[:, 128:256], func=AF.Exp), reads=[rb3], writes=[r_ex[i]])
        P.op("act", lambda e: e.activation(out=gcc[i][:, 0:1], in_=b3[:, 256:257], func=AF.Exp), reads=[rb3], writes=[r_gcc[i]])
        P.op("act", lambda e: e.activation(out=ex[i][:, 384:512], in_=LW[:, :], func=AF.Exp, scale=-1.0), reads=[r_LW], writes=[r_ex[i]])
        EI = ex[i][:, 0:128]
        EN = ex[i][:, 128:256]
        P.op("dve", lambda e: e.tensor_tensor(out=ee[i][:, 0:128], in0=EI, in1=ex[i][:, 384:512], op=ALU.mult), reads=[r_ex[i]], writes=[r_ee[i]])
        P.op("pool", lambda e: e.tensor_tensor(out=ee[i][:, 128:256], in0=ex[i][:, 256:384], in1=EN, op=ALU.mult), reads=[r_ex[i]], writes=[r_ee[i]])
        P.op("dve", lambda e: e.tensor_tensor(out=At[i][:], in0=Ab[:, :], in1=ee[i][:, 0:128], op=ALU.mult), reads=[r_Ab, r_ee[i]], writes=[r_At[i]])
        P.op("pool", lambda e: e.tensor_tensor(out=BKt[i][:, 0:128], in0=Bb[:, :], in1=EN, op=ALU.mult), reads=[r_Bb, r_ex[i]], writes=[r_BKt[i]])
        P.op("dve", lambda e: e.tensor_tensor(out=BKt[i][:, 128:256], in0=KDb[:, :], in1=EN, op=ALU.mult), reads=[r_KDb, r_ex[i]], writes=[r_BKt[i]])
        P.op("pool", lambda e: e.tensor_tensor(out=Rt[i][:], in0=Rb[:, :], in1=EI, op=ALU.mult), reads=[r_Rb, r_ex[i]], writes=[r_Rt[i]])
        P.op("dve", lambda e: e.tensor_tensor(out=BKG[i][:, 0:128], in0=Bb[:, :], in1=ee[i][:, 128:256], op=ALU.mult), reads=[r_Bb, r_ee[i]], writes=[r_BKG[i]])
        P.op("pool", lambda e: e.tensor_tensor(out=BKG[i][:, 128:256], in0=KDb[:, :], in1=ee[i][:, 128:256], op=ALU.mult), reads=[r_KDb, r_ee[i]], writes=[r_BKG[i]])
        if stop <= 5:
            continue
        b4, rb4 = nb()
        P.op("pe", lambda e: e.transpose(b4[:, 0:128], BKt[i][:, 0:128], ident[:, :]), reads=[r_BKt[i], r_id], writes=[rb4], inc=False)
        P.op("pe", lambda e: e.transpose(b4[:, 128:256], BKt[i][:, 128:256], ident[:, :]), reads=[r_BKt[i], r_id], writes=[rb4])
        evac(BKT[i][:, :], b4[:, 0:256], [rb4], [r_BKT[i]])
        b5, rb5 = nb()
        P.op("pe", lambda e: e.transpose(b5[:, 0:128], At[i][:, :], ident[:, :]), reads=[r_At[i], r_id], writes=[rb5], inc=False)
        P.op("pe", lambda e: e.transpose(b5[:, 128:256], Rt[i][:, :], ident[:, :]), reads=[r_Rt[i], r_id], writes=[rb5])
        evac(ART[i][:, :], b5[:, 0:256], [rb5], [r_ART[i]])
        b6, rb6 = nb()
        P.op("pe", lambda e: e.matmul(b6[:, 0:256], BKT[i][:, 0:128], ART[i][:, :], start=True, stop=True), reads=[r_BKT[i], r_ART[i]], writes=[rb6])
        P.op("dve", lambda e: e.tensor_tensor(out=LA[i][:], in0=b6[:, 0:256], in1=MM, op=ALU.mult), reads=[rb6, r_cm], writes=[r_LA[i]])
        b7, rb7 = nb()
        P.op("pe", lambda e: e.matmul(b7[:, 0:256], BKT[i][:, 128:256], ART[i][:, :], start=True, stop=True), reads=[r_BKT[i], r_ART[i]], writes=[rb7])
        P.op("dve", lambda e: e.tensor_tensor(out=LK[i][:], in0=b7[:, 0:256], in1=MM, op=ALU.mult), reads=[rb7, r_cm], writes=[r_LK[i]])
        b8, rb8 = nb()
        P.op("pe", lambda e: e.matmul(b8[:, 0:128], ART[i][:, 0:128], BKT[i][:, 0:128], start=True, stop=True), reads=[r_BKT[i], r_ART[i]], writes=[rb8])
        P.op("dve", lambda e: e.tensor_tensor(out=XT[i][:], in0=b8[:, 0:128], in1=MS, op=ALU.mult), reads=[rb8, r_cm], writes=[r_XT[i]])
        if stop <= 6:
            continue
        P.op("pool", lambda e: e.tensor_tensor(out=TT[i][:], in0=LA[i][:, 0:128], in1=ident[:, :], op=ALU.add), reads=[r_LA[i], r_id], writes=[r_TT[i]])
        cx, r_cx = LA[i][:, 0:128], r_LA[i]
        cxt, r_cxt = XT[i][:, :], r_XT[i]
        nxt = [(X2[i], r_X2[i], XT2[i], r_XT2[i]), (X[i], r_X[i], XT[i], r_XT[i])]
        for lv in range(5):
            nX, r_nX, nXT, r_nXT = nxt[lv % 2]
            if lv < 4:
                ba, rba = nb()
                P.op("pe", lambda e, cx=cx, cxt=cxt, ba=ba: e.matmul(ba[:, 0:128], cxt, cx, start=True, stop=True), reads=[r_cx, r_cxt], writes=[rba])
            bb, rbb = nb()
            P.op("pe", lambda e, cx=cx, cxt=cxt, bb=bb: e.matmul(bb[:, 0:128], cx, cxt, start=True, stop=True), reads=[r_cx, r_cxt], writes=[rbb])
            if lv < 4:
                P.op("act", lambda e, nX=nX, ba=ba: e.copy(out=nX[:, :], in_=ba[:, 0:128]), reads=[rba], writes=[r_nX])
            P.op("dve", lambda e, nXT=nXT, bb=bb: e.tensor_copy(out=nXT[:, :], in_=bb[:, 0:128]), reads=[rbb], writes=[r_nXT])
            bc, rbc = nb()
            P.op("pe", lambda e, nXT=nXT, bc=bc: e.matmul(bc[:, 0:128], nXT[:, :], TT[i][:, :], start=True, stop=True), reads=[r_nXT, r_TT[i]], writes=[rbc])
            P.op("dve", lambda e, bc=bc: e.tensor_tensor(out=TT[i][:], in0=bc[:, 0:128], in1=TT[i][:], op=ALU.add), reads=[rbc, r_TT[i]], writes=[r_TT[i]])
            cx, r_cx, cxt, r_cxt = nX[:, :], r_nX, nXT[:, :], r_nXT
        if stop <= 7:
            continue
        b9, rb9 = nb()
        P.op("pe", lambda e: e.matmul(b9[:, 0:128], TT[i][:, :], At[i][:, :], start=True, stop=True), reads=[r_TT[i], r_At[i]], writes=[rb9], inc=False)
        P.op("pe", lambda e: e.matmul(b9[:, 128:256], LK[i][:, 0:128], Vb[:, :], start=True, stop=True), reads=[r_LK[i], r_Vb], writes=[rb9])
        P.op("act", lambda e: e.copy(out=Pm[i][:, :], in_=b9[:, 0:128]), reads=[rb9], writes=[r_Pm[i]])
        P.op("dve", lambda e: e.tensor_copy(out=LV[i][:, :], in_=b9[:, 128:256]), reads=[rb9], writes=[r_LV[i]])
        b10, rb10 = nb()
        P.op("pe", lambda e: e.matmul(b10[:, 0:128], TT[i][:, :], LV[i][:, :], start=True, stop=True), reads=[r_TT[i], r_LV[i]], writes=[rb10], inc=False)
        P.op("pe", lambda e: e.matmul(b10[:, 128:256], Pm[i][:, :], LA[i][:, 128:256], start=True, stop=True), reads=[r_Pm[i], r_LA[i]], writes=[rb10], inc=False)
        P.op("pe", lambda e: e.matmul(b10[:, 256:384], Pm[i][:, :], BKG[i][:, 0:128], start=True, stop=True), reads=[r_Pm[i], r_BKG[i]], writes=[rb10])
        P.op("act", lambda e: e.copy(out=Q[i][:, :], in_=b10[:, 0:128]), reads=[rb10], writes=[r_Q[i]])
        P.op("dve", lambda e: e.tensor_tensor(out=RpT[i][:], in0=b10[:, 128:256], in1=ART[i][:, 128:256], op=ALU.add), reads=[rb10, r_ART[i]], writes=[r_RpT[i]])
        P.op("dve", lambda e: e.scalar_tensor_tensor(out=Mm[i][:], in0=ident[:, :], scalar=gcc[i][:, 0:1], in1=b10[:, 256:384], op0=ALU.mult, op1=ALU.add),
             reads=[rb10, r_id, r_gcc[i]], writes=[r_Mm[i]])
        if stop <= 8:
            continue
        sc, sn = s % 2, (s + 1) % 2
        b11, rb11 = nb()
        P.op("pe", lambda e: e.matmul(b11[:, 0:128], LA[i][:, 128:256], Q[i][:, :], start=True, stop=False), reads=[r_LA[i], r_Q[i]], writes=[rb11], inc=False)
        P.op("pe", lambda e: e.matmul(b11[:, 0:128], LK[i][:, 128:256], Vb[:, :], start=False, stop=False), reads=[r_LK[i], r_Vb], writes=[rb11], inc=False)
        P.op("pe", lambda e: e.matmul(b11[:, 0:128], RpT[i][:, :], ST[sc][:, :], start=False, stop=True), reads=[r_RpT[i], r_ST[sc]], writes=[rb11])
        b12, rb12 = nb()
        P.op("pe", lambda e: e.matmul(b12[:, 0:128], BKG[i][:, 0:128], Q[i][:, :], start=True, stop=False), reads=[r_BKG[i], r_Q[i]], writes=[rb12], inc=False)
        P.op("pe", lambda e: e.matmul(b12[:, 0:128], BKG[i][:, 128:256], Vb[:, :], start=False, stop=False), reads=[r_BKG[i], r_Vb], writes=[rb12], inc=False)
        P.op("pe", lambda e: e.matmul(b12[:, 0:128], Mm[i][:, :], ST[sc][:, :], start=False, stop=True), reads=[r_Mm[i], r_ST[sc]], writes=[rb12])
        P.op("act", lambda e: e.copy(out=ST[sn][:, :], in_=b12[:, 0:128]), reads=[rb12], writes=[r_ST[sn]])
        P.op("dve", lambda e: e.scalar_tensor_tensor(out=yo[i][:], in0=Vb[:, :], scalar=sm[i][:, 3:4], in1=b11[:, 0:128], op0=ALU.mult, op1=ALU.add),
             reads=[rb11, r_Vb, r_sm[i]], writes=[r_yo[i]])
        tf, tb_ = fwd[s], bwd[s]
        P.dma("sp", yfs[tf:tf + 64, :], yo[i][0:64, 0:64], reads=[r_yo[i]], writes=[r_yfs])
        P.dma("sp", ybs[tb_:tb_ + 64, :], yo[i][64:128, 64:128], reads=[r_yo[i]], writes=[r_ybs])
        P.dma("sp", gs[tf:tf + 64, :], gg[i][0:64, :], reads=[r_gg[i]], writes=[r_gs])
        if REC:
            lists.append(P.stop_record())
            if s == NS - 1:
                P.replay_skewed(lists, NBUF)
                lists = []

    if stop < 99:
        P.finish([])
        return P
    yf_, r_yf_ = T((128, 64), 2)
    yb_, r_yb_ = T((128, 64), 2)
    g_, r_g_ = T((128, 64), 2)
    st, r_st = T((128, 16), 2)
    yn, r_yn = T((128, 64), 2)
    for t in range(ntok // 128):
        i = t % 2
        t0 = t * 128
        P.dma("sp", yf_[i][:], yfs[t0:t0 + 128, :], reads=[r_yfs], writes=[r_yf_[i]])
        P.dma("sp", yb_[i][:], ybs[t0:t0 + 128, :], reads=[r_ybs], writes=[r_yb_[i]])
        P.dma("sp", g_[i][:], gs[t0:t0 + 128, :], reads=[r_gs], writes=[r_g_[i]])
        P.op("dve", lambda e: e.tensor_tensor(out=yf_[i][:], in0=yf_[i][:], in1=yb_[i][:], op=ALU.add), reads=[r_yf_[i], r_yb_[i]], writes=[r_yf_[i]])
        P.op("dve", lambda e: e.bn_stats(out=st[i][:, 0:6], in_=yf_[i][:]), reads=[r_yf_[i]], writes=[r_st[i]])
        P.op("dve", lambda e: e.bn_aggr(out=st[i][:, 6:8], in_=st[i][:, 0:6]), reads=[r_st[i]], writes=[r_st[i]])
        P.op("dve", lambda e: e.tensor_scalar_add(out=st[i][:, 8:9], in0=st[i][:, 7:8], scalar1=GN_EPS), reads=[r_st[i]], writes=[r_st[i]])
        P.op("act", lambda e: e.activation(out=st[i][:, 8:9], in_=st[i][:, 8:9], func=AF.Sqrt), reads=[r_st[i]], writes=[r_st[i]])
        P.op("dve", lambda e: e.reciprocal(out=st[i][:, 9:10], in_=st[i][:, 8:9]), reads=[r_st[i]], writes=[r_st[i]])
        P.op("dve", lambda e: e.tensor_scalar(out=yn[i][:], in0=yf_[i][:], scalar1=st[i][:, 6:7], scalar2=st[i][:, 9:10], op0=ALU.subtract, op1=ALU.mult),
             reads=[r_yf_[i], r_st[i]], writes=[r_yn[i]])
        P.op("pool", lambda e: e.tensor_tensor(out=yn[i][:], in0=yn[i][:], in1=GNG, op=ALU.mult), reads=[r_yn[i], r_par], writes=[r_yn[i]])
        P.op("pool", lambda e: e.tensor_tensor(out=yn[i][:], in0=yn[i][:], in1=GNB, op=ALU.add), reads=[r_yn[i], r_par], writes=[r_yn[i]])
        P.op("dve", lambda e: e.tensor_tensor(out=yn[i][:], in0=yn[i][:], in1=g_[i][:], op=ALU.mult), reads=[r_yn[i], r_g_[i]], writes=[r_yn[i]])
        P.dma("sp", rw[t0:t0 + 128, :], yn[i][:], reads=[r_yn[i]], writes=[r_rw])
    P.finish([r_rw])
    return P


PI = math.pi


def build_a3(n_lat=16384, n_ctx=256, stop=99):
    P = Prog()
    ntok = n_lat + n_ctx
    NT = ntok // 128
    H3 = P.dram("H3", [ntok, 576], F32, "ExternalInput")
    cw = P.dram("cw", [128, 768], F32, "ExternalInput")
    ztl = P.dram("ztl", [2, 33, n_lat], F32, "ExternalInput")
    ztc = P.dram("ztc", [2, 33, n_ctx], F32, "ExternalInput")
    w1d = P.dram("w1", [33, 64], F32, "ExternalInput")
    w2d = P.dram("w2", [64, 64], F32, "ExternalInput")
    w3d = P.dram("w3", [64, 256], F32, "ExternalInput")
    colp = P.dram("colp", [64, 4], F32, "ExternalInput")
    decd = P.dram("dec", [1, 256], F32, "ExternalInput")
    hbd = P.dram("hb", [128, 128], F32, "ExternalInput")
    hy = P.dram("hy", [ntok, 64], F32, "ExternalOutput")
    WL = 2 * n_lat - 1
    WC = 2 * n_ctx - 1
    KDl = P.dram("KDl", [128, WL + 1], BF16, "Internal")
    KDc = P.dram("KDc", [128, WC + 1], BF16, "Internal")
    r_KD = {0: Reg(), 1: Reg()}
    r_hy = Reg()

    identf, r_idf = make_ident(P, F32)
    J = P.sb([128, 128], BF16)
    r_J = Reg()
    P.op("pool", lambda e: e.memset(J[:], 0.0), writes=[r_J])
    P.op("pool", lambda e: e.affine_select(out=J[:], in_=J[:], pattern=[[1, 128]], compare_op=ALU.not_equal, fill=1.0, base=-127, channel_multiplier=1),
         reads=[r_J], writes=[r_J])
    ones = P.sb([128, 128], F32)
    r_ones = Reg()
    P.op("pool", lambda e: e.memset(ones[:], 1.0), writes=[r_ones])
    npi = P.sb([128, 1], F32)
    P.op("pool", lambda e: e.memset(npi[:], PI / 2), writes=[r_ones])

    cws = P.sb([128, 768], F32)
    w1s = P.sb([33, 64], F32)
    w2s = P.sb([64, 64], F32)
    w3s = P.sb([64, 256], F32)
    cps = P.sb([64, 4], F32)
    decs = P.sb([1, 256], F32)
    hbs = P.sb([128, 128], F32)
    r_par = Reg()
    for dst, src in ((cws, cw), (w1s, w1d), (w2s, w2d), (w3s, w3d), (cps, colp), (decs, decd), (hbs, hbd)):
        P.dma("sp", dst[:], src[:, :], writes=[r_par])
    P.op("dve", lambda e: e.tensor_scalar(out=decs[:], in0=decs[:], scalar1=-1.0, scalar2=None, op0=ALU.mult), reads=[r_par], writes=[r_par])

    banks = [P.ps([128, 512], F32) for _ in range(8)]
    bk = [0]

    def nb():
        b = banks[bk[0] % 6]
        bk[0] += 1
        return b
    ybanks = banks[6:8]

    SC = P.sb([128, 192, NT], F32)
    r_SC = Reg()
    RN = P.sb([128, 2, 128], F32)
    r_RN = Reg()
    P.push_scope()
    h3 = [P.sb([128, 576], F32) for _ in range(2)]
    r_h3 = [Reg(), Reg()]
    pr = [P.sb([128, 576], F32) for _ in range(2)]
    r_pr = [Reg(), Reg()]
    s1 = [P.sb([128, 192], F32) for _ in range(2)]
    r_s1 = [Reg(), Reg()]
    for t in range(NT):
        i = t % 2
        P.dma("sp", h3[i][:], H3[t * 128:(t + 1) * 128, :], writes=[r_h3[i]])
        P.op("dve", lambda e: e.tensor_tensor(out=pr[i][:], in0=h3[i][:], in1=cws[:, 0:576], op=ALU.mult), reads=[r_h3[i], r_par], writes=[r_pr[i]])
        P.op("pool", lambda e: e.tensor_tensor(out=s1[i][:], in0=pr[i][:, 0:192], in1=pr[i][:, 192:384], op=ALU.add), reads=[r_pr[i]], writes=[r_s1[i]])
        P.op("pool", lambda e: e.tensor_tensor(out=s1[i][:], in0=s1[i][:], in1=pr[i][:, 384:576], op=ALU.add), reads=[r_pr[i], r_s1[i]], writes=[r_s1[i]])
        P.op("dve", lambda e: e.tensor_tensor(out=SC[:, :, t], in0=s1[i][:], in1=cws[:, 576:768], op=ALU.add), reads=[r_s1[i], r_par], writes=[r_SC])

    nacc = 2 * max(n_lat // 512, 1) + 2
    acc = P.sb([128, 2, 2 * (n_lat // 512 + 1)], F32)
    r_acc = Reg()
    P.op("pool", lambda e: e.memset(acc[:], 0.0), writes=[r_acc])
    zt = [P.sb([33, 512], F32) for _ in range(2)]
    r_zt = [Reg(), Reg()]
    hA = [P.sb([64, 512], F32) for _ in range(2)]
    r_hA = [Reg(), Reg()]
    hB = [P.sb([64, 512], F32) for _ in range(2)]
    r_hB = [Reg(), Reg()]
    win = [P.sb([128, 512], F32) for _ in range(2)]
    r_win = [Reg(), Reg()]
    hw = [P.sb([128, 512], F32) for _ in range(2)]
    r_hw = [Reg(), Reg()]
    hwb = [P.sb([128, 512], BF16) for _ in range(2)]
    r_hwb = [Reg(), Reg()]
    junk = [P.sb([128, 512], F32) for _ in range(2)]
    r_junk = [Reg(), Reg()]
    sS = [P.sb([64, 512], F32) for _ in range(2)]
    sC = [P.sb([64, 512], F32) for _ in range(2)]
    sQ = [P.sb([64, 512], F32) for _ in range(2)]
    r_sS = [Reg(), Reg()]
    r_sC = [Reg(), Reg()]
    r_sQ = [Reg(), Reg()]

    def sin_big(h, r_h, N, i):
        S, C, Q = sS[i], sC[i], sQ[i]
        P.op("act", lambda e: e.activation(out=S[:, 0:N], in_=h[:, 0:N], func=AF.Sin, scale=0.125), reads=[r_h], writes=[r_sS[i]])
        P.op("act", lambda e: e.activation(out=C[:, 0:N], in_=h[:, 0:N], func=AF.Sin, scale=0.125, bias=npi[0:64, 0:1]), reads=[r_h, r_ones], writes=[r_sC[i]])
        for lv in range(3):
            dst = h if lv == 2 else S
            r_dst = r_h if lv == 2 else r_sS[i]
            if lv < 2:
                P.op("pool", lambda e: e.tensor_tensor(out=Q[:, 0:N], in0=S[:, 0:N], in1=S[:, 0:N], op=ALU.mult), reads=[r_sS[i]], writes=[r_sQ[i]])
            P.op("dve", lambda e, dst=dst: e.scalar_tensor_tensor(out=dst[:, 0:N], in0=S[:, 0:N], scalar=2.0, in1=C[:, 0:N], op0=ALU.mult, op1=ALU.mult),
                 reads=[r_sS[i], r_sC[i]], writes=[r_dst])
            if lv < 2:
                P.op("dve", lambda e: e.tensor_scalar(out=C[:, 0:N], in0=Q[:, 0:N], scalar1=-2.0, scalar2=1.0, op0=ALU.mult, op1=ALU.add),
                     reads=[r_sQ[i]], writes=[r_sC[i]])

    it = 0
    for seq, (L, ztd, KD) in enumerate(((n_lat, ztl, KDl), (n_ctx, ztc, KDc))):
        N = min(512, L)
        nblk = L // N
        for ps in range(2):
            for bl in range(nblk):
                i = it % 2
                it += 1
                P.dma("sp", zt[i][:, 0:N], ztd[ps, :, bl * N:(bl + 1) * N], writes=[r_zt[i]])
                b1, rb1 = nb()
                P.op("pe", lambda e: e.matmul(b1[0:64, 0:N], w1s[:, :], zt[i][:, 0:N], start=True, stop=True), reads=[r_par, r_zt[i]], writes=[rb1])
                P.op("dve", lambda e: e.tensor_scalar(out=hA[i][:, 0:N], in0=b1[0:64, 0:N], scalar1=cps[:, 0:1], scalar2=cps[:, 1:2], op0=ALU.add, op1=ALU.mult),
                     reads=[rb1, r_par], writes=[r_hA[i]])
                sin_big(hA[i], r_hA[i], N, i)
                b2, rb2 = nb()
                P.op("pe", lambda e: e.matmul(b2[0:64, 0:N], w2s[:, :], hA[i][:, 0:N], start=True, stop=True), reads=[r_par, r_hA[i]], writes=[rb2])
                P.op("dve", lambda e: e.tensor_scalar(out=hB[i][:, 0:N], in0=b2[0:64, 0:N], scalar1=cps[:, 2:3], scalar2=cps[:, 3:4], op0=ALU.add, op1=ALU.mult),
                     reads=[rb2, r_par], writes=[r_hB[i]])
                sin_big(hB[i], r_hB[i], N, i)
                b3, rb3 = nb()
                P.op("pe", lambda e: e.matmul(b3[:, 0:N], w3s[:, ps * 128:(ps + 1) * 128], hB[i][:, 0:N], start=True, stop=True), reads=[r_par, r_hB[i]], writes=[rb3])
                b4, rb4 = nb()
                P.op("pe", lambda e: e.matmul(b4[:, 0:N], decs[0:1, ps * 128:(ps + 1) * 128], zt[i][0:1, 0:N], start=True, stop=True), reads=[r_par, r_zt[i]], writes=[rb4])
                P.op("act", lambda e: e.activation(out=win[i][:, 0:N], in_=b4[:, 0:N], func=AF.Exp), reads=[rb4], writes=[r_win[i]])
                P.op("dve", lambda e: e.tensor_tensor(out=hw[i][:, 0:N], in0=b3[:, 0:N], in1=win[i][:, 0:N], op=ALU.mult), reads=[rb3, r_win[i]], writes=[r_hw[i]])
                if ps == 1 and bl == nblk - 1:
                    P.op("dve", lambda e: e.memset(hw[i][:, N - 1:N], 0.0), reads=[r_hw[i]], writes=[r_hw[i]])
                P.op("act", lambda e: e.activation(out=junk[i][:, 0:N], in_=hw[i][:, 0:N], func=AF.Abs, accum_out=acc[:, seq, ps * nblk + bl:ps * nblk + bl + 1]),
                     reads=[r_hw[i]], writes=[r_junk[i], r_acc])
                P.op("pool", lambda e: e.tensor_copy(out=hwb[i][:, 0:N], in_=hw[i][:, 0:N]), reads=[r_hw[i]], writes=[r_hwb[i]])
                if ps == 0:
                    q0 = L - 1 + bl * N
                    P.dma("sp", KD[:, q0:q0 + N], hwb[i][:, 0:N], reads=[r_hwb[i]], writes=[r_KD[seq]])
                else:
                    q0 = bl * N
                    nw = N - 1 if bl == nblk - 1 else N
                    P.dma("sp", KD[:, q0:q0 + nw], hwb[i][:, 0:nw], reads=[r_hwb[i]], writes=[r_KD[seq]])
    nrm = P.sb([128, 2], F32)
    r_nrm = Reg()
    dg = P.sb([128, 128], F32)
    r_dg = Reg()
    for seq in range(2):
        P.op("dve", lambda e: e.tensor_reduce(out=nrm[:, seq:seq + 1], in_=acc[:, seq, :], axis=AX.X, op=ALU.add), reads=[r_acc], writes=[r_nrm])
        P.op("dve", lambda e: e.tensor_scalar(out=dg[:], in0=identf[:], scalar1=nrm[:, seq:seq + 1], scalar2=None, op0=ALU.mult), reads=[r_nrm, r_idf], writes=[r_dg])
        b5, rb5 = nb()
        P.op("pe", lambda e: e.matmul(b5[:, 0:128], ones[:, :], dg[:, :], start=True, stop=True), reads=[r_ones, r_dg], writes=[rb5])
        P.op("dve", lambda e: e.reciprocal(out=RN[:, seq, :], in_=b5[:, 0:128]), reads=[rb5], writes=[r_RN])

    P.pop_scope([r_KD[0], r_KD[1], r_RN, r_SC])
    hsk = [P.sb([128, n_lat], BF16) for _ in range(2)]
    r_hsk = [Reg(), Reg()]
    zb = [P.sb([128, 128], BF16) for _ in range(2)]
    r_zb = [Reg(), Reg()]
    zr = [P.sb([128, 128], BF16) for _ in range(2)]
    r_zr = [Reg(), Reg()]
    tm = [P.sb([128, 128], F32) for _ in range(2)]
    r_tm = [Reg(), Reg()]
    hk = 0
    cv = 0
    for seq, (L, KD, W, j0) in enumerate(((n_lat, KDl, WL + 1, 0), (n_ctx, KDc, WC + 1, n_lat // 128))):
        NB = L // 128
        for ch in range(64):
            for o in range(2):
                c2 = cv % 2
                cv += 1
                row = o * 64 + ch
                src_c = (128 + ch) if o == 0 else ch
                Zs = SC[:, src_c, j0:j0 + NB]
                P.op("pool", lambda e: e.tensor_copy(out=zb[c2][:, 0:NB], in_=Zs), reads=[r_SC], writes=[r_zb[c2]])
                bz, rbz = nb()
                P.op("pe", lambda e: e.matmul(bz[:, 0:NB], J[:, :], zb[c2][:, 0:NB], start=True, stop=True), reads=[r_J, r_zb[c2]], writes=[rbz])
                P.op("act", lambda e: e.copy(out=zr[c2][:, 0:NB], in_=bz[:, 0:NB]), reads=[rbz], writes=[r_zr[c2]])
                yb_, r_yb = ybanks[c2]
                first = True
                for h in (1, 0):
                    hb_ = hk % 2
                    hk += 1
                    if h == 1:
                        x0, wd = L - 128, L
                        deltas = list(range(0, NB))
                    else:
                        x0, wd = 0, L - 128
                        deltas = list(range(-(NB - 1), 0))
                    if wd == 0:
                        continue
                    src = bass.AP(KD.tensor, row * W + x0, [[1, 128], [1, wd]])
                    P.dma("sp", hsk[hb_][:, 0:wd], src, reads=[r_KD[seq]], writes=[r_hsk[hb_]])
                    for di, d in enumerate(deltas):
                        xo = 128 * d + L - 128 - x0
                        lo_i, hi_i = max(0, d), NB + min(0, d)
                        lo_j, hi_j = max(0, -d), NB - max(0, d)
                        last = (h == 0 and di == len(deltas) - 1) or (NB == 1)
                        P.op("pe", lambda e, xo=xo, lo_i=lo_i, hi_i=hi_i, lo_j=lo_j, hi_j=hi_j, first=first, last=last: e.matmul(
                            yb_[:, lo_i:hi_i], hsk[hb_][:, xo:xo + 128], zr[c2][:, lo_j:hi_j], start=first, stop=last),
                            reads=[r_hsk[hb_], r_zr[c2]], writes=[r_yb], inc=(di == len(deltas) - 1))
                        first = False
                col = o * 64 + ch
                P.op("dve", lambda e: e.tensor_scalar(out=tm[c2][:, 0:NB], in0=yb_[:, 0:NB], scalar1=RN[:, seq, col:col + 1], scalar2=None, op0=ALU.mult),
                     reads=[r_yb, r_RN], writes=[r_tm[c2]])
                P.op("dve", lambda e: e.scalar_tensor_tensor(out=tm[c2][:, 0:NB], in0=Zs, scalar=hbs[:, col:col + 1], in1=tm[c2][:, 0:NB], op0=ALU.mult, op1=ALU.add),
                     reads=[r_SC, r_par, r_tm[c2]], writes=[r_tm[c2]])
                if o == 0:
                    P.op("dve", lambda e: e.tensor_tensor(out=SC[:, ch, j0:j0 + NB], in0=SC[:, ch, j0:j0 + NB], in1=tm[c2][:, 0:NB], op=ALU.mult),
                         reads=[r_SC, r_tm[c2]], writes=[r_SC])
                else:
                    P.op("dve", lambda e: e.tensor_tensor(out=SC[:, 64 + ch, j0:j0 + NB], in0=SC[:, 64 + ch, j0:j0 + NB], in1=tm[c2][:, 0:NB], op=ALU.mult),
                         reads=[r_SC, r_tm[c2]], writes=[r_SC])
    if stop <= 4:
        P.finish([])
        return P
    ot = [P.sb([128, 64], F32) for _ in range(2)]
    r_ot = [Reg(), Reg()]
    for t in range(NT):
        i = t % 2
        eng = "act" if t % 2 == 0 else "pool"
        if eng == "act":
            P.op("act", lambda e: e.copy(out=ot[i][:], in_=SC[:, 64:128, t]), reads=[r_SC], writes=[r_ot[i]])
        else:
            P.op("pool", lambda e: e.tensor_copy(out=ot[i][:], in_=SC[:, 64:128, t]), reads=[r_SC], writes=[r_ot[i]])
        P.dma("sp", hy[t * 128:(t + 1) * 128, :], ot[i][:], reads=[r_ot[i]], writes=[r_hy])
    P.finish([r_hy])
    return P


D = 1024
FF = 2816
ALPHA = float(4 ** 0.25)


def build_p2(n_lat=4096, n_ctx=64, moe=False, SB=None):
    P = Prog()
    E = 8 if moe else 1
    ntok = n_lat + n_ctx
    mix = P.dram("mix", [ntok, D], F32, "ExternalInput")
    x = P.dram("x", [ntok, D], F32, "ExternalInput")
    cT = P.dram("cT", [128, 16], F32, "ExternalInput")
    adaw = P.dram("adaw", [D, 4096], F32, "ExternalInput")
    adab = P.dram("adab", [1, 4096], F32, "ExternalInput")
    wout = P.dram("wout", [D, D], F32, "ExternalInput")
    lnp = P.dram("lnp", [128, 4096], F32, "ExternalInput")
    w1 = P.dram("w1", [E, D, FF], F32, "ExternalInput")
    w3 = P.dram("w3", [E, D, FF], F32, "ExternalInput")
    w2 = P.dram("w2", [E, FF, D], F32, "ExternalInput")
    if moe:
        wr = P.dram("wr", [D, 8], F32, "ExternalInput")
    xo = P.dram("xo", [ntok, D], F32, "ExternalOutput")
    r_xo = Reg()
    w1b = P.dram("w1b", [E, D, FF], BF16, "Internal")
    w3b = P.dram("w3b", [E, D, FF], BF16, "Internal")
    w2b = P.dram("w2b", [E, FF, D], BF16, "Internal")
    r_wbf = [Reg() for _ in range(E)]
    for e_ in range(E):
        for k in range(8):
            P.dma("pool", w1b[e_, k * 128:(k + 1) * 128, :], w1[e_, k * 128:(k + 1) * 128, :], writes=[r_wbf[e_]])
            P.dma("pool", w3b[e_, k * 128:(k + 1) * 128, :], w3[e_, k * 128:(k + 1) * 128, :], writes=[r_wbf[e_]])
        for f in range(22):
            P.dma("pool", w2b[e_, f * 128:(f + 1) * 128, :], w2[e_, f * 128:(f + 1) * 128, :], writes=[r_wbf[e_]])

    fb = [P.ps([128, 512], F32) for _ in range(8)]
    tb = [(fb[6][0][:, :].bitcast(BF16).rearrange("p (k c) -> p k c", k=8), fb[6][1])]
    tf = [(fb[7][0][:, :].rearrange("p (k c) -> p k c", k=4), fb[7][1])]
    ob_rr = [0]
    ones_row = P.sb([1, 128], F32)
    P.op("dve", lambda e: e.memset(ones_row[:], 1.0), writes=[Reg()])
    ident, r_id = make_ident(P)
    if moe:
        identf, r_idf = make_ident(P, F32)
        wrs = P.sb([128, 8, 8], F32)
        r_wrs = Reg()
        P.dma("sp", wrs[:], wr.rearrange("(k p) e -> p k e", p=128), writes=[r_wrs])
    wob = P.sb([128, 8, D], BF16)
    r_wob = Reg()
    for k in range(8):
        P.dma("pool", wob[:, k, :], wout[k * 128:(k + 1) * 128, :], writes=[r_wob])
    lns = P.sb([128, 4096], F32)
    r_lns = Reg()
    P.dma("sp", lns[:], lnp[:, :], writes=[r_lns])
    bcA, r_bcA = mods_block(P, cT, adaw[:, 0:2048], adab[:, 0:2048], 2048, ones_row, fb[:4], [])
    bcB, r_bcB = mods_block(P, cT, adaw[:, 2048:4096], adab[:, 2048:4096], 2048, ones_row, fb[:4], [(0, 1024)])

    tiles_all = [(i * 128, 128, 0) for i in range(n_lat // 128)]
    if n_ctx:
        tiles_all.append((n_lat, n_ctx, 1))
    SB = SB or (7 if moe else 4)
    sblocks = [tiles_all[i:i + SB] for i in range(0, n_lat // 128, SB)]
    if n_ctx:
        sblocks[-1] = sblocks[-1] + [tiles_all[-1]]
    MT = SB + 1
    NTOK = SB * 128 + n_ctx

    def T(shape, dt=F32, n=2):
        return [P.sb(list(shape), dt) for _ in range(n)], [Reg() for _ in range(n)]
    xs, r_xs = T((128, D))
    ms, r_ms = T((128, D), n=1)
    ms, r_ms = ms * 2, r_ms * 2
    mb, r_mb = T((128, D), BF16, n=1)
    mb, r_mb = mb * 2, r_mb * 2
    mT, r_mT = T((128, 8, 128), BF16)
    yv, r_yv = T((128, D))
    tmp, r_tmp = T((128, D))
    stat, r_stat = T((128, 16))
    hb, r_hb = T((128, D), BF16, n=1)
    hb, r_hb = hb * 2, r_hb * 2
    x1d = P.dram("x1d", [ntok, D], F32, "Internal")
    r_x1d = Reg()
    x1t, r_x1t = T((128, D))
    acc = P.sb([128, MT, D], F32)
    r_acc = [Reg() for _ in range(MT)]
    r_acc2 = r_acc
    evt, r_evt = [None, None], [Reg(), Reg()]
    evq = [0]
    h2T = P.sb([128, 8, NTOK], BF16)
    r_h2T = Reg()
    if moe:
        hf, r_hf = T((128, D), n=1)
        hf, r_hf = hf * 2, r_hf * 2
        hTf, r_hTf = T((128, 8, 128), n=1)
        hTf, r_hTf = hTf * 2, r_hTf * 2
        lg, r_lg = T((128, 32))
        gates = P.sb([128, MT, 8], F32)
        r_gates = [Reg() for _ in range(MT)]
    UF = 2
    units = [(f0, min(UF, 22 - f0)) for f0 in range(0, 22, UF)]
    w1u, r_w1u = T((128, 8, UF * 128), BF16)
    w3u, r_w3u = T((128, 8, UF * 128), BF16)
    w2u, r_w2u = T((128, UF, D), BF16)
    GT, r_GT = T((128, UF, NTOK), BF16)
    sa, r_sa = T((128, 512))
    wq = [0]
    it = [0]

    for sbk in sblocks:
        ntk = sum(r for (_, r, _) in sbk)
        col = 0
        for ti, (t0, rows, s) in enumerate(sbk):
            i = it[0] % 2
            it[0] += 1
            P.dma("sp", ms[i][:rows, :], mix[t0:t0 + rows, :], writes=[r_ms[i]])
            P.dma("sp", xs[i][:rows, :], x[t0:t0 + rows, :], writes=[r_xs[i]])
            P.op("pool", lambda e: e.tensor_copy(out=mb[i][:rows, :], in_=ms[i][:rows, :]), reads=[r_ms[i]], writes=[r_mb[i]])
            tp, r_tp = tb[0]
            for k in range(8):
                P.op("pe", lambda e, k=k: e.transpose(tp[:, k, :rows], mb[i][:rows, k * 128:(k + 1) * 128], ident[:rows, :rows]),
                     reads=[r_mb[i], r_id], writes=[r_tp], inc=(k == 7))
            P.op("act", lambda e: e.copy(out=mT[i][:, :, :rows], in_=tp[:, :, :rows]), reads=[r_tp], writes=[r_mT[i]])
            P.op("act", lambda e: e.mul(out=yv[i][:rows, :], in_=xs[i][:rows, :], mul=ALPHA), reads=[r_xs[i]], writes=[r_yv[i]])
            for cb in range(2):
                bank, rb = fb[4 + cb]
                for k in range(8):
                    P.op("pe", lambda e, k=k, cb=cb, bank=bank: e.matmul(bank[:rows, :], mT[i][:, k, :rows], wob[:, k, cb * 512:(cb + 1) * 512],
                                                                   start=(k == 0), stop=(k == 7)), reads=[r_mT[i], r_wob], writes=[rb], inc=(k == 7))
                P.op("dve", lambda e, cb=cb, bank=bank: e.tensor_tensor(out=tmp[i][:rows, cb * 512:(cb + 1) * 512], in0=bank[:rows, :],
                                                                in1=bcA[s][:rows, cb * 512:(cb + 1) * 512], op=ALU.mult),
                     reads=[rb, r_bcA[s]], writes=[r_tmp[i]])
            P.op("pool", lambda e: e.tensor_tensor(out=yv[i][:rows, :], in0=yv[i][:rows, :], in1=tmp[i][:rows, :], op=ALU.add), reads=[r_yv[i], r_tmp[i]], writes=[r_yv[i]])
            ln_tile(P, rows, yv[i][:rows, :], r_yv[i], tmp[i], r_tmp[i], stat[i], r_stat[i])
            P.op("pool", lambda e: e.tensor_tensor(out=tmp[i][:rows, :], in0=tmp[i][:rows, :], in1=lns[:rows, 0:1024], op=ALU.mult), reads=[r_tmp[i], r_lns], writes=[r_tmp[i]])
            P.op("dve", lambda e: e.tensor_tensor(out=x1t[i][:rows, :], in0=tmp[i][:rows, :], in1=lns[:rows, 1024:2048], op=ALU.add), reads=[r_tmp[i], r_lns], writes=[r_x1t[i]])
            P.dma("sp", x1d[t0:t0 + rows, :], x1t[i][:rows, :], reads=[r_x1t[i]], writes=[r_x1d])
            ln_tile(P, rows, x1t[i][:rows, :], r_x1t[i], tmp[i], r_tmp[i], stat[i], r_stat[i])
            P.op("pool", lambda e: e.tensor_tensor(out=tmp[i][:rows, :], in0=tmp[i][:rows, :], in1=bcB[s][:rows, 0:1024], op=ALU.mult), reads=[r_tmp[i], r_bcB[s]], writes=[r_tmp[i]])
            if moe:
                P.op("dve", lambda e: e.tensor_tensor(out=hf[i][:rows, :], in0=tmp[i][:rows, :], in1=bcA[s][:rows, 1024:2048], op=ALU.add), reads=[r_tmp[i], r_bcA[s]], writes=[r_hf[i]])
                P.op("pool", lambda e: e.tensor_copy(out=hb[i][:rows, :], in_=hf[i][:rows, :]), reads=[r_hf[i]], writes=[r_hb[i]])
            else:
                P.op("dve", lambda e: e.tensor_tensor(out=hb[i][:rows, :], in0=tmp[i][:rows, :], in1=bcA[s][:rows, 1024:2048], op=ALU.add), reads=[r_tmp[i], r_bcA[s]], writes=[r_hb[i]])
            for k in range(8):
                P.op("pe", lambda e, k=k: e.transpose(tp[:, k, :rows], hb[i][:rows, k * 128:(k + 1) * 128], ident[:rows, :rows]),
                     reads=[r_hb[i], r_id], writes=[r_tp], inc=(k == 7))
            P.op("act", lambda e, col=col: e.copy(out=h2T[:, :, col:col + rows], in_=tp[:, :, :rows]), reads=[r_tp], writes=[r_h2T])
            if moe:
                tq, r_tq = tf[0]
                for half in range(2):
                    for k in range(4):
                        kk = half * 4 + k
                        P.op("pe", lambda e, k=k, kk=kk: e.transpose(tq[:, k, :rows], hf[i][:rows, kk * 128:(kk + 1) * 128], identf[:rows, :rows]),
                             reads=[r_hf[i], r_idf], writes=[r_tq], inc=(k == 3))
                    P.op("dve", lambda e, half=half: e.tensor_copy(out=hTf[i][:, half * 4:half * 4 + 4, :rows], in_=tq[:, :, :rows]), reads=[r_tq], writes=[r_hTf[i]])
                bank, rb = fb[4]
                for k in range(8):
                    P.op("pe", lambda e, k=k, bank=bank: e.matmul(bank[:rows, 0:8], hTf[i][:, k, :rows], wrs[:, k, :], start=(k == 0), stop=(k == 7)),
                         reads=[r_hTf[i], r_wrs], writes=[rb], inc=(k == 7))
                L = lg[i]
                rl = r_lg[i]
                P.op("dve", lambda e, bank=bank: e.tensor_copy(out=L[:rows, 0:8], in_=bank[:rows, 0:8]), reads=[rb], writes=[rl])
                P.op("dve", lambda e: e.tensor_reduce(out=L[:rows, 24:25], in_=L[:rows, 0:8], axis=AX.X, op=ALU.max), reads=[rl], writes=[rl])
                P.op("dve", lambda e: e.tensor_scalar(out=L[:rows, 8:16], in0=L[:rows, 0:8], scalar1=L[:rows, 24:25], scalar2=None, op0=ALU.is_equal), reads=[rl], writes=[rl])
                P.op("dve", lambda e: e.scalar_tensor_tensor(out=L[:rows, 16:24], in0=L[:rows, 8:16], scalar=-1e30, in1=L[:rows, 0:8], op0=ALU.mult, op1=ALU.add), reads=[rl], writes=[rl])
                P.op("dve", lambda e: e.tensor_reduce(out=L[:rows, 25:26], in_=L[:rows, 16:24], axis=AX.X, op=ALU.max), reads=[rl], writes=[rl])
                P.op("dve", lambda e: e.tensor_scalar(out=L[:rows, 16:24], in0=L[:rows, 16:24], scalar1=L[:rows, 25:26], scalar2=None, op0=ALU.is_equal), reads=[rl], writes=[rl])
                P.op("dve", lambda e: e.tensor_tensor(out=L[:rows, 26:27], in0=L[:rows, 25:26], in1=L[:rows, 24:25], op=ALU.subtract), reads=[rl], writes=[rl])
                P.op("act", lambda e: e.activation(out=L[:rows, 26:27], in_=L[:rows, 26:27], func=AF.Exp), reads=[rl], writes=[rl])
                P.op("dve", lambda e: e.tensor_scalar_add(out=L[:rows, 27:28], in0=L[:rows, 26:27], scalar1=1.0), reads=[rl], writes=[rl])
                P.op("dve", lambda e: e.reciprocal(out=L[:rows, 27:28], in_=L[:rows, 27:28]), reads=[rl], writes=[rl])
                P.op("dve", lambda e: e.tensor_tensor(out=L[:rows, 28:29], in0=L[:rows, 26:27], in1=L[:rows, 27:28], op=ALU.mult), reads=[rl], writes=[rl])
                P.op("dve", lambda e: e.tensor_scalar(out=L[:rows, 8:16], in0=L[:rows, 8:16], scalar1=L[:rows, 27:28], scalar2=None, op0=ALU.mult), reads=[rl], writes=[rl])
                P.op("dve", lambda e, ti=ti: e.scalar_tensor_tensor(out=gates[:rows, ti, :], in0=L[:rows, 16:24], scalar=L[:rows, 28:29], in1=L[:rows, 8:16],
                                                                    op0=ALU.mult, op1=ALU.add), reads=[rl], writes=[r_gates[ti]])
            col += rows
        tblocks = []
        c0 = 0
        while c0 < ntk:
            n = min(512, ntk - c0)
            tblocks.append((c0, n))
            c0 += n
        for e_ in range(E):
            for (f0, nf) in units:
                q = wq[0] % 2
                wq[0] += 1
                P.dma("sp", w1u[q][:, :, 0:nf * 128], w1b[e_, :, f0 * 128:(f0 + nf) * 128].rearrange("(k p) f -> p k f", p=128),
                      reads=[r_wbf[e_]], writes=[r_w1u[q]])
                P.dma("act", w3u[q][:, :, 0:nf * 128], w3b[e_, :, f0 * 128:(f0 + nf) * 128].rearrange("(k p) f -> p k f", p=128),
                      reads=[r_wbf[e_]], writes=[r_w3u[q]])
                P.dma("sp", w2u[q][:, 0:nf, :], w2b[e_, f0 * 128:(f0 + nf) * 128, :].rearrange("(f p) d -> p f d", p=128),
                      reads=[r_wbf[e_]], writes=[r_w2u[q]])
                for (c0, n) in tblocks:
                    for f in range(nf):
                        ba, rba = fb[0 + (f % 2) * 2]
                        bb, rbb = fb[1 + (f % 2) * 2]
                        for k in range(8):
                            P.op("pe", lambda e, k=k, f=f, ba=ba: e.matmul(ba[:, 0:n], w1u[q][:, k, f * 128:(f + 1) * 128], h2T[:, k, c0:c0 + n],
                                                                     start=(k == 0), stop=(k == 7)), reads=[r_w1u[q], r_h2T], writes=[rba], inc=(k == 7))
                        for k in range(8):
                            P.op("pe", lambda e, k=k, f=f, bb=bb: e.matmul(bb[:, 0:n], w3u[q][:, k, f * 128:(f + 1) * 128], h2T[:, k, c0:c0 + n],
                                                                     start=(k == 0), stop=(k == 7)), reads=[r_w3u[q], r_h2T], writes=[rbb], inc=(k == 7))
                        j = f % 2
                        P.op("act", lambda e, ba=ba, j=j: e.activation(out=sa[j][:, 0:n], in_=ba[:, 0:n], func=AF.Silu), reads=[rba], writes=[r_sa[j]])
                        P.op("dve", lambda e, bb=bb, j=j, f=f: e.tensor_tensor(out=GT[q][:, f, c0:c0 + n], in0=bb[:, 0:n], in1=sa[j][:, 0:n], op=ALU.mult),
                             reads=[rbb, r_sa[j]], writes=[r_GT[q]])
                col = 0
                for ti, (t0, rows, s) in enumerate(sbk):
                    for cb in range(2):
                        bank, rb = fb[4 + ob_rr[0] % 4]
                        ob_rr[0] += 1
                        for f in range(nf):
                            P.op("pe", lambda e, f=f, cb=cb, bank=bank, col=col: e.matmul(bank[:rows, :], GT[q][:, f, col:col + rows], w2u[q][:, f, cb * 512:(cb + 1) * 512],
                                                                                  start=(f == 0), stop=(f == nf - 1)),
                                 reads=[r_GT[q], r_w2u[q]], writes=[rb], inc=(f == nf - 1))
                        first = (e_ == 0 and f0 == 0)
                        dst = acc[:rows, ti, cb * 512:(cb + 1) * 512]
                        if moe:
                            gsc = gates[:rows, ti, e_:e_ + 1]
                            if first:
                                P.op("dve", lambda e, bank=bank, dst=dst, gsc=gsc: e.tensor_scalar(out=dst, in0=bank[:rows, :], scalar1=gsc, scalar2=None, op0=ALU.mult),
                                     reads=[rb, r_gates[ti]], writes=[r_acc[ti]])
                            elif True:
                                P.op("dve", lambda e, bank=bank, dst=dst, gsc=gsc: e.scalar_tensor_tensor(out=dst, in0=bank[:rows, :], scalar=gsc, in1=dst, op0=ALU.mult, op1=ALU.add),
                                     reads=[rb, r_gates[ti], r_acc[ti]], writes=[r_acc[ti]])
                            else:
                                ev_i = evq[0] % 2
                                evq[0] += 1
                                P.op("act", lambda e, bank=bank, gsc=gsc, ev_i=ev_i: e.activation(out=evt[ev_i][:rows, :], in_=bank[:rows, :], func=AF.Copy, scale=gsc),
                                     reads=[rb, r_gates[ti]], writes=[r_evt[ev_i]])
                                P.op("pool", lambda e, dst=dst, ev_i=ev_i: e.tensor_tensor(out=dst, in0=dst, in1=evt[ev_i][:rows, :], op=ALU.add),
                                     reads=[r_evt[ev_i], r_acc2[ti]], writes=[r_acc2[ti]])
                        else:
                            if first:
                                P.op("act", lambda e, bank=bank, dst=dst: e.copy(out=dst, in_=bank[:rows, :]), reads=[rb], writes=[r_acc[ti]])
                            else:
                                P.op("dve", lambda e, bank=bank, dst=dst: e.tensor_tensor(out=dst, in0=bank[:rows, :], in1=dst, op=ALU.add),
                                     reads=[rb, r_acc[ti]], writes=[r_acc[ti]])
                    col += rows
        for ti, (t0, rows, s) in enumerate(sbk):
            i = it[0] % 2
            it[0] += 1
            P.op("pool", lambda e: e.tensor_tensor(out=acc[:rows, ti, :], in0=acc[:rows, ti, :], in1=bcB[s][:rows, 1024:2048], op=ALU.mult), reads=[r_acc[ti], r_bcB[s]], writes=[r_acc[ti]])
            P.dma("sp", x1t[i][:rows, :], x1d[t0:t0 + rows, :], reads=[r_x1d], writes=[r_x1t[i]])
            P.op("dve", lambda e: e.scalar_tensor_tensor(out=yv[i][:rows, :], in0=x1t[i][:rows, :], scalar=ALPHA, in1=acc[:rows, ti, :], op0=ALU.mult, op1=ALU.add),
                 reads=[r_x1t[i], r_acc[ti]], writes=[r_yv[i]])
            ln_tile(P, rows, yv[i][:rows, :], r_yv[i], tmp[i], r_tmp[i], stat[i], r_stat[i])
            P.op("pool", lambda e: e.tensor_tensor(out=tmp[i][:rows, :], in0=tmp[i][:rows, :], in1=lns[:rows, 2048:3072], op=ALU.mult), reads=[r_tmp[i], r_lns], writes=[r_tmp[i]])
            P.op("dve", lambda e: e.tensor_tensor(out=yv[i][:rows, :], in0=tmp[i][:rows, :], in1=lns[:rows, 3072:4096], op=ALU.add), reads=[r_tmp[i], r_lns], writes=[r_yv[i]])
            P.dma("sp", xo[t0:t0 + rows, :], yv[i][:rows, :], reads=[r_yv[i]], writes=[r_xo])
    P.finish([r_xo])
    return P


def a2_consts():
    i = np.arange(128); half = i // 64
    same = half[:, None] == half[None, :]
    MST = (same & np.where(half[:, None] == 0, i[:, None] < i[None, :], i[:, None] > i[None, :])).astype(np.float32)
    MIT = MST + np.eye(128, dtype=np.float32)
    MS = np.ascontiguousarray(MST.T)
    LM = np.zeros((128, 96), np.float32)
    LM[:64, 0:16] = 1; LM[64:, 16:32] = 1; LM[:64, 32:48] = 1; LM[64:, 48:64] = 1; LM[:, 64:96] = 1
    return np.concatenate([MIT, MST, MIT, MS, LM], 1).astype(np.float32)

def a2_inputs(ur, n_lat, n_ctx, hd, mu, w0, wB, a0, aB, gB, kkw, ka, rk, gng, gnb):
    C = 256
    cols = np.concatenate([np.arange(hd * 64, hd * 64 + 64), C + np.arange(hd * 64, hd * 64 + 64), 2 * C + np.arange(hd * 64, hd * 64 + 64),
                           np.arange(768, 864)])
    u = ur[:, cols]
    def shifted(x, d):
        o = np.zeros_like(x)
        if d == 1: o[1:] = x[:-1]
        else: o[:-1] = x[1:]
        return o
    prev = np.concatenate([shifted(u[:n_lat], 1), shifted(u[n_lat:], 1)], 0)
    nxt = np.concatenate([shifted(u[:n_lat], -1), shifted(u[n_lat:], -1)], 0)
    u3 = np.concatenate([u, prev, nxt], 1)
    fwd, bwd = rwkv_orders(n_lat, n_ctx)
    idx = np.concatenate([np.concatenate([np.arange(f, f + 64), np.arange(b, b + 64)]) for f, b in zip(fwd, bwd)])
    U3 = np.ascontiguousarray(u3[idx])
    coefmu = np.tile(np.concatenate([mu[0][cols], mu[1][cols]])[None], (128, 1)).astype(np.float32)
    hs = slice(hd * 64, hd * 64 + 64)
    rowp = np.zeros((128, 128), np.float32)
    rowp[:64, :64] = w0[0][hs]; rowp[64:, :64] = w0[1][hs]; rowp[:64, 64:] = a0[0][hs]; rowp[64:, 64:] = a0[1][hs]
    hv = np.tile(np.concatenate([kkw[hs], ka[hs], rk[hd], gng[hs], gnb[hs]])[None], (128, 1)).astype(np.float32)
    Wl = np.zeros((96, 192), np.float32)
    Wl[0:16, 0:64] = wB[0][:, hs]; Wl[16:32, 0:64] = wB[1][:, hs]; Wl[32:48, 64:128] = aB[0][:, hs]; Wl[48:64, 64:128] = aB[1][:, hs]
    Wl[64:96, 128:192] = gB[:, hs]
    return dict(U3=U3, coefmu=coefmu, rowp=rowp, hv=hv, Wl=Wl, cmask=a2_consts())


def hy_ztab(L):
    bands = 16
    t = np.linspace(0.0, 1.0, L, dtype=np.float32)[:, None]
    f = np.linspace(1e-4, bands - 1, bands, dtype=np.float32)[None, :]
    wt = (np.float32(2.0 * math.pi) * np.arange(L, dtype=np.float32)[:, None] / np.float32(L)).astype(np.float32)
    z = np.concatenate([t, np.cos(f * wt), -np.sin(f * wt)], -1).astype(np.float32)
    return np.ascontiguousarray(np.stack([z.T, z[::-1].T], 0))

def a3_inputs(uh, n_lat, n_ctx, j, sw, sb, w1, b1, f1, w2, b2, f2, w3, dec, hbias):
    cs = slice(j * 64, j * 64 + 64)
    cols = np.concatenate([np.arange(256)[cs], 256 + np.arange(256)[cs], 512 + np.arange(256)[cs]])
    u = uh[:, cols]
    def shifted(x, d):
        o = np.zeros_like(x)
        if d == 1: o[1:] = x[:-1]
        else: o[:-1] = x[1:]
        return o
    prev = np.concatenate([shifted(u[:n_lat], 1), shifted(u[n_lat:], 1)], 0)
    nxt = np.concatenate([shifted(u[:n_lat], -1), shifted(u[n_lat:], -1)], 0)
    H3 = np.ascontiguousarray(np.concatenate([u, prev, nxt], 1), dtype=np.float32)
    cw = np.tile(np.concatenate([sw[1][cols], sw[0][cols], sw[2][cols], sb[cols]])[None], (128, 1)).astype(np.float32)
    fc = np.array([o * 512 + d * 256 + j * 64 + c for d in range(2) for o in range(2) for c in range(64)])
    colp = np.stack([b1, f1, b2, f2], 1).astype(np.float32)
    hb = np.tile(np.concatenate([hbias[0][cs], hbias[1][cs]])[None], (128, 1)).astype(np.float32)
    return dict(H3=H3, cw=cw, ztl=hy_ztab(n_lat), ztc=hy_ztab(n_ctx), w1=np.ascontiguousarray(w1, dtype=np.float32),
                w2=np.ascontiguousarray(w2, dtype=np.float32), w3=np.ascontiguousarray(w3[:, fc], dtype=np.float32), colp=colp,
                dec=np.ascontiguousarray(dec[fc][None], dtype=np.float32), hb=hb)


N_LAT = 16384
N_CTX = 256
_PROGS = {}


def _prog(name):
    if name not in _PROGS:
        if name == "p1":
            P = build_p1(4096, 64)
        elif name == "a1":
            P = build_a1(N_LAT, N_CTX)
        elif name == "a2":
            P = build_a2(N_LAT, N_CTX)
        elif name == "a3":
            P = build_a3(N_LAT, N_CTX)
        elif name == "p2d":
            P = build_p2(4096, 64, False)
        elif name == "p2m":
            P = build_p2(4096, 64, True)
        _PROGS[name] = P.close()
    return _PROGS[name]


def _run(name, in_maps, out_name):
    nc = _prog(name)
    in_maps = [{k: np.ascontiguousarray(v, dtype=np.float32) for k, v in m.items()} for m in in_maps]
    res = run_bass_kernel_spmd(nc, in_maps, core_ids=list(range(8)))
    return [np.asarray(r[out_name]) for r in res.results]


def _rope_cs(L):
    rows = L // 64
    row = np.repeat(np.arange(rows, dtype=np.float32), 64)
    col = np.tile(np.arange(64, dtype=np.float32), rows)
    inv = (np.float32(10000.0) ** (-np.arange(16, dtype=np.float32) / np.float32(16))).astype(np.float32)
    ang = np.concatenate([row[:, None] * inv, col[:, None] * inv], -1).astype(np.float32)
    cos, sin = np.cos(ang).astype(np.float32), np.sin(ang).astype(np.float32)
    return np.concatenate([cos, cos, cos, sin, sin, sin], -1).astype(np.float32)


def _cT(cb, cctx):
    c2 = np.stack([cb, cctx], 0).astype(np.float32)
    return np.ascontiguousarray(c2.reshape(2, 8, 128).transpose(2, 0, 1).reshape(128, 16))


def kernel(x, c, ctx, c_ctx, ada_w, ada_b, w_in, w_out, q_gain, k_gain,
           rwkv_mu, rwkv_w0, rwkv_wB, rwkv_a0, rwkv_aB, rwkv_gB, rwkv_kk, rwkv_ka, rwkv_rk,
           rwkv_gn_g, rwkv_gn_b, hy_short_w, hy_short_b, hy_w1, hy_b1, hy_freq1, hy_w2, hy_b2,
           hy_freq2, hy_w3, hy_decay, hy_bias, ln1_g, ln1_b, ln2_g, ln2_b,
           ffn_w1, ffn_w3, ffn_w2, moe_router, moe_w1, moe_w3, moe_w2):
    f32 = lambda a: np.asarray(a, dtype=np.float32)
    x = f32(x).copy()
    xc = f32(ctx).copy()
    c, c_ctx = f32(c), f32(c_ctx)
    cs = _rope_cs(N_LAT)
    depth = 2
    for l in range(depth):
        aw, ab = f32(ada_w[l]), f32(ada_b[l])
        cores = [(b, q) for b in range(2) for q in range(4)]
        ins = []
        for (b, q) in cores:
            xt = np.concatenate([x[b, q * 4096:(q + 1) * 4096], xc[b, q * 64:(q + 1) * 64]], 0)
            ins.append(dict(x=xt, cT=_cT(c[b], c_ctx), adaw=aw[:, 0:2048], adab=ab[None, 0:2048], win=f32(w_in[l])))
        us = _run("p1", ins, "u")
        u = np.empty((2, N_LAT + N_CTX, 2400), np.float32)
        for (b, q), uu in zip(cores, us):
            u[b, q * 4096:(q + 1) * 4096] = uu[:4096]
            u[b, N_LAT + q * 64:N_LAT + (q + 1) * 64] = uu[4096:]
        mixo = np.empty((2, N_LAT + N_CTX, 1024), np.float32)
        gains = np.tile(np.concatenate([f32(q_gain[l]), f32(q_gain[l]), f32(k_gain[l])])[None], (128, 1))
        ins = []
        for (b, j) in cores:
            g = j // 2
            qk = np.concatenate([u[b][:, 128 * j:128 * j + 128], u[b][:, 512 + 64 * g:512 + 64 * g + 64]], 1)
            ins.append(dict(qk=qk, v=u[b][:, 640 + 64 * g:640 + 64 * g + 64], gains=gains, cs=cs))
        for (b, j), o in zip(cores, _run("a1", ins, "att")):
            mixo[b][:, 128 * j:128 * j + 128] = o
        ins = []
        for (b, j) in cores:
            ins.append(a2_inputs(u[b][:, 768:1632], N_LAT, N_CTX, j, f32(rwkv_mu[l]), f32(rwkv_w0[l]), f32(rwkv_wB[l]), f32(rwkv_a0[l]),
                                 f32(rwkv_aB[l]), f32(rwkv_gB[l]), f32(rwkv_kk[l]), f32(rwkv_ka[l]), f32(rwkv_rk[l]),
                                 f32(rwkv_gn_g[l]), f32(rwkv_gn_b[l])))
        for (b, j), o in zip(cores, _run("a2", ins, "rw")):
            mixo[b][:, 512 + 64 * j:512 + 64 * j + 64] = o
        ins = []
        for (b, j) in cores:
            ins.append(a3_inputs(u[b][:, 1632:2400], N_LAT, N_CTX, j, f32(hy_short_w[l]), f32(hy_short_b[l]), f32(hy_w1[l]), f32(hy_b1[l]),
                                 f32(hy_freq1[l]), f32(hy_w2[l]), f32(hy_b2[l]), f32(hy_freq2[l]), f32(hy_w3[l]), f32(hy_decay[l]),
                                 f32(hy_bias[l])))
        for (b, j), o in zip(cores, _run("a3", ins, "hy")):
            mixo[b][:, 768 + 64 * j:768 + 64 * j + 64] = o
        lnp = np.tile(np.concatenate([f32(ln1_g[l]), f32(ln1_b[l]), f32(ln2_g[l]), f32(ln2_b[l])])[None], (128, 1))
        jj = l // 2
        ins = []
        for (b, q) in cores:
            mt = np.concatenate([mixo[b, q * 4096:(q + 1) * 4096], mixo[b, N_LAT + q * 64:N_LAT + (q + 1) * 64]], 0)
            xt = np.concatenate([x[b, q * 4096:(q + 1) * 4096], xc[b, q * 64:(q + 1) * 64]], 0)
            d = dict(mix=mt, x=xt, cT=_cT(c[b], c_ctx), adaw=aw[:, 2048:6144], adab=ab[None, 2048:6144], wout=f32(w_out[l]), lnp=lnp)
            if l % 2 == 0:
                d.update(w1=f32(ffn_w1[jj])[None], w3=f32(ffn_w3[jj])[None], w2=f32(ffn_w2[jj])[None])
            else:
                d.update(w1=f32(moe_w1[jj]), w3=f32(moe_w3[jj]), w2=f32(moe_w2[jj]), wr=f32(moe_router[jj]))
            ins.append(d)
        outs = _run("p2d" if l % 2 == 0 else "p2m", ins, "xo")
        xn = np.empty_like(x)
        xcn = np.empty_like(xc)
        for (b, q), o in zip(cores, outs):
            xn[b, q * 4096:(q + 1) * 4096] = o[:4096]
            xcn[b, q * 64:(q + 1) * 64] = o[4096:]
        x, xc = xn, xcn
    return x
```
